# Optimizing a Trainium2 kernel written in Bass

```python
import math
import jax
import jax.numpy as jnp
from jax import lax
import numpy as np

D_MODEL = 1024
BATCH = 4
SEQ = 4096
DEPTH = 2

GRID_W = 64
CTX_LEN = 256

S5_WIDTH = 256
S5_GROUP = 16
S5_GROUPS = S5_WIDTH // S5_GROUP
S5_STATE = 64
S5_DT_MIN = 1e-3
S5_DT_MAX = 1e-1

GM_WIDTH = 256
GM_HEADS = 4
GM_CHUNK = 128

DA_HEADS = 4
DA_HEAD_DIM = 64
DA_QK = DA_HEADS * 2 * DA_HEAD_DIM
DA_V = DA_HEADS * 2 * DA_HEAD_DIM
Q_BLOCK = 128
ROPE_BASE = 10000.0

N_BRANCH = 3
IN_SPLITS = (S5_WIDTH,
             S5_WIDTH + 2 * GM_WIDTH,
             S5_WIDTH + 2 * GM_WIDTH + DA_QK,
             S5_WIDTH + 2 * GM_WIDTH + 2 * DA_QK,
             S5_WIDTH + 2 * GM_WIDTH + 2 * DA_QK + DA_V)
IN_WIDTH = IN_SPLITS[-1] + N_BRANCH * D_MODEL

N_EXPERTS = 64
TOP_K = 8
N_GROUPS = 8
TOPK_GROUPS = 4
EXPERT_HIDDEN = 256
SHARED_HIDDEN = 256
ROUTED_SCALE = 2.5
MOE_BLOCK = 128

DN_ALPHA = (2 * DEPTH) ** 0.25
DN_BETA = (8 * DEPTH) ** -0.25
LN_EPS = 1e-5

kernel_name = 'hybrid_s5_gmlp_diffattn_moe_dit'


def layer_norm(x, g=None, b=None):
    xf = x.astype(jnp.float32)
    mu = jnp.mean(xf, -1, keepdims=True)
    var = jnp.mean(jnp.square(xf - mu), -1, keepdims=True)
    y = ((xf - mu) * lax.rsqrt(var + LN_EPS)).astype(x.dtype)
    return y if g is None else y * g + b


def rms_norm(x, g):
    xf = x.astype(jnp.float32)
    y = xf * lax.rsqrt(jnp.mean(jnp.square(xf), -1, keepdims=True) + LN_EPS)
    return y.astype(x.dtype) * g


def modulate(x, shift, scale):
    return x * (1 + scale) + shift


def axial_rope_tables(n_tokens):
    rows = n_tokens // GRID_W
    r, col = jnp.meshgrid(jnp.arange(rows), jnp.arange(GRID_W), indexing='ij')
    axis_dim = DA_HEAD_DIM // 2
    inv_freq = ROPE_BASE ** (-jnp.arange(0, axis_dim, 2, dtype=jnp.float32) / axis_dim)
    ang = jnp.concatenate([r.reshape(-1, 1).astype(jnp.float32) * inv_freq,
                           col.reshape(-1, 1).astype(jnp.float32) * inv_freq], -1)
    return jnp.cos(ang), jnp.sin(ang)


def _rotate(x, cos, sin):
    h = x.shape[-1] // 2
    x1, x2 = x[..., :h], x[..., h:]
    return jnp.concatenate([x1 * cos - x2 * sin, x2 * cos + x1 * sin], -1)


def apply_axial_rope(x, cos, sin):
    bshape = (1, x.shape[1]) + (1,) * (x.ndim - 3) + (cos.shape[-1],)
    cos = cos.reshape(bshape).astype(x.dtype)
    sin = sin.reshape(bshape).astype(x.dtype)
    half = DA_HEAD_DIM // 2
    nf = half // 2
    return jnp.concatenate([_rotate(x[..., :half], cos[..., :nf], sin[..., :nf]),
                            _rotate(x[..., half:], cos[..., nf:], sin[..., nf:])], -1)


def s5_discretize(lam_re, lam_im, log_step, b_re, b_im):
    dt = jnp.exp(log_step.astype(jnp.float32))[:, None]
    lr = lam_re.astype(jnp.float32)
    li = lam_im.astype(jnp.float32)
    mag = jnp.exp(lr * dt)
    a_re = mag * jnp.cos(li * dt)
    a_im = mag * jnp.sin(li * dt)
    den = lr * lr + li * li
    n_re = a_re - 1.0
    z_re = (n_re * lr + a_im * li) / den
    z_im = (a_im * lr - n_re * li) / den
    br = b_re.astype(jnp.float32)
    bi = b_im.astype(jnp.float32)
    bb_re = z_re[..., None] * br - z_im[..., None] * bi
    bb_im = z_re[..., None] * bi + z_im[..., None] * br
    return a_re, a_im, bb_re, bb_im


def _complex_affine_combine(e1, e2):
    a1r, a1i, b1r, b1i = e1
    a2r, a2i, b2r, b2i = e2
    return (a1r * a2r - a1i * a2i,
            a1r * a2i + a1i * a2r,
            a2r * b1r - a2i * b1i + b2r,
            a2r * b1i + a2i * b1r + b2i)


def s5_scan(u, a_re, a_im, bb_re, bb_im, h0, reverse):
    bu_re = jnp.einsum('blgc,gpc->blgp', u, bb_re)
    bu_im = jnp.einsum('blgc,gpc->blgp', u, bb_im)
    if h0 is not None:
        h_re, h_im = h0
        edge = -1 if reverse else 0
        bu_re = bu_re.at[:, edge].add(a_re * h_re - a_im * h_im)
        bu_im = bu_im.at[:, edge].add(a_re * h_im + a_im * h_re)
    a_re_t = jnp.broadcast_to(a_re, bu_re.shape)
    a_im_t = jnp.broadcast_to(a_im, bu_re.shape)
    _, _, s_re, s_im = lax.associative_scan(_complex_affine_combine, (a_re_t, a_im_t, bu_re, bu_im),
                                            reverse=reverse, axis=1)
    return s_re, s_im


def s5_readout(s_re, s_im, c_re, c_im):
    return jnp.einsum('blgp,gcp->blgc', s_re, c_re) - jnp.einsum('blgp,gcp->blgc', s_im, c_im)


def s5_branch(a_lat, a_ctx, lam_re, lam_im, log_step, b_re, b_im, c_re, c_im, d_skip, ctx_out):
    def grouped(a):
        return a.astype(jnp.float32).reshape(a.shape[:2] + (S5_GROUPS, S5_GROUP))
    u_lat, u_ctx = grouped(a_lat), grouped(a_ctx)
    d = d_skip.astype(jnp.float32).reshape(S5_GROUPS, S5_GROUP)
    y_lat = d * u_lat
    y_ctx = d * u_ctx
    for direction in range(2):
        reverse = direction == 1
        a_re, a_im, bb_re, bb_im = s5_discretize(lam_re[direction], lam_im[direction],
                                                 log_step[direction], b_re[direction], b_im[direction])
        cr = c_re[direction].astype(jnp.float32)
        ci = c_im[direction].astype(jnp.float32)
        sc_re, sc_im = s5_scan(u_ctx, a_re, a_im, bb_re, bb_im, None, reverse)
        edge = 0 if reverse else -1
        sl_re, sl_im = s5_scan(u_lat, a_re, a_im, bb_re, bb_im, (sc_re[:, edge], sc_im[:, edge]), reverse)
        y_lat = y_lat + s5_readout(sl_re, sl_im, cr, ci)
        if ctx_out:
            y_ctx = y_ctx + s5_readout(sc_re, sc_im, cr, ci)

    def finish(y, like):
        return jax.nn.gelu(y).reshape(like.shape).astype(like.dtype)
    return finish(y_lat, a_lat), (finish(y_ctx, a_ctx) if ctx_out else None)


def chunk_gmlp(z, w_s, b_s):
    bsz, n, _ = z.shape
    u, v = jnp.split(jax.nn.gelu(z), 2, axis=-1)
    v = layer_norm(v).reshape(bsz, n // GM_CHUNK, GM_CHUNK, GM_HEADS, GM_WIDTH // GM_HEADS)
    s = jnp.einsum('hts,bnshc->bnthc', w_s, v) + b_s.T[None, None, :, :, None]
    return u * s.reshape(bsz, n, GM_WIDTH)


def diff_attend(q, k, v, lam):
    s = jnp.einsum('bqhmd,bkhmd->bhmqk', q, k).astype(jnp.float32) * (DA_HEAD_DIM ** -0.5)
    p = jax.nn.softmax(s, axis=-1)
    attn = p[:, :, 0] - lam * p[:, :, 1]
    return jnp.einsum('bhqk,bkhe->bqhe', attn.astype(v.dtype), v)


def diff_attention(q_lat, k_lat, v_lat, q_ctx, k_ctx, v_ctx, da_lam, subln_g, cos, sin, lam_init, ctx_out):
    bsz, n, _ = q_lat.shape

    def qk_heads(t):
        return t.reshape(t.shape[:2] + (DA_HEADS, 2, DA_HEAD_DIM))

    def v_heads(t):
        return t.reshape(t.shape[:2] + (DA_HEADS, 2 * DA_HEAD_DIM))
    q_l = apply_axial_rope(qk_heads(q_lat), cos, sin)
    k_l = apply_axial_rope(qk_heads(k_lat), cos, sin)
    q_c, k_c = qk_heads(q_ctx), qk_heads(k_ctx)
    v_l, v_c = v_heads(v_lat), v_heads(v_ctx)
    lf = da_lam.astype(jnp.float32)
    lam = jnp.exp(jnp.sum(lf[0] * lf[1])) - jnp.exp(jnp.sum(lf[2] * lf[3])) + lam_init
    k_all = jnp.concatenate([k_c, k_l], axis=1)
    v_all = jnp.concatenate([v_c, v_l], axis=1)
    nb = n // Q_BLOCK
    qb = jnp.moveaxis(q_l.reshape(bsz, nb, Q_BLOCK, DA_HEADS, 2, DA_HEAD_DIM), 1, 0)
    o = lax.map(lambda qq: diff_attend(qq, k_all, v_all, lam), qb)
    o_lat = jnp.moveaxis(o, 0, 1).reshape(bsz, n, DA_HEADS, 2 * DA_HEAD_DIM)

    def finish(t):
        return (rms_norm(t, subln_g) * (1.0 - lam_init)).reshape(t.shape[:2] + (DA_V,))
    y_ctx = finish(diff_attend(q_c, k_c, v_c, lam)) if ctx_out else None
    return finish(o_lat), y_ctx


def token_mixer(u_lat, u_ctx, w_in, s5_lam_re, s5_lam_im, s5_log_step, s5_b_re, s5_b_im, s5_c_re, s5_c_im,
                s5_d, gm_w_s, gm_b_s, da_lam, da_subln_g, w_glu_val, w_glu_gate, w_proj_gm, w_proj_da,
                w_out, cos, sin, lam_init, ctx_out):
    p_lat = u_lat @ w_in
    p_ctx = u_ctx @ w_in
    a_lat, z_lat, q_lat, k_lat, v_lat, g_lat = jnp.split(p_lat, IN_SPLITS, axis=-1)
    a_ctx, z_ctx, q_ctx, k_ctx, v_ctx, g_ctx = jnp.split(p_ctx, IN_SPLITS, axis=-1)
    ya_lat, ya_ctx = s5_branch(a_lat, a_ctx, s5_lam_re, s5_lam_im, s5_log_step, s5_b_re, s5_b_im,
                               s5_c_re, s5_c_im, s5_d, ctx_out)
    yc_lat, yc_ctx = diff_attention(q_lat, k_lat, v_lat, q_ctx, k_ctx, v_ctx, da_lam, da_subln_g,
                                    cos, sin, lam_init, ctx_out)

    def merge(y_a, y_b, y_c, gates):
        gate_a, gate_b, gate_c = jnp.split(jax.nn.sigmoid(gates), N_BRANCH, axis=-1)
        branch_a = (y_a @ w_glu_val) * jax.nn.sigmoid(y_a @ w_glu_gate)
        m = gate_a * branch_a + gate_b * (y_b @ w_proj_gm) + gate_c * (y_c @ w_proj_da)
        return m @ w_out
    out_lat = merge(ya_lat, chunk_gmlp(z_lat, gm_w_s, gm_b_s), yc_lat, g_lat)
    out_ctx = merge(ya_ctx, chunk_gmlp(z_ctx, gm_w_s, gm_b_s), yc_ctx, g_ctx) if ctx_out else None
    return out_lat, out_ctx


def swiglu(x, wg, wu, wd):
    return (jax.nn.silu(x @ wg) * (x @ wu)) @ wd


def routed_experts(h, eidx, w, w_gate, w_up, w_down):
    t, d = h.shape
    n = t * TOP_K
    flat_e = eidx.reshape(-1)
    flat_t = jnp.arange(n, dtype=jnp.int32) // TOP_K
    order = jnp.argsort(flat_e)
    se, st, sw = flat_e[order], flat_t[order], w.reshape(-1)[order]
    counts = jnp.bincount(flat_e, length=N_EXPERTS)
    padded = (counts + MOE_BLOCK - 1) // MOE_BLOCK * MOE_BLOCK
    start = jnp.cumsum(counts) - counts
    pend = jnp.cumsum(padded)
    pstart = pend - padded
    dest = pstart[se] + jnp.arange(n, dtype=jnp.int32) - start[se]
    n_blk = -(-n // MOE_BLOCK) + N_EXPERTS
    n_rows = n_blk * MOE_BLOCK
    row_tok = jnp.full((n_rows,), t, jnp.int32).at[dest].set(st)
    row_w = jnp.zeros((n_rows,), h.dtype).at[dest].set(sw)
    blk_e = jnp.minimum(jnp.searchsorted(pend, jnp.arange(n_blk) * MOE_BLOCK, side='right'), N_EXPERTS - 1)
    h_pad = jnp.concatenate([h, jnp.zeros((1, d), h.dtype)], axis=0)

    def block_ffn(args):
        tok, wt, e = args
        return swiglu(h_pad[tok], w_gate[e], w_up[e], w_down[e]) * wt[:, None]
    yb = lax.map(block_ffn, (row_tok.reshape(n_blk, MOE_BLOCK), row_w.reshape(n_blk, MOE_BLOCK), blk_e))
    return jax.ops.segment_sum(yb.reshape(n_rows, d), row_tok, num_segments=t + 1)[:t]


def moe_ffn(h, w_router, b_router, w_exp_gate, w_exp_up, w_exp_down, w_sh_gate, w_sh_up, w_sh_down):
    t = h.shape[0]
    scores = jax.nn.sigmoid((h @ w_router).astype(jnp.float32))
    sel = scores + b_router.astype(jnp.float32)
    group_score = jnp.sum(lax.top_k(sel.reshape(t, N_GROUPS, N_EXPERTS // N_GROUPS), 2)[0], axis=-1)
    _, gidx = lax.top_k(group_score, TOPK_GROUPS)
    gmask = jnp.sum(jax.nn.one_hot(gidx, N_GROUPS, dtype=jnp.float32), axis=1) > 0
    emask = jnp.repeat(gmask, N_EXPERTS // N_GROUPS, axis=1)
    _, eidx = lax.top_k(jnp.where(emask, sel, -jnp.inf), TOP_K)
    wts = jnp.take_along_axis(scores, eidx, axis=1)
    wts = wts / jnp.sum(wts, -1, keepdims=True) * ROUTED_SCALE
    routed = routed_experts(h, eidx, wts.astype(h.dtype), w_exp_gate, w_exp_up, w_exp_down)
    return routed + swiglu(h, w_sh_gate, w_sh_up, w_sh_down)


def setup_inputs(seed: int = 0) -> dict:
    key = jax.random.key(seed)
    ks = iter(jax.random.split(key, 48))
    f32 = jnp.float32

    def nrm(shape, scale):
        return jax.random.normal(next(ks), shape, f32) * scale
    D = D_MODEL
    G, P, C = S5_GROUPS, S5_STATE, S5_GROUP
    return {
        'x': nrm((BATCH, SEQ, D), 1.0),
        'c': nrm((BATCH, D), 1.0),
        'ctx': nrm((BATCH, CTX_LEN, D), 1.0),
        'c_ctx': nrm((D,), 1.0),
        'w_mod': nrm((DEPTH, D, 6 * D), 0.5 * D ** -0.5),
        'b_mod': nrm((DEPTH, 6 * D), 0.02),
        'w_in': nrm((DEPTH, D, IN_WIDTH), D ** -0.5),
        's5_lam_re': -0.5 + nrm((DEPTH, 2, G, P), 0.01),
        's5_lam_im': jnp.pi * jnp.arange(P, dtype=f32) + nrm((DEPTH, 2, G, P), 0.01),
        's5_log_step': jax.random.uniform(next(ks), (DEPTH, 2, G), f32,
                                          math.log(S5_DT_MIN), math.log(S5_DT_MAX)),
        's5_b_re': nrm((DEPTH, 2, G, P, C), (2 * C) ** -0.5),
        's5_b_im': nrm((DEPTH, 2, G, P, C), (2 * C) ** -0.5),
        's5_c_re': nrm((DEPTH, 2, G, C, P), P ** -0.5),
        's5_c_im': nrm((DEPTH, 2, G, C, P), P ** -0.5),
        's5_d': nrm((DEPTH, S5_WIDTH), 1.0),
        'gm_w_s': nrm((DEPTH, GM_HEADS, GM_CHUNK, GM_CHUNK), GM_CHUNK ** -0.5),
        'gm_b_s': 1.0 + nrm((DEPTH, GM_HEADS, GM_CHUNK), 0.01),
        'da_lam': nrm((DEPTH, 4, DA_HEAD_DIM), 0.1),
        'da_subln_g': 1.0 + nrm((DEPTH, 2 * DA_HEAD_DIM), 0.01),
        'w_glu_val': nrm((DEPTH, S5_WIDTH, D), S5_WIDTH ** -0.5),
        'w_glu_gate': nrm((DEPTH, S5_WIDTH, D), S5_WIDTH ** -0.5),
        'w_proj_gm': nrm((DEPTH, GM_WIDTH, D), GM_WIDTH ** -0.5),
        'w_proj_da': nrm((DEPTH, DA_V, D), DA_V ** -0.5),
        'w_out': nrm((DEPTH, D, D), DN_BETA * D ** -0.5),
        'ln1_g': 1.0 + nrm((DEPTH, D), 0.01),
        'ln1_b': nrm((DEPTH, D), 0.01),
        'ln2_g': 1.0 + nrm((DEPTH, D), 0.01),
        'ln2_b': nrm((DEPTH, D), 0.01),
        'w_router': nrm((DEPTH, D, N_EXPERTS), D ** -0.5),
        'b_router': nrm((DEPTH, N_EXPERTS), 0.01),
        'w_exp_gate': nrm((DEPTH, N_EXPERTS, D, EXPERT_HIDDEN), D ** -0.5),
        'w_exp_up': nrm((DEPTH, N_EXPERTS, D, EXPERT_HIDDEN), D ** -0.5),
        'w_exp_down': nrm((DEPTH, N_EXPERTS, EXPERT_HIDDEN, D), DN_BETA * EXPERT_HIDDEN ** -0.5),
        'w_sh_gate': nrm((DEPTH, D, SHARED_HIDDEN), D ** -0.5),
        'w_sh_up': nrm((DEPTH, D, SHARED_HIDDEN), D ** -0.5),
        'w_sh_down': nrm((DEPTH, SHARED_HIDDEN, D), DN_BETA * SHARED_HIDDEN ** -0.5),
    }


def reference(x, c, ctx, c_ctx, w_mod, b_mod, w_in, s5_lam_re, s5_lam_im, s5_log_step, s5_b_re, s5_b_im,
              s5_c_re, s5_c_im, s5_d, gm_w_s, gm_b_s, da_lam, da_subln_g, w_glu_val, w_glu_gate,
              w_proj_gm, w_proj_da, w_out, ln1_g, ln1_b, ln2_g, ln2_b, w_router, b_router,
              w_exp_gate, w_exp_up, w_exp_down, w_sh_gate, w_sh_up, w_sh_down):
    bsz, n, d = x.shape
    cos, sin = axial_rope_tables(n)
    silu_c = jax.nn.silu(c)
    silu_cc = jax.nn.silu(c_ctx)
    h_lat, h_ctx = x, ctx
    for l in range(DEPTH):
        last = l == DEPTH - 1
        mod_lat = (silu_c @ w_mod[l] + b_mod[l])[:, None, :]
        mod_ctx = (silu_cc @ w_mod[l] + b_mod[l])[None, None, :]
        sh1, sc1, g1, sh2, sc2, g2 = jnp.split(mod_lat, 6, axis=-1)
        csh1, csc1, cg1, csh2, csc2, cg2 = jnp.split(mod_ctx, 6, axis=-1)
        lam_init = 0.8 - 0.6 * math.exp(-0.3 * l)
        mix_lat, mix_ctx = token_mixer(
            modulate(layer_norm(h_lat), sh1, sc1), modulate(layer_norm(h_ctx), csh1, csc1),
            w_in[l], s5_lam_re[l], s5_lam_im[l], s5_log_step[l], s5_b_re[l], s5_b_im[l],
            s5_c_re[l], s5_c_im[l], s5_d[l], gm_w_s[l], gm_b_s[l], da_lam[l], da_subln_g[l],
            w_glu_val[l], w_glu_gate[l], w_proj_gm[l], w_proj_da[l], w_out[l],
            cos, sin, lam_init, not last)
        h_lat = layer_norm(DN_ALPHA * h_lat + g1 * mix_lat, ln1_g[l], ln1_b[l])
        f_lat = modulate(layer_norm(h_lat), sh2, sc2).reshape(-1, d)
        moe_args = (w_router[l], b_router[l], w_exp_gate[l], w_exp_up[l], w_exp_down[l],
                    w_sh_gate[l], w_sh_up[l], w_sh_down[l])
        if last:
            y_lat = moe_ffn(f_lat, *moe_args).reshape(bsz, n, d)
        else:
            h_ctx = layer_norm(DN_ALPHA * h_ctx + cg1 * mix_ctx, ln1_g[l], ln1_b[l])
            f_ctx = modulate(layer_norm(h_ctx), csh2, csc2).reshape(-1, d)
            y = moe_ffn(jnp.concatenate([f_lat, f_ctx], axis=0), *moe_args)
            y_lat = y[:bsz * n].reshape(bsz, n, d)
            h_ctx = layer_norm(DN_ALPHA * h_ctx + cg2 * y[bsz * n:].reshape(h_ctx.shape), ln2_g[l], ln2_b[l])
        h_lat = layer_norm(DN_ALPHA * h_lat + g2 * y_lat, ln2_g[l], ln2_b[l])
    return h_lat
```

```python
import contextlib, math
import numpy as np
import concourse.bass as bass
import concourse.mybir as mybir
from concourse.bass_utils import run_bass_kernel_spmd

F32 = mybir.dt.float32
BF16 = mybir.dt.bfloat16
AF = mybir.ActivationFunctionType
ALU = mybir.AluOpType
AX = mybir.AxisListType

P = 128
D = 1024
NCTX = 256
NLAT = 4096
NTOK = NCTX + NLAT
NT = NTOK // P
DEPTH = 2
LN_EPS = 1e-5
DN_ALPHA = (2 * DEPTH) ** 0.25
IN_WIDTH = 5376


class Sched:
    def __init__(self, nc, stack):
        self.nc = nc
        self.stack = stack
        self.eng = {"pe": nc.tensor, "act": nc.scalar, "dve": nc.vector,
                    "pool": nc.gpsimd, "sp": nc.sync}
        self.sem = {}
        self.cnt = {}
        for e in self.eng:
            self.sem[e] = stack.enter_context(nc.semaphore("s_" + e))
            self.cnt[e] = 0
        self.waited = {e: {} for e in self.eng}
        self.res = {}
        self.dsem = {}
        self.free_dsems = {}
        self.dtype_of = {}
        self.ninst = 0

    def _r(self, key):
        r = self.res.get(key)
        if r is None:
            r = self.res[key] = {"w": {}, "r": {}}
        return r

    def _need(self, deps, reads, writes):
        for k in reads:
            for s, v in self._r(k)["w"].items():
                if deps.get(s, 0) < v:
                    deps[s] = v
        for k in writes:
            r = self._r(k)
            for s, v in r["w"].items():
                if deps.get(s, 0) < v:
                    deps[s] = v
            for s, v in r["r"].items():
                if deps.get(s, 0) < v:
                    deps[s] = v

    def _emit_waits(self, e, deps, skip_self=False):
        w = self.waited[e]
        for s, v in deps.items():
            if skip_self and s == e:
                continue
            if w.get(s, 0) >= v:
                continue
            self.eng[e].wait_ge(self.sem[s], v)
            w[s] = v
            self.ninst += 1

    def _record(self, ev, reads, writes):
        s, v = ev
        for k in reads:
            r = self._r(k)["r"]
            if r.get(s, 0) < v:
                r[s] = v
        for k in writes:
            r = self._r(k)
            r["w"] = {s: v}
            r["r"] = {}

    def op(self, e, fn, reads=(), writes=(), pe_chain=False):
        deps = {}
        self._need(deps, reads, writes)
        self._emit_waits(e, deps, skip_self=(e == "pe" and pe_chain))
        ins = fn()
        self.cnt[e] += 1
        ins.then_inc(self.sem[e], 1)
        self._record((e, self.cnt[e]), reads, writes)
        self.ninst += 1
        return ins

    def dma(self, q, out, in_, reads=(), writes=(), semkey=None, **kw):
        if semkey is None:
            semkey = writes[0]
        sname = self.dsem.get(semkey)
        qt = "sw" if q == "pool" else "hw"
        if sname is None:
            pool_ = self.free_dsems.setdefault(qt, [])
            if pool_:
                sname = pool_.pop()
            else:
                sname = "d%d" % (len(self.sem))
                self.sem[sname] = self.stack.enter_context(self.nc.semaphore(sname))
                self.cnt[sname] = 0
                self.dtype_of[sname] = qt
            self.dsem[semkey] = sname
        assert self.dtype_of[sname] == qt, (semkey, sname, qt)
        deps = {}
        self._need(deps, reads, writes)
        self._emit_waits(q, deps)
        ins = self.eng[q].dma_start(out=out, in_=in_, **kw)
        self.cnt[sname] += 16
        ins.then_inc(self.sem[sname], 16)
        self._record((sname, self.cnt[sname]), reads, writes)
        self.ninst += 1
        return ins

    def barrier(self, final=False):
        engs = ["sp"] if final else list(self.eng)
        for e in engs:
            w = self.waited[e]
            for s, v in self.cnt.items():
                if v > 0 and w.get(s, 0) < v:
                    self.eng[e].wait_ge(self.sem[s], v)
                    w[s] = v
                    self.ninst += 1
        if not final:
            self.res = {}
            for sn in self.dsem.values():
                self.free_dsems.setdefault(self.dtype_of[sn], []).append(sn)
            self.dsem = {}


def _rope_tables():
    rows = NLAT // 64
    r, col = np.meshgrid(np.arange(rows), np.arange(64), indexing="ij")
    inv_freq = (10000.0 ** (-np.arange(0, 32, 2, dtype=np.float32) / 32)).astype(np.float32)
    ang = np.concatenate([r.reshape(-1, 1).astype(np.float32) * inv_freq,
                          col.reshape(-1, 1).astype(np.float32) * inv_freq], -1)
    cos = np.cos(ang).astype(np.float32)
    sin = np.sin(ang).astype(np.float32)
    cosT = np.ones((128, NTOK), np.float32)
    sinT = np.zeros((128, NTOK), np.float32)
    for m in range(2):
        for d in range(64):
            f = (d % 16) if d < 32 else 16 + (d % 16)
            sgn = -1.0 if (d % 32) < 16 else 1.0
            cosT[m * 64 + d, NCTX:] = cos[:, f]
            sinT[m * 64 + d, NCTX:] = sgn * sin[:, f]
    return cosT, sinT


def _rope_perm():
    perm = np.zeros(128, np.int64)
    for m in range(2):
        for d in range(64):
            pd = d + 16 if (d % 32) < 16 else d - 16
            perm[m * 64 + d] = m * 64 + pd
    return perm


def _consts():
    c = {}
    c["ident"] = np.eye(128, dtype=np.float32)
    tt = np.arange(128)
    c["mfwd"] = (tt[:, None] <= tt[None, :]).astype(np.float32)
    c["mbwd"] = (tt[:, None] >= tt[None, :]).astype(np.float32)
    sig = np.stack([tt, 127 - tt], 1).astype(np.float32)
    c["sigp"] = sig
    c["sigf"] = np.broadcast_to(np.stack([tt, 127 - tt], 0).astype(np.float32)[None], (128, 2, 128)).copy()
    e8 = np.zeros((128, 8, 128), np.float32)
    for k in range(8):
        e8[k, k, :] = 1.0
    c["e8"] = e8
    sel = np.zeros((128, 64, 128), np.float32)
    for k in range(64):
        sel[k, k, :] = 1.0
    c["sel"] = sel
    return c


class Builder:
    def __init__(self, nlayers=DEPTH, dbg=()):
        self.nlayers = nlayers
        self.dbg = set(dbg)
        self.nc = bass.Bass("TRN2", target_bir_lowering=False)
        self.dbg_out = {}

    def din(self, name, shape, dt=F32):
        return self.nc.dram_tensor(name, list(shape), dt, kind="ExternalInput").ap()

    def dscr(self, name, shape, dt):
        if name in self.dbg:
            t = self.nc.dram_tensor("dbg_" + name, list(shape), dt, kind="ExternalOutput").ap()
            self.dbg_out[name] = "dbg_" + name
            return t
        return self.nc.dram_tensor(name, list(shape), dt, kind="Internal").ap()

    def declare(self):
        L = DEPTH
        shapes = {"x": [NLAT, D], "ctx": [NCTX, D], "c": [D], "c_ctx": [D], "w_mod": [L, D, 6 * D], "b_mod": [L, 6 * D],
                  "w_in": [L, D, IN_WIDTH], "w_qkp": [L, D, 1024], "rope_cos": [128, NTOK], "rope_sin": [128, NTOK]}
        for n, sh in [("s5_lam_re", [L, 2, 16, 64]), ("s5_lam_im", [L, 2, 16, 64]), ("s5_log_step", [L, 2, 16]),
                      ("s5_b_re", [L, 2, 16, 64, 16]), ("s5_b_im", [L, 2, 16, 64, 16]),
                      ("s5_c_re", [L, 2, 16, 16, 64]), ("s5_c_im", [L, 2, 16, 16, 64]), ("s5_d", [L, 256]),
                      ("gm_w_s", [L, 4, 128, 128]), ("gm_b_s", [L, 4, 128]), ("da_lam", [L, 4, 64]),
                      ("da_subln_g", [L, 128]), ("w_glu_val", [L, 256, D]), ("w_glu_gate", [L, 256, D]),
                      ("w_proj_gm", [L, 256, D]), ("w_proj_da", [L, 512, D]), ("w_out", [L, D, D]),
                      ("ln1_g", [L, D]), ("ln1_b", [L, D]), ("ln2_g", [L, D]), ("ln2_b", [L, D]),
                      ("w_router", [L, D, 64]), ("b_router", [L, 64]),
                      ("w_exp_gate", [L, 64, D, 256]), ("w_exp_up", [L, 64, D, 256]), ("w_exp_down", [L, 64, 256, D]),
                      ("w_sh_gate", [L, D, 256]), ("w_sh_up", [L, D, 256]), ("w_sh_down", [L, 256, D])]:
            shapes[n] = sh
        for n, a in _consts().items():
            shapes["k_" + n] = list(a.shape)
        bld = self

        class Lazy(dict):
            def __missing__(self, n):
                self[n] = bld.din(n, shapes[n])
                return self[n]
        self.I = Lazy()
        self.out = self.nc.dram_tensor("out", [NLAT, D], F32, kind="ExternalOutput").ap()
        Sx = {}
        Sx["h"] = self.dscr("h", [NTOK, D], F32)
        Sx["uT"] = self.dscr("uT", [D, NTOK], BF16)
        Sx["aT"] = self.dscr("aT", [256, NTOK], BF16)
        Sx["qT"] = self.dscr("qT", [512, NTOK], BF16)
        Sx["kT"] = self.dscr("kT", [512, NTOK], BF16)
        Sx["v"] = self.dscr("v", [NTOK, 512], BF16)
        Sx["yaT"] = self.dscr("yaT", [256, NTOK], BF16)
        Sx["ybT"] = self.dscr("ybT", [256, NTOK], BF16)
        Sx["ycT"] = self.dscr("ycT", [512, NTOK], BF16)
        Sx["fT"] = self.dscr("fT", [D, NTOK], BF16)
        self.X = Sx

    def sb(self, ph, name, shape, dt=F32):
        return ph.enter_context(self.nc.sbuf_tensor(name + getattr(self, "sfx", ""), list(shape), dt))

    def psum(self, ph, name, shape, dt=F32):
        return ph.enter_context(self.nc.psum_tensor(name + getattr(self, "sfx", ""), list(shape), dt))

    def hsrc(self, l, i):
        if l == 0:
            if i < 2:
                return self.I["ctx"][i * P:(i + 1) * P, :]
            return self.I["x"][(i - 2) * P:(i - 1) * P, :]
        return self.X["h"][i * P:(i + 1) * P, :]

    def build(self):
        nc = self.nc
        self.declare()
        with contextlib.ExitStack() as st:
            self.S = S = Sched(nc, st)
            self.ident = self.sb(st, "ident", [P, P], F32)
            self.identb = self.sb(st, "identb", [P, P], BF16)
            S.dma("sp", self.ident[:], self.I["k_ident"], writes=["ident"])
            S.op("dve", lambda: nc.vector.tensor_copy(out=self.identb[:], in_=self.ident[:]), reads=["ident"], writes=["identb"])
            self.modc = self.sb(st, "modc", [P, 2, 48], F32)
            self.gb = self.sb(st, "gb", [P, 4, D], F32)
            for l in range(self.nlayers):
                self.layer(l)
            S.barrier(final=True)
        return nc

    def layer(self, l):
        S = self.S
        last = (l == DEPTH - 1)
        self.sfx = '_L%d' % l
        self.phase_mod(l)
        S.barrier()
        if 'noproj' not in self.dbg:
            self.phase_proj(l)
            S.barrier()
        if 'nos5' not in self.dbg:
            self.phase_s5(l)
            S.barrier()
        if 'noattn' not in self.dbg:
            self.phase_attn(l)
            S.barrier()
        if 'nomerge' not in self.dbg:
            self.phase_merge(l)
            S.barrier()
        if 'nomoe' not in self.dbg:
            self.phase_moe(l)
            S.barrier()

    def phase_mod(self, l):
        nc, S, I = self.nc, self.S, self.I
        with contextlib.ExitStack() as ph:
            cs = self.sb(ph, "cs", [P, 2, 8])
            scs = self.sb(ph, "scs", [P, 8, 2])
            rep = self.sb(ph, "rep", [P, 2, 8, P])
            bm = self.sb(ph, "bm", [P, 48])
            bmb = self.sb(ph, "bmb", [P, 2, D])
            wm = [self.sb(ph, "wm%d" % i, [P, 8, 512]) for i in range(2)]
            pmod = self.psum(ph, "pmod", [P, 48, 2])
            pg = [self.psum(ph, "pg%d" % i, [P, 512]) for i in range(2)]
            S.dma("sp", cs[:, 0, :], I["c"].rearrange("(k p) -> p k", p=P), writes=["cs0"], allow_slow_non_contiguous=True)
            S.dma("sp", cs[:, 1, :], I["c_ctx"].rearrange("(k p) -> p k", p=P), writes=["cs1"], allow_slow_non_contiguous=True)
            S.dma("sp", bm[:], I["b_mod"][l].rearrange("(j p) -> p j", p=P), writes=["bm"], allow_slow_non_contiguous=True)
            S.dma("sp", bmb[:, 0, :], I["b_mod"][l, 2 * D:3 * D].partition_broadcast(P), writes=["bmb0"])
            S.dma("sp", bmb[:, 1, :], I["b_mod"][l, 5 * D:6 * D].partition_broadcast(P), writes=["bmb1"])
            for s in range(2):
                S.op("act", lambda: nc.scalar.activation(out=scs[:, :, s], in_=cs[:, s, :], func=AF.Silu),
                     reads=["cs%d" % s], writes=[("scs", s)])
                S.op("dve", lambda: nc.vector.tensor_copy(out=rep[:, s, :, :], in_=scs[:, :, s].unsqueeze(2).to_broadcast([P, 8, P])),
                     reads=[("scs", s)], writes=[("rep", s)])
            for c in range(12):
                w = wm[c % 2]
                S.dma("sp", w[:], I["w_mod"][l][:, c * 512:(c + 1) * 512].rearrange("(kt p) n -> p kt n", p=P),
                      writes=[("wm", c % 2)])
                for j in range(4):
                    jj = c * 4 + j
                    for kt in range(8):
                        S.op("pe", lambda: nc.tensor.matmul(pmod[:, jj, :], lhsT=w[:, kt, j * P:(j + 1) * P], rhs=scs[:, kt, :],
                                                            start=(kt == 0), stop=(kt == 7)),
                             reads=[("wm", c % 2), ("scs", 0), ("scs", 1)], writes=["pmod"], pe_chain=True)
                if c in (4, 5, 10, 11):
                    gi = 0 if c < 6 else 1
                    half = c % 2
                    for s in range(2):
                        pgt = pg[s]
                        for kt in range(8):
                            S.op("pe", lambda: nc.tensor.matmul(pgt[:], lhsT=rep[:, s, kt, :], rhs=w[:, kt, :],
                                                                start=(kt == 0), stop=(kt == 7)),
                                 reads=[("wm", c % 2), ("rep", s)], writes=[("pg", s)], pe_chain=True)
                        S.op("dve", lambda: nc.vector.tensor_tensor(out=self.gb[:, 2 * s + gi, half * 512:(half + 1) * 512], in0=pgt[:],
                                                                    in1=bmb[:, gi, half * 512:(half + 1) * 512], op=ALU.add),
                             reads=[("pg", s), "bmb%d" % gi], writes=[("gb", 2 * s + gi, half)])
            for s in range(2):
                S.op("dve", lambda: nc.vector.tensor_tensor(out=self.modc[:, s, :], in0=pmod[:, :, s], in1=bm[:], op=ALU.add),
                     reads=["pmod", "bm"], writes=[("modc", s)])
                for c0 in (8, 32):
                    S.op("dve", lambda: nc.vector.tensor_scalar(out=self.modc[:, s, c0:c0 + 8], in0=self.modc[:, s, c0:c0 + 8], scalar1=1.0, scalar2=None, op0=ALU.add),
                         reads=[("modc", s)], writes=[("modc", s)])
            if "modc" in self.dbg:
                t = self.nc.dram_tensor("dbg_modc%d" % l, [P, 2, 48], F32, kind="ExternalOutput").ap()
                S.dma("sp", t, self.modc[:], reads=[("modc", 0), ("modc", 1)], writes=["dbg_modc"])
                t2 = self.nc.dram_tensor("dbg_gb%d" % l, [P, 4, D], F32, kind="ExternalOutput").ap()
                S.dma("sp", t2, self.gb[:], reads=[("gb", a, b) for a in range(4) for b in range(2)], writes=["dbg_gb"])

    def phase_proj(self, l):
        nc, S, I, X = self.nc, self.S, self.I, self.X
        with contextlib.ExitStack() as ph:
            wsplit = {"a": (0, 256), "zu": (256, 512), "zv": (512, 768), "q": (768, 1280), "k": (1280, 1792), "v": (1792, 2304)}
            W = {}
            for n, (c0, c1) in wsplit.items():
                W[n] = self.sb(ph, "w_" + n, [P, 8, c1 - c0], BF16)
                S.dma("pool", W[n][:], I["w_in"][l][:, c0:c1].rearrange("(kt p) n -> p kt n", p=P), writes=["w_" + n])
            for i, n in enumerate(("qp", "kp")):
                W[n] = self.sb(ph, "w_" + n, [P, 8, 512], BF16)
                S.dma("pool", W[n][:], I["w_qkp"][l][:, i * 512:(i + 1) * 512].rearrange("(kt p) n -> p kt n", p=P), writes=["w_" + n])
            wsn = self.sb(ph, "wsn", [P, 4, P], BF16)
            wsT = self.sb(ph, "wsT", [P, 4, P], BF16)
            bsT = self.sb(ph, "bsT", [P, 2, P], F32)
            S.dma("pool", wsn[:], I["gm_w_s"][l].rearrange("h t s -> t h s"), writes=["wsn"])
            for mt in range(2):
                for hh in range(2):
                    S.dma("sp", bsT[64 * hh:64 * hh + 64, mt, :], I["gm_b_s"][l, 2 * mt + hh, :].partition_broadcast(64),
                          writes=[("bsT", mt, hh)])
            bsT_keys = [("bsT", mt, hh) for mt in range(2) for hh in range(2)]
            tpw = self.psum(ph, "tpw", [P, 4, P], BF16)
            for h in range(4):
                S.op("pe", lambda: nc.tensor.transpose(tpw[:, h, :], wsn[:, h, :], self.identb[:]), reads=["wsn", "identb"], writes=["tpw"], pe_chain=True)
            S.op("dve", lambda: nc.vector.tensor_copy(out=wsT[:], in_=tpw[:]), reads=["tpw"], writes=["wsT"])

            NH = 3
            hx = [self.sb(ph, "hx%d" % i, [P, D]) for i in range(NH)]
            xn = [self.sb(ph, "xn%d" % i, [P, D], BF16) for i in range(2)]
            st6 = self.sb(ph, "st6", [P, 2, 2, 6])
            mv = self.sb(ph, "mv", [P, 2, 2])
            rs = self.sb(ph, "rs", [P, 2, 2])
            uT = [self.sb(ph, "uTb%d" % i, [P, 8, 512], BF16) for i in range(2)]
            cosb = [self.sb(ph, "cos%d" % i, [P, 512]) for i in range(2)]
            sinb = [self.sb(ph, "sin%d" % i, [P, 512]) for i in range(2)]
            aTb = [self.sb(ph, "aTb%d" % i, [P, 2, 512], BF16) for i in range(2)]
            zuT = [self.sb(ph, "zuT%d" % i, [P, 2, 512], BF16) for i in range(2)]
            ybT = [self.sb(ph, "ybT%d" % i, [P, 2, 512], BF16) for i in range(2)]
            qkT = [self.sb(ph, "qkT%d" % i, [P, 8, 512], BF16) for i in range(2)]
            vb = [self.sb(ph, "vb%d" % i, [P, 4, 512], BF16) for i in range(2)]
            zg = [self.sb(ph, "zg%d" % i, [P, 256]) for i in range(2)]
            zvn = [self.sb(ph, "zvn%d" % i, [P, 256], BF16) for i in range(2)]
            zst = self.sb(ph, "zst", [P, 2, 6])
            zmv = self.sb(ph, "zmv", [P, 2, 2])
            zrs = self.sb(ph, "zrs", [P, 2, 2])
            t1 = [self.sb(ph, "t1_%d" % i, [P, 512]) for i in range(2)]
            t2 = [self.sb(ph, "t2_%d" % i, [P, 512]) for i in range(2)]
            tg = [self.sb(ph, "tg%d" % i, [P, 2, P]) for i in range(2)]
            tp = [self.psum(ph, "tp%d" % i, [P, 8, P], BF16) for i in range(2)]
            pa = [self.psum(ph, "pa%d" % i, [P, 512]) for i in range(4)]
            pz = self.psum(ph, "pz", [P, 2, 256])

            chunks = [(0, 2)] + [(2 + 4 * c, 4) for c in range(8)]
            tcount = 0
            pcount = 0

            def emit_ln(ci):
                nonlocal tcount
                t0, ntl = chunks[ci]
                s = 1 if ci == 0 else 0
                Wd = ntl * P
                tok0 = t0 * P
                cb = ci % 2
                ub = uT[cb]
                S.dma("sp", cosb[cb][:, :Wd], I["rope_cos"][:, tok0:tok0 + Wd], writes=[("cos", cb)])
                S.dma("sp", sinb[cb][:, :Wd], I["rope_sin"][:, tok0:tok0 + Wd], writes=[("sin", cb)])
                for ti in range(ntl):
                    i = t0 + ti
                    hb = tcount % NH
                    xb = tcount % 2
                    tcount += 1
                    S.dma("sp", hx[hb][:], self.hsrc(l, i), writes=[("hx", hb)])
                    for hf in range(2):
                        S.op("dve", lambda: nc.vector.bn_stats(out=st6[:, xb, hf, :], in_=hx[hb][:, hf * 512:(hf + 1) * 512]),
                             reads=[("hx", hb)], writes=[("st6", xb, hf)])
                    S.op("dve", lambda: nc.vector.bn_aggr(out=mv[:, xb, :], in_=st6[:, xb, :, :].rearrange("p a b -> p (a b)")),
                         reads=[("st6", xb, 0), ("st6", xb, 1)], writes=[("mv", xb)])
                    S.op("act", lambda: nc.scalar.activation(out=rs[:, xb, 0:1], in_=mv[:, xb, 1:2], func=AF.Sqrt, bias=LN_EPS, scale=1.0),
                         reads=[("mv", xb)], writes=[("rs0", xb)])
                    S.op("dve", lambda: nc.vector.reciprocal(out=rs[:, xb, 1:2], in_=rs[:, xb, 0:1]), reads=[("rs0", xb)], writes=[("rs1", xb)])
                    S.op("dve", lambda: nc.vector.tensor_scalar(out=xn[xb][:], in0=hx[hb][:], scalar1=mv[:, xb, 0:1], scalar2=rs[:, xb, 1:2],
                                                                op0=ALU.subtract, op1=ALU.mult),
                         reads=[("hx", hb), ("mv", xb), ("rs1", xb)], writes=[("xn", xb)])
                    tpb = tp[xb]
                    for kt in range(8):
                        S.op("pe", lambda: nc.tensor.transpose(tpb[:, kt, :], xn[xb][:, kt * P:(kt + 1) * P], self.identb[:]),
                             reads=[("xn", xb), "identb"], writes=[("tp", xb)], pe_chain=True)
                    for kt in range(8):
                        S.op("act", lambda: nc.scalar.activation(out=ub[:, kt, ti * P:(ti + 1) * P], in_=tpb[:, kt, :], func=AF.Identity,
                                                                 scale=self.modc[:, s, 8 + kt:9 + kt], bias=self.modc[:, s, kt:kt + 1]),
                             reads=[("tp", xb), ("modc", s)], writes=[("uT", cb, ti)])
                ukeys_ = [("uT", cb, ti) for ti in range(ntl)]
                S.dma("sp", X["uT"].rearrange("(kt p) t -> p kt t", p=P)[:, :, tok0:tok0 + Wd], ub[:, :, :Wd], reads=ukeys_, writes=[("d_uT", ci % 2)])

            emit_ln(0)
            for ci, (t0, ntl) in enumerate(chunks):
                s = 1 if ci == 0 else 0
                Wd = ntl * P
                tok0 = t0 * P
                cb = ci % 2
                ub = uT[cb]
                ukeys = [("uT", cb, ti) for ti in range(ntl)]
                if ci + 1 < len(chunks):
                    emit_ln(ci + 1)

                def mm_fm(pt, wt, c0):
                    for kt in range(8):
                        S.op("pe", lambda: nc.tensor.matmul(pt[:, :Wd], lhsT=wt[:, kt, c0:c0 + P], rhs=ub[:, kt, :Wd], start=(kt == 0), stop=(kt == 7)),
                             reads=ukeys + [wt_key[id(wt)]], writes=[pkey[id(pt)]], pe_chain=True)

                wt_key = {id(W[n]): "w_" + n for n in W}
                pkey = {id(pa[i]): ("pa", i) for i in range(4)}

                for mt in range(2):
                    pt = pa[pcount % 4]; pcount += 1
                    mm_fm(pt, W["a"], mt * P)
                    S.op("act", lambda: nc.scalar.copy(out=aTb[cb][:, mt, :Wd], in_=pt[:, :Wd]), reads=[pkey[id(pt)]], writes=[("aTb", cb, mt)])
                S.dma("sp", X["aT"].rearrange("(mt p) t -> p mt t", p=P)[:, :, tok0:tok0 + Wd], aTb[cb][:, :, :Wd],
                      reads=[("aTb", cb, 0), ("aTb", cb, 1)], writes=[("d_aT", ci)])
                for mt in range(2):
                    pt = pa[pcount % 4]; pcount += 1
                    mm_fm(pt, W["zu"], mt * P)
                    S.op("act", lambda: nc.scalar.activation(out=zuT[cb][:, mt, :Wd], in_=pt[:, :Wd], func=AF.Gelu_apprx_tanh),
                         reads=[pkey[id(pt)]], writes=[("zuT", cb, mt)])
                for qi, (wn, wpn) in enumerate((("q", "qp"), ("k", "kp"))):
                    for hd in range(4):
                        p0 = pa[pcount % 4]; pcount += 1
                        p1 = pa[pcount % 4]; pcount += 1
                        mm_fm(p0, W[wn], hd * P)
                        mm_fm(p1, W[wpn], hd * P)
                        tb = (qi * 4 + hd) % 2
                        S.op("dve", lambda: nc.vector.tensor_tensor(out=t1[tb][:, :Wd], in0=p0[:, :Wd], in1=cosb[cb][:, :Wd], op=ALU.mult),
                             reads=[pkey[id(p0)], ("cos", cb)], writes=[("t1", tb)])
                        S.op("dve", lambda: nc.vector.tensor_tensor(out=t2[tb][:, :Wd], in0=p1[:, :Wd], in1=sinb[cb][:, :Wd], op=ALU.mult),
                             reads=[pkey[id(p1)], ("sin", cb)], writes=[("t2", tb)])
                        S.op("pool", lambda: nc.gpsimd.tensor_tensor(out=qkT[cb][:, qi * 4 + hd, :Wd], in0=t1[tb][:, :Wd], in1=t2[tb][:, :Wd], op=ALU.add),
                             reads=[("t1", tb), ("t2", tb)], writes=[("qkT", cb, qi * 4 + hd)])
                    dst = X["qT"] if qi == 0 else X["kT"]
                    S.dma("sp", dst.rearrange("(h p) t -> p h t", p=P)[:, :, tok0:tok0 + Wd], qkT[cb][:, qi * 4:qi * 4 + 4, :Wd],
                          reads=[("qkT", cb, qi * 4 + hd) for hd in range(4)], writes=[("d_qk", ci, qi)])
                for ti in range(ntl):
                    i = t0 + ti
                    pt = pa[pcount % 4]; pcount += 1
                    for kt in range(8):
                        S.op("pe", lambda: nc.tensor.matmul(pt[:], lhsT=ub[:, kt, ti * P:(ti + 1) * P], rhs=W["v"][:, kt, :], start=(kt == 0), stop=(kt == 7)),
                             reads=[("uT", cb, ti), "w_v"], writes=[pkey[id(pt)]], pe_chain=True)
                    S.op("act", lambda: nc.scalar.copy(out=vb[cb][:, ti, :], in_=pt[:]), reads=[pkey[id(pt)]], writes=[("vb", cb, ti)])
                    zb = i % 2
                    for kt in range(8):
                        S.op("pe", lambda: nc.tensor.matmul(pz[:, zb, :], lhsT=ub[:, kt, ti * P:(ti + 1) * P], rhs=W["zv"][:, kt, :], start=(kt == 0), stop=(kt == 7)),
                             reads=[("uT", cb, ti), "w_zv"], writes=[("pz", zb)], pe_chain=True)
                    S.op("act", lambda: nc.scalar.activation(out=zg[zb][:], in_=pz[:, zb, :], func=AF.Gelu_apprx_tanh), reads=[("pz", zb)], writes=[("zg", zb)])
                    S.op("dve", lambda: nc.vector.bn_stats(out=zst[:, zb, :], in_=zg[zb][:]), reads=[("zg", zb)], writes=[("zst", zb)])
                    S.op("dve", lambda: nc.vector.bn_aggr(out=zmv[:, zb, :], in_=zst[:, zb, :]), reads=[("zst", zb)], writes=[("zmv", zb)])
                    S.op("act", lambda: nc.scalar.activation(out=zrs[:, zb, 0:1], in_=zmv[:, zb, 1:2], func=AF.Sqrt, bias=LN_EPS, scale=1.0),
                         reads=[("zmv", zb)], writes=[("zrs0", zb)])
                    S.op("dve", lambda: nc.vector.reciprocal(out=zrs[:, zb, 1:2], in_=zrs[:, zb, 0:1]), reads=[("zrs0", zb)], writes=[("zrs1", zb)])
                    S.op("dve", lambda: nc.vector.tensor_scalar(out=zvn[zb][:], in0=zg[zb][:], scalar1=zmv[:, zb, 0:1], scalar2=zrs[:, zb, 1:2],
                                                                op0=ALU.subtract, op1=ALU.mult),
                         reads=[("zg", zb), ("zmv", zb), ("zrs1", zb)], writes=[("zvn", zb)])
                    for h in range(4):
                        S.op("pe", lambda: nc.tensor.matmul(pz[64 * (h % 2):64 * (h % 2) + 64, zb, (h // 2) * P:(h // 2 + 1) * P],
                                                            lhsT=zvn[zb][:, h * 64:(h + 1) * 64], rhs=wsT[:, h, :], start=True, stop=True),
                             reads=[("zvn", zb), "wsT", ("zg", zb)], writes=[("pz", zb)], pe_chain=True)
                    S.op("dve", lambda: nc.vector.tensor_tensor(out=tg[zb][:], in0=pz[:, zb, :].rearrange("p (a b) -> p a b", a=2), in1=bsT[:], op=ALU.add),
                         reads=[("pz", zb)] + bsT_keys, writes=[("tg", zb)])
                    S.op("pool", lambda: nc.gpsimd.tensor_tensor(out=ybT[cb][:, :, ti * P:(ti + 1) * P], in0=tg[zb][:], in1=zuT[cb][:, :, ti * P:(ti + 1) * P], op=ALU.mult),
                         reads=[("tg", zb), ("zuT", cb, 0), ("zuT", cb, 1)], writes=[("ybT", cb, ti)])
                S.dma("sp", X["v"][tok0:tok0 + Wd, :].rearrange("(a p) n -> p a n", p=P), vb[cb][:, :ntl, :],
                      reads=[("vb", cb, ti) for ti in range(ntl)], writes=[("d_v", ci)])
                S.dma("sp", X["ybT"].rearrange("(mt p) t -> p mt t", p=P)[:, :, tok0:tok0 + Wd], ybT[cb][:, :, :Wd],
                      reads=[("ybT", cb, ti) for ti in range(ntl)], writes=[("d_yb", ci)])


    def phase_attn(self, l):
        nc, S, I, X = self.nc, self.S, self.I, self.X
        last = (l == DEPTH - 1)
        lam_init = 0.8 - 0.6 * math.exp(-0.3 * l)
        with contextlib.ExitStack() as ph:
            lamb = self.sb(ph, "lamb", [P, 4, 64])
            lt = self.sb(ph, "lt", [P, 2, 64])
            ls = self.sb(ph, "ls", [P, 4])
            neglam = self.sb(ph, "neglam", [P, 1])
            gsc = self.sb(ph, "gsc", [P, 1])
            onesb = self.sb(ph, "onesb", [P, P], BF16)
            S.dma("sp", lamb[:], I["da_lam"][l].partition_broadcast(P), writes=["lamb"])
            S.dma("sp", gsc[:], I["da_subln_g"][l].rearrange("(p o) -> p o", o=1), writes=["gsc"])
            S.op("dve", lambda: nc.vector.memset(onesb[:], 1.0), writes=["onesb"])
            for a in range(2):
                S.op("dve", lambda: nc.vector.tensor_tensor(out=lt[:, a, :], in0=lamb[:, 2 * a, :], in1=lamb[:, 2 * a + 1, :], op=ALU.mult),
                     reads=["lamb"], writes=[("lt", a)])
                S.op("dve", lambda: nc.vector.tensor_reduce(out=ls[:, a:a + 1], in_=lt[:, a, :], axis=AX.X, op=ALU.add),
                     reads=[("lt", a)], writes=[("ls", a)])
                S.op("act", lambda: nc.scalar.activation(out=ls[:, 2 + a:3 + a], in_=ls[:, a:a + 1], func=AF.Exp), reads=[("ls", a)], writes=[("le", a)])
            S.op("dve", lambda: nc.vector.tensor_tensor(out=neglam[:], in0=ls[:, 3:4], in1=ls[:, 2:3], op=ALU.subtract),
                 reads=[("le", 0), ("le", 1)], writes=["neglam"])
            S.op("dve", lambda: nc.vector.tensor_scalar(out=neglam[:], in0=neglam[:], scalar1=-lam_init, scalar2=None, op0=ALU.add),
                 reads=["neglam"], writes=["neglam"])
            S.op("dve", lambda: nc.vector.tensor_scalar(out=gsc[:], in0=gsc[:], scalar1=(1.0 - lam_init), scalar2=None, op0=ALU.mult),
                 reads=["gsc"], writes=["gsc"])

            kTz = [[self.sb(ph, "kTz%d_%d" % (b, m), [P, NTOK], BF16) for m in range(2)] for b in range(2)]
            vh = [self.sb(ph, "vh%d" % b, [P, NT, P], BF16) for b in range(2)]
            for b in range(2):
                for m in range(2):
                    S.op("pool", lambda: nc.gpsimd.memset(kTz[b][m][:], 0.0), writes=[("kTz", b, m)])
            qt = [self.sb(ph, "qt%d" % i, [P, 512], BF16) for i in range(2)]
            NPT = 6
            pT = [self.sb(ph, "pT%d" % i, [P, 512], BF16) for i in range(NPT)]
            rd = [self.sb(ph, "rd%d" % i, [P, 512]) for i in range(2)]
            o0 = self.sb(ph, "o0", [P, 512])
            o1 = self.sb(ph, "o1", [P, 512])
            oo = self.sb(ph, "oo", [P, 512])
            sq = self.sb(ph, "sq", [P, 512], BF16)
            rt = self.sb(ph, "rt", [P, 512])
            yc = [self.sb(ph, "yc%d" % i, [P, 512], BF16) for i in range(2)]
            NSC = 4
            sc = [self.psum(ph, "sc%d" % i, [P, 512]) for i in range(NSC)]
            acco = [self.psum(ph, "acco%d" % i, [P, 512]) for i in range(2)]
            accd = [self.psum(ph, "accd%d" % i, [P, 512]) for i in range(2)]
            nq = 0
            nstep = 0
            for h in range(4):
                hb = h % 2
                for m in range(2):
                    S.dma("sp", kTz[hb][m][64 * m:64 * m + 64, :], X["kT"][h * P + 64 * m:h * P + 64 * m + 64, :], writes=[("kTz", hb, m)])
                S.dma("sp", vh[hb][:], X["v"][:, h * P:(h + 1) * P].rearrange("(j p) e -> p j e", p=P), writes=[("vh", hb)])
                qchunks = [(NCTX + 512 * c, 512, list(range(NT))) for c in range(8)]
                if not last:
                    qchunks = [(0, NCTX, [0, 1])] + qchunks
                for (tok0, Wq, keys) in qchunks:
                    qb = nq % 2
                    nq += 1
                    S.dma("sp", qt[qb][:, :Wq], X["qT"][h * P:(h + 1) * P, tok0:tok0 + Wq], writes=[("qt", qb)])
                    steps = [(j, m) for j in keys for m in range(2)]
                    LAG = 3

                    def emit_qk(si):
                        j, m = steps[si]
                        g = (nstep + si)
                        S.op("pe", lambda: nc.tensor.matmul(sc[g % NSC][:, :Wq], lhsT=kTz[hb][m][:, j * P:(j + 1) * P], rhs=qt[qb][:, :Wq], start=True, stop=True),
                             reads=[("kTz", hb, m), ("qt", qb)], writes=[("sc", g % NSC)], pe_chain=True)
                        S.op("act", lambda: nc.scalar.activation(out=pT[g % NPT][:, :Wq], in_=sc[g % NSC][:, :Wq], func=AF.Exp, scale=0.125),
                             reads=[("sc", g % NSC)], writes=[("pT", g % NPT)])

                    def emit_pv(si):
                        j, m = steps[si]
                        g = (nstep + si)
                        first = (j == keys[0])
                        lastk = (j == keys[-1])
                        S.op("pe", lambda: nc.tensor.matmul(acco[m][:, :Wq], lhsT=vh[hb][:, j, :], rhs=pT[g % NPT][:, :Wq], start=first, stop=lastk),
                             reads=[("vh", hb), ("pT", g % NPT)], writes=[("acco", m)], pe_chain=True)
                        S.op("pe", lambda: nc.tensor.matmul(accd[m][:, :Wq], lhsT=onesb[:], rhs=pT[g % NPT][:, :Wq], start=first, stop=lastk),
                             reads=["onesb", ("pT", g % NPT)], writes=[("accd", m)], pe_chain=True)

                    for si in range(len(steps) + LAG):
                        if si < len(steps):
                            emit_qk(si)
                        if si - LAG >= 0:
                            emit_pv(si - LAG)
                    nstep += len(steps)
                    for m in range(2):
                        S.op("dve", lambda: nc.vector.reciprocal(out=rd[m][:, :Wq], in_=accd[m][:, :Wq]), reads=[("accd", m)], writes=[("rd", m)])
                    S.op("dve", lambda: nc.vector.tensor_tensor(out=o0[:, :Wq], in0=acco[0][:, :Wq], in1=rd[0][:, :Wq], op=ALU.mult),
                         reads=[("acco", 0), ("rd", 0)], writes=["o0"])
                    S.op("dve", lambda: nc.vector.tensor_tensor(out=o1[:, :Wq], in0=acco[1][:, :Wq], in1=rd[1][:, :Wq], op=ALU.mult),
                         reads=[("acco", 1), ("rd", 1)], writes=["o1"])
                    S.op("dve", lambda: nc.vector.scalar_tensor_tensor(out=oo[:, :Wq], in0=o1[:, :Wq], scalar=neglam[:, 0:1], in1=o0[:, :Wq], op0=ALU.mult, op1=ALU.add),
                         reads=["o0", "o1", "neglam"], writes=["oo"])
                    S.op("pool", lambda: nc.gpsimd.tensor_tensor(out=sq[:, :Wq], in0=oo[:, :Wq], in1=oo[:, :Wq], op=ALU.mult), reads=["oo"], writes=["sq"])
                    g = nstep % NSC
                    S.op("pe", lambda: nc.tensor.matmul(sc[g][:, :Wq], lhsT=onesb[:], rhs=sq[:, :Wq], start=True, stop=True),
                         reads=["onesb", "sq"], writes=[("sc", g)], pe_chain=True)
                    S.op("act", lambda: nc.scalar.activation(out=rt[:, :Wq], in_=sc[g][:, :Wq], func=AF.Sqrt, scale=1.0 / 128, bias=LN_EPS),
                         reads=[("sc", g)], writes=["rt"])
                    nstep += 1
                    S.op("dve", lambda: nc.vector.reciprocal(out=rt[:, :Wq], in_=rt[:, :Wq]), reads=["rt"], writes=["rt"])
                    yb_ = nq % 2
                    S.op("dve", lambda: nc.vector.scalar_tensor_tensor(out=yc[yb_][:, :Wq], in0=oo[:, :Wq], scalar=gsc[:, 0:1], in1=rt[:, :Wq], op0=ALU.mult, op1=ALU.mult),
                         reads=["oo", "gsc", "rt"], writes=[("yc", yb_)])
                    S.dma("sp", X["ycT"][h * P:(h + 1) * P, tok0:tok0 + Wq], yc[yb_][:, :Wq], reads=[("yc", yb_)], writes=[("d_yc", yb_)])

    def phase_s5(self, l):
        nc, S, I, X = self.nc, self.S, self.I, self.X
        TWO_PI = 2.0 * math.pi
        INV2PI = 1.0 / TWO_PI
        MAGIC = 12582912.0
        V = nc.vector

        def dve(fn, reads, writes):
            return S.op("dve", fn, reads=reads, writes=writes)

        def reduce_angle(out, x, tmp, shift, kx, kout, ktmp):
            dve(lambda: V.tensor_scalar(out=tmp, in0=x, scalar1=INV2PI, scalar2=shift * INV2PI + MAGIC, op0=ALU.mult, op1=ALU.add), [kx], [ktmp])
            dve(lambda: V.tensor_scalar(out=tmp, in0=tmp, scalar1=-MAGIC, scalar2=None, op0=ALU.add), [ktmp], [ktmp])
            dve(lambda: V.scalar_tensor_tensor(out=out, in0=tmp, scalar=-TWO_PI, in1=x, op0=ALU.mult, op1=ALU.add), [ktmp, kx], [kout])

        def cossin(cos_o, sin_o, x, tmp, tmp2, kx, kc_, ks_, ktmp, ktmp2):
            reduce_angle(tmp2, x, tmp, 0.0, kx, ktmp2, ktmp)
            S.op("act", lambda: nc.scalar.activation(out=sin_o, in_=tmp2, func=AF.Sin), reads=[ktmp2], writes=[ks_])
            reduce_angle(tmp2, x, tmp, math.pi / 2, kx, ktmp2, ktmp)
            S.op("act", lambda: nc.scalar.activation(out=cos_o, in_=tmp2, func=AF.Sin, bias=halfpi[:, 0:1]), reads=[ktmp2, "halfpi"], writes=[kc_])

        with contextlib.ExitStack() as ph:
            aT_sb = self.sb(ph, "aT_sb", [P, 2, NTOK], BF16)
            yacc = self.sb(ph, "yacc", [P, 2, NTOK], F32)
            dcol = self.sb(ph, "dcol", [P, 2])
            halfpi = self.sb(ph, "halfpi", [P, 1])
            sigp = self.sb(ph, "sigp", [P, 2])
            nsig = self.sb(ph, "nsig", [P, 2])
            sigf = self.sb(ph, "sigf", [P, 2, P])
            Bmat = [self.sb(ph, "Bmat%d" % d, [P, 2, 1024], BF16) for d in range(2)]
            WpreR = [self.sb(ph, "WpreR%d" % d, [P, 2, 512]) for d in range(2)]
            WpreI = [self.sb(ph, "WpreI%d" % d, [P, 2, 512]) for d in range(2)]
            WpostR = [self.sb(ph, "WpostR%d" % d, [P, 8, P]) for d in range(2)]
            WpostI = [self.sb(ph, "WpostI%d" % d, [P, 8, P]) for d in range(2)]
            A128R = [self.sb(ph, "A128R%d" % d, [P, 8]) for d in range(2)]
            A128I = [self.sb(ph, "A128I%d" % d, [P, 8]) for d in range(2)]
            Cm = [self.sb(ph, "Cm%d" % d, [P, 16, P], BF16) for d in range(2)]
            Mdir = [self.sb(ph, "Mdir%d" % d, [P, P], BF16) for d in range(2)]
            E8 = self.sb(ph, "E8", [P, 8, P], BF16)
            cTh = [[self.sb(ph, "cTh%d%d" % (d, k), [P, P], BF16) for k in range(2)] for d in range(2)]
            cTl = [[self.sb(ph, "cTl%d%d" % (d, k), [P, P], BF16) for k in range(2)] for d in range(2)]

            S.dma("sp", aT_sb[:], X["aT"].rearrange("(kc p) t -> p kc t", p=P), writes=["aT_sb"])
            S.dma("sp", dcol[:], I["s5_d"][l].rearrange("(kc p) -> p kc", p=P), writes=["dcol"], allow_slow_non_contiguous=True)
            S.dma("sp", sigp[:], I["k_sigp"], writes=["sigp"])
            S.dma("sp", sigf[:], I["k_sigf"], writes=["sigf"])
            S.dma("pool", Mdir[0][:], I["k_mfwd"], writes=[("Mdir", 0)])
            S.dma("pool", Mdir[1][:], I["k_mbwd"], writes=[("Mdir", 1)])
            S.dma("pool", E8[:], I["k_e8"], writes=["E8"])
            dve(lambda: V.memset(halfpi[:], math.pi / 2), [], ["halfpi"])
            dve(lambda: V.tensor_scalar(out=nsig[:], in0=sigp[:], scalar1=-1.0, scalar2=None, op0=ALU.mult), ["sigp"], ["nsig"])
            for d in range(2):
                for k in range(2):
                    S.op("pool", lambda: nc.gpsimd.memset(cTh[d][k][:], 0.0), writes=[("cTh", d, k)])
                    S.op("pool", lambda: nc.gpsimd.memset(cTl[d][k][:], 0.0), writes=[("cTl", d, k)])
            for kc in range(2):
                dve(lambda: V.tensor_scalar(out=yacc[:, kc, :], in0=aT_sb[:, kc, :], scalar1=dcol[:, kc:kc + 1], scalar2=None, op0=ALU.mult),
                    ["aT_sb", "dcol"], [("yacc", kc, i) for i in range(NT)])

            with contextlib.ExitStack() as su:
                R = {}
                for n in ("LR", "LI", "lrdt", "ang", "mag", "cs", "sn", "tA", "tB", "are", "aim", "zre", "zim", "x", "t3", "t4"):
                    R[n] = self.sb(su, "r_" + n, [P, 1024])
                LS = self.sb(su, "r_LS", [P, 16])
                braw = [self.sb(su, "braw%d" % i, [P, 512]) for i in range(2)]
                CmS = self.sb(su, "CmS", [P, 16, P])
                Cc = {}
                for n in ("LR", "LI", "LS", "lrdt", "ang", "angr", "t", "e", "x", "cs", "sn", "mg"):
                    Cc[n] = self.sb(su, "c_" + n, [P, 8])
                for d in range(2):
                    k_ = lambda n: ("su", n)
                    S.dma("sp", R["LR"][:], I["s5_lam_re"][l, d].rearrange("g p -> (g p)").partition_broadcast(P), writes=[k_("LR")])
                    S.dma("sp", R["LI"][:], I["s5_lam_im"][l, d].rearrange("g p -> (g p)").partition_broadcast(P), writes=[k_("LI")])
                    S.dma("sp", LS[:], I["s5_log_step"][l, d].partition_broadcast(P), writes=[k_("LS")])
                    S.op("act", lambda: nc.scalar.activation(out=LS[:], in_=LS[:], func=AF.Exp), reads=[k_("LS")], writes=[k_("LS")])
                    dtb = LS[:, :].unsqueeze(2).to_broadcast([P, 16, 64])
                    v3 = lambda t: t[:].rearrange("p (g q) -> p g q", g=16)
                    dve(lambda: V.tensor_tensor(out=v3(R["lrdt"]), in0=v3(R["LR"]), in1=dtb, op=ALU.mult), [k_("LR"), k_("LS")], [k_("lrdt")])
                    dve(lambda: V.tensor_tensor(out=v3(R["ang"]), in0=v3(R["LI"]), in1=dtb, op=ALU.mult), [k_("LI"), k_("LS")], [k_("ang")])
                    S.op("act", lambda: nc.scalar.activation(out=R["mag"][:], in_=R["lrdt"][:], func=AF.Exp), reads=[k_("lrdt")], writes=[k_("mag")])
                    reduce_angle(R["tB"][:], R["ang"][:], R["tA"][:], 0.0, k_("ang"), k_("tB"), k_("tA"))
                    dve(lambda: V.tensor_copy(out=R["ang"][:], in_=R["tB"][:]), [k_("tB")], [k_("ang")])
                    cossin(R["cs"][:], R["sn"][:], R["ang"][:], R["tA"][:], R["tB"][:], k_("ang"), k_("cs"), k_("sn"), k_("tA"), k_("tB"))
                    dve(lambda: V.tensor_tensor(out=R["are"][:], in0=R["mag"][:], in1=R["cs"][:], op=ALU.mult), [k_("mag"), k_("cs")], [k_("are")])
                    dve(lambda: V.tensor_tensor(out=R["aim"][:], in0=R["mag"][:], in1=R["sn"][:], op=ALU.mult), [k_("mag"), k_("sn")], [k_("aim")])
                    dve(lambda: V.tensor_tensor(out=R["tA"][:], in0=R["LR"][:], in1=R["LR"][:], op=ALU.mult), [k_("LR")], [k_("tA")])
                    dve(lambda: V.tensor_tensor(out=R["tB"][:], in0=R["LI"][:], in1=R["LI"][:], op=ALU.mult), [k_("LI")], [k_("tB")])
                    dve(lambda: V.tensor_tensor(out=R["cs"][:], in0=R["tA"][:], in1=R["tB"][:], op=ALU.add), [k_("tA"), k_("tB")], [k_("cs")])
                    dve(lambda: V.reciprocal(out=R["cs"][:], in_=R["cs"][:]), [k_("cs")], [k_("cs")])
                    dve(lambda: V.tensor_scalar(out=R["are"][:], in0=R["are"][:], scalar1=-1.0, scalar2=None, op0=ALU.add), [k_("are")], [k_("are")])
                    dve(lambda: V.tensor_tensor(out=R["tA"][:], in0=R["are"][:], in1=R["LR"][:], op=ALU.mult), [k_("are"), k_("LR")], [k_("tA")])
                    dve(lambda: V.tensor_tensor(out=R["tB"][:], in0=R["aim"][:], in1=R["LI"][:], op=ALU.mult), [k_("aim"), k_("LI")], [k_("tB")])
                    dve(lambda: V.tensor_tensor(out=R["tA"][:], in0=R["tA"][:], in1=R["tB"][:], op=ALU.add), [k_("tA"), k_("tB")], [k_("tA")])
                    dve(lambda: V.tensor_tensor(out=R["zre"][:], in0=R["tA"][:], in1=R["cs"][:], op=ALU.mult), [k_("tA"), k_("cs")], [k_("zre")])
                    dve(lambda: V.tensor_tensor(out=R["tA"][:], in0=R["aim"][:], in1=R["LR"][:], op=ALU.mult), [k_("aim"), k_("LR")], [k_("tA")])
                    dve(lambda: V.tensor_tensor(out=R["tB"][:], in0=R["are"][:], in1=R["LI"][:], op=ALU.mult), [k_("are"), k_("LI")], [k_("tB")])
                    dve(lambda: V.tensor_tensor(out=R["tA"][:], in0=R["tA"][:], in1=R["tB"][:], op=ALU.subtract), [k_("tA"), k_("tB")], [k_("tA")])
                    dve(lambda: V.tensor_tensor(out=R["zim"][:], in0=R["tA"][:], in1=R["cs"][:], op=ALU.mult), [k_("tA"), k_("cs")], [k_("zim")])
                    for kc in range(2):
                        hs = slice(kc * 512, (kc + 1) * 512)
                        for ri, src in enumerate((I["s5_b_re"], I["s5_b_im"])):
                            dve(lambda: V.memset(braw[ri][:], 0.0), [], [("braw", ri, gl) for gl in range(8)])
                            for gl in range(8):
                                g = kc * 8 + gl
                                S.dma("sp", braw[ri][16 * gl:16 * gl + 16, gl * 64:(gl + 1) * 64], src[l, d, g].rearrange("p c -> c p"),
                                      reads=[], writes=[("braw", ri, gl)], allow_slow_non_contiguous=True)
                        tA = R["tA"][:, 0:512]; tB = R["tB"][:, 0:512]
                        dve(lambda: V.tensor_tensor(out=tA, in0=braw[0][:], in1=R["zre"][:, hs], op=ALU.mult), [("braw", 0, gl) for gl in range(8)] + [k_("zre")], [k_("tA")])
                        dve(lambda: V.tensor_tensor(out=tB, in0=braw[1][:], in1=R["zim"][:, hs], op=ALU.mult), [("braw", 1, gl) for gl in range(8)] + [k_("zim")], [k_("tB")])
                        dve(lambda: V.tensor_tensor(out=Bmat[d][:, kc, 0:512], in0=tA, in1=tB, op=ALU.subtract), [k_("tA"), k_("tB")], [("Bmat", d, kc, 0)])
                        dve(lambda: V.tensor_tensor(out=tA, in0=braw[0][:], in1=R["zim"][:, hs], op=ALU.mult), [("braw", 0, gl) for gl in range(8)] + [k_("zim")], [k_("tA")])
                        dve(lambda: V.tensor_tensor(out=tB, in0=braw[1][:], in1=R["zre"][:, hs], op=ALU.mult), [("braw", 1, gl) for gl in range(8)] + [k_("zre")], [k_("tB")])
                        dve(lambda: V.tensor_tensor(out=Bmat[d][:, kc, 512:1024], in0=tA, in1=tB, op=ALU.add), [k_("tA"), k_("tB")], [("Bmat", d, kc, 1)])
                        S.op("act", lambda: nc.scalar.activation(out=R["mag"][:, 0:512], in_=R["lrdt"][:, hs], func=AF.Exp, scale=nsig[:, d:d + 1]),
                             reads=[k_("lrdt"), "nsig"], writes=[k_("mag")])
                        dve(lambda: V.tensor_scalar(out=R["x"][:, 0:512], in0=R["ang"][:, hs], scalar1=sigp[:, d:d + 1], scalar2=None, op0=ALU.mult),
                            [k_("ang"), "sigp"], [k_("x")])
                        cossin(R["cs"][:, 0:512], R["sn"][:, 0:512], R["x"][:, 0:512], R["t3"][:, 0:512], R["t4"][:, 0:512],
                               k_("x"), k_("cs"), k_("sn"), k_("t3"), k_("t4"))
                        dve(lambda: V.tensor_tensor(out=WpreR[d][:, kc, :], in0=R["mag"][:, 0:512], in1=R["cs"][:, 0:512], op=ALU.mult),
                            [k_("mag"), k_("cs")], [("WpreR", d, kc)])
                        dve(lambda: V.scalar_tensor_tensor(out=WpreI[d][:, kc, :], in0=R["mag"][:, 0:512], scalar=-1.0, in1=R["sn"][:, 0:512], op0=ALU.mult, op1=ALU.mult),
                            [k_("mag"), k_("sn")], [("WpreI", d, kc)])
                    S.dma("sp", Cc["LR"][:], I["s5_lam_re"][l, d].rearrange("g p -> (g p)").rearrange("(c q) -> q c", q=P), writes=[k_("cLR")], allow_slow_non_contiguous=True)
                    S.dma("sp", Cc["LI"][:], I["s5_lam_im"][l, d].rearrange("g p -> (g p)").rearrange("(c q) -> q c", q=P), writes=[k_("cLI")], allow_slow_non_contiguous=True)
                    for hh in range(2):
                        S.dma("sp", Cc["LS"][64 * hh:64 * hh + 64, :], I["s5_log_step"][l, d].rearrange("(c two) -> two c", two=2)[hh].partition_broadcast(64),
                              writes=[k_("cLS%d" % hh)], allow_slow_non_contiguous=True)
                    S.op("act", lambda: nc.scalar.activation(out=Cc["LS"][:], in_=Cc["LS"][:], func=AF.Exp), reads=[k_("cLS0"), k_("cLS1")], writes=[k_("cdt")])
                    dve(lambda: V.tensor_tensor(out=Cc["lrdt"][:], in0=Cc["LR"][:], in1=Cc["LS"][:], op=ALU.mult), [k_("cLR"), k_("cdt")], [k_("clrdt")])
                    dve(lambda: V.tensor_tensor(out=Cc["ang"][:], in0=Cc["LI"][:], in1=Cc["LS"][:], op=ALU.mult), [k_("cLI"), k_("cdt")], [k_("cang")])
                    reduce_angle(Cc["angr"][:], Cc["ang"][:], Cc["t"][:], 0.0, k_("cang"), k_("cangr"), k_("ct"))
                    E3 = R["tA"][:].rearrange("p (c t) -> p c t", c=8)
                    X3 = R["tB"][:].rearrange("p (c t) -> p c t", c=8)
                    sgb = sigf[:, d, :].unsqueeze(1).to_broadcast([P, 8, P])
                    dve(lambda: V.tensor_tensor(out=E3, in0=Cc["lrdt"][:, :].unsqueeze(2).to_broadcast([P, 8, P]), in1=sgb, op=ALU.mult),
                        [k_("clrdt"), "sigf"], [k_("tA")])
                    S.op("act", lambda: nc.scalar.activation(out=R["mag"][:], in_=R["tA"][:], func=AF.Exp), reads=[k_("tA")], writes=[k_("mag")])
                    dve(lambda: V.tensor_tensor(out=X3, in0=Cc["angr"][:, :].unsqueeze(2).to_broadcast([P, 8, P]), in1=sgb, op=ALU.mult),
                        [k_("cangr"), "sigf"], [k_("tB")])
                    dve(lambda: V.tensor_copy(out=R["x"][:], in_=R["tB"][:]), [k_("tB")], [k_("x")])
                    cossin(R["cs"][:], R["sn"][:], R["x"][:], R["t3"][:], R["t4"][:], k_("x"), k_("cs"), k_("sn"), k_("t3"), k_("t4"))
                    dve(lambda: V.tensor_tensor(out=WpostR[d][:].rearrange("p c t -> p (c t)"), in0=R["mag"][:], in1=R["cs"][:], op=ALU.mult),
                        [k_("mag"), k_("cs")], [("WpostR", d)])
                    dve(lambda: V.tensor_tensor(out=WpostI[d][:].rearrange("p c t -> p (c t)"), in0=R["mag"][:], in1=R["sn"][:], op=ALU.mult),
                        [k_("mag"), k_("sn")], [("WpostI", d)])
                    dve(lambda: V.tensor_scalar(out=Cc["e"][:], in0=Cc["lrdt"][:], scalar1=128.0, scalar2=None, op0=ALU.mult), [k_("clrdt")], [k_("ce")])
                    S.op("act", lambda: nc.scalar.activation(out=Cc["mg"][:], in_=Cc["e"][:], func=AF.Exp), reads=[k_("ce")], writes=[k_("cmg")])
                    dve(lambda: V.tensor_scalar(out=Cc["x"][:], in0=Cc["angr"][:], scalar1=128.0, scalar2=None, op0=ALU.mult), [k_("cangr")], [k_("cx")])
                    cossin(Cc["cs"][:], Cc["sn"][:], Cc["x"][:], Cc["t"][:], Cc["e"][:], k_("cx"), k_("ccs"), k_("csn"), k_("ct"), k_("ce"))
                    dve(lambda: V.tensor_tensor(out=A128R[d][:], in0=Cc["mg"][:], in1=Cc["cs"][:], op=ALU.mult), [k_("cmg"), k_("ccs")], [("A128R", d)])
                    dve(lambda: V.tensor_tensor(out=A128I[d][:], in0=Cc["mg"][:], in1=Cc["sn"][:], op=ALU.mult), [k_("cmg"), k_("csn")], [("A128I", d)])
                    cmk = [("CmS", g, ri) for g in range(16) for ri in range(2)]
                    dve(lambda: V.memset(CmS[:], 0.0), [], cmk)
                    for g in range(16):
                        kc, gl = g // 8, g % 8
                        j, hh = gl // 2, gl % 2
                        for ri, src in enumerate((I["s5_c_re"], I["s5_c_im"])):
                            S.dma("sp", CmS[64 * hh:64 * hh + 64, kc * 8 + ri * 4 + j, 16 * gl:16 * gl + 16], src[l, d, g].rearrange("c p -> p c"),
                                  reads=[], writes=[("CmS", g, ri)], allow_slow_non_contiguous=True)
                    Cm4 = Cm[d][:].rearrange("p (k r j) c -> p k r (j c)", k=2, r=2)
                    CmS4 = CmS[:].rearrange("p (k r j) c -> p k r (j c)", k=2, r=2)
                    dve(lambda: V.tensor_copy(out=Cm4[:, :, 0, :], in_=CmS4[:, :, 0, :]), cmk, [("Cm", d, 0)])
                    dve(lambda: V.tensor_scalar(out=Cm4[:, :, 1, :], in0=CmS4[:, :, 1, :], scalar1=-1.0, scalar2=None, op0=ALU.mult), cmk, [("Cm", d, 1)])
            S.barrier()

            tA = [self.sb(ph, "s5tA%d" % d, [P, 2, 512]) for d in range(2)]
            tB = [self.sb(ph, "s5tB%d" % d, [P, 2, 512]) for d in range(2)]
            Xb = [self.sb(ph, "s5Xb%d" % d, [P, 1024], BF16) for d in range(2)]
            Hb = [self.sb(ph, "s5Hb%d" % d, [P, 8, P], BF16) for d in range(2)]
            tc1 = self.sb(ph, "s5tc1", [P, 2, 4])
            tc2 = self.sb(ph, "s5tc2", [P, 2, 4])
            cn = self.sb(ph, "s5cn", [P, 8])
            bu = [self.psum(ph, "s5bu%d" % d, [P, 2, 512]) for d in range(2)]
            st = self.psum(ph, "s5st", [P, 8, P])
            ctp = self.psum(ph, "s5ctp", [P, 512])
            yo = self.psum(ph, "s5yo", [P, 512])
            orders = [list(range(NT)), [1, 0] + list(range(NT - 1, 1, -1))]
            for step in range(NT):
                for d in range(2):
                    ti = orders[d][step]
                    cols = slice(ti * P, (ti + 1) * P)
                    tl = 127 if d == 0 else 0
                    for kc in range(2):
                        for ri in range(2):
                            S.op("pe", lambda: nc.tensor.matmul(bu[d][:, ri, :], lhsT=aT_sb[:, kc, cols], rhs=Bmat[d][:, kc, ri * 512:(ri + 1) * 512], start=True, stop=True),
                                 reads=["aT_sb"], writes=[("bu", d)], pe_chain=True)
                        dve(lambda: V.tensor_tensor(out=tA[d][:], in0=bu[d][:], in1=WpreR[d][:, kc, :].unsqueeze(1).to_broadcast([P, 2, 512]), op=ALU.mult),
                            [("bu", d)], [("tA", d)])
                        dve(lambda: V.tensor_tensor(out=tB[d][:], in0=bu[d][:], in1=WpreI[d][:, kc, :].unsqueeze(1).to_broadcast([P, 2, 512]), op=ALU.mult),
                            [("bu", d)], [("tB", d)])
                        S.op("pool", lambda: nc.gpsimd.tensor_tensor(out=Xb[d][:, 0:512], in0=tA[d][:, 0, :], in1=tB[d][:, 1, :], op=ALU.subtract),
                             reads=[("tA", d), ("tB", d)], writes=[("Xb", d, 0)])
                        S.op("pool", lambda: nc.gpsimd.tensor_tensor(out=Xb[d][:, 512:1024], in0=tB[d][:, 0, :], in1=tA[d][:, 1, :], op=ALU.add),
                             reads=[("tA", d), ("tB", d)], writes=[("Xb", d, 1)])
                        for jj in range(8):
                            S.op("pe", lambda: nc.tensor.matmul(st[:, jj, :], lhsT=Xb[d][:, jj * P:(jj + 1) * P], rhs=Mdir[d][:], start=True, stop=(step == 0)),
                                 reads=[("Xb", d, jj // 4)], writes=["st"], pe_chain=True)
                            if step > 0:
                                S.op("pe", lambda: nc.tensor.matmul(st[:, jj, :], lhsT=cTh[d][kc][:], rhs=E8[:, jj, :], start=False, stop=False),
                                     reads=[("cTh", d, kc)], writes=["st"], pe_chain=True)
                                S.op("pe", lambda: nc.tensor.matmul(st[:, jj, :], lhsT=cTl[d][kc][:], rhs=E8[:, jj, :], start=False, stop=True),
                                     reads=[("cTl", d, kc)], writes=["st"], pe_chain=True)
                        st3 = st[:].rearrange("p (r j) t -> p r (j t)", r=2)
                        wr = WpostR[d][:, kc * 4:(kc + 1) * 4, :].rearrange("p j t -> p (j t)").unsqueeze(1).to_broadcast([P, 2, 512])
                        wi = WpostI[d][:, kc * 4:(kc + 1) * 4, :].rearrange("p j t -> p (j t)").unsqueeze(1).to_broadcast([P, 2, 512])
                        dve(lambda: V.tensor_tensor(out=tA[d][:], in0=st3, in1=wr, op=ALU.mult), ["st"], [("tA", d)])
                        dve(lambda: V.tensor_tensor(out=tB[d][:], in0=st3, in1=wi, op=ALU.mult), ["st"], [("tB", d)])
                        Hf = Hb[d][:].rearrange("p j t -> p (j t)")
                        S.op("pool", lambda: nc.gpsimd.tensor_tensor(out=Hf[:, 0:512], in0=tA[d][:, 0, :], in1=tB[d][:, 1, :], op=ALU.subtract),
                             reads=[("tA", d), ("tB", d)], writes=[("Hb", d, 0)])
                        S.op("pool", lambda: nc.gpsimd.tensor_tensor(out=Hf[:, 512:1024], in0=tB[d][:, 0, :], in1=tA[d][:, 1, :], op=ALU.add),
                             reads=[("tA", d), ("tB", d)], writes=[("Hb", d, 1)])
                        if step < NT - 1:
                            sl = st[:, :, tl].rearrange("p (r j) -> p r j", r=2)
                            ar = A128R[d][:, kc * 4:(kc + 1) * 4].unsqueeze(1).to_broadcast([P, 2, 4])
                            ai = A128I[d][:, kc * 4:(kc + 1) * 4].unsqueeze(1).to_broadcast([P, 2, 4])
                            dve(lambda: V.tensor_tensor(out=tc1[:], in0=sl, in1=ar, op=ALU.mult), ["st"], ["tc1"])
                            dve(lambda: V.tensor_tensor(out=tc2[:], in0=sl, in1=ai, op=ALU.mult), ["st"], ["tc2"])
                            dve(lambda: V.tensor_tensor(out=cn[:, 0:4], in0=tc1[:, 0, :], in1=tc2[:, 1, :], op=ALU.subtract), ["tc1", "tc2"], ["cn0"])
                            dve(lambda: V.tensor_tensor(out=cn[:, 4:8], in0=tc2[:, 0, :], in1=tc1[:, 1, :], op=ALU.add), ["tc1", "tc2"], ["cn1"])
                            S.op("pe", lambda: nc.tensor.transpose(ctp[0:8, 0:P], cn[:, :], self.ident[:]), reads=["cn0", "cn1", "ident"], writes=["ctp"], pe_chain=True)
                            S.op("act", lambda: nc.scalar.copy(out=cTh[d][kc][0:8, :], in_=ctp[0:8, 0:P]), reads=["ctp"], writes=[("cTh", d, kc)])
                            dve(lambda: V.tensor_tensor(out=cTl[d][kc][0:8, :], in0=ctp[0:8, 0:P], in1=cTh[d][kc][0:8, :], op=ALU.subtract),
                                ["ctp", ("cTh", d, kc)], [("cTl", d, kc)])
                        for jj in range(8):
                            S.op("pe", lambda: nc.tensor.matmul(yo[:, 0:P], lhsT=Cm[d][:, kc * 8 + jj, :], rhs=Hb[d][:, jj, :], start=(jj == 0), stop=(jj == 7)),
                                 reads=[("Hb", d, jj // 4)], writes=["yo"], pe_chain=True)
                        dve(lambda: V.tensor_tensor(out=yacc[:, kc, cols], in0=yo[:, 0:P], in1=yacc[:, kc, cols], op=ALU.add),
                            ["yo", ("yacc", kc, ti)], [("yacc", kc, ti)])
            for kc in range(2):
                S.op("act", lambda: nc.scalar.activation(out=aT_sb[:, kc, :], in_=yacc[:, kc, :], func=AF.Gelu_apprx_tanh),
                     reads=[("yacc", kc, i) for i in range(NT)] + ["aT_sb"], writes=["aT_sb"])
            S.dma("sp", X["yaT"].rearrange("(kc p) t -> p kc t", p=P), aT_sb[:], reads=["aT_sb"], writes=["d_yaT"])

    def ln_stats(self, src, st6, mv, rs, key):
        nc, S = self.nc, self.S
        for hf in range(2):
            S.op("dve", lambda: nc.vector.bn_stats(out=st6[:, hf, :], in_=src[:, hf * 512:(hf + 1) * 512]), reads=[key], writes=[("st6", id(st6), hf)])
        S.op("dve", lambda: nc.vector.bn_aggr(out=mv[:, :], in_=st6[:, :, :].rearrange("p a b -> p (a b)")),
             reads=[("st6", id(st6), 0), ("st6", id(st6), 1)], writes=[("mv", id(mv))])
        S.op("act", lambda: nc.scalar.activation(out=rs[:, 0:1], in_=mv[:, 1:2], func=AF.Sqrt, bias=LN_EPS, scale=1.0),
             reads=[("mv", id(mv))], writes=[("rs0", id(rs))])
        S.op("dve", lambda: nc.vector.reciprocal(out=rs[:, 1:2], in_=rs[:, 0:1]), reads=[("rs0", id(rs))], writes=[("rs1", id(rs))])
        return [("mv", id(mv)), ("rs1", id(rs))]

    def phase_merge(self, l):
        nc, S, I, X = self.nc, self.S, self.I, self.X
        last = (l == DEPTH - 1)
        with contextlib.ExitStack() as ph:
            wG = self.sb(ph, "wG", [P, 8, 3072], BF16)
            for br in range(3):
                S.dma("pool", wG[:, :, br * 1024:(br + 1) * 1024],
                      I["w_in"][l][:, 2304 + br * 1024:2304 + (br + 1) * 1024].rearrange("(kt p) n -> p kt n", p=P), writes=[("wG", br)])
            wsm = {}
            for n, kts in (("w_glu_val", 2), ("w_glu_gate", 2), ("w_proj_gm", 2), ("w_proj_da", 4), ("w_out", 8)):
                wsm[n] = self.sb(ph, "m_" + n, [P, kts, D], BF16)
                S.dma("pool", wsm[n][:], I[n][l].rearrange("(kt p) n -> p kt n", p=P), writes=[n])
            lng = self.sb(ph, "lng", [P, D])
            lnb = self.sb(ph, "lnb", [P, D])
            S.dma("sp", lng[:], I["ln1_g"][l].partition_broadcast(P), writes=["lng"])
            S.dma("sp", lnb[:], I["ln1_b"][l].partition_broadcast(P), writes=["lnb"])
            ya = [self.sb(ph, "g_ya%d" % i, [P, 2, 512], BF16) for i in range(2)]
            yb = [self.sb(ph, "g_yb%d" % i, [P, 2, 512], BF16) for i in range(2)]
            yc = [self.sb(ph, "g_yc%d" % i, [P, 4, 512], BF16) for i in range(2)]
            uu = [self.sb(ph, "g_uu%d" % i, [P, 8, 512], BF16) for i in range(2)]
            mT = [self.sb(ph, "g_mT%d" % i, [P, 8, 512], BF16) for i in range(2)]
            fT = [self.sb(ph, "g_fT%d" % i, [P, 8, 512], BF16) for i in range(1)] * 2
            sg = [self.sb(ph, "g_sg%d" % i, [P, 4, 512]) for i in range(1)] * 2
            tm = [self.sb(ph, "g_tm%d" % i, [P, 4, 512]) for i in range(1)] * 2
            hx = [self.sb(ph, "g_hx%d" % i, [P, D]) for i in range(2)]
            tt_ = [self.sb(ph, "g_tt%d" % i, [P, D]) for i in range(2)]
            xn = [self.sb(ph, "g_xn%d" % i, [P, D], BF16) for i in range(2)]
            st6 = [self.sb(ph, "g_st6%d" % i, [P, 2, 6]) for i in range(2)]
            mv = [self.sb(ph, "g_mv%d" % i, [P, 2]) for i in range(2)]
            rs = [self.sb(ph, "g_rs%d" % i, [P, 2]) for i in range(2)]
            NPB = 5
            pb_ = [self.psum(ph, "g_p%d" % i, [P, 512]) for i in range(NPB)]
            po = self.psum(ph, "g_po", [P, 2, 512])
            tp = self.psum(ph, "g_tp", [P, 8, P], BF16)
            chunks = [(2 + 4 * c, 4) for c in range(8)]
            if not last:
                chunks = [(0, 2)] + chunks
            pc_ = 0
            tcount = 0

            def load(ci):
                t0, ntl = chunks[ci]
                Wd = ntl * P
                tok0 = t0 * P
                cb = ci % 2
                S.dma("sp", ya[cb][:, :, :Wd], X["yaT"].rearrange("(kt p) t -> p kt t", p=P)[:, :, tok0:tok0 + Wd], writes=[("ya", cb)])
                S.dma("sp", yb[cb][:, :, :Wd], X["ybT"].rearrange("(kt p) t -> p kt t", p=P)[:, :, tok0:tok0 + Wd], writes=[("yb", cb)])
                S.dma("sp", yc[cb][:, :, :Wd], X["ycT"].rearrange("(kt p) t -> p kt t", p=P)[:, :, tok0:tok0 + Wd], writes=[("yc", cb)])
                S.dma("sp", uu[cb][:, :, :Wd], X["uT"].rearrange("(kt p) t -> p kt t", p=P)[:, :, tok0:tok0 + Wd], writes=[("uu", cb)])

            def dc_step(ci, dc):
                nonlocal pc_
                t0, ntl = chunks[ci]
                Wd = ntl * P
                cb = ci % 2

                def prod(wn, kts, src, skey):
                    nonlocal pc_
                    pi = pc_ % NPB; pc_ += 1
                    pt = pb_[pi]
                    for kt in range(kts):
                        if wn[0] == "g":
                            br = "ABC".index(wn[1])
                            lhsT = wG[:, kt, br * 1024 + dc * P:br * 1024 + (dc + 1) * P]
                            wkey = ("wG", br)
                        else:
                            lhsT = wsm[wn][:, kt, dc * P:(dc + 1) * P]
                            wkey = wn
                        S.op("pe", lambda: nc.tensor.matmul(pt[:, :Wd], lhsT=lhsT, rhs=src[:, kt, :Wd], start=(kt == 0), stop=(kt == kts - 1)),
                             reads=[wkey, skey], writes=[("gp", pi)], pe_chain=True)
                    return pt, ("gp", pi)

                def sig(pt, pk, sgi):
                    S.op("act", lambda: nc.scalar.activation(out=sg[0][:, sgi, :Wd], in_=pt[:, :Wd], func=AF.Sigmoid), reads=[pk], writes=[("sg", sgi)])

                pt, pk = prod("w_glu_gate", 2, ya[cb], ("ya", cb)); sig(pt, pk, 0)
                pt, pk = prod("gA", 8, uu[cb], ("uu", cb)); sig(pt, pk, 1)
                pt, pk = prod("w_glu_val", 2, ya[cb], ("ya", cb))
                S.op("dve", lambda: nc.vector.tensor_tensor(out=tm[0][:, 0, :Wd], in0=pt[:, :Wd], in1=sg[0][:, 0, :Wd], op=ALU.mult),
                     reads=[pk, ("sg", 0)], writes=[("tm", 0)])
                S.op("pool", lambda: nc.gpsimd.tensor_tensor(out=tm[0][:, 0, :Wd], in0=tm[0][:, 0, :Wd], in1=sg[0][:, 1, :Wd], op=ALU.mult),
                     reads=[("tm", 0), ("sg", 1)], writes=[("tm", 0)])
                pt, pk = prod("gB", 8, uu[cb], ("uu", cb)); sig(pt, pk, 2)
                pt, pk = prod("w_proj_gm", 2, yb[cb], ("yb", cb))
                S.op("dve", lambda: nc.vector.tensor_tensor(out=tm[0][:, 1, :Wd], in0=pt[:, :Wd], in1=sg[0][:, 2, :Wd], op=ALU.mult),
                     reads=[pk, ("sg", 2)], writes=[("tm", 1)])
                pt, pk = prod("gC", 8, uu[cb], ("uu", cb)); sig(pt, pk, 3)
                pt, pk = prod("w_proj_da", 4, yc[cb], ("yc", cb))
                S.op("dve", lambda: nc.vector.tensor_tensor(out=tm[0][:, 2, :Wd], in0=pt[:, :Wd], in1=sg[0][:, 3, :Wd], op=ALU.mult),
                     reads=[pk, ("sg", 3)], writes=[("tm", 2)])
                S.op("pool", lambda: nc.gpsimd.tensor_tensor(out=tm[0][:, 3, :Wd], in0=tm[0][:, 0, :Wd], in1=tm[0][:, 1, :Wd], op=ALU.add),
                     reads=[("tm", 0), ("tm", 1)], writes=[("tm", 3)])
                S.op("pool", lambda: nc.gpsimd.tensor_tensor(out=mT[cb][:, dc, :Wd], in0=tm[0][:, 3, :Wd], in1=tm[0][:, 2, :Wd], op=ALU.add),
                     reads=[("tm", 3), ("tm", 2)], writes=[("mT", cb, dc)])

            def tail_tile(ci, ti):
                nonlocal tcount
                t0, ntl = chunks[ci]
                s = 1 if t0 == 0 else 0
                cb = ci % 2
                i = t0 + ti
                hb = tcount % 2
                tcount += 1
                S.dma("sp", hx[hb][:], self.hsrc(l, i), writes=[("ghx", hb)])
                for nh in range(2):
                    for kt in range(8):
                        S.op("pe", lambda: nc.tensor.matmul(po[:, nh, :], lhsT=mT[cb][:, kt, ti * P:(ti + 1) * P], rhs=wsm["w_out"][:, kt, nh * 512:(nh + 1) * 512],
                                                            start=(kt == 0), stop=(kt == 7)),
                             reads=[("mT", cb, kt), "w_out"], writes=[("po", nh)], pe_chain=True)
                pof = po[:].rearrange("p a b -> p (a b)")
                S.op("dve", lambda: nc.vector.tensor_tensor(out=tt_[hb][:], in0=pof, in1=self.gb[:, 2 * s + 0, :], op=ALU.mult),
                     reads=[("po", 0), ("po", 1)], writes=[("gtt", hb)])
                S.op("dve", lambda: nc.vector.scalar_tensor_tensor(out=hx[hb][:], in0=hx[hb][:], scalar=DN_ALPHA, in1=tt_[hb][:], op0=ALU.mult, op1=ALU.add),
                     reads=[("ghx", hb), ("gtt", hb)], writes=[("ghx", hb)])
                ks = self.ln_stats(hx[hb], st6[hb], mv[hb], rs[hb], ("ghx", hb))
                S.op("dve", lambda: nc.vector.tensor_scalar(out=tt_[hb][:], in0=hx[hb][:], scalar1=mv[hb][:, 0:1], scalar2=rs[hb][:, 1:2], op0=ALU.subtract, op1=ALU.mult),
                     reads=[("ghx", hb)] + ks, writes=[("gtt", hb)])
                S.op("pool", lambda: nc.gpsimd.tensor_tensor(out=tt_[hb][:], in0=tt_[hb][:], in1=lng[:], op=ALU.mult), reads=[("gtt", hb), "lng"], writes=[("gtt", hb)])
                S.op("pool", lambda: nc.gpsimd.tensor_tensor(out=hx[hb][:], in0=tt_[hb][:], in1=lnb[:], op=ALU.add), reads=[("gtt", hb), "lnb"], writes=[("ghx", hb)])
                S.dma("sp", X["h"][i * P:(i + 1) * P, :], hx[hb][:], reads=[("ghx", hb)], writes=[("d_h", hb)])
                ks = self.ln_stats(hx[hb], st6[hb], mv[hb], rs[hb], ("ghx", hb))
                S.op("dve", lambda: nc.vector.tensor_scalar(out=xn[hb][:], in0=hx[hb][:], scalar1=mv[hb][:, 0:1], scalar2=rs[hb][:, 1:2], op0=ALU.subtract, op1=ALU.mult),
                     reads=[("ghx", hb)] + ks, writes=[("gxn", hb)])
                for kt in range(8):
                    S.op("pe", lambda: nc.tensor.transpose(tp[:, kt, :], xn[hb][:, kt * P:(kt + 1) * P], self.identb[:]),
                         reads=[("gxn", hb), "identb"], writes=["gtp"], pe_chain=True)
                for kt in range(8):
                    S.op("act", lambda: nc.scalar.activation(out=fT[0][:, kt, ti * P:(ti + 1) * P], in_=tp[:, kt, :], func=AF.Identity,
                                                             scale=self.modc[:, s, 32 + kt:33 + kt], bias=self.modc[:, s, 24 + kt:25 + kt]),
                         reads=["gtp"], writes=[("gfT", 0, ti)])

            def tail_store(ci):
                t0, ntl = chunks[ci]
                Wd = ntl * P
                tok0 = t0 * P
                S.dma("sp", X["fT"].rearrange("(kt p) t -> p kt t", p=P)[:, :, tok0:tok0 + Wd], fT[0][:, :, :Wd],
                      reads=[("gfT", 0, ti) for ti in range(ntl)], writes=[("d_fT", 0)])

            nch = len(chunks)
            load(0)
            for dc in range(8):
                dc_step(0, dc)
            for ci in range(nch):
                ntl = chunks[ci][1]
                if ci + 1 < nch:
                    load(ci + 1)
                done = 0
                for dc in range(8):
                    if ci + 1 < nch:
                        dc_step(ci + 1, dc)
                    if dc % 2 == 1 and done < ntl:
                        tail_tile(ci, done)
                        done += 1
                while done < ntl:
                    tail_tile(ci, done)
                    done += 1
                tail_store(ci)

    def phase_moe(self, l):
        nc, S, I, X = self.nc, self.S, self.I, self.X
        V = nc.vector
        last = (l == DEPTH - 1)

        def dve(fn, reads, writes):
            return S.op("dve", fn, reads=reads, writes=writes)

        with contextlib.ExitStack() as ph:
            MAXT = 18
            wrt = self.sb(ph, "wrt", [P, 8, 64], BF16)
            S.dma("pool", wrt[:], I["w_router"][l].rearrange("(kt p) n -> p kt n", p=P), writes=["wrt"])
            brb = self.sb(ph, "brb", [P, 64])
            S.dma("sp", brb[:], I["b_router"][l].partition_broadcast(P), writes=["brb"])
            lng = self.sb(ph, "lng2", [P, D])
            lnb = self.sb(ph, "lnb2", [P, D])
            S.dma("sp", lng[:], I["ln2_g"][l].partition_broadcast(P), writes=["lng"])
            S.dma("sp", lnb[:], I["ln2_b"][l].partition_broadcast(P), writes=["lnb"])
            fTb = self.sb(ph, "fTb", [P, 8, MAXT * P], BF16)
            wts = self.sb(ph, "wts", [P, MAXT, 64])
            acc = self.sb(ph, "acc", [P, MAXT, D])
            NW = 3
            wg = [self.sb(ph, "wg%d" % i, [P, 8, 256], BF16) for i in range(NW)]
            wu = [self.sb(ph, "wu%d" % i, [P, 8, 256], BF16) for i in range(NW)]
            wd = [self.sb(ph, "wd%d" % i, [P, 2, D], BF16) for i in range(NW)]
            sgt = [self.sb(ph, "sgt%d" % i, [P, 512]) for i in range(2)]
            hT = [[self.sb(ph, "hT%d%d" % (i, m), [P, 512], BF16) for m in range(2)] for i in range(2)]
            r_sc = self.sb(ph, "r_sc", [P, 64]); r_sel = self.sb(ph, "r_sel", [P, 64]); r_eq = self.sb(ph, "r_eq", [P, 64])
            r_s2 = self.sb(ph, "r_s2", [P, 64]); r_m1 = self.sb(ph, "r_m1", [P, 8]); r_m2 = self.sb(ph, "r_m2", [P, 8])
            r_gs = self.sb(ph, "r_gs", [P, 8]); r_t8 = self.sb(ph, "r_t8", [P, 8]); r_gm = self.sb(ph, "r_gm", [P, 8])
            r_gt = self.sb(ph, "r_gt", [P, 8]); r_sm = self.sb(ph, "r_sm", [P, 64]); r_e8 = self.sb(ph, "r_e8", [P, 8])
            r_em = self.sb(ph, "r_em", [P, 64]); r_w = self.sb(ph, "r_w", [P, 64]); r_ss = self.sb(ph, "r_ss", [P, 2])
            hx = [self.sb(ph, "h_hx%d" % i, [P, D]) for i in range(2)]
            tt_ = [self.sb(ph, "h_tt%d" % i, [P, D]) for i in range(2)]
            st6 = [self.sb(ph, "h_st6%d" % i, [P, 2, 6]) for i in range(2)]
            mv = [self.sb(ph, "h_mv%d" % i, [P, 2]) for i in range(2)]
            rs = [self.sb(ph, "h_rs%d" % i, [P, 2]) for i in range(2)]
            pgu = [self.psum(ph, "h_pgu%d" % i, [P, 512]) for i in range(4)]
            py = [self.psum(ph, "h_py%d" % i, [P, 2, 512]) for i in range(2)]

            if last:
                blocks = [(2, 16), (18, 16)]
            else:
                blocks = [(0, 18), (18, 16)]
            wcount = 0
            ccount = 0
            fcount = 0
            ycount = 0
            for (t0, ntl) in blocks:
                BW = ntl * P
                tok0 = t0 * P
                for kt in range(8):
                    S.dma("sp", fTb[:, kt, :BW], X["fT"][kt * P:(kt + 1) * P, tok0:tok0 + BW], writes=[("fTb", kt)])
                fkeys = [("fTb", kt) for kt in range(8)]
                for ti in range(ntl):
                    cols = slice(ti * P, (ti + 1) * P)
                    prt = pgu[ti % 4]
                    pk = ("pgu", ti % 4)
                    for kt in range(8):
                        S.op("pe", lambda: nc.tensor.matmul(prt[:, 0:64], lhsT=fTb[:, kt, cols], rhs=wrt[:, kt, :], start=(kt == 0), stop=(kt == 7)),
                             reads=[("fTb", kt), "wrt"], writes=[pk], pe_chain=True)
                    S.op("act", lambda: nc.scalar.activation(out=r_sc[:], in_=prt[:, 0:64], func=AF.Sigmoid), reads=[pk], writes=["r_sc"])
                    dve(lambda: V.tensor_tensor(out=r_sel[:], in0=r_sc[:], in1=brb[:], op=ALU.add), ["r_sc", "brb"], ["r_sel"])
                    sel3 = r_sel[:].rearrange("p (g e) -> p g e", g=8)
                    dve(lambda: V.tensor_reduce(out=r_m1[:], in_=sel3, axis=AX.X, op=ALU.max), ["r_sel"], ["r_m1"])
                    dve(lambda: V.tensor_tensor(out=r_eq[:].rearrange("p (g e) -> p g e", g=8), in0=sel3, in1=r_m1[:, :].unsqueeze(2).to_broadcast([P, 8, 8]), op=ALU.is_equal),
                        ["r_sel", "r_m1"], ["r_eq"])
                    dve(lambda: V.scalar_tensor_tensor(out=r_s2[:], in0=r_eq[:], scalar=-4.0, in1=r_sel[:], op0=ALU.mult, op1=ALU.add), ["r_eq", "r_sel"], ["r_s2"])
                    dve(lambda: V.tensor_reduce(out=r_m2[:], in_=r_s2[:].rearrange("p (g e) -> p g e", g=8), axis=AX.X, op=ALU.max), ["r_s2"], ["r_m2"])
                    dve(lambda: V.tensor_tensor(out=r_gs[:], in0=r_m1[:], in1=r_m2[:], op=ALU.add), ["r_m1", "r_m2"], ["r_gs"])
                    dve(lambda: V.max(out=r_t8[:], in_=r_gs[:]), ["r_gs"], ["r_t8"])
                    dve(lambda: V.tensor_scalar(out=r_gm[:], in0=r_gs[:], scalar1=r_t8[:, 3:4], scalar2=None, op0=ALU.is_ge), ["r_gs", "r_t8"], ["r_gm"])
                    dve(lambda: V.tensor_scalar(out=r_gt[:], in0=r_gm[:], scalar1=4.0, scalar2=-4.0, op0=ALU.mult, op1=ALU.add), ["r_gm"], ["r_gt"])
                    sm3 = r_sm[:].rearrange("p (g e) -> p g e", g=8)
                    dve(lambda: V.tensor_tensor(out=sm3, in0=sel3, in1=r_gm[:, :].unsqueeze(2).to_broadcast([P, 8, 8]), op=ALU.mult), ["r_sel", "r_gm"], ["r_sm"])
                    dve(lambda: V.tensor_tensor(out=sm3, in0=sm3, in1=r_gt[:, :].unsqueeze(2).to_broadcast([P, 8, 8]), op=ALU.add), ["r_sm", "r_gt"], ["r_sm"])
                    dve(lambda: V.max(out=r_e8[:], in_=r_sm[:]), ["r_sm"], ["r_e8"])
                    dve(lambda: V.tensor_scalar(out=r_em[:], in0=r_sm[:], scalar1=r_e8[:, 7:8], scalar2=None, op0=ALU.is_ge), ["r_sm", "r_e8"], ["r_em"])
                    dve(lambda: V.tensor_tensor(out=r_w[:], in0=r_sc[:], in1=r_em[:], op=ALU.mult), ["r_sc", "r_em"], ["r_w"])
                    dve(lambda: V.tensor_reduce(out=r_ss[:, 0:1], in_=r_w[:], axis=AX.X, op=ALU.add), ["r_w"], ["r_ss0"])
                    dve(lambda: V.reciprocal(out=r_ss[:, 1:2], in_=r_ss[:, 0:1]), ["r_ss0"], ["r_ss1"])
                    dve(lambda: V.tensor_scalar(out=wts[:, ti, :], in0=r_w[:], scalar1=r_ss[:, 1:2], scalar2=2.5, op0=ALU.mult, op1=ALU.mult), ["r_w", "r_ss1"], [("wts", ti)])
                chunks = [(c * 512, min(512, BW - c * 512)) for c in range((BW + 511) // 512)]
                items = [(e, c0, Wd) for e in range(65) for (c0, Wd) in chunks]

                def wsrc(e):
                    if e < 64:
                        return (I["w_exp_gate"][l, e], I["w_exp_up"][l, e], I["w_exp_down"][l, e])
                    return (I["w_sh_gate"][l], I["w_sh_up"][l], I["w_sh_down"][l])

                def load_w(e):
                    ws = (wbase + e) % NW
                    srcs = wsrc(e)
                    S.dma("pool", wg[ws][:], srcs[0].rearrange("(kt p) n -> p kt n", p=P), writes=[("wg", ws)])
                    S.dma("pool", wu[ws][:], srcs[1].rearrange("(kt p) n -> p kt n", p=P), writes=[("wu", ws)])
                    S.dma("pool", wd[ws][:], srcs[2].rearrange("(kt p) n -> p kt n", p=P), writes=[("wd", ws)])

                def emit_gu(k):
                    nonlocal fcount
                    e, c0, Wd = items[k]
                    ws = (wbase + e) % NW
                    cb = k % 2
                    cs_ = slice(c0, c0 + Wd)
                    if c0 == 0 and e + 1 < 65:
                        load_w(e + 1)
                    for mt in range(2):
                        pg_i = 2 * mt
                        pu_i = 2 * mt + 1
                        for (pi, wt_, wk) in ((pg_i, wg[ws], ("wg", ws)), (pu_i, wu[ws], ("wu", ws))):
                            for kt in range(8):
                                S.op("pe", lambda: nc.tensor.matmul(pgu[pi][:, :Wd], lhsT=wt_[:, kt, mt * P:(mt + 1) * P], rhs=fTb[:, kt, cs_], start=(kt == 0), stop=(kt == 7)),
                                     reads=[wk, ("fTb", kt)], writes=[("pgu", pi)], pe_chain=True)
                        fb = fcount % 2
                        fcount += 1
                        S.op("act", lambda: nc.scalar.activation(out=sgt[fb][:, :Wd], in_=pgu[pg_i][:, :Wd], func=AF.Silu), reads=[("pgu", pg_i)], writes=[("sgt", fb)])
                        dve(lambda: V.tensor_tensor(out=hT[cb][mt][:, :Wd], in0=pgu[pu_i][:, :Wd], in1=sgt[fb][:, :Wd], op=ALU.mult),
                            [("pgu", pu_i), ("sgt", fb)], [("hT", cb, mt)])

                def emit_down(k):
                    nonlocal ycount
                    e, c0, Wd = items[k]
                    ws = (wbase + e) % NW
                    cb = k % 2
                    for tl_ in range(Wd // P):
                        ti = c0 // P + tl_
                        yb_ = ycount % 2
                        path = (ycount // 2) % 2
                        ab = ycount % 2
                        ycount += 1
                        for nh in range(2):
                            for mt in range(2):
                                S.op("pe", lambda: nc.tensor.matmul(py[yb_][:, nh, :], lhsT=hT[cb][mt][:, tl_ * P:(tl_ + 1) * P], rhs=wd[ws][:, mt, nh * 512:(nh + 1) * 512],
                                                                    start=(mt == 0), stop=(mt == 1)),
                                     reads=[("hT", cb, mt), ("wd", ws)], writes=[("py", yb_, nh)], pe_chain=True)
                        pyf = py[yb_][:].rearrange("p a b -> p (a b)")
                        pyk = [("py", yb_, 0), ("py", yb_, 1)]
                        wcol = wts[:, ti, min(e, 63):min(e, 63) + 1]
                        if e == 64:
                            if path == 0:
                                S.op("act", lambda: nc.scalar.copy(out=tt_[ab][:], in_=pyf), reads=pyk, writes=[("htt", ab)])
                                S.op("pool", lambda: nc.gpsimd.tensor_tensor(out=acc[:, ti, :], in0=tt_[ab][:], in1=acc[:, ti, :], op=ALU.add),
                                     reads=[("htt", ab), ("acc", ti)], writes=[("acc", ti)])
                            else:
                                dve(lambda: V.tensor_tensor(out=acc[:, ti, :], in0=pyf, in1=acc[:, ti, :], op=ALU.add), pyk + [("acc", ti)], [("acc", ti)])
                        elif path == 0:
                            dst = acc[:, ti, :] if e == 0 else tt_[ab][:]
                            dkey = ("acc", ti) if e == 0 else ("htt", ab)
                            S.op("act", lambda: nc.scalar.activation(out=dst, in_=pyf, func=AF.Identity, scale=wcol),
                                 reads=pyk + [("wts", ti)], writes=[dkey])
                            if e > 0:
                                S.op("pool", lambda: nc.gpsimd.tensor_tensor(out=acc[:, ti, :], in0=tt_[ab][:], in1=acc[:, ti, :], op=ALU.add),
                                     reads=[("htt", ab), ("acc", ti)], writes=[("acc", ti)])
                        else:
                            dst = acc[:, ti, :] if e == 0 else hx[ab][:]
                            dkey = ("acc", ti) if e == 0 else ("hhx", ab)
                            dve(lambda: V.tensor_tensor(out=dst, in0=pyf, in1=wcol.to_broadcast([P, D]), op=ALU.mult), pyk + [("wts", ti)], [dkey])
                            if e > 0:
                                dve(lambda: V.tensor_tensor(out=acc[:, ti, :], in0=hx[ab][:], in1=acc[:, ti, :], op=ALU.add),
                                    [("hhx", ab), ("acc", ti)], [("acc", ti)])

                wbase = wcount
                wcount += 65
                load_w(0)
                for k in range(len(items) + 1):
                    if k < len(items):
                        emit_gu(k)
                    if k >= 1:
                        emit_down(k - 1)
                for ti in range(ntl):
                    i = t0 + ti
                    s = 1 if i < 2 else 0
                    hb = i % 2
                    S.dma("sp", hx[hb][:], X["h"][i * P:(i + 1) * P, :], writes=[("hhx", hb)])
                    S.op("pool", lambda: nc.gpsimd.tensor_tensor(out=tt_[hb][:], in0=acc[:, ti, :], in1=self.gb[:, 2 * s + 1, :], op=ALU.mult),
                         reads=[("acc", ti)], writes=[("htt", hb)])
                    dve(lambda: V.scalar_tensor_tensor(out=hx[hb][:], in0=hx[hb][:], scalar=DN_ALPHA, in1=tt_[hb][:], op0=ALU.mult, op1=ALU.add),
                        [("hhx", hb), ("htt", hb)], [("hhx", hb)])
                    ks = self.ln_stats(hx[hb], st6[hb], mv[hb], rs[hb], ("hhx", hb))
                    dve(lambda: V.tensor_scalar(out=tt_[hb][:], in0=hx[hb][:], scalar1=mv[hb][:, 0:1], scalar2=rs[hb][:, 1:2], op0=ALU.subtract, op1=ALU.mult),
                        [("hhx", hb)] + ks, [("htt", hb)])
                    S.op("pool", lambda: nc.gpsimd.tensor_tensor(out=tt_[hb][:], in0=tt_[hb][:], in1=lng[:], op=ALU.mult), reads=[("htt", hb), "lng"], writes=[("htt", hb)])
                    S.op("pool", lambda: nc.gpsimd.tensor_tensor(out=hx[hb][:], in0=tt_[hb][:], in1=lnb[:], op=ALU.add), reads=[("htt", hb), "lnb"], writes=[("hhx", hb)])
                    if last:
                        dst = self.out[(i - 2) * P:(i - 1) * P, :]
                    else:
                        dst = X["h"][i * P:(i + 1) * P, :]
                    S.dma("sp", dst, hx[hb][:], reads=[("hhx", hb)], writes=[("d_h2", hb)])

_ROPE = None


def make_in_maps(inputs, ncores=8, used=None):
    global _ROPE
    if _ROPE is None:
        _ROPE = _rope_tables()
    cosT, sinT = _ROPE
    perm = _rope_perm()
    w_in = np.ascontiguousarray(inputs["w_in"], dtype=np.float32)
    qcols = np.concatenate([768 + h * 128 + perm for h in range(4)])
    kcols = np.concatenate([1280 + h * 128 + perm for h in range(4)])
    w_qkp = np.ascontiguousarray(w_in[:, :, np.concatenate([qcols, kcols])])
    shared = {k: np.ascontiguousarray(v, dtype=np.float32) for k, v in inputs.items() if k not in ("x", "c", "ctx")}
    shared["w_qkp"] = w_qkp
    shared["rope_cos"] = cosT
    shared["rope_sin"] = sinT
    for n, a in _consts().items():
        shared["k_" + n] = a
    maps = []
    for core in range(ncores):
        b = core % 4
        m = dict(shared)
        m["x"] = np.ascontiguousarray(inputs["x"][b], dtype=np.float32)
        m["ctx"] = np.ascontiguousarray(inputs["ctx"][b], dtype=np.float32)
        m["c"] = np.ascontiguousarray(inputs["c"][b], dtype=np.float32)
        if used is not None:
            m = {k: v for k, v in m.items() if k in used}
        maps.append(m)
    return maps


def kernel(**inputs):
    bld = Builder()
    nc = bld.build()
    maps = make_in_maps(inputs, used=set(bld.I.keys()))
    res = run_bass_kernel_spmd(nc, maps, core_ids=list(range(8)))
    out = np.stack([res.results[b]["out"] for b in range(4)], 0)
    return out.astype(np.float32)
```

```python
import contextlib, math
import numpy as np
import concourse.bass as bass
import concourse.mybir as mybir
from concourse.bass_utils import run_bass_kernel_spmd

F32 = mybir.dt.float32
BF16 = mybir.dt.bfloat16
AF = mybir.ActivationFunctionType
ALU = mybir.AluOpType
AX = mybir.AxisListType

P = 128
D = 1024
NCTX = 256
NLAT = 4096
NTOK = NCTX + NLAT
NT = NTOK // P
DEPTH = 2
LN_EPS = 1e-5
DN_ALPHA = (2 * DEPTH) ** 0.25
IN_WIDTH = 5376


class Sched:
    def __init__(self, nc, stack):
        self.nc = nc
        self.stack = stack
        self.eng = {"pe": nc.tensor, "act": nc.scalar, "dve": nc.vector,
                    "pool": nc.gpsimd, "sp": nc.sync}
        self.sem = {}
        self.cnt = {}
        for e in self.eng:
            self.sem[e] = stack.enter_context(nc.semaphore("s_" + e))
            self.cnt[e] = 0
        self.waited = {e: {} for e in self.eng}
        self.res = {}
        self.dsem = {}
        self.free_dsems = {}
        self.dtype_of = {}
        self.ninst = 0

    def _r(self, key):
        r = self.res.get(key)
        if r is None:
            r = self.res[key] = {"w": {}, "r": {}}
        return r

    def _need(self, deps, reads, writes):
        for k in reads:
            for s, v in self._r(k)["w"].items():
                if deps.get(s, 0) < v:
                    deps[s] = v
        for k in writes:
            r = self._r(k)
            for s, v in r["w"].items():
                if deps.get(s, 0) < v:
                    deps[s] = v
            for s, v in r["r"].items():
                if deps.get(s, 0) < v:
                    deps[s] = v

    def _emit_waits(self, e, deps, skip_self=False):
        w = self.waited[e]
        for s, v in deps.items():
            if skip_self and s == e:
                continue
            if w.get(s, 0) >= v:
                continue
            self.eng[e].wait_ge(self.sem[s], v)
            w[s] = v
            self.ninst += 1

    def _record(self, ev, reads, writes):
        s, v = ev
        for k in reads:
            r = self._r(k)["r"]
            if r.get(s, 0) < v:
                r[s] = v
        for k in writes:
            r = self._r(k)
            r["w"] = {s: v}
            r["r"] = {}

    def op(self, e, fn, reads=(), writes=(), pe_chain=False):
        deps = {}
        self._need(deps, reads, writes)
        self._emit_waits(e, deps, skip_self=(e == "pe" and pe_chain))
        ins = fn()
        self.cnt[e] += 1
        ins.then_inc(self.sem[e], 1)
        self._record((e, self.cnt[e]), reads, writes)
        self.ninst += 1
        return ins

    def dma(self, q, out, in_, reads=(), writes=(), semkey=None, **kw):
        if semkey is None:
            semkey = writes[0]
        sname = self.dsem.get(semkey)
        qt = "sw" if q == "pool" else "hw"
        if sname is None:
            pool_ = self.free_dsems.setdefault(qt, [])
            if pool_:
                sname = pool_.pop()
            else:
                sname = "d%d" % (len(self.sem))
                self.sem[sname] = self.stack.enter_context(self.nc.semaphore(sname))
                self.cnt[sname] = 0
                self.dtype_of[sname] = qt
            self.dsem[semkey] = sname
        assert self.dtype_of[sname] == qt, (semkey, sname, qt)
        deps = {}
        self._need(deps, reads, writes)
        self._emit_waits(q, deps)
        ins = self.eng[q].dma_start(out=out, in_=in_, **kw)
        self.cnt[sname] += 16
        ins.then_inc(self.sem[sname], 16)
        self._record((sname, self.cnt[sname]), reads, writes)
        self.ninst += 1
        return ins

    def barrier(self, final=False):
        engs = ["sp"] if final else list(self.eng)
        for e in engs:
            w = self.waited[e]
            for s, v in self.cnt.items():
                if v > 0 and w.get(s, 0) < v:
                    self.eng[e].wait_ge(self.sem[s], v)
                    w[s] = v
                    self.ninst += 1
        if not final:
            self.res = {}
            for sn in self.dsem.values():
                self.free_dsems.setdefault(self.dtype_of[sn], []).append(sn)
            self.dsem = {}


def _rope_tables():
    rows = NLAT // 64
    r, col = np.meshgrid(np.arange(rows), np.arange(64), indexing="ij")
    inv_freq = (10000.0 ** (-np.arange(0, 32, 2, dtype=np.float32) / 32)).astype(np.float32)
    ang = np.concatenate([r.reshape(-1, 1).astype(np.float32) * inv_freq,
                          col.reshape(-1, 1).astype(np.float32) * inv_freq], -1)
    cos = np.cos(ang).astype(np.float32)
    sin = np.sin(ang).astype(np.float32)
    cosT = np.ones((128, NTOK), np.float32)
    sinT = np.zeros((128, NTOK), np.float32)
    for m in range(2):
        for d in range(64):
            f = (d % 16) if d < 32 else 16 + (d % 16)
            sgn = -1.0 if (d % 32) < 16 else 1.0
            cosT[m * 64 + d, NCTX:] = cos[:, f]
            sinT[m * 64 + d, NCTX:] = sgn * sin[:, f]
    return cosT, sinT


def _rope_perm():
    perm = np.zeros(128, np.int64)
    for m in range(2):
        for d in range(64):
            pd = d + 16 if (d % 32) < 16 else d - 16
            perm[m * 64 + d] = m * 64 + pd
    return perm


def _consts():
    c = {}
    c["ident"] = np.eye(128, dtype=np.float32)
    tt = np.arange(128)
    c["mfwd"] = (tt[:, None] <= tt[None, :]).astype(np.float32)
    c["mbwd"] = (tt[:, None] >= tt[None, :]).astype(np.float32)
    sig = np.stack([tt, 127 - tt], 1).astype(np.float32)
    c["sigp"] = sig
    c["sigf"] = np.broadcast_to(np.stack([tt, 127 - tt], 0).astype(np.float32)[None], (128, 2, 128)).copy()
    e8 = np.zeros((128, 8, 128), np.float32)
    for k in range(8):
        e8[k, k, :] = 1.0
    c["e8"] = e8
    sel = np.zeros((128, 64, 128), np.float32)
    for k in range(64):
        sel[k, k, :] = 1.0
    c["sel"] = sel
    return c


class Builder:
    def __init__(self, nlayers=DEPTH, dbg=()):
        self.nlayers = nlayers
        self.dbg = set(dbg)
        self.nc = bass.Bass("TRN2", target_bir_lowering=False)
        self.dbg_out = {}

    def din(self, name, shape, dt=F32):
        return self.nc.dram_tensor(name, list(shape), dt, kind="ExternalInput").ap()

    def dscr(self, name, shape, dt):
        if name in self.dbg:
            t = self.nc.dram_tensor("dbg_" + name, list(shape), dt, kind="ExternalOutput").ap()
            self.dbg_out[name] = "dbg_" + name
            return t
        return self.nc.dram_tensor(name, list(shape), dt, kind="Internal").ap()

    def declare(self):
        L = DEPTH
        shapes = {"x": [NLAT, D], "ctx": [NCTX, D], "c": [D], "c_ctx": [D], "w_mod": [L, D, 6 * D], "b_mod": [L, 6 * D],
                  "w_in": [L, D, IN_WIDTH], "w_qkp": [L, D, 1024], "rope_cos": [128, NTOK], "rope_sin": [128, NTOK]}
        for n, sh in [("s5_lam_re", [L, 2, 16, 64]), ("s5_lam_im", [L, 2, 16, 64]), ("s5_log_step", [L, 2, 16]),
                      ("s5_b_re", [L, 2, 16, 64, 16]), ("s5_b_im", [L, 2, 16, 64, 16]),
                      ("s5_c_re", [L, 2, 16, 16, 64]), ("s5_c_im", [L, 2, 16, 16, 64]), ("s5_d", [L, 256]),
                      ("gm_w_s", [L, 4, 128, 128]), ("gm_b_s", [L, 4, 128]), ("da_lam", [L, 4, 64]),
                      ("da_subln_g", [L, 128]), ("w_glu_val", [L, 256, D]), ("w_glu_gate", [L, 256, D]),
                      ("w_proj_gm", [L, 256, D]), ("w_proj_da", [L, 512, D]), ("w_out", [L, D, D]),
                      ("ln1_g", [L, D]), ("ln1_b", [L, D]), ("ln2_g", [L, D]), ("ln2_b", [L, D]),
                      ("w_router", [L, D, 64]), ("b_router", [L, 64]),
                      ("w_exp_gate", [L, 64, D, 256]), ("w_exp_up", [L, 64, D, 256]), ("w_exp_down", [L, 64, 256, D]),
                      ("w_sh_gate", [L, D, 256]), ("w_sh_up", [L, D, 256]), ("w_sh_down", [L, 256, D])]:
            shapes[n] = sh
        for n, a in _consts().items():
            shapes["k_" + n] = list(a.shape)
        bld = self

        class Lazy(dict):
            def __missing__(self, n):
                self[n] = bld.din(n, shapes[n])
                return self[n]
        self.I = Lazy()
        self.out = self.nc.dram_tensor("out", [NLAT, D], F32, kind="ExternalOutput").ap()
        Sx = {}
        Sx["h"] = self.dscr("h", [NTOK, D], F32)
        Sx["uT"] = self.dscr("uT", [D, NTOK], BF16)
        Sx["aT"] = self.dscr("aT", [256, NTOK], BF16)
        Sx["qT"] = self.dscr("qT", [512, NTOK], BF16)
        Sx["kT"] = self.dscr("kT", [512, NTOK], BF16)
        Sx["v"] = self.dscr("v", [NTOK, 512], BF16)
        Sx["yaT"] = self.dscr("yaT", [256, NTOK], BF16)
        Sx["ybT"] = self.dscr("ybT", [256, NTOK], BF16)
        Sx["ycT"] = self.dscr("ycT", [512, NTOK], BF16)
        Sx["fT"] = self.dscr("fT", [D, NTOK], BF16)
        self.X = Sx

    def sb(self, ph, name, shape, dt=F32):
        return ph.enter_context(self.nc.sbuf_tensor(name + getattr(self, "sfx", ""), list(shape), dt))

    def psum(self, ph, name, shape, dt=F32):
        return ph.enter_context(self.nc.psum_tensor(name + getattr(self, "sfx", ""), list(shape), dt))

    def hsrc(self, l, i):
        if l == 0:
            if i < 2:
                return self.I["ctx"][i * P:(i + 1) * P, :]
            return self.I["x"][(i - 2) * P:(i - 1) * P, :]
        return self.X["h"][i * P:(i + 1) * P, :]

    def build(self):
        nc = self.nc
        self.declare()
        with contextlib.ExitStack() as st:
            self.S = S = Sched(nc, st)
            self.ident = self.sb(st, "ident", [P, P], F32)
            self.identb = self.sb(st, "identb", [P, P], BF16)
            S.dma("sp", self.ident[:], self.I["k_ident"], writes=["ident"])
            S.op("dve", lambda: nc.vector.tensor_copy(out=self.identb[:], in_=self.ident[:]), reads=["ident"], writes=["identb"])
            self.modc = self.sb(st, "modc", [P, 2, 48], F32)
            self.gb = self.sb(st, "gb", [P, 4, D], F32)
            for l in range(self.nlayers):
                self.layer(l)
            S.barrier(final=True)
        return nc

    def layer(self, l):
        S = self.S
        last = (l == DEPTH - 1)
        self.sfx = '_L%d' % l
        self.phase_mod(l)
        S.barrier()
        if 'noproj' not in self.dbg:
            self.phase_proj(l)
            S.barrier()
        if 'nos5' not in self.dbg:
            self.phase_s5(l)
            S.barrier()
        if 'noattn' not in self.dbg:
            self.phase_attn(l)
            S.barrier()
        if 'nomerge' not in self.dbg:
            self.phase_merge(l)
            S.barrier()
        if 'nomoe' not in self.dbg:
            self.phase_moe(l)
            S.barrier()

    def phase_mod(self, l):
        nc, S, I = self.nc, self.S, self.I
        with contextlib.ExitStack() as ph:
            cs = self.sb(ph, "cs", [P, 2, 8])
            scs = self.sb(ph, "scs", [P, 8, 2])
            rep = self.sb(ph, "rep", [P, 2, 8, P])
            bm = self.sb(ph, "bm", [P, 48])
            bmb = self.sb(ph, "bmb", [P, 2, D])
            wm = [self.sb(ph, "wm%d" % i, [P, 8, 512]) for i in range(2)]
            pmod = self.psum(ph, "pmod", [P, 48, 2])
            pg = [self.psum(ph, "pg%d" % i, [P, 512]) for i in range(2)]
            S.dma("sp", cs[:, 0, :], I["c"].rearrange("(k p) -> p k", p=P), writes=["cs0"], allow_slow_non_contiguous=True)
            S.dma("sp", cs[:, 1, :], I["c_ctx"].rearrange("(k p) -> p k", p=P), writes=["cs1"], allow_slow_non_contiguous=True)
            S.dma("sp", bm[:], I["b_mod"][l].rearrange("(j p) -> p j", p=P), writes=["bm"], allow_slow_non_contiguous=True)
            S.dma("sp", bmb[:, 0, :], I["b_mod"][l, 2 * D:3 * D].partition_broadcast(P), writes=["bmb0"])
            S.dma("sp", bmb[:, 1, :], I["b_mod"][l, 5 * D:6 * D].partition_broadcast(P), writes=["bmb1"])
            for s in range(2):
                S.op("act", lambda: nc.scalar.activation(out=scs[:, :, s], in_=cs[:, s, :], func=AF.Silu),
                     reads=["cs%d" % s], writes=[("scs", s)])
                S.op("dve", lambda: nc.vector.tensor_copy(out=rep[:, s, :, :], in_=scs[:, :, s].unsqueeze(2).to_broadcast([P, 8, P])),
                     reads=[("scs", s)], writes=[("rep", s)])
            for c in range(12):
                w = wm[c % 2]
                S.dma("sp", w[:], I["w_mod"][l][:, c * 512:(c + 1) * 512].rearrange("(kt p) n -> p kt n", p=P),
                      writes=[("wm", c % 2)])
                for j in range(4):
                    jj = c * 4 + j
                    for kt in range(8):
                        S.op("pe", lambda: nc.tensor.matmul(pmod[:, jj, :], lhsT=w[:, kt, j * P:(j + 1) * P], rhs=scs[:, kt, :],
                                                            start=(kt == 0), stop=(kt == 7)),
                             reads=[("wm", c % 2), ("scs", 0), ("scs", 1)], writes=["pmod"], pe_chain=True)
                if c in (4, 5, 10, 11):
                    gi = 0 if c < 6 else 1
                    half = c % 2
                    for s in range(2):
                        pgt = pg[s]
                        for kt in range(8):
                            S.op("pe", lambda: nc.tensor.matmul(pgt[:], lhsT=rep[:, s, kt, :], rhs=w[:, kt, :],
                                                                start=(kt == 0), stop=(kt == 7)),
                                 reads=[("wm", c % 2), ("rep", s)], writes=[("pg", s)], pe_chain=True)
                        S.op("dve", lambda: nc.vector.tensor_tensor(out=self.gb[:, 2 * s + gi, half * 512:(half + 1) * 512], in0=pgt[:],
                                                                    in1=bmb[:, gi, half * 512:(half + 1) * 512], op=ALU.add),
                             reads=[("pg", s), "bmb%d" % gi], writes=[("gb", 2 * s + gi, half)])
            for s in range(2):
                S.op("dve", lambda: nc.vector.tensor_tensor(out=self.modc[:, s, :], in0=pmod[:, :, s], in1=bm[:], op=ALU.add),
                     reads=["pmod", "bm"], writes=[("modc", s)])
                for c0 in (8, 32):
                    S.op("dve", lambda: nc.vector.tensor_scalar(out=self.modc[:, s, c0:c0 + 8], in0=self.modc[:, s, c0:c0 + 8], scalar1=1.0, scalar2=None, op0=ALU.add),
                         reads=[("modc", s)], writes=[("modc", s)])
            if "modc" in self.dbg:
                t = self.nc.dram_tensor("dbg_modc%d" % l, [P, 2, 48], F32, kind="ExternalOutput").ap()
                S.dma("sp", t, self.modc[:], reads=[("modc", 0), ("modc", 1)], writes=["dbg_modc"])
                t2 = self.nc.dram_tensor("dbg_gb%d" % l, [P, 4, D], F32, kind="ExternalOutput").ap()
                S.dma("sp", t2, self.gb[:], reads=[("gb", a, b) for a in range(4) for b in range(2)], writes=["dbg_gb"])

    def phase_proj(self, l):
        nc, S, I, X = self.nc, self.S, self.I, self.X
        with contextlib.ExitStack() as ph:
            wsplit = {"a": (0, 256), "zu": (256, 512), "zv": (512, 768), "q": (768, 1280), "k": (1280, 1792), "v": (1792, 2304)}
            W = {}
            for n, (c0, c1) in wsplit.items():
                W[n] = self.sb(ph, "w_" + n, [P, 8, c1 - c0], BF16)
                S.dma("pool", W[n][:], I["w_in"][l][:, c0:c1].rearrange("(kt p) n -> p kt n", p=P), writes=["w_" + n])
            for i, n in enumerate(("qp", "kp")):
                W[n] = self.sb(ph, "w_" + n, [P, 8, 512], BF16)
                S.dma("pool", W[n][:], I["w_qkp"][l][:, i * 512:(i + 1) * 512].rearrange("(kt p) n -> p kt n", p=P), writes=["w_" + n])
            wsn = self.sb(ph, "wsn", [P, 4, P], BF16)
            wsT = self.sb(ph, "wsT", [P, 4, P], BF16)
            bsT = self.sb(ph, "bsT", [P, 2, P], F32)
            S.dma("pool", wsn[:], I["gm_w_s"][l].rearrange("h t s -> t h s"), writes=["wsn"])
            for mt in range(2):
                for hh in range(2):
                    S.dma("sp", bsT[64 * hh:64 * hh + 64, mt, :], I["gm_b_s"][l, 2 * mt + hh, :].partition_broadcast(64),
                          writes=[("bsT", mt, hh)])
            bsT_keys = [("bsT", mt, hh) for mt in range(2) for hh in range(2)]

            NH = 3
            hx = [self.sb(ph, "hx%d" % i, [P, D]) for i in range(NH)]
            xn = [self.sb(ph, "xn%d" % i, [P, D], BF16) for i in range(2)]
            st6 = self.sb(ph, "st6", [P, 2, 2, 6])
            mv = self.sb(ph, "mv", [P, 2, 2])
            rs = self.sb(ph, "rs", [P, 2, 2])
            uT = [self.sb(ph, "uTb%d" % i, [P, 8, 512], BF16) for i in range(2)]
            cosb = [self.sb(ph, "cos%d" % i, [P, 512]) for i in range(2)]
            sinb = [self.sb(ph, "sin%d" % i, [P, 512]) for i in range(2)]
            aTb = [self.sb(ph, "aTb%d" % i, [P, 2, 512], BF16) for i in range(2)]
            zuT = [self.sb(ph, "zuT%d" % i, [P, 2, 512], BF16) for i in range(2)]
            ybT = [self.sb(ph, "ybT%d" % i, [P, 2, 512], BF16) for i in range(2)]
            qkT = [self.sb(ph, "qkT%d" % i, [P, 8, 512], BF16) for i in range(2)]
            vb = [self.sb(ph, "vb%d" % i, [P, 4, 512], BF16) for i in range(2)]
            zg = [self.sb(ph, "zg%d" % i, [P, 256]) for i in range(2)]
            zvn = [self.sb(ph, "zvn%d" % i, [P, 256], BF16) for i in range(2)]
            zst = self.sb(ph, "zst", [P, 2, 6])
            zmv = self.sb(ph, "zmv", [P, 2, 2])
            zrs = self.sb(ph, "zrs", [P, 2, 2])
            t1 = [self.sb(ph, "t1_%d" % i, [P, 512]) for i in range(2)]
            t2 = [self.sb(ph, "t2_%d" % i, [P, 512]) for i in range(2)]
            tg = [self.sb(ph, "tg%d" % i, [P, 2, P]) for i in range(2)]
            tp = [self.psum(ph, "tp%d" % i, [P, 8, P], BF16) for i in range(2)]
            for h in range(4):
                S.op("pe", lambda: nc.tensor.transpose(tp[0][:, h, :], wsn[:, h, :], self.identb[:]), reads=["wsn", "identb"], writes=[("tp", 0)], pe_chain=True)
            S.op("dve", lambda: nc.vector.tensor_copy(out=wsT[:], in_=tp[0][:, 0:4, :]), reads=[("tp", 0)], writes=["wsT"])
            pa = [self.psum(ph, "pa%d" % i, [P, 512]) for i in range(4)]
            pz_ = [self.psum(ph, "pz%d" % i, [P, 256]) for i in range(2)]

            chunks = [(0, 2)] + [(2 + 4 * c, 4) for c in range(8)]
            tcount = 0
            pcount = 0

            def emit_ln(ci):
                nonlocal tcount
                t0, ntl = chunks[ci]
                s = 1 if ci == 0 else 0
                Wd = ntl * P
                tok0 = t0 * P
                cb = ci % 2
                ub = uT[cb]
                S.dma("sp", cosb[cb][:, :Wd], I["rope_cos"][:, tok0:tok0 + Wd], writes=[("cos", cb)])
                S.dma("sp", sinb[cb][:, :Wd], I["rope_sin"][:, tok0:tok0 + Wd], writes=[("sin", cb)])
                for ti in range(ntl):
                    i = t0 + ti
                    hb = tcount % NH
                    xb = tcount % 2
                    tcount += 1
                    S.dma("sp", hx[hb][:], self.hsrc(l, i), writes=[("hx", hb)])
                    for hf in range(2):
                        S.op("dve", lambda: nc.vector.bn_stats(out=st6[:, xb, hf, :], in_=hx[hb][:, hf * 512:(hf + 1) * 512]),
                             reads=[("hx", hb)], writes=[("st6", xb, hf)])
                    S.op("dve", lambda: nc.vector.bn_aggr(out=mv[:, xb, :], in_=st6[:, xb, :, :].rearrange("p a b -> p (a b)")),
                         reads=[("st6", xb, 0), ("st6", xb, 1)], writes=[("mv", xb)])
                    S.op("act", lambda: nc.scalar.activation(out=rs[:, xb, 0:1], in_=mv[:, xb, 1:2], func=AF.Sqrt, bias=LN_EPS, scale=1.0),
                         reads=[("mv", xb)], writes=[("rs0", xb)])
                    S.op("dve", lambda: nc.vector.reciprocal(out=rs[:, xb, 1:2], in_=rs[:, xb, 0:1]), reads=[("rs0", xb)], writes=[("rs1", xb)])
                    S.op("dve", lambda: nc.vector.tensor_scalar(out=xn[xb][:], in0=hx[hb][:], scalar1=mv[:, xb, 0:1], scalar2=rs[:, xb, 1:2],
                                                                op0=ALU.subtract, op1=ALU.mult),
                         reads=[("hx", hb), ("mv", xb), ("rs1", xb)], writes=[("xn", xb)])
                    tpb = tp[xb]
                    for kt in range(8):
                        S.op("pe", lambda: nc.tensor.transpose(tpb[:, kt, :], xn[xb][:, kt * P:(kt + 1) * P], self.identb[:]),
                             reads=[("xn", xb), "identb"], writes=[("tp", xb)], pe_chain=True)
                    for kt in range(8):
                        S.op("act", lambda: nc.scalar.activation(out=ub[:, kt, ti * P:(ti + 1) * P], in_=tpb[:, kt, :], func=AF.Identity,
                                                                 scale=self.modc[:, s, 8 + kt:9 + kt], bias=self.modc[:, s, kt:kt + 1]),
                             reads=[("tp", xb), ("modc", s)], writes=[("uT", cb, ti)])
                ukeys_ = [("uT", cb, ti) for ti in range(ntl)]
                S.dma("sp", X["uT"].rearrange("(kt p) t -> p kt t", p=P)[:, :, tok0:tok0 + Wd], ub[:, :, :Wd], reads=ukeys_, writes=[("d_uT", ci % 2)])

            emit_ln(0)
            for ci, (t0, ntl) in enumerate(chunks):
                s = 1 if ci == 0 else 0
                Wd = ntl * P
                tok0 = t0 * P
                cb = ci % 2
                ub = uT[cb]
                ukeys = [("uT", cb, ti) for ti in range(ntl)]
                if ci + 1 < len(chunks):
                    emit_ln(ci + 1)

                def mm_fm(pt, wt, c0):
                    for kt in range(8):
                        S.op("pe", lambda: nc.tensor.matmul(pt[:, :Wd], lhsT=wt[:, kt, c0:c0 + P], rhs=ub[:, kt, :Wd], start=(kt == 0), stop=(kt == 7)),
                             reads=ukeys + [wt_key[id(wt)]], writes=[pkey[id(pt)]], pe_chain=True)

                wt_key = {id(W[n]): "w_" + n for n in W}
                pkey = {id(pa[i]): ("pa", i) for i in range(4)}

                for mt in range(2):
                    pt = pa[pcount % 4]; pcount += 1
                    mm_fm(pt, W["a"], mt * P)
                    S.op("act", lambda: nc.scalar.copy(out=aTb[cb][:, mt, :Wd], in_=pt[:, :Wd]), reads=[pkey[id(pt)]], writes=[("aTb", cb, mt)])
                S.dma("sp", X["aT"].rearrange("(mt p) t -> p mt t", p=P)[:, :, tok0:tok0 + Wd], aTb[cb][:, :, :Wd],
                      reads=[("aTb", cb, 0), ("aTb", cb, 1)], writes=[("d_aT", ci)])
                for mt in range(2):
                    pt = pa[pcount % 4]; pcount += 1
                    mm_fm(pt, W["zu"], mt * P)
                    S.op("act", lambda: nc.scalar.activation(out=zuT[cb][:, mt, :Wd], in_=pt[:, :Wd], func=AF.Gelu_apprx_tanh),
                         reads=[pkey[id(pt)]], writes=[("zuT", cb, mt)])
                for qi, (wn, wpn) in enumerate((("q", "qp"), ("k", "kp"))):
                    for hd in range(4):
                        p0 = pa[pcount % 4]; pcount += 1
                        p1 = pa[pcount % 4]; pcount += 1
                        mm_fm(p0, W[wn], hd * P)
                        mm_fm(p1, W[wpn], hd * P)
                        tb = (qi * 4 + hd) % 2
                        S.op("dve", lambda: nc.vector.tensor_tensor(out=t1[tb][:, :Wd], in0=p0[:, :Wd], in1=cosb[cb][:, :Wd], op=ALU.mult),
                             reads=[pkey[id(p0)], ("cos", cb)], writes=[("t1", tb)])
                        S.op("dve", lambda: nc.vector.tensor_tensor(out=t2[tb][:, :Wd], in0=p1[:, :Wd], in1=sinb[cb][:, :Wd], op=ALU.mult),
                             reads=[pkey[id(p1)], ("sin", cb)], writes=[("t2", tb)])
                        S.op("pool", lambda: nc.gpsimd.tensor_tensor(out=qkT[cb][:, qi * 4 + hd, :Wd], in0=t1[tb][:, :Wd], in1=t2[tb][:, :Wd], op=ALU.add),
                             reads=[("t1", tb), ("t2", tb)], writes=[("qkT", cb, qi * 4 + hd)])
                    dst = X["qT"] if qi == 0 else X["kT"]
                    S.dma("sp", dst.rearrange("(h p) t -> p h t", p=P)[:, :, tok0:tok0 + Wd], qkT[cb][:, qi * 4:qi * 4 + 4, :Wd],
                          reads=[("qkT", cb, qi * 4 + hd) for hd in range(4)], writes=[("d_qk", ci, qi)])
                for ti in range(ntl):
                    i = t0 + ti
                    pt = pa[pcount % 4]; pcount += 1
                    for kt in range(8):
                        S.op("pe", lambda: nc.tensor.matmul(pt[:], lhsT=ub[:, kt, ti * P:(ti + 1) * P], rhs=W["v"][:, kt, :], start=(kt == 0), stop=(kt == 7)),
                             reads=[("uT", cb, ti), "w_v"], writes=[pkey[id(pt)]], pe_chain=True)
                    S.op("act", lambda: nc.scalar.copy(out=vb[cb][:, ti, :], in_=pt[:]), reads=[pkey[id(pt)]], writes=[("vb", cb, ti)])
                    zb = i % 2
                    for kt in range(8):
                        S.op("pe", lambda: nc.tensor.matmul(pz_[zb][:, :], lhsT=ub[:, kt, ti * P:(ti + 1) * P], rhs=W["zv"][:, kt, :], start=(kt == 0), stop=(kt == 7)),
                             reads=[("uT", cb, ti), "w_zv"], writes=[("pz", zb)], pe_chain=True)
                    S.op("act", lambda: nc.scalar.activation(out=zg[zb][:], in_=pz_[zb][:, :], func=AF.Gelu_apprx_tanh), reads=[("pz", zb)], writes=[("zg", zb)])
                    S.op("dve", lambda: nc.vector.bn_stats(out=zst[:, zb, :], in_=zg[zb][:]), reads=[("zg", zb)], writes=[("zst", zb)])
                    S.op("dve", lambda: nc.vector.bn_aggr(out=zmv[:, zb, :], in_=zst[:, zb, :]), reads=[("zst", zb)], writes=[("zmv", zb)])
                    S.op("act", lambda: nc.scalar.activation(out=zrs[:, zb, 0:1], in_=zmv[:, zb, 1:2], func=AF.Sqrt, bias=LN_EPS, scale=1.0),
                         reads=[("zmv", zb)], writes=[("zrs0", zb)])
                    S.op("dve", lambda: nc.vector.reciprocal(out=zrs[:, zb, 1:2], in_=zrs[:, zb, 0:1]), reads=[("zrs0", zb)], writes=[("zrs1", zb)])
                    S.op("dve", lambda: nc.vector.tensor_scalar(out=zvn[zb][:], in0=zg[zb][:], scalar1=zmv[:, zb, 0:1], scalar2=zrs[:, zb, 1:2],
                                                                op0=ALU.subtract, op1=ALU.mult),
                         reads=[("zg", zb), ("zmv", zb), ("zrs1", zb)], writes=[("zvn", zb)])
                    for h in range(4):
                        S.op("pe", lambda: nc.tensor.matmul(pz_[zb][64 * (h % 2):64 * (h % 2) + 64, (h // 2) * P:(h // 2 + 1) * P],
                                                            lhsT=zvn[zb][:, h * 64:(h + 1) * 64], rhs=wsT[:, h, :], start=True, stop=True),
                             reads=[("zvn", zb), "wsT", ("zg", zb)], writes=[("pz", zb)], pe_chain=True)
                    S.op("dve", lambda: nc.vector.tensor_tensor(out=tg[zb][:], in0=pz_[zb][:, :].rearrange("p (a b) -> p a b", a=2), in1=bsT[:], op=ALU.add),
                         reads=[("pz", zb)] + bsT_keys, writes=[("tg", zb)])
                    S.op("pool", lambda: nc.gpsimd.tensor_tensor(out=ybT[cb][:, :, ti * P:(ti + 1) * P], in0=tg[zb][:], in1=zuT[cb][:, :, ti * P:(ti + 1) * P], op=ALU.mult),
                         reads=[("tg", zb), ("zuT", cb, 0), ("zuT", cb, 1)], writes=[("ybT", cb, ti)])
                S.dma("sp", X["v"][tok0:tok0 + Wd, :].rearrange("(a p) n -> p a n", p=P), vb[cb][:, :ntl, :],
                      reads=[("vb", cb, ti) for ti in range(ntl)], writes=[("d_v", ci)])
                S.dma("sp", X["ybT"].rearrange("(mt p) t -> p mt t", p=P)[:, :, tok0:tok0 + Wd], ybT[cb][:, :, :Wd],
                      reads=[("ybT", cb, ti) for ti in range(ntl)], writes=[("d_yb", ci)])


    def phase_attn(self, l):
        nc, S, I, X = self.nc, self.S, self.I, self.X
        last = (l == DEPTH - 1)
        lam_init = 0.8 - 0.6 * math.exp(-0.3 * l)
        with contextlib.ExitStack() as ph:
            lamb = self.sb(ph, "lamb", [P, 4, 64])
            lt = self.sb(ph, "lt", [P, 2, 64])
            ls = self.sb(ph, "ls", [P, 4])
            neglam = self.sb(ph, "neglam", [P, 1])
            gsc = self.sb(ph, "gsc", [P, 1])
            onesb = self.sb(ph, "onesb", [P, P], BF16)
            S.dma("sp", lamb[:], I["da_lam"][l].partition_broadcast(P), writes=["lamb"])
            S.dma("sp", gsc[:], I["da_subln_g"][l].rearrange("(p o) -> p o", o=1), writes=["gsc"])
            S.op("dve", lambda: nc.vector.memset(onesb[:], 1.0), writes=["onesb"])
            for a in range(2):
                S.op("dve", lambda: nc.vector.tensor_tensor(out=lt[:, a, :], in0=lamb[:, 2 * a, :], in1=lamb[:, 2 * a + 1, :], op=ALU.mult),
                     reads=["lamb"], writes=[("lt", a)])
                S.op("dve", lambda: nc.vector.tensor_reduce(out=ls[:, a:a + 1], in_=lt[:, a, :], axis=AX.X, op=ALU.add),
                     reads=[("lt", a)], writes=[("ls", a)])
                S.op("act", lambda: nc.scalar.activation(out=ls[:, 2 + a:3 + a], in_=ls[:, a:a + 1], func=AF.Exp), reads=[("ls", a)], writes=[("le", a)])
            S.op("dve", lambda: nc.vector.tensor_tensor(out=neglam[:], in0=ls[:, 3:4], in1=ls[:, 2:3], op=ALU.subtract),
                 reads=[("le", 0), ("le", 1)], writes=["neglam"])
            S.op("dve", lambda: nc.vector.tensor_scalar(out=neglam[:], in0=neglam[:], scalar1=-lam_init, scalar2=None, op0=ALU.add),
                 reads=["neglam"], writes=["neglam"])
            S.op("dve", lambda: nc.vector.tensor_scalar(out=gsc[:], in0=gsc[:], scalar1=(1.0 - lam_init), scalar2=None, op0=ALU.mult),
                 reads=["gsc"], writes=["gsc"])

            kTz = [[self.sb(ph, "kTz%d_%d" % (b, m), [P, NTOK], BF16) for m in range(2)] for b in range(2)]
            vh = [self.sb(ph, "vh%d" % b, [P, NT, P], BF16) for b in range(2)]
            for b in range(2):
                for m in range(2):
                    S.op("pool", lambda: nc.gpsimd.memset(kTz[b][m][:], 0.0), writes=[("kTz", b, m)])
            qt = [self.sb(ph, "qt%d" % i, [P, 512], BF16) for i in range(2)]
            NPT = 6
            pT = [self.sb(ph, "pT%d" % i, [P, 512], BF16) for i in range(NPT)]
            rd = [self.sb(ph, "rd%d" % i, [P, 512]) for i in range(2)]
            o0 = self.sb(ph, "o0", [P, 512])
            o1 = self.sb(ph, "o1", [P, 512])
            oo = self.sb(ph, "oo", [P, 512])
            sq = self.sb(ph, "sq", [P, 512], BF16)
            rt = self.sb(ph, "rt", [P, 512])
            yc = [self.sb(ph, "yc%d" % i, [P, 512], BF16) for i in range(2)]
            NSC = 4
            sc = [self.psum(ph, "sc%d" % i, [P, 512]) for i in range(NSC)]
            acco = [self.psum(ph, "acco%d" % i, [P, 512]) for i in range(2)]
            accd = [self.psum(ph, "accd%d" % i, [P, 512]) for i in range(2)]
            nq = 0
            nstep = 0
            for h in range(4):
                hb = h % 2
                for m in range(2):
                    S.dma("sp", kTz[hb][m][64 * m:64 * m + 64, :], X["kT"][h * P + 64 * m:h * P + 64 * m + 64, :], writes=[("kTz", hb, m)])
                S.dma("sp", vh[hb][:], X["v"][:, h * P:(h + 1) * P].rearrange("(j p) e -> p j e", p=P), writes=[("vh", hb)])
                qchunks = [(NCTX + 512 * c, 512, list(range(NT))) for c in range(8)]
                if not last:
                    qchunks = [(0, NCTX, [0, 1])] + qchunks
                for (tok0, Wq, keys) in qchunks:
                    qb = nq % 2
                    nq += 1
                    S.dma("sp", qt[qb][:, :Wq], X["qT"][h * P:(h + 1) * P, tok0:tok0 + Wq], writes=[("qt", qb)])
                    steps = [(j, m) for j in keys for m in range(2)]
                    LAG = 3

                    def emit_qk(si):
                        j, m = steps[si]
                        g = (nstep + si)
                        S.op("pe", lambda: nc.tensor.matmul(sc[g % NSC][:, :Wq], lhsT=kTz[hb][m][:, j * P:(j + 1) * P], rhs=qt[qb][:, :Wq], start=True, stop=True),
                             reads=[("kTz", hb, m), ("qt", qb)], writes=[("sc", g % NSC)], pe_chain=True)
                        S.op("act", lambda: nc.scalar.activation(out=pT[g % NPT][:, :Wq], in_=sc[g % NSC][:, :Wq], func=AF.Exp, scale=0.125),
                             reads=[("sc", g % NSC)], writes=[("pT", g % NPT)])

                    def emit_pv(si):
                        j, m = steps[si]
                        g = (nstep + si)
                        first = (j == keys[0])
                        lastk = (j == keys[-1])
                        S.op("pe", lambda: nc.tensor.matmul(acco[m][:, :Wq], lhsT=vh[hb][:, j, :], rhs=pT[g % NPT][:, :Wq], start=first, stop=lastk),
                             reads=[("vh", hb), ("pT", g % NPT)], writes=[("acco", m)], pe_chain=True)
                        S.op("pe", lambda: nc.tensor.matmul(accd[m][:, :Wq], lhsT=onesb[:], rhs=pT[g % NPT][:, :Wq], start=first, stop=lastk),
                             reads=["onesb", ("pT", g % NPT)], writes=[("accd", m)], pe_chain=True)

                    for si in range(len(steps) + LAG):
                        if si < len(steps):
                            emit_qk(si)
                        if si - LAG >= 0:
                            emit_pv(si - LAG)
                    nstep += len(steps)
                    for m in range(2):
                        S.op("dve", lambda: nc.vector.reciprocal(out=rd[m][:, :Wq], in_=accd[m][:, :Wq]), reads=[("accd", m)], writes=[("rd", m)])
                    S.op("dve", lambda: nc.vector.tensor_tensor(out=o0[:, :Wq], in0=acco[0][:, :Wq], in1=rd[0][:, :Wq], op=ALU.mult),
                         reads=[("acco", 0), ("rd", 0)], writes=["o0"])
                    S.op("dve", lambda: nc.vector.tensor_tensor(out=o1[:, :Wq], in0=acco[1][:, :Wq], in1=rd[1][:, :Wq], op=ALU.mult),
                         reads=[("acco", 1), ("rd", 1)], writes=["o1"])
                    S.op("dve", lambda: nc.vector.scalar_tensor_tensor(out=oo[:, :Wq], in0=o1[:, :Wq], scalar=neglam[:, 0:1], in1=o0[:, :Wq], op0=ALU.mult, op1=ALU.add),
                         reads=["o0", "o1", "neglam"], writes=["oo"])
                    S.op("pool", lambda: nc.gpsimd.tensor_tensor(out=sq[:, :Wq], in0=oo[:, :Wq], in1=oo[:, :Wq], op=ALU.mult), reads=["oo"], writes=["sq"])
                    g = nstep % NSC
                    S.op("pe", lambda: nc.tensor.matmul(sc[g][:, :Wq], lhsT=onesb[:], rhs=sq[:, :Wq], start=True, stop=True),
                         reads=["onesb", "sq"], writes=[("sc", g)], pe_chain=True)
                    S.op("act", lambda: nc.scalar.activation(out=rt[:, :Wq], in_=sc[g][:, :Wq], func=AF.Sqrt, scale=1.0 / 128, bias=LN_EPS),
                         reads=[("sc", g)], writes=["rt"])
                    nstep += 1
                    S.op("dve", lambda: nc.vector.reciprocal(out=rt[:, :Wq], in_=rt[:, :Wq]), reads=["rt"], writes=["rt"])
                    yb_ = nq % 2
                    S.op("dve", lambda: nc.vector.scalar_tensor_tensor(out=yc[yb_][:, :Wq], in0=oo[:, :Wq], scalar=gsc[:, 0:1], in1=rt[:, :Wq], op0=ALU.mult, op1=ALU.mult),
                         reads=["oo", "gsc", "rt"], writes=[("yc", yb_)])
                    S.dma("sp", X["ycT"][h * P:(h + 1) * P, tok0:tok0 + Wq], yc[yb_][:, :Wq], reads=[("yc", yb_)], writes=[("d_yc", yb_)])

    def phase_s5(self, l):
        nc, S, I, X = self.nc, self.S, self.I, self.X
        TWO_PI = 2.0 * math.pi
        INV2PI = 1.0 / TWO_PI
        MAGIC = 12582912.0
        V = nc.vector

        def dve(fn, reads, writes):
            return S.op("dve", fn, reads=reads, writes=writes)

        def reduce_angle(out, x, tmp, shift, kx, kout, ktmp):
            dve(lambda: V.tensor_scalar(out=tmp, in0=x, scalar1=INV2PI, scalar2=shift * INV2PI + MAGIC, op0=ALU.mult, op1=ALU.add), [kx], [ktmp])
            dve(lambda: V.tensor_scalar(out=tmp, in0=tmp, scalar1=-MAGIC, scalar2=None, op0=ALU.add), [ktmp], [ktmp])
            dve(lambda: V.scalar_tensor_tensor(out=out, in0=tmp, scalar=-TWO_PI, in1=x, op0=ALU.mult, op1=ALU.add), [ktmp, kx], [kout])

        def cossin(cos_o, sin_o, x, tmp, tmp2, kx, kc_, ks_, ktmp, ktmp2):
            reduce_angle(tmp2, x, tmp, 0.0, kx, ktmp2, ktmp)
            S.op("act", lambda: nc.scalar.activation(out=sin_o, in_=tmp2, func=AF.Sin), reads=[ktmp2], writes=[ks_])
            reduce_angle(tmp2, x, tmp, math.pi / 2, kx, ktmp2, ktmp)
            S.op("act", lambda: nc.scalar.activation(out=cos_o, in_=tmp2, func=AF.Sin, bias=halfpi[:, 0:1]), reads=[ktmp2, "halfpi"], writes=[kc_])

        with contextlib.ExitStack() as ph:
            aT_sb = self.sb(ph, "aT_sb", [P, 2, NTOK], BF16)
            yacc = self.sb(ph, "yacc", [P, 2, NTOK], F32)
            dcol = self.sb(ph, "dcol", [P, 2])
            halfpi = self.sb(ph, "halfpi", [P, 1])
            sigp = self.sb(ph, "sigp", [P, 2])
            nsig = self.sb(ph, "nsig", [P, 2])
            sigf = self.sb(ph, "sigf", [P, 2, P])
            Bmat = [self.sb(ph, "Bmat%d" % d, [P, 2, 1024], BF16) for d in range(2)]
            WpreR = [self.sb(ph, "WpreR%d" % d, [P, 2, 512]) for d in range(2)]
            WpreI = [self.sb(ph, "WpreI%d" % d, [P, 2, 512]) for d in range(2)]
            WpostR = [self.sb(ph, "WpostR%d" % d, [P, 8, P]) for d in range(2)]
            WpostI = [self.sb(ph, "WpostI%d" % d, [P, 8, P]) for d in range(2)]
            A128R = [self.sb(ph, "A128R%d" % d, [P, 8]) for d in range(2)]
            A128I = [self.sb(ph, "A128I%d" % d, [P, 8]) for d in range(2)]
            Cm = [self.sb(ph, "Cm%d" % d, [P, 16, P], BF16) for d in range(2)]
            Mdir = [self.sb(ph, "Mdir%d" % d, [P, P], BF16) for d in range(2)]
            E8 = self.sb(ph, "E8", [P, 8, P], BF16)
            cTh = [[self.sb(ph, "cTh%d%d" % (d, k), [P, P], BF16) for k in range(2)] for d in range(2)]
            cTl = [[self.sb(ph, "cTl%d%d" % (d, k), [P, P], BF16) for k in range(2)] for d in range(2)]

            S.dma("sp", aT_sb[:], X["aT"].rearrange("(kc p) t -> p kc t", p=P), writes=["aT_sb"])
            S.dma("sp", dcol[:], I["s5_d"][l].rearrange("(kc p) -> p kc", p=P), writes=["dcol"], allow_slow_non_contiguous=True)
            S.dma("sp", sigp[:], I["k_sigp"], writes=["sigp"])
            S.dma("sp", sigf[:], I["k_sigf"], writes=["sigf"])
            S.dma("pool", Mdir[0][:], I["k_mfwd"], writes=[("Mdir", 0)])
            S.dma("pool", Mdir[1][:], I["k_mbwd"], writes=[("Mdir", 1)])
            S.dma("pool", E8[:], I["k_e8"], writes=["E8"])
            Mneg = [self.sb(ph, "Mneg%d" % d, [P, P], BF16) for d in range(2)]
            CmN = [self.sb(ph, "CmN%d" % d, [P, 8, P], BF16) for d in range(2)]
            for d in range(2):
                dve(lambda: V.tensor_scalar(out=Mneg[d][:], in0=Mdir[d][:], scalar1=-1.0, scalar2=None, op0=ALU.mult), [("Mdir", d)], [("Mneg", d)])
            dve(lambda: V.memset(halfpi[:], math.pi / 2), [], ["halfpi"])
            dve(lambda: V.tensor_scalar(out=nsig[:], in0=sigp[:], scalar1=-1.0, scalar2=None, op0=ALU.mult), ["sigp"], ["nsig"])
            for d in range(2):
                for k in range(2):
                    S.op("pool", lambda: nc.gpsimd.memset(cTh[d][k][:], 0.0), writes=[("cTh", d, k)])
                    S.op("pool", lambda: nc.gpsimd.memset(cTl[d][k][:], 0.0), writes=[("cTl", d, k)])
            for kc in range(2):
                dve(lambda: V.tensor_scalar(out=yacc[:, kc, :], in0=aT_sb[:, kc, :], scalar1=dcol[:, kc:kc + 1], scalar2=None, op0=ALU.mult),
                    ["aT_sb", "dcol"], [("yacc", kc, i) for i in range(NT)])

            with contextlib.ExitStack() as su:
                R = {}
                for n in ("LR", "LI", "lrdt", "ang", "mag", "cs", "sn", "tA", "tB", "are", "aim", "zre", "zim", "x", "t3", "t4"):
                    R[n] = self.sb(su, "r_" + n, [P, 1024])
                LS = self.sb(su, "r_LS", [P, 16])
                braw = [self.sb(su, "braw%d" % i, [P, 512]) for i in range(2)]
                CmS = self.sb(su, "CmS", [P, 16, P])
                Cc = {}
                for n in ("LR", "LI", "LS", "lrdt", "ang", "angr", "t", "e", "x", "cs", "sn", "mg"):
                    Cc[n] = self.sb(su, "c_" + n, [P, 8])
                for d in range(2):
                    k_ = lambda n: ("su", n)
                    S.dma("sp", R["LR"][:], I["s5_lam_re"][l, d].rearrange("g p -> (g p)").partition_broadcast(P), writes=[k_("LR")])
                    S.dma("sp", R["LI"][:], I["s5_lam_im"][l, d].rearrange("g p -> (g p)").partition_broadcast(P), writes=[k_("LI")])
                    S.dma("sp", LS[:], I["s5_log_step"][l, d].partition_broadcast(P), writes=[k_("LS")])
                    S.op("act", lambda: nc.scalar.activation(out=LS[:], in_=LS[:], func=AF.Exp), reads=[k_("LS")], writes=[k_("LS")])
                    dtb = LS[:, :].unsqueeze(2).to_broadcast([P, 16, 64])
                    v3 = lambda t: t[:].rearrange("p (g q) -> p g q", g=16)
                    dve(lambda: V.tensor_tensor(out=v3(R["lrdt"]), in0=v3(R["LR"]), in1=dtb, op=ALU.mult), [k_("LR"), k_("LS")], [k_("lrdt")])
                    dve(lambda: V.tensor_tensor(out=v3(R["ang"]), in0=v3(R["LI"]), in1=dtb, op=ALU.mult), [k_("LI"), k_("LS")], [k_("ang")])
                    S.op("act", lambda: nc.scalar.activation(out=R["mag"][:], in_=R["lrdt"][:], func=AF.Exp), reads=[k_("lrdt")], writes=[k_("mag")])
                    reduce_angle(R["tB"][:], R["ang"][:], R["tA"][:], 0.0, k_("ang"), k_("tB"), k_("tA"))
                    dve(lambda: V.tensor_copy(out=R["ang"][:], in_=R["tB"][:]), [k_("tB")], [k_("ang")])
                    cossin(R["cs"][:], R["sn"][:], R["ang"][:], R["tA"][:], R["tB"][:], k_("ang"), k_("cs"), k_("sn"), k_("tA"), k_("tB"))
                    dve(lambda: V.tensor_tensor(out=R["are"][:], in0=R["mag"][:], in1=R["cs"][:], op=ALU.mult), [k_("mag"), k_("cs")], [k_("are")])
                    dve(lambda: V.tensor_tensor(out=R["aim"][:], in0=R["mag"][:], in1=R["sn"][:], op=ALU.mult), [k_("mag"), k_("sn")], [k_("aim")])
                    dve(lambda: V.tensor_tensor(out=R["tA"][:], in0=R["LR"][:], in1=R["LR"][:], op=ALU.mult), [k_("LR")], [k_("tA")])
                    dve(lambda: V.tensor_tensor(out=R["tB"][:], in0=R["LI"][:], in1=R["LI"][:], op=ALU.mult), [k_("LI")], [k_("tB")])
                    dve(lambda: V.tensor_tensor(out=R["cs"][:], in0=R["tA"][:], in1=R["tB"][:], op=ALU.add), [k_("tA"), k_("tB")], [k_("cs")])
                    dve(lambda: V.reciprocal(out=R["cs"][:], in_=R["cs"][:]), [k_("cs")], [k_("cs")])
                    dve(lambda: V.tensor_scalar(out=R["are"][:], in0=R["are"][:], scalar1=-1.0, scalar2=None, op0=ALU.add), [k_("are")], [k_("are")])
                    dve(lambda: V.tensor_tensor(out=R["tA"][:], in0=R["are"][:], in1=R["LR"][:], op=ALU.mult), [k_("are"), k_("LR")], [k_("tA")])
                    dve(lambda: V.tensor_tensor(out=R["tB"][:], in0=R["aim"][:], in1=R["LI"][:], op=ALU.mult), [k_("aim"), k_("LI")], [k_("tB")])
                    dve(lambda: V.tensor_tensor(out=R["tA"][:], in0=R["tA"][:], in1=R["tB"][:], op=ALU.add), [k_("tA"), k_("tB")], [k_("tA")])
                    dve(lambda: V.tensor_tensor(out=R["zre"][:], in0=R["tA"][:], in1=R["cs"][:], op=ALU.mult), [k_("tA"), k_("cs")], [k_("zre")])
                    dve(lambda: V.tensor_tensor(out=R["tA"][:], in0=R["aim"][:], in1=R["LR"][:], op=ALU.mult), [k_("aim"), k_("LR")], [k_("tA")])
                    dve(lambda: V.tensor_tensor(out=R["tB"][:], in0=R["are"][:], in1=R["LI"][:], op=ALU.mult), [k_("are"), k_("LI")], [k_("tB")])
                    dve(lambda: V.tensor_tensor(out=R["tA"][:], in0=R["tA"][:], in1=R["tB"][:], op=ALU.subtract), [k_("tA"), k_("tB")], [k_("tA")])
                    dve(lambda: V.tensor_tensor(out=R["zim"][:], in0=R["tA"][:], in1=R["cs"][:], op=ALU.mult), [k_("tA"), k_("cs")], [k_("zim")])
                    for kc in range(2):
                        hs = slice(kc * 512, (kc + 1) * 512)
                        for ri, src in enumerate((I["s5_b_re"], I["s5_b_im"])):
                            dve(lambda: V.memset(braw[ri][:], 0.0), [], [("braw", ri, gl) for gl in range(8)])
                            for gl in range(8):
                                g = kc * 8 + gl
                                S.dma("sp", braw[ri][16 * gl:16 * gl + 16, gl * 64:(gl + 1) * 64], src[l, d, g].rearrange("p c -> c p"),
                                      reads=[], writes=[("braw", ri, gl)], allow_slow_non_contiguous=True)
                        tA = R["tA"][:, 0:512]; tB = R["tB"][:, 0:512]
                        dve(lambda: V.tensor_tensor(out=tA, in0=braw[0][:], in1=R["zre"][:, hs], op=ALU.mult), [("braw", 0, gl) for gl in range(8)] + [k_("zre")], [k_("tA")])
                        dve(lambda: V.tensor_tensor(out=tB, in0=braw[1][:], in1=R["zim"][:, hs], op=ALU.mult), [("braw", 1, gl) for gl in range(8)] + [k_("zim")], [k_("tB")])
                        dve(lambda: V.tensor_tensor(out=Bmat[d][:, kc, 0:512], in0=tA, in1=tB, op=ALU.subtract), [k_("tA"), k_("tB")], [("Bmat", d, kc, 0)])
                        dve(lambda: V.tensor_tensor(out=tA, in0=braw[0][:], in1=R["zim"][:, hs], op=ALU.mult), [("braw", 0, gl) for gl in range(8)] + [k_("zim")], [k_("tA")])
                        dve(lambda: V.tensor_tensor(out=tB, in0=braw[1][:], in1=R["zre"][:, hs], op=ALU.mult), [("braw", 1, gl) for gl in range(8)] + [k_("zre")], [k_("tB")])
                        dve(lambda: V.tensor_tensor(out=Bmat[d][:, kc, 512:1024], in0=tA, in1=tB, op=ALU.add), [k_("tA"), k_("tB")], [("Bmat", d, kc, 1)])
                        S.op("act", lambda: nc.scalar.activation(out=R["mag"][:, 0:512], in_=R["lrdt"][:, hs], func=AF.Exp, scale=nsig[:, d:d + 1]),
                             reads=[k_("lrdt"), "nsig"], writes=[k_("mag")])
                        dve(lambda: V.tensor_scalar(out=R["x"][:, 0:512], in0=R["ang"][:, hs], scalar1=sigp[:, d:d + 1], scalar2=None, op0=ALU.mult),
                            [k_("ang"), "sigp"], [k_("x")])
                        cossin(R["cs"][:, 0:512], R["sn"][:, 0:512], R["x"][:, 0:512], R["t3"][:, 0:512], R["t4"][:, 0:512],
                               k_("x"), k_("cs"), k_("sn"), k_("t3"), k_("t4"))
                        dve(lambda: V.tensor_tensor(out=WpreR[d][:, kc, :], in0=R["mag"][:, 0:512], in1=R["cs"][:, 0:512], op=ALU.mult),
                            [k_("mag"), k_("cs")], [("WpreR", d, kc)])
                        dve(lambda: V.scalar_tensor_tensor(out=WpreI[d][:, kc, :], in0=R["mag"][:, 0:512], scalar=-1.0, in1=R["sn"][:, 0:512], op0=ALU.mult, op1=ALU.mult),
                            [k_("mag"), k_("sn")], [("WpreI", d, kc)])
                    S.dma("sp", Cc["LR"][:], I["s5_lam_re"][l, d].rearrange("g p -> (g p)").rearrange("(c q) -> q c", q=P), writes=[k_("cLR")], allow_slow_non_contiguous=True)
                    S.dma("sp", Cc["LI"][:], I["s5_lam_im"][l, d].rearrange("g p -> (g p)").rearrange("(c q) -> q c", q=P), writes=[k_("cLI")], allow_slow_non_contiguous=True)
                    for hh in range(2):
                        S.dma("sp", Cc["LS"][64 * hh:64 * hh + 64, :], I["s5_log_step"][l, d].rearrange("(c two) -> two c", two=2)[hh].partition_broadcast(64),
                              writes=[k_("cLS%d" % hh)], allow_slow_non_contiguous=True)
                    S.op("act", lambda: nc.scalar.activation(out=Cc["LS"][:], in_=Cc["LS"][:], func=AF.Exp), reads=[k_("cLS0"), k_("cLS1")], writes=[k_("cdt")])
                    dve(lambda: V.tensor_tensor(out=Cc["lrdt"][:], in0=Cc["LR"][:], in1=Cc["LS"][:], op=ALU.mult), [k_("cLR"), k_("cdt")], [k_("clrdt")])
                    dve(lambda: V.tensor_tensor(out=Cc["ang"][:], in0=Cc["LI"][:], in1=Cc["LS"][:], op=ALU.mult), [k_("cLI"), k_("cdt")], [k_("cang")])
                    reduce_angle(Cc["angr"][:], Cc["ang"][:], Cc["t"][:], 0.0, k_("cang"), k_("cangr"), k_("ct"))
                    E3 = R["tA"][:].rearrange("p (c t) -> p c t", c=8)
                    X3 = R["tB"][:].rearrange("p (c t) -> p c t", c=8)
                    sgb = sigf[:, d, :].unsqueeze(1).to_broadcast([P, 8, P])
                    dve(lambda: V.tensor_tensor(out=E3, in0=Cc["lrdt"][:, :].unsqueeze(2).to_broadcast([P, 8, P]), in1=sgb, op=ALU.mult),
                        [k_("clrdt"), "sigf"], [k_("tA")])
                    S.op("act", lambda: nc.scalar.activation(out=R["mag"][:], in_=R["tA"][:], func=AF.Exp), reads=[k_("tA")], writes=[k_("mag")])
                    dve(lambda: V.tensor_tensor(out=X3, in0=Cc["angr"][:, :].unsqueeze(2).to_broadcast([P, 8, P]), in1=sgb, op=ALU.mult),
                        [k_("cangr"), "sigf"], [k_("tB")])
                    dve(lambda: V.tensor_copy(out=R["x"][:], in_=R["tB"][:]), [k_("tB")], [k_("x")])
                    cossin(R["cs"][:], R["sn"][:], R["x"][:], R["t3"][:], R["t4"][:], k_("x"), k_("cs"), k_("sn"), k_("t3"), k_("t4"))
                    dve(lambda: V.tensor_tensor(out=WpostR[d][:].rearrange("p c t -> p (c t)"), in0=R["mag"][:], in1=R["cs"][:], op=ALU.mult),
                        [k_("mag"), k_("cs")], [("WpostR", d)])
                    dve(lambda: V.tensor_tensor(out=WpostI[d][:].rearrange("p c t -> p (c t)"), in0=R["mag"][:], in1=R["sn"][:], op=ALU.mult),
                        [k_("mag"), k_("sn")], [("WpostI", d)])
                    dve(lambda: V.tensor_scalar(out=Cc["e"][:], in0=Cc["lrdt"][:], scalar1=128.0, scalar2=None, op0=ALU.mult), [k_("clrdt")], [k_("ce")])
                    S.op("act", lambda: nc.scalar.activation(out=Cc["mg"][:], in_=Cc["e"][:], func=AF.Exp), reads=[k_("ce")], writes=[k_("cmg")])
                    dve(lambda: V.tensor_scalar(out=Cc["x"][:], in0=Cc["angr"][:], scalar1=128.0, scalar2=None, op0=ALU.mult), [k_("cangr")], [k_("cx")])
                    cossin(Cc["cs"][:], Cc["sn"][:], Cc["x"][:], Cc["t"][:], Cc["e"][:], k_("cx"), k_("ccs"), k_("csn"), k_("ct"), k_("ce"))
                    dve(lambda: V.tensor_tensor(out=A128R[d][:], in0=Cc["mg"][:], in1=Cc["cs"][:], op=ALU.mult), [k_("cmg"), k_("ccs")], [("A128R", d)])
                    dve(lambda: V.tensor_tensor(out=A128I[d][:], in0=Cc["mg"][:], in1=Cc["sn"][:], op=ALU.mult), [k_("cmg"), k_("csn")], [("A128I", d)])
                    cmk = [("CmS", g, ri) for g in range(16) for ri in range(2)]
                    dve(lambda: V.memset(CmS[:], 0.0), [], cmk)
                    for g in range(16):
                        kc, gl = g // 8, g % 8
                        j, hh = gl // 2, gl % 2
                        for ri, src in enumerate((I["s5_c_re"], I["s5_c_im"])):
                            S.dma("sp", CmS[64 * hh:64 * hh + 64, kc * 8 + ri * 4 + j, 16 * gl:16 * gl + 16], src[l, d, g].rearrange("c p -> p c"),
                                  reads=[], writes=[("CmS", g, ri)], allow_slow_non_contiguous=True)
                    Cm4 = Cm[d][:].rearrange("p (k r j) c -> p k r (j c)", k=2, r=2)
                    CmS4 = CmS[:].rearrange("p (k r j) c -> p k r (j c)", k=2, r=2)
                    dve(lambda: V.tensor_copy(out=Cm4[:, :, 0, :], in_=CmS4[:, :, 0, :]), cmk, [("Cm", d, 0)])
                    dve(lambda: V.tensor_scalar(out=Cm4[:, :, 1, :], in0=CmS4[:, :, 1, :], scalar1=-1.0, scalar2=None, op0=ALU.mult), cmk, [("Cm", d, 1)])
                    dve(lambda: V.tensor_scalar(out=CmN[d][:].rearrange("p (k j) c -> p k (j c)", k=2), in0=CmS4[:, :, 0, :], scalar1=-1.0, scalar2=None, op0=ALU.mult),
                        cmk, [("CmN", d)])
            S.barrier()

            P1 = [[self.sb(ph, "s5P1%d%d" % (d, k), [P, 2, 512], BF16) for k in range(2)] for d in range(2)]
            P2 = [[self.sb(ph, "s5P2%d%d" % (d, k), [P, 2, 512], BF16) for k in range(2)] for d in range(2)]
            Q1 = [[self.sb(ph, "s5Q1%d%d" % (d, k), [P, 2, 512], BF16) for k in range(2)] for d in range(2)]
            Q2 = [[self.sb(ph, "s5Q2%d%d" % (d, k), [P, 2, 512], BF16) for k in range(2)] for d in range(2)]
            tc1 = self.sb(ph, "s5tc1", [P, 2, 4])
            tc2 = self.sb(ph, "s5tc2", [P, 2, 4])
            cn = self.sb(ph, "s5cn", [P, 8])
            bu = self.psum(ph, "s5bu", [P, 2, 512])
            st_ = [self.psum(ph, "s5st%d" % d, [P, 8, P]) for d in range(2)]
            ctp = self.psum(ph, "s5ctp", [P, 512])
            yo = self.psum(ph, "s5yo", [P, 512])
            orders = [list(range(NT)), [1, 0] + list(range(NT - 1, 1, -1))]
            for step in range(NT):
                for d in range(2):
                    ti = orders[d][step]
                    cols = slice(ti * P, (ti + 1) * P)
                    tl = 127 if d == 0 else 0
                    st = st_[d]
                    for kc in range(2):
                        p1, p2, q1, q2 = P1[d][kc], P2[d][kc], Q1[d][kc], Q2[d][kc]
                        for ri in range(2):
                            S.op("pe", lambda: nc.tensor.matmul(bu[:, ri, :], lhsT=aT_sb[:, kc, cols], rhs=Bmat[d][:, kc, ri * 512:(ri + 1) * 512], start=True, stop=True),
                                 reads=["aT_sb"], writes=["bu"], pe_chain=True)
                        dve(lambda: V.tensor_tensor(out=p1[:], in0=bu[:], in1=WpreR[d][:, kc, :].unsqueeze(1).to_broadcast([P, 2, 512]), op=ALU.mult),
                            ["bu"], [("P1", d, kc)])
                        dve(lambda: V.tensor_tensor(out=p2[:], in0=bu[:], in1=WpreI[d][:, kc, :].unsqueeze(1).to_broadcast([P, 2, 512]), op=ALU.mult),
                            ["bu"], [("P2", d, kc)])
                        for jj in range(8):
                            j = jj % 4
                            js = slice(j * P, (j + 1) * P)
                            if jj < 4:
                                terms = [(p1[:, 0, js], Mdir[d]), (p2[:, 1, js], Mneg[d])]
                            else:
                                terms = [(p2[:, 0, js], Mdir[d]), (p1[:, 1, js], Mdir[d])]
                            for tix, (lh, rh) in enumerate(terms):
                                S.op("pe", lambda: nc.tensor.matmul(st[:, jj, :], lhsT=lh, rhs=rh[:], start=(tix == 0), stop=(tix == 1 and step == 0)),
                                     reads=[("P1", d, kc), ("P2", d, kc)], writes=[("st", d)], pe_chain=True)
                            if step > 0:
                                S.op("pe", lambda: nc.tensor.matmul(st[:, jj, :], lhsT=cTh[d][kc][:], rhs=E8[:, jj, :], start=False, stop=False),
                                     reads=[("cTh", d, kc)], writes=[("st", d)], pe_chain=True)
                                S.op("pe", lambda: nc.tensor.matmul(st[:, jj, :], lhsT=cTl[d][kc][:], rhs=E8[:, jj, :], start=False, stop=True),
                                     reads=[("cTl", d, kc)], writes=[("st", d)], pe_chain=True)
                        st3 = st[:].rearrange("p (r j) t -> p r (j t)", r=2)
                        wr = WpostR[d][:, kc * 4:(kc + 1) * 4, :].rearrange("p j t -> p (j t)").unsqueeze(1).to_broadcast([P, 2, 512])
                        wi = WpostI[d][:, kc * 4:(kc + 1) * 4, :].rearrange("p j t -> p (j t)").unsqueeze(1).to_broadcast([P, 2, 512])
                        dve(lambda: V.tensor_tensor(out=q1[:], in0=st3, in1=wr, op=ALU.mult), [("st", d)], [("Q1", d, kc)])
                        dve(lambda: V.tensor_tensor(out=q2[:], in0=st3, in1=wi, op=ALU.mult), [("st", d)], [("Q2", d, kc)])
                        if step < NT - 1:
                            sl = st[:, :, tl].rearrange("p (r j) -> p r j", r=2)
                            ar = A128R[d][:, kc * 4:(kc + 1) * 4].unsqueeze(1).to_broadcast([P, 2, 4])
                            ai = A128I[d][:, kc * 4:(kc + 1) * 4].unsqueeze(1).to_broadcast([P, 2, 4])
                            dve(lambda: V.tensor_tensor(out=tc1[:], in0=sl, in1=ar, op=ALU.mult), [("st", d)], ["tc1"])
                            dve(lambda: V.tensor_tensor(out=tc2[:], in0=sl, in1=ai, op=ALU.mult), [("st", d)], ["tc2"])
                            dve(lambda: V.tensor_tensor(out=cn[:, 0:4], in0=tc1[:, 0, :], in1=tc2[:, 1, :], op=ALU.subtract), ["tc1", "tc2"], ["cn0"])
                            dve(lambda: V.tensor_tensor(out=cn[:, 4:8], in0=tc2[:, 0, :], in1=tc1[:, 1, :], op=ALU.add), ["tc1", "tc2"], ["cn1"])
                            S.op("pe", lambda: nc.tensor.transpose(ctp[0:8, 0:P], cn[:, :], self.ident[:]), reads=["cn0", "cn1", "ident"], writes=["ctp"], pe_chain=True)
                            S.op("act", lambda: nc.scalar.copy(out=cTh[d][kc][0:8, :], in_=ctp[0:8, 0:P]), reads=["ctp"], writes=[("cTh", d, kc)])
                            dve(lambda: V.tensor_tensor(out=cTl[d][kc][0:8, :], in0=ctp[0:8, 0:P], in1=cTh[d][kc][0:8, :], op=ALU.subtract),
                                ["ctp", ("cTh", d, kc)], [("cTl", d, kc)])
                        rterms = []
                        for j in range(4):
                            js = slice(j * P, (j + 1) * P)
                            rterms += [(Cm[d][:, kc * 8 + j, :], q1[:, 0, js], ("Q1", d, kc)), (CmN[d][:, kc * 4 + j, :], q2[:, 1, js], ("Q2", d, kc)),
                                       (Cm[d][:, kc * 8 + 4 + j, :], q2[:, 0, js], ("Q2", d, kc)), (Cm[d][:, kc * 8 + 4 + j, :], q1[:, 1, js], ("Q1", d, kc))]
                        for tix, (lh, rh, rk) in enumerate(rterms):
                            S.op("pe", lambda: nc.tensor.matmul(yo[:, 0:P], lhsT=lh, rhs=rh, start=(tix == 0), stop=(tix == len(rterms) - 1)),
                                 reads=[rk], writes=["yo"], pe_chain=True)
                        dve(lambda: V.tensor_tensor(out=yacc[:, kc, cols], in0=yo[:, 0:P], in1=yacc[:, kc, cols], op=ALU.add),
                            ["yo", ("yacc", kc, ti)], [("yacc", kc, ti)])
            for kc in range(2):
                S.op("act", lambda: nc.scalar.activation(out=aT_sb[:, kc, :], in_=yacc[:, kc, :], func=AF.Gelu_apprx_tanh),
                     reads=[("yacc", kc, i) for i in range(NT)] + ["aT_sb"], writes=["aT_sb"])
            S.dma("sp", X["yaT"].rearrange("(kc p) t -> p kc t", p=P), aT_sb[:], reads=["aT_sb"], writes=["d_yaT"])

    def ln_stats(self, src, st6, mv, rs, key):
        nc, S = self.nc, self.S
        for hf in range(2):
            S.op("dve", lambda: nc.vector.bn_stats(out=st6[:, hf, :], in_=src[:, hf * 512:(hf + 1) * 512]), reads=[key], writes=[("st6", id(st6), hf)])
        S.op("dve", lambda: nc.vector.bn_aggr(out=mv[:, :], in_=st6[:, :, :].rearrange("p a b -> p (a b)")),
             reads=[("st6", id(st6), 0), ("st6", id(st6), 1)], writes=[("mv", id(mv))])
        S.op("act", lambda: nc.scalar.activation(out=rs[:, 0:1], in_=mv[:, 1:2], func=AF.Sqrt, bias=LN_EPS, scale=1.0),
             reads=[("mv", id(mv))], writes=[("rs0", id(rs))])
        S.op("dve", lambda: nc.vector.reciprocal(out=rs[:, 1:2], in_=rs[:, 0:1]), reads=[("rs0", id(rs))], writes=[("rs1", id(rs))])
        return [("mv", id(mv)), ("rs1", id(rs))]

    def phase_merge(self, l):
        nc, S, I, X = self.nc, self.S, self.I, self.X
        last = (l == DEPTH - 1)
        with contextlib.ExitStack() as ph:
            wG = self.sb(ph, "wG", [P, 8, 3072], BF16)
            for br in range(3):
                S.dma("pool", wG[:, :, br * 1024:(br + 1) * 1024],
                      I["w_in"][l][:, 2304 + br * 1024:2304 + (br + 1) * 1024].rearrange("(kt p) n -> p kt n", p=P), writes=[("wG", br)])
            wsm = {}
            for n, kts in (("w_glu_val", 2), ("w_glu_gate", 2), ("w_proj_gm", 2), ("w_proj_da", 4), ("w_out", 8)):
                wsm[n] = self.sb(ph, "m_" + n, [P, kts, D], BF16)
                S.dma("pool", wsm[n][:], I[n][l].rearrange("(kt p) n -> p kt n", p=P), writes=[n])
            lng = self.sb(ph, "lng", [P, D])
            lnb = self.sb(ph, "lnb", [P, D])
            S.dma("sp", lng[:], I["ln1_g"][l].partition_broadcast(P), writes=["lng"])
            S.dma("sp", lnb[:], I["ln1_b"][l].partition_broadcast(P), writes=["lnb"])
            ya = [self.sb(ph, "g_ya%d" % i, [P, 2, 512], BF16) for i in range(2)]
            yb = [self.sb(ph, "g_yb%d" % i, [P, 2, 512], BF16) for i in range(2)]
            yc = [self.sb(ph, "g_yc%d" % i, [P, 4, 512], BF16) for i in range(2)]
            uu = [self.sb(ph, "g_uu%d" % i, [P, 8, 512], BF16) for i in range(2)]
            mT = [self.sb(ph, "g_mT%d" % i, [P, 8, 512], BF16) for i in range(2)]
            fT = [self.sb(ph, "g_fT%d" % i, [P, 8, 512], BF16) for i in range(1)] * 2
            sg = [self.sb(ph, "g_sg%d" % i, [P, 4, 512]) for i in range(1)] * 2
            tm = [self.sb(ph, "g_tm%d" % i, [P, 4, 512]) for i in range(1)] * 2
            hx = [self.sb(ph, "g_hx%d" % i, [P, D]) for i in range(2)]
            tt_ = [self.sb(ph, "g_tt%d" % i, [P, D]) for i in range(2)]
            xn = [self.sb(ph, "g_xn%d" % i, [P, D], BF16) for i in range(2)]
            st6 = [self.sb(ph, "g_st6%d" % i, [P, 2, 6]) for i in range(2)]
            mv = [self.sb(ph, "g_mv%d" % i, [P, 2]) for i in range(2)]
            rs = [self.sb(ph, "g_rs%d" % i, [P, 2]) for i in range(2)]
            NPB = 5
            pb_ = [self.psum(ph, "g_p%d" % i, [P, 512]) for i in range(NPB)]
            po = self.psum(ph, "g_po", [P, 2, 512])
            tp = self.psum(ph, "g_tp", [P, 8, P], BF16)
            chunks = [(2 + 4 * c, 4) for c in range(8)]
            if not last:
                chunks = [(0, 2)] + chunks
            pc_ = 0
            tcount = 0

            def load(ci):
                t0, ntl = chunks[ci]
                Wd = ntl * P
                tok0 = t0 * P
                cb = ci % 2
                S.dma("sp", ya[cb][:, :, :Wd], X["yaT"].rearrange("(kt p) t -> p kt t", p=P)[:, :, tok0:tok0 + Wd], writes=[("ya", cb)])
                S.dma("sp", yb[cb][:, :, :Wd], X["ybT"].rearrange("(kt p) t -> p kt t", p=P)[:, :, tok0:tok0 + Wd], writes=[("yb", cb)])
                S.dma("sp", yc[cb][:, :, :Wd], X["ycT"].rearrange("(kt p) t -> p kt t", p=P)[:, :, tok0:tok0 + Wd], writes=[("yc", cb)])
                S.dma("sp", uu[cb][:, :, :Wd], X["uT"].rearrange("(kt p) t -> p kt t", p=P)[:, :, tok0:tok0 + Wd], writes=[("uu", cb)])

            def dc_step(ci, dc):
                nonlocal pc_
                t0, ntl = chunks[ci]
                Wd = ntl * P
                cb = ci % 2

                def prod(wn, kts, src, skey):
                    nonlocal pc_
                    pi = pc_ % NPB; pc_ += 1
                    pt = pb_[pi]
                    for kt in range(kts):
                        if wn[0] == "g":
                            br = "ABC".index(wn[1])
                            lhsT = wG[:, kt, br * 1024 + dc * P:br * 1024 + (dc + 1) * P]
                            wkey = ("wG", br)
                        else:
                            lhsT = wsm[wn][:, kt, dc * P:(dc + 1) * P]
                            wkey = wn
                        S.op("pe", lambda: nc.tensor.matmul(pt[:, :Wd], lhsT=lhsT, rhs=src[:, kt, :Wd], start=(kt == 0), stop=(kt == kts - 1)),
                             reads=[wkey, skey], writes=[("gp", pi)], pe_chain=True)
                    return pt, ("gp", pi)

                def sig(pt, pk, sgi):
                    S.op("act", lambda: nc.scalar.activation(out=sg[0][:, sgi, :Wd], in_=pt[:, :Wd], func=AF.Sigmoid), reads=[pk], writes=[("sg", sgi)])

                pt, pk = prod("w_glu_gate", 2, ya[cb], ("ya", cb)); sig(pt, pk, 0)
                pt, pk = prod("gA", 8, uu[cb], ("uu", cb)); sig(pt, pk, 1)
                pt, pk = prod("w_glu_val", 2, ya[cb], ("ya", cb))
                S.op("dve", lambda: nc.vector.tensor_tensor(out=tm[0][:, 0, :Wd], in0=pt[:, :Wd], in1=sg[0][:, 0, :Wd], op=ALU.mult),
                     reads=[pk, ("sg", 0)], writes=[("tm", 0)])
                S.op("pool", lambda: nc.gpsimd.tensor_tensor(out=tm[0][:, 0, :Wd], in0=tm[0][:, 0, :Wd], in1=sg[0][:, 1, :Wd], op=ALU.mult),
                     reads=[("tm", 0), ("sg", 1)], writes=[("tm", 0)])
                pt, pk = prod("gB", 8, uu[cb], ("uu", cb)); sig(pt, pk, 2)
                pt, pk = prod("w_proj_gm", 2, yb[cb], ("yb", cb))
                S.op("dve", lambda: nc.vector.tensor_tensor(out=tm[0][:, 1, :Wd], in0=pt[:, :Wd], in1=sg[0][:, 2, :Wd], op=ALU.mult),
                     reads=[pk, ("sg", 2)], writes=[("tm", 1)])
                pt, pk = prod("gC", 8, uu[cb], ("uu", cb)); sig(pt, pk, 3)
                pt, pk = prod("w_proj_da", 4, yc[cb], ("yc", cb))
                S.op("dve", lambda: nc.vector.tensor_tensor(out=tm[0][:, 2, :Wd], in0=pt[:, :Wd], in1=sg[0][:, 3, :Wd], op=ALU.mult),
                     reads=[pk, ("sg", 3)], writes=[("tm", 2)])
                S.op("pool", lambda: nc.gpsimd.tensor_tensor(out=tm[0][:, 3, :Wd], in0=tm[0][:, 0, :Wd], in1=tm[0][:, 1, :Wd], op=ALU.add),
                     reads=[("tm", 0), ("tm", 1)], writes=[("tm", 3)])
                S.op("pool", lambda: nc.gpsimd.tensor_tensor(out=mT[cb][:, dc, :Wd], in0=tm[0][:, 3, :Wd], in1=tm[0][:, 2, :Wd], op=ALU.add),
                     reads=[("tm", 3), ("tm", 2)], writes=[("mT", cb, dc)])

            def tail_tile(ci, ti):
                nonlocal tcount
                t0, ntl = chunks[ci]
                s = 1 if t0 == 0 else 0
                cb = ci % 2
                i = t0 + ti
                hb = tcount % 2
                tcount += 1
                S.dma("sp", hx[hb][:], self.hsrc(l, i), writes=[("ghx", hb)])
                for nh in range(2):
                    for kt in range(8):
                        S.op("pe", lambda: nc.tensor.matmul(po[:, nh, :], lhsT=mT[cb][:, kt, ti * P:(ti + 1) * P], rhs=wsm["w_out"][:, kt, nh * 512:(nh + 1) * 512],
                                                            start=(kt == 0), stop=(kt == 7)),
                             reads=[("mT", cb, kt), "w_out"], writes=[("po", nh)], pe_chain=True)
                pof = po[:].rearrange("p a b -> p (a b)")
                S.op("dve", lambda: nc.vector.tensor_tensor(out=tt_[hb][:], in0=pof, in1=self.gb[:, 2 * s + 0, :], op=ALU.mult),
                     reads=[("po", 0), ("po", 1)], writes=[("gtt", hb)])
                S.op("dve", lambda: nc.vector.scalar_tensor_tensor(out=hx[hb][:], in0=hx[hb][:], scalar=DN_ALPHA, in1=tt_[hb][:], op0=ALU.mult, op1=ALU.add),
                     reads=[("ghx", hb), ("gtt", hb)], writes=[("ghx", hb)])
                ks = self.ln_stats(hx[hb], st6[hb], mv[hb], rs[hb], ("ghx", hb))
                S.op("dve", lambda: nc.vector.tensor_scalar(out=tt_[hb][:], in0=hx[hb][:], scalar1=mv[hb][:, 0:1], scalar2=rs[hb][:, 1:2], op0=ALU.subtract, op1=ALU.mult),
                     reads=[("ghx", hb)] + ks, writes=[("gtt", hb)])
                S.op("pool", lambda: nc.gpsimd.tensor_tensor(out=tt_[hb][:], in0=tt_[hb][:], in1=lng[:], op=ALU.mult), reads=[("gtt", hb), "lng"], writes=[("gtt", hb)])
                S.op("pool", lambda: nc.gpsimd.tensor_tensor(out=hx[hb][:], in0=tt_[hb][:], in1=lnb[:], op=ALU.add), reads=[("gtt", hb), "lnb"], writes=[("ghx", hb)])
                S.dma("sp", X["h"][i * P:(i + 1) * P, :], hx[hb][:], reads=[("ghx", hb)], writes=[("d_h", hb)])
                ks = self.ln_stats(hx[hb], st6[hb], mv[hb], rs[hb], ("ghx", hb))
                S.op("dve", lambda: nc.vector.tensor_scalar(out=xn[hb][:], in0=hx[hb][:], scalar1=mv[hb][:, 0:1], scalar2=rs[hb][:, 1:2], op0=ALU.subtract, op1=ALU.mult),
                     reads=[("ghx", hb)] + ks, writes=[("gxn", hb)])
                for kt in range(8):
                    S.op("pe", lambda: nc.tensor.transpose(tp[:, kt, :], xn[hb][:, kt * P:(kt + 1) * P], self.identb[:]),
                         reads=[("gxn", hb), "identb"], writes=["gtp"], pe_chain=True)
                for kt in range(8):
                    S.op("act", lambda: nc.scalar.activation(out=fT[0][:, kt, ti * P:(ti + 1) * P], in_=tp[:, kt, :], func=AF.Identity,
                                                             scale=self.modc[:, s, 32 + kt:33 + kt], bias=self.modc[:, s, 24 + kt:25 + kt]),
                         reads=["gtp"], writes=[("gfT", 0, ti)])

            def tail_store(ci):
                t0, ntl = chunks[ci]
                Wd = ntl * P
                tok0 = t0 * P
                S.dma("sp", X["fT"].rearrange("(kt p) t -> p kt t", p=P)[:, :, tok0:tok0 + Wd], fT[0][:, :, :Wd],
                      reads=[("gfT", 0, ti) for ti in range(ntl)], writes=[("d_fT", 0)])

            nch = len(chunks)
            load(0)
            for dc in range(8):
                dc_step(0, dc)
            for ci in range(nch):
                ntl = chunks[ci][1]
                if ci + 1 < nch:
                    load(ci + 1)
                done = 0
                for dc in range(8):
                    if ci + 1 < nch:
                        dc_step(ci + 1, dc)
                    if dc % 2 == 1 and done < ntl:
                        tail_tile(ci, done)
                        done += 1
                while done < ntl:
                    tail_tile(ci, done)
                    done += 1
                tail_store(ci)

    def phase_moe(self, l):
        nc, S, I, X = self.nc, self.S, self.I, self.X
        V = nc.vector
        last = (l == DEPTH - 1)

        def dve(fn, reads, writes):
            return S.op("dve", fn, reads=reads, writes=writes)

        with contextlib.ExitStack() as ph:
            MAXT = 18
            wrt = self.sb(ph, "wrt", [P, 8, 64], BF16)
            S.dma("pool", wrt[:], I["w_router"][l].rearrange("(kt p) n -> p kt n", p=P), writes=["wrt"])
            brb = self.sb(ph, "brb", [P, 64])
            S.dma("sp", brb[:], I["b_router"][l].partition_broadcast(P), writes=["brb"])
            lng = self.sb(ph, "lng2", [P, D])
            lnb = self.sb(ph, "lnb2", [P, D])
            S.dma("sp", lng[:], I["ln2_g"][l].partition_broadcast(P), writes=["lng"])
            S.dma("sp", lnb[:], I["ln2_b"][l].partition_broadcast(P), writes=["lnb"])
            fTb = self.sb(ph, "fTb", [P, 8, MAXT * P], BF16)
            wts = self.sb(ph, "wts", [P, MAXT, 64])
            acc = self.sb(ph, "acc", [P, MAXT, D])
            NW = 3
            wg = [self.sb(ph, "wg%d" % i, [P, 8, 256], BF16) for i in range(NW)]
            wu = [self.sb(ph, "wu%d" % i, [P, 8, 256], BF16) for i in range(NW)]
            wd = [self.sb(ph, "wd%d" % i, [P, 2, D], BF16) for i in range(NW)]
            sgt = [self.sb(ph, "sgt%d" % i, [P, 512]) for i in range(2)]
            hT = [[self.sb(ph, "hT%d%d" % (i, m), [P, 512], BF16) for m in range(2)] for i in range(2)]
            r_sc = self.sb(ph, "r_sc", [P, 64]); r_sel = self.sb(ph, "r_sel", [P, 64]); r_eq = self.sb(ph, "r_eq", [P, 64])
            r_s2 = self.sb(ph, "r_s2", [P, 64]); r_m1 = self.sb(ph, "r_m1", [P, 8]); r_m2 = self.sb(ph, "r_m2", [P, 8])
            r_gs = self.sb(ph, "r_gs", [P, 8]); r_t8 = self.sb(ph, "r_t8", [P, 8]); r_gm = self.sb(ph, "r_gm", [P, 8])
            r_gt = self.sb(ph, "r_gt", [P, 8]); r_sm = self.sb(ph, "r_sm", [P, 64]); r_e8 = self.sb(ph, "r_e8", [P, 8])
            r_em = self.sb(ph, "r_em", [P, 64]); r_w = self.sb(ph, "r_w", [P, 64]); r_ss = self.sb(ph, "r_ss", [P, 2])
            hx = [self.sb(ph, "h_hx%d" % i, [P, D]) for i in range(2)]
            tt_ = [self.sb(ph, "h_tt%d" % i, [P, D]) for i in range(2)]
            st6 = [self.sb(ph, "h_st6%d" % i, [P, 2, 6]) for i in range(2)]
            mv = [self.sb(ph, "h_mv%d" % i, [P, 2]) for i in range(2)]
            rs = [self.sb(ph, "h_rs%d" % i, [P, 2]) for i in range(2)]
            pgu = [self.psum(ph, "h_pgu%d" % i, [P, 512]) for i in range(4)]
            py = [self.psum(ph, "h_py%d" % i, [P, 2, 512]) for i in range(2)]

            if last:
                blocks = [(2, 16), (18, 16)]
            else:
                blocks = [(0, 18), (18, 16)]
            wcount = 0
            ccount = 0
            fcount = 0
            ycount = 0
            for (t0, ntl) in blocks:
                BW = ntl * P
                tok0 = t0 * P
                for kt in range(8):
                    S.dma("sp", fTb[:, kt, :BW], X["fT"][kt * P:(kt + 1) * P, tok0:tok0 + BW], writes=[("fTb", kt)])
                fkeys = [("fTb", kt) for kt in range(8)]
                for ti in range(ntl):
                    cols = slice(ti * P, (ti + 1) * P)
                    prt = pgu[ti % 4]
                    pk = ("pgu", ti % 4)
                    for kt in range(8):
                        S.op("pe", lambda: nc.tensor.matmul(prt[:, 0:64], lhsT=fTb[:, kt, cols], rhs=wrt[:, kt, :], start=(kt == 0), stop=(kt == 7)),
                             reads=[("fTb", kt), "wrt"], writes=[pk], pe_chain=True)
                    S.op("act", lambda: nc.scalar.activation(out=r_sc[:], in_=prt[:, 0:64], func=AF.Sigmoid), reads=[pk], writes=["r_sc"])
                    dve(lambda: V.tensor_tensor(out=r_sel[:], in0=r_sc[:], in1=brb[:], op=ALU.add), ["r_sc", "brb"], ["r_sel"])
                    sel3 = r_sel[:].rearrange("p (g e) -> p g e", g=8)
                    dve(lambda: V.tensor_reduce(out=r_m1[:], in_=sel3, axis=AX.X, op=ALU.max), ["r_sel"], ["r_m1"])
                    dve(lambda: V.tensor_tensor(out=r_eq[:].rearrange("p (g e) -> p g e", g=8), in0=sel3, in1=r_m1[:, :].unsqueeze(2).to_broadcast([P, 8, 8]), op=ALU.is_equal),
                        ["r_sel", "r_m1"], ["r_eq"])
                    dve(lambda: V.scalar_tensor_tensor(out=r_s2[:], in0=r_eq[:], scalar=-4.0, in1=r_sel[:], op0=ALU.mult, op1=ALU.add), ["r_eq", "r_sel"], ["r_s2"])
                    dve(lambda: V.tensor_reduce(out=r_m2[:], in_=r_s2[:].rearrange("p (g e) -> p g e", g=8), axis=AX.X, op=ALU.max), ["r_s2"], ["r_m2"])
                    dve(lambda: V.tensor_tensor(out=r_gs[:], in0=r_m1[:], in1=r_m2[:], op=ALU.add), ["r_m1", "r_m2"], ["r_gs"])
                    dve(lambda: V.max(out=r_t8[:], in_=r_gs[:]), ["r_gs"], ["r_t8"])
                    dve(lambda: V.tensor_scalar(out=r_gm[:], in0=r_gs[:], scalar1=r_t8[:, 3:4], scalar2=None, op0=ALU.is_ge), ["r_gs", "r_t8"], ["r_gm"])
                    dve(lambda: V.tensor_scalar(out=r_gt[:], in0=r_gm[:], scalar1=4.0, scalar2=-4.0, op0=ALU.mult, op1=ALU.add), ["r_gm"], ["r_gt"])
                    sm3 = r_sm[:].rearrange("p (g e) -> p g e", g=8)
                    dve(lambda: V.tensor_tensor(out=sm3, in0=sel3, in1=r_gm[:, :].unsqueeze(2).to_broadcast([P, 8, 8]), op=ALU.mult), ["r_sel", "r_gm"], ["r_sm"])
                    dve(lambda: V.tensor_tensor(out=sm3, in0=sm3, in1=r_gt[:, :].unsqueeze(2).to_broadcast([P, 8, 8]), op=ALU.add), ["r_sm", "r_gt"], ["r_sm"])
                    dve(lambda: V.max(out=r_e8[:], in_=r_sm[:]), ["r_sm"], ["r_e8"])
                    dve(lambda: V.tensor_scalar(out=r_em[:], in0=r_sm[:], scalar1=r_e8[:, 7:8], scalar2=None, op0=ALU.is_ge), ["r_sm", "r_e8"], ["r_em"])
                    dve(lambda: V.tensor_tensor(out=r_w[:], in0=r_sc[:], in1=r_em[:], op=ALU.mult), ["r_sc", "r_em"], ["r_w"])
                    dve(lambda: V.tensor_reduce(out=r_ss[:, 0:1], in_=r_w[:], axis=AX.X, op=ALU.add), ["r_w"], ["r_ss0"])
                    dve(lambda: V.reciprocal(out=r_ss[:, 1:2], in_=r_ss[:, 0:1]), ["r_ss0"], ["r_ss1"])
                    dve(lambda: V.tensor_scalar(out=wts[:, ti, :], in0=r_w[:], scalar1=r_ss[:, 1:2], scalar2=2.5, op0=ALU.mult, op1=ALU.mult), ["r_w", "r_ss1"], [("wts", ti)])
                chunks = [(c * 512, min(512, BW - c * 512)) for c in range((BW + 511) // 512)]
                items = [(e, c0, Wd) for e in range(65) for (c0, Wd) in chunks]

                def wsrc(e):
                    if e < 64:
                        return (I["w_exp_gate"][l, e], I["w_exp_up"][l, e], I["w_exp_down"][l, e])
                    return (I["w_sh_gate"][l], I["w_sh_up"][l], I["w_sh_down"][l])

                def load_w(e):
                    ws = (wbase + e) % NW
                    srcs = wsrc(e)
                    S.dma("pool", wg[ws][:], srcs[0].rearrange("(kt p) n -> p kt n", p=P), writes=[("wg", ws)])
                    S.dma("pool", wu[ws][:], srcs[1].rearrange("(kt p) n -> p kt n", p=P), writes=[("wu", ws)])
                    S.dma("pool", wd[ws][:], srcs[2].rearrange("(kt p) n -> p kt n", p=P), writes=[("wd", ws)])

                def emit_gu(k):
                    nonlocal fcount
                    e, c0, Wd = items[k]
                    ws = (wbase + e) % NW
                    cb = k % 2
                    cs_ = slice(c0, c0 + Wd)
                    if c0 == 0 and e + 1 < 65:
                        load_w(e + 1)
                    for mt in range(2):
                        pg_i = 2 * mt
                        pu_i = 2 * mt + 1
                        for (pi, wt_, wk) in ((pg_i, wg[ws], ("wg", ws)), (pu_i, wu[ws], ("wu", ws))):
                            for kt in range(8):
                                S.op("pe", lambda: nc.tensor.matmul(pgu[pi][:, :Wd], lhsT=wt_[:, kt, mt * P:(mt + 1) * P], rhs=fTb[:, kt, cs_], start=(kt == 0), stop=(kt == 7)),
                                     reads=[wk, ("fTb", kt)], writes=[("pgu", pi)], pe_chain=True)
                        fb = fcount % 2
                        fcount += 1
                        S.op("act", lambda: nc.scalar.activation(out=sgt[fb][:, :Wd], in_=pgu[pg_i][:, :Wd], func=AF.Silu), reads=[("pgu", pg_i)], writes=[("sgt", fb)])
                        dve(lambda: V.tensor_tensor(out=hT[cb][mt][:, :Wd], in0=pgu[pu_i][:, :Wd], in1=sgt[fb][:, :Wd], op=ALU.mult),
                            [("pgu", pu_i), ("sgt", fb)], [("hT", cb, mt)])

                def emit_down(k):
                    nonlocal ycount
                    e, c0, Wd = items[k]
                    ws = (wbase + e) % NW
                    cb = k % 2
                    for tl_ in range(Wd // P):
                        ti = c0 // P + tl_
                        yb_ = ycount % 2
                        path = (ycount // 2) % 2
                        ab = ycount % 2
                        ycount += 1
                        for nh in range(2):
                            for mt in range(2):
                                S.op("pe", lambda: nc.tensor.matmul(py[yb_][:, nh, :], lhsT=hT[cb][mt][:, tl_ * P:(tl_ + 1) * P], rhs=wd[ws][:, mt, nh * 512:(nh + 1) * 512],
                                                                    start=(mt == 0), stop=(mt == 1)),
                                     reads=[("hT", cb, mt), ("wd", ws)], writes=[("py", yb_, nh)], pe_chain=True)
                        pyf = py[yb_][:].rearrange("p a b -> p (a b)")
                        pyk = [("py", yb_, 0), ("py", yb_, 1)]
                        wcol = wts[:, ti, min(e, 63):min(e, 63) + 1]
                        if e == 64:
                            if path == 0:
                                S.op("act", lambda: nc.scalar.copy(out=tt_[ab][:], in_=pyf), reads=pyk, writes=[("htt", ab)])
                                S.op("pool", lambda: nc.gpsimd.tensor_tensor(out=acc[:, ti, :], in0=tt_[ab][:], in1=acc[:, ti, :], op=ALU.add),
                                     reads=[("htt", ab), ("acc", ti)], writes=[("acc", ti)])
                            else:
                                dve(lambda: V.tensor_tensor(out=acc[:, ti, :], in0=pyf, in1=acc[:, ti, :], op=ALU.add), pyk + [("acc", ti)], [("acc", ti)])
                        elif path == 0:
                            dst = acc[:, ti, :] if e == 0 else tt_[ab][:]
                            dkey = ("acc", ti) if e == 0 else ("htt", ab)
                            S.op("act", lambda: nc.scalar.activation(out=dst, in_=pyf, func=AF.Identity, scale=wcol),
                                 reads=pyk + [("wts", ti)], writes=[dkey])
                            if e > 0:
                                S.op("pool", lambda: nc.gpsimd.tensor_tensor(out=acc[:, ti, :], in0=tt_[ab][:], in1=acc[:, ti, :], op=ALU.add),
                                     reads=[("htt", ab), ("acc", ti)], writes=[("acc", ti)])
                        else:
                            dst = acc[:, ti, :] if e == 0 else hx[ab][:]
                            dkey = ("acc", ti) if e == 0 else ("hhx", ab)
                            dve(lambda: V.tensor_tensor(out=dst, in0=pyf, in1=wcol.to_broadcast([P, D]), op=ALU.mult), pyk + [("wts", ti)], [dkey])
                            if e > 0:
                                dve(lambda: V.tensor_tensor(out=acc[:, ti, :], in0=hx[ab][:], in1=acc[:, ti, :], op=ALU.add),
                                    [("hhx", ab), ("acc", ti)], [("acc", ti)])

                wbase = wcount
                wcount += 65
                load_w(0)
                for k in range(len(items) + 1):
                    if k < len(items):
                        emit_gu(k)
                    if k >= 1:
                        emit_down(k - 1)
                for ti in range(ntl):
                    i = t0 + ti
                    s = 1 if i < 2 else 0
                    hb = i % 2
                    S.dma("sp", hx[hb][:], X["h"][i * P:(i + 1) * P, :], writes=[("hhx", hb)])
                    S.op("pool", lambda: nc.gpsimd.tensor_tensor(out=tt_[hb][:], in0=acc[:, ti, :], in1=self.gb[:, 2 * s + 1, :], op=ALU.mult),
                         reads=[("acc", ti)], writes=[("htt", hb)])
                    dve(lambda: V.scalar_tensor_tensor(out=hx[hb][:], in0=hx[hb][:], scalar=DN_ALPHA, in1=tt_[hb][:], op0=ALU.mult, op1=ALU.add),
                        [("hhx", hb), ("htt", hb)], [("hhx", hb)])
                    ks = self.ln_stats(hx[hb], st6[hb], mv[hb], rs[hb], ("hhx", hb))
                    dve(lambda: V.tensor_scalar(out=tt_[hb][:], in0=hx[hb][:], scalar1=mv[hb][:, 0:1], scalar2=rs[hb][:, 1:2], op0=ALU.subtract, op1=ALU.mult),
                        [("hhx", hb)] + ks, [("htt", hb)])
                    S.op("pool", lambda: nc.gpsimd.tensor_tensor(out=tt_[hb][:], in0=tt_[hb][:], in1=lng[:], op=ALU.mult), reads=[("htt", hb), "lng"], writes=[("htt", hb)])
                    S.op("pool", lambda: nc.gpsimd.tensor_tensor(out=hx[hb][:], in0=tt_[hb][:], in1=lnb[:], op=ALU.add), reads=[("htt", hb), "lnb"], writes=[("hhx", hb)])
                    if last:
                        dst = self.out[(i - 2) * P:(i - 1) * P, :]
                    else:
                        dst = X["h"][i * P:(i + 1) * P, :]
                    S.dma("sp", dst, hx[hb][:], reads=[("hhx", hb)], writes=[("d_h2", hb)])

_ROPE = None


def make_in_maps(inputs, ncores=8, used=None):
    global _ROPE
    if _ROPE is None:
        _ROPE = _rope_tables()
    cosT, sinT = _ROPE
    perm = _rope_perm()
    w_in = np.ascontiguousarray(inputs["w_in"], dtype=np.float32)
    qcols = np.concatenate([768 + h * 128 + perm for h in range(4)])
    kcols = np.concatenate([1280 + h * 128 + perm for h in range(4)])
    w_qkp = np.ascontiguousarray(w_in[:, :, np.concatenate([qcols, kcols])])
    shared = {k: np.ascontiguousarray(v, dtype=np.float32) for k, v in inputs.items() if k not in ("x", "c", "ctx")}
    shared["w_qkp"] = w_qkp
    shared["rope_cos"] = cosT
    shared["rope_sin"] = sinT
    for n, a in _consts().items():
        shared["k_" + n] = a
    maps = []
    for core in range(ncores):
        b = core % 4
        m = dict(shared)
        m["x"] = np.ascontiguousarray(inputs["x"][b], dtype=np.float32)
        m["ctx"] = np.ascontiguousarray(inputs["ctx"][b], dtype=np.float32)
        m["c"] = np.ascontiguousarray(inputs["c"][b], dtype=np.float32)
        if used is not None:
            m = {k: v for k, v in m.items() if k in used}
        maps.append(m)
    return maps


def kernel(**inputs):
    bld = Builder()
    nc = bld.build()
    maps = make_in_maps(inputs, used=set(bld.I.keys()))
    res = run_bass_kernel_spmd(nc, maps, core_ids=list(range(8)))
    out = np.stack([res.results[b]["out"] for b in range(4)], 0)
    return out.astype(np.float32)
```

```python
import contextlib, math
import numpy as np
import concourse.bass as bass
import concourse.mybir as mybir
from concourse.bass_utils import run_bass_kernel_spmd

F32 = mybir.dt.float32
BF16 = mybir.dt.bfloat16
AF = mybir.ActivationFunctionType
ALU = mybir.AluOpType
AX = mybir.AxisListType

P = 128
D = 1024
NCTX = 256
NLAT = 4096
NTOK = NCTX + NLAT
NT = NTOK // P
DEPTH = 2
LN_EPS = 1e-5
DN_ALPHA = (2 * DEPTH) ** 0.25
IN_WIDTH = 5376


class Sched:
    def __init__(self, nc, stack):
        self.nc = nc
        self.stack = stack
        self.eng = {"pe": nc.tensor, "act": nc.scalar, "dve": nc.vector,
                    "pool": nc.gpsimd, "sp": nc.sync}
        self.sem = {}
        self.cnt = {}
        for e in self.eng:
            self.sem[e] = stack.enter_context(nc.semaphore("s_" + e))
            self.cnt[e] = 0
        self.waited = {e: {} for e in self.eng}
        self.res = {}
        self.dsem = {}
        self.free_dsems = {}
        self.dtype_of = {}
        self.ninst = 0

    def _r(self, key):
        r = self.res.get(key)
        if r is None:
            r = self.res[key] = {"w": {}, "r": {}}
        return r

    def _need(self, deps, reads, writes):
        for k in reads:
            for s, v in self._r(k)["w"].items():
                if deps.get(s, 0) < v:
                    deps[s] = v
        for k in writes:
            r = self._r(k)
            for s, v in r["w"].items():
                if deps.get(s, 0) < v:
                    deps[s] = v
            for s, v in r["r"].items():
                if deps.get(s, 0) < v:
                    deps[s] = v

    def _emit_waits(self, e, deps, skip_self=False):
        w = self.waited[e]
        for s, v in deps.items():
            if skip_self and s == e:
                continue
            if w.get(s, 0) >= v:
                continue
            self.eng[e].wait_ge(self.sem[s], v)
            w[s] = v
            self.ninst += 1

    def _record(self, ev, reads, writes):
        s, v = ev
        for k in reads:
            r = self._r(k)["r"]
            if r.get(s, 0) < v:
                r[s] = v
        for k in writes:
            r = self._r(k)
            r["w"] = {s: v}
            r["r"] = {}

    def op(self, e, fn, reads=(), writes=(), pe_chain=False):
        deps = {}
        self._need(deps, reads, writes)
        self._emit_waits(e, deps, skip_self=(e == "pe" and pe_chain))
        ins = fn()
        self.cnt[e] += 1
        ins.then_inc(self.sem[e], 1)
        self._record((e, self.cnt[e]), reads, writes)
        self.ninst += 1
        return ins

    def dma(self, q, out, in_, reads=(), writes=(), semkey=None, **kw):
        if semkey is None:
            semkey = writes[0]
        sname = self.dsem.get(semkey)
        qt = "sw" if q == "pool" else "hw"
        if sname is None:
            pool_ = self.free_dsems.setdefault(qt, [])
            if pool_:
                sname = pool_.pop()
            else:
                sname = "d%d" % (len(self.sem))
                self.sem[sname] = self.stack.enter_context(self.nc.semaphore(sname))
                self.cnt[sname] = 0
                self.dtype_of[sname] = qt
            self.dsem[semkey] = sname
        assert self.dtype_of[sname] == qt, (semkey, sname, qt)
        deps = {}
        self._need(deps, reads, writes)
        self._emit_waits(q, deps)
        ins = self.eng[q].dma_start(out=out, in_=in_, **kw)
        self.cnt[sname] += 16
        ins.then_inc(self.sem[sname], 16)
        self._record((sname, self.cnt[sname]), reads, writes)
        self.ninst += 1
        return ins

    def barrier(self, final=False):
        engs = ["sp"] if final else list(self.eng)
        for e in engs:
            w = self.waited[e]
            for s, v in self.cnt.items():
                if v > 0 and w.get(s, 0) < v:
                    self.eng[e].wait_ge(self.sem[s], v)
                    w[s] = v
                    self.ninst += 1
        if not final:
            self.res = {}
            for sn in self.dsem.values():
                self.free_dsems.setdefault(self.dtype_of[sn], []).append(sn)
            self.dsem = {}


def _rope_tables():
    rows = NLAT // 64
    r, col = np.meshgrid(np.arange(rows), np.arange(64), indexing="ij")
    inv_freq = (10000.0 ** (-np.arange(0, 32, 2, dtype=np.float32) / 32)).astype(np.float32)
    ang = np.concatenate([r.reshape(-1, 1).astype(np.float32) * inv_freq,
                          col.reshape(-1, 1).astype(np.float32) * inv_freq], -1)
    cos = np.cos(ang).astype(np.float32)
    sin = np.sin(ang).astype(np.float32)
    cosT = np.ones((128, NTOK), np.float32)
    sinT = np.zeros((128, NTOK), np.float32)
    for m in range(2):
        for d in range(64):
            f = (d % 16) if d < 32 else 16 + (d % 16)
            sgn = -1.0 if (d % 32) < 16 else 1.0
            cosT[m * 64 + d, NCTX:] = cos[:, f]
            sinT[m * 64 + d, NCTX:] = sgn * sin[:, f]
    return cosT, sinT


def _rope_perm():
    perm = np.zeros(128, np.int64)
    for m in range(2):
        for d in range(64):
            pd = d + 16 if (d % 32) < 16 else d - 16
            perm[m * 64 + d] = m * 64 + pd
    return perm


def _consts():
    c = {}
    c["ident"] = np.eye(128, dtype=np.float32)
    tt = np.arange(128)
    c["mfwd"] = (tt[:, None] <= tt[None, :]).astype(np.float32)
    c["mbwd"] = (tt[:, None] >= tt[None, :]).astype(np.float32)
    sig = np.stack([tt, 127 - tt], 1).astype(np.float32)
    c["sigp"] = sig
    c["sigf"] = np.broadcast_to(np.stack([tt, 127 - tt], 0).astype(np.float32)[None], (128, 2, 128)).copy()
    e8 = np.zeros((128, 8, 128), np.float32)
    for k in range(8):
        e8[k, k, :] = 1.0
    c["e8"] = e8
    sel = np.zeros((128, 64, 128), np.float32)
    for k in range(64):
        sel[k, k, :] = 1.0
    c["sel"] = sel
    return c


class Builder:
    def __init__(self, nlayers=DEPTH, dbg=()):
        self.nlayers = nlayers
        self.dbg = set(dbg)
        self.nc = bass.Bass("TRN2", target_bir_lowering=False)
        self.dbg_out = {}

    def din(self, name, shape, dt=F32):
        return self.nc.dram_tensor(name, list(shape), dt, kind="ExternalInput").ap()

    def dscr(self, name, shape, dt):
        if name in self.dbg:
            t = self.nc.dram_tensor("dbg_" + name, list(shape), dt, kind="ExternalOutput").ap()
            self.dbg_out[name] = "dbg_" + name
            return t
        return self.nc.dram_tensor(name, list(shape), dt, kind="Internal").ap()

    def declare(self):
        L = DEPTH
        shapes = {"x": [NLAT, D], "ctx": [NCTX, D], "c": [D], "c_ctx": [D], "w_mod": [L, D, 6 * D], "b_mod": [L, 6 * D],
                  "w_in": [L, D, IN_WIDTH], "w_qkp": [L, D, 1024], "rope_cos": [128, NTOK], "rope_sin": [128, NTOK]}
        for n, sh in [("s5_lam_re", [L, 2, 16, 64]), ("s5_lam_im", [L, 2, 16, 64]), ("s5_log_step", [L, 2, 16]),
                      ("s5_b_re", [L, 2, 16, 64, 16]), ("s5_b_im", [L, 2, 16, 64, 16]),
                      ("s5_c_re", [L, 2, 16, 16, 64]), ("s5_c_im", [L, 2, 16, 16, 64]), ("s5_d", [L, 256]),
                      ("gm_w_s", [L, 4, 128, 128]), ("gm_b_s", [L, 4, 128]), ("da_lam", [L, 4, 64]),
                      ("da_subln_g", [L, 128]), ("w_glu_val", [L, 256, D]), ("w_glu_gate", [L, 256, D]),
                      ("w_proj_gm", [L, 256, D]), ("w_proj_da", [L, 512, D]), ("w_out", [L, D, D]),
                      ("ln1_g", [L, D]), ("ln1_b", [L, D]), ("ln2_g", [L, D]), ("ln2_b", [L, D]),
                      ("w_router", [L, D, 64]), ("b_router", [L, 64]),
                      ("w_exp_gate", [L, 64, D, 256]), ("w_exp_up", [L, 64, D, 256]), ("w_exp_down", [L, 64, 256, D]),
                      ("w_sh_gate", [L, D, 256]), ("w_sh_up", [L, D, 256]), ("w_sh_down", [L, 256, D])]:
            shapes[n] = sh
        for n, a in _consts().items():
            shapes["k_" + n] = list(a.shape)
        bld = self

        class Lazy(dict):
            def __missing__(self, n):
                self[n] = bld.din(n, shapes[n])
                return self[n]
        self.I = Lazy()
        self.out = self.nc.dram_tensor("out", [NLAT, D], F32, kind="ExternalOutput").ap()
        Sx = {}
        Sx["h"] = self.dscr("h", [NTOK, D], F32)
        Sx["uT"] = self.dscr("uT", [D, NTOK], BF16)
        Sx["aT"] = self.dscr("aT", [256, NTOK], BF16)
        Sx["qT"] = self.dscr("qT", [512, NTOK], BF16)
        Sx["kT"] = self.dscr("kT", [512, NTOK], BF16)
        Sx["v"] = self.dscr("v", [NTOK, 512], BF16)
        Sx["yaT"] = self.dscr("yaT", [256, NTOK], BF16)
        Sx["ybT"] = self.dscr("ybT", [256, NTOK], BF16)
        Sx["ycT"] = self.dscr("ycT", [512, NTOK], BF16)
        Sx["fT"] = self.dscr("fT", [D, NTOK], BF16)
        self.X = Sx

    def sb(self, ph, name, shape, dt=F32):
        return ph.enter_context(self.nc.sbuf_tensor(name + getattr(self, "sfx", ""), list(shape), dt))

    def psum(self, ph, name, shape, dt=F32):
        return ph.enter_context(self.nc.psum_tensor(name + getattr(self, "sfx", ""), list(shape), dt))

    def hsrc(self, l, i):
        if l == 0:
            if i < 2:
                return self.I["ctx"][i * P:(i + 1) * P, :]
            return self.I["x"][(i - 2) * P:(i - 1) * P, :]
        return self.X["h"][i * P:(i + 1) * P, :]

    def build(self):
        nc = self.nc
        self.declare()
        with contextlib.ExitStack() as st:
            self.S = S = Sched(nc, st)
            self.ident = self.sb(st, "ident", [P, P], F32)
            self.identb = self.sb(st, "identb", [P, P], BF16)
            S.dma("sp", self.ident[:], self.I["k_ident"], writes=["ident"])
            S.op("dve", lambda: nc.vector.tensor_copy(out=self.identb[:], in_=self.ident[:]), reads=["ident"], writes=["identb"])
            self.modc = self.sb(st, "modc", [P, 2, 48], F32)
            self.gb = self.sb(st, "gb", [P, 4, D], F32)
            for l in range(self.nlayers):
                self.layer(l)
            S.barrier(final=True)
        return nc

    def layer(self, l):
        S = self.S
        last = (l == DEPTH - 1)
        self.sfx = '_L%d' % l
        self.phase_mod(l)
        S.barrier()
        if 'noproj' not in self.dbg:
            self.phase_proj(l)
            S.barrier()
        if 'nos5' not in self.dbg:
            self.phase_s5(l)
            S.barrier()
        if 'noattn' not in self.dbg:
            self.phase_attn(l)
            S.barrier()
        if 'nomerge' not in self.dbg:
            self.phase_merge(l)
            S.barrier()
        if 'nomoe' not in self.dbg:
            self.phase_moe(l)
            S.barrier()

    def phase_mod(self, l):
        nc, S, I = self.nc, self.S, self.I
        with contextlib.ExitStack() as ph:
            cs = self.sb(ph, "cs", [P, 2, 8])
            scs = self.sb(ph, "scs", [P, 8, 2])
            rep = self.sb(ph, "rep", [P, 2, 8, P])
            bm = self.sb(ph, "bm", [P, 48])
            bmb = self.sb(ph, "bmb", [P, 2, D])
            wm = [self.sb(ph, "wm%d" % i, [P, 8, 512]) for i in range(2)]
            pmod = self.psum(ph, "pmod", [P, 48, 2])
            pg = [self.psum(ph, "pg%d" % i, [P, 512]) for i in range(2)]
            S.dma("sp", cs[:, 0, :], I["c"].rearrange("(k p) -> p k", p=P), writes=["cs0"], allow_slow_non_contiguous=True)
            S.dma("sp", cs[:, 1, :], I["c_ctx"].rearrange("(k p) -> p k", p=P), writes=["cs1"], allow_slow_non_contiguous=True)
            S.dma("sp", bm[:], I["b_mod"][l].rearrange("(j p) -> p j", p=P), writes=["bm"], allow_slow_non_contiguous=True)
            S.dma("sp", bmb[:, 0, :], I["b_mod"][l, 2 * D:3 * D].partition_broadcast(P), writes=["bmb0"])
            S.dma("sp", bmb[:, 1, :], I["b_mod"][l, 5 * D:6 * D].partition_broadcast(P), writes=["bmb1"])
            for s in range(2):
                S.op("act", lambda: nc.scalar.activation(out=scs[:, :, s], in_=cs[:, s, :], func=AF.Silu),
                     reads=["cs%d" % s], writes=[("scs", s)])
                S.op("dve", lambda: nc.vector.tensor_copy(out=rep[:, s, :, :], in_=scs[:, :, s].unsqueeze(2).to_broadcast([P, 8, P])),
                     reads=[("scs", s)], writes=[("rep", s)])
            for c in range(12):
                w = wm[c % 2]
                S.dma("sp", w[:], I["w_mod"][l][:, c * 512:(c + 1) * 512].rearrange("(kt p) n -> p kt n", p=P),
                      writes=[("wm", c % 2)])
                for j in range(4):
                    jj = c * 4 + j
                    for kt in range(8):
                        S.op("pe", lambda: nc.tensor.matmul(pmod[:, jj, :], lhsT=w[:, kt, j * P:(j + 1) * P], rhs=scs[:, kt, :],
                                                            start=(kt == 0), stop=(kt == 7)),
                             reads=[("wm", c % 2), ("scs", 0), ("scs", 1)], writes=["pmod"], pe_chain=True)
                if c in (4, 5, 10, 11):
                    gi = 0 if c < 6 else 1
                    half = c % 2
                    for s in range(2):
                        pgt = pg[s]
                        for kt in range(8):
                            S.op("pe", lambda: nc.tensor.matmul(pgt[:], lhsT=rep[:, s, kt, :], rhs=w[:, kt, :],
                                                                start=(kt == 0), stop=(kt == 7)),
                                 reads=[("wm", c % 2), ("rep", s)], writes=[("pg", s)], pe_chain=True)
                        S.op("dve", lambda: nc.vector.tensor_tensor(out=self.gb[:, 2 * s + gi, half * 512:(half + 1) * 512], in0=pgt[:],
                                                                    in1=bmb[:, gi, half * 512:(half + 1) * 512], op=ALU.add),
                             reads=[("pg", s), "bmb%d" % gi], writes=[("gb", 2 * s + gi, half)])
            for s in range(2):
                S.op("dve", lambda: nc.vector.tensor_tensor(out=self.modc[:, s, :], in0=pmod[:, :, s], in1=bm[:], op=ALU.add),
                     reads=["pmod", "bm"], writes=[("modc", s)])
                for c0 in (8, 32):
                    S.op("dve", lambda: nc.vector.tensor_scalar(out=self.modc[:, s, c0:c0 + 8], in0=self.modc[:, s, c0:c0 + 8], scalar1=1.0, scalar2=None, op0=ALU.add),
                         reads=[("modc", s)], writes=[("modc", s)])
            if "modc" in self.dbg:
                t = self.nc.dram_tensor("dbg_modc%d" % l, [P, 2, 48], F32, kind="ExternalOutput").ap()
                S.dma("sp", t, self.modc[:], reads=[("modc", 0), ("modc", 1)], writes=["dbg_modc"])
                t2 = self.nc.dram_tensor("dbg_gb%d" % l, [P, 4, D], F32, kind="ExternalOutput").ap()
                S.dma("sp", t2, self.gb[:], reads=[("gb", a, b) for a in range(4) for b in range(2)], writes=["dbg_gb"])

    def phase_proj(self, l):
        nc, S, I, X = self.nc, self.S, self.I, self.X
        with contextlib.ExitStack() as ph:
            wsplit = {"a": (0, 256), "zu": (256, 512), "zv": (512, 768), "q": (768, 1280), "k": (1280, 1792), "v": (1792, 2304)}
            W = {}
            for n, (c0, c1) in wsplit.items():
                W[n] = self.sb(ph, "w_" + n, [P, 8, c1 - c0], BF16)
                S.dma("pool", W[n][:], I["w_in"][l][:, c0:c1].rearrange("(kt p) n -> p kt n", p=P), writes=["w_" + n])
            for i, n in enumerate(("qp", "kp")):
                W[n] = self.sb(ph, "w_" + n, [P, 8, 512], BF16)
                S.dma("pool", W[n][:], I["w_qkp"][l][:, i * 512:(i + 1) * 512].rearrange("(kt p) n -> p kt n", p=P), writes=["w_" + n])
            wsn = self.sb(ph, "wsn", [P, 4, P], BF16)
            wsT = self.sb(ph, "wsT", [P, 4, P], BF16)
            bsT = self.sb(ph, "bsT", [P, 2, P], F32)
            S.dma("pool", wsn[:], I["gm_w_s"][l].rearrange("h t s -> t h s"), writes=["wsn"])
            for mt in range(2):
                for hh in range(2):
                    S.dma("sp", bsT[64 * hh:64 * hh + 64, mt, :], I["gm_b_s"][l, 2 * mt + hh, :].partition_broadcast(64),
                          writes=[("bsT", mt, hh)])
            bsT_keys = [("bsT", mt, hh) for mt in range(2) for hh in range(2)]

            NH = 3
            hx = [self.sb(ph, "hx%d" % i, [P, D]) for i in range(NH)]
            xn = [self.sb(ph, "xn%d" % i, [P, D], BF16) for i in range(2)]
            st6 = self.sb(ph, "st6", [P, 2, 2, 6])
            mv = self.sb(ph, "mv", [P, 2, 2])
            rs = self.sb(ph, "rs", [P, 2, 2])
            uT = [self.sb(ph, "uTb%d" % i, [P, 8, 512], BF16) for i in range(2)]
            cosb = [self.sb(ph, "cos%d" % i, [P, 512]) for i in range(2)]
            sinb = [self.sb(ph, "sin%d" % i, [P, 512]) for i in range(2)]
            aTb = [self.sb(ph, "aTb%d" % i, [P, 2, 512], BF16) for i in range(2)]
            zuT = [self.sb(ph, "zuT%d" % i, [P, 2, 512], BF16) for i in range(2)]
            ybT = [self.sb(ph, "ybT%d" % i, [P, 2, 512], BF16) for i in range(2)]
            qkT = [self.sb(ph, "qkT%d" % i, [P, 8, 512], BF16) for i in range(2)]
            vb = [self.sb(ph, "vb%d" % i, [P, 4, 512], BF16) for i in range(2)]
            zg = [self.sb(ph, "zg%d" % i, [P, 256]) for i in range(2)]
            zvn = [self.sb(ph, "zvn%d" % i, [P, 256], BF16) for i in range(2)]
            zst = self.sb(ph, "zst", [P, 2, 6])
            zmv = self.sb(ph, "zmv", [P, 2, 2])
            zrs = self.sb(ph, "zrs", [P, 2, 2])
            t1 = [self.sb(ph, "t1_%d" % i, [P, 512]) for i in range(2)]
            t2 = [self.sb(ph, "t2_%d" % i, [P, 512]) for i in range(2)]
            tg = [self.sb(ph, "tg%d" % i, [P, 2, P]) for i in range(2)]
            tp = [self.psum(ph, "tp%d" % i, [P, 8, P], BF16) for i in range(2)]
            for h in range(4):
                S.op("pe", lambda: nc.tensor.transpose(tp[0][:, h, :], wsn[:, h, :], self.identb[:]), reads=["wsn", "identb"], writes=[("tp", 0)], pe_chain=True)
            S.op("dve", lambda: nc.vector.tensor_copy(out=wsT[:], in_=tp[0][:, 0:4, :]), reads=[("tp", 0)], writes=["wsT"])
            pa = [self.psum(ph, "pa%d" % i, [P, 512]) for i in range(4)]
            pz_ = [self.psum(ph, "pz%d" % i, [P, 256]) for i in range(2)]

            chunks = [(0, 2)] + [(2 + 4 * c, 4) for c in range(8)]
            tcount = 0
            pcount = 0

            def emit_ln(ci):
                nonlocal tcount
                t0, ntl = chunks[ci]
                s = 1 if ci == 0 else 0
                Wd = ntl * P
                tok0 = t0 * P
                cb = ci % 2
                ub = uT[cb]
                S.dma("sp", cosb[cb][:, :Wd], I["rope_cos"][:, tok0:tok0 + Wd], writes=[("cos", cb)])
                S.dma("sp", sinb[cb][:, :Wd], I["rope_sin"][:, tok0:tok0 + Wd], writes=[("sin", cb)])
                for ti in range(ntl):
                    i = t0 + ti
                    hb = tcount % NH
                    xb = tcount % 2
                    tcount += 1
                    S.dma("sp", hx[hb][:], self.hsrc(l, i), writes=[("hx", hb)])
                    for hf in range(2):
                        S.op("dve", lambda: nc.vector.bn_stats(out=st6[:, xb, hf, :], in_=hx[hb][:, hf * 512:(hf + 1) * 512]),
                             reads=[("hx", hb)], writes=[("st6", xb, hf)])
                    S.op("dve", lambda: nc.vector.bn_aggr(out=mv[:, xb, :], in_=st6[:, xb, :, :].rearrange("p a b -> p (a b)")),
                         reads=[("st6", xb, 0), ("st6", xb, 1)], writes=[("mv", xb)])
                    S.op("act", lambda: nc.scalar.activation(out=rs[:, xb, 0:1], in_=mv[:, xb, 1:2], func=AF.Sqrt, bias=LN_EPS, scale=1.0),
                         reads=[("mv", xb)], writes=[("rs0", xb)])
                    S.op("dve", lambda: nc.vector.reciprocal(out=rs[:, xb, 1:2], in_=rs[:, xb, 0:1]), reads=[("rs0", xb)], writes=[("rs1", xb)])
                    S.op("dve", lambda: nc.vector.tensor_scalar(out=xn[xb][:], in0=hx[hb][:], scalar1=mv[:, xb, 0:1], scalar2=rs[:, xb, 1:2],
                                                                op0=ALU.subtract, op1=ALU.mult),
                         reads=[("hx", hb), ("mv", xb), ("rs1", xb)], writes=[("xn", xb)])
                    tpb = tp[xb]
                    for kt in range(8):
                        S.op("pe", lambda: nc.tensor.transpose(tpb[:, kt, :], xn[xb][:, kt * P:(kt + 1) * P], self.identb[:]),
                             reads=[("xn", xb), "identb"], writes=[("tp", xb)], pe_chain=True)
                    for kt in range(8):
                        S.op("act", lambda: nc.scalar.activation(out=ub[:, kt, ti * P:(ti + 1) * P], in_=tpb[:, kt, :], func=AF.Identity,
                                                                 scale=self.modc[:, s, 8 + kt:9 + kt], bias=self.modc[:, s, kt:kt + 1]),
                             reads=[("tp", xb), ("modc", s)], writes=[("uT", cb, ti)])
                ukeys_ = [("uT", cb, ti) for ti in range(ntl)]
                S.dma("sp", X["uT"].rearrange("(kt p) t -> p kt t", p=P)[:, :, tok0:tok0 + Wd], ub[:, :, :Wd], reads=ukeys_, writes=[("d_uT", ci % 2)])

            emit_ln(0)
            for ci, (t0, ntl) in enumerate(chunks):
                s = 1 if ci == 0 else 0
                Wd = ntl * P
                tok0 = t0 * P
                cb = ci % 2
                ub = uT[cb]
                ukeys = [("uT", cb, ti) for ti in range(ntl)]
                if ci + 1 < len(chunks):
                    emit_ln(ci + 1)

                def mm_fm(pt, wt, c0):
                    for kt in range(8):
                        S.op("pe", lambda: nc.tensor.matmul(pt[:, :Wd], lhsT=wt[:, kt, c0:c0 + P], rhs=ub[:, kt, :Wd], start=(kt == 0), stop=(kt == 7)),
                             reads=ukeys + [wt_key[id(wt)]], writes=[pkey[id(pt)]], pe_chain=True)

                wt_key = {id(W[n]): "w_" + n for n in W}
                pkey = {id(pa[i]): ("pa", i) for i in range(4)}

                for mt in range(2):
                    pt = pa[pcount % 4]; pcount += 1
                    mm_fm(pt, W["a"], mt * P)
                    S.op("act", lambda: nc.scalar.copy(out=aTb[cb][:, mt, :Wd], in_=pt[:, :Wd]), reads=[pkey[id(pt)]], writes=[("aTb", cb, mt)])
                S.dma("sp", X["aT"].rearrange("(mt p) t -> p mt t", p=P)[:, :, tok0:tok0 + Wd], aTb[cb][:, :, :Wd],
                      reads=[("aTb", cb, 0), ("aTb", cb, 1)], writes=[("d_aT", ci)])
                for mt in range(2):
                    pt = pa[pcount % 4]; pcount += 1
                    mm_fm(pt, W["zu"], mt * P)
                    S.op("act", lambda: nc.scalar.activation(out=zuT[cb][:, mt, :Wd], in_=pt[:, :Wd], func=AF.Gelu_apprx_tanh),
                         reads=[pkey[id(pt)]], writes=[("zuT", cb, mt)])
                for qi, (wn, wpn) in enumerate((("q", "qp"), ("k", "kp"))):
                    for hd in range(4):
                        p0 = pa[pcount % 4]; pcount += 1
                        p1 = pa[pcount % 4]; pcount += 1
                        mm_fm(p0, W[wn], hd * P)
                        mm_fm(p1, W[wpn], hd * P)
                        tb = (qi * 4 + hd) % 2
                        S.op("dve", lambda: nc.vector.tensor_tensor(out=t1[tb][:, :Wd], in0=p0[:, :Wd], in1=cosb[cb][:, :Wd], op=ALU.mult),
                             reads=[pkey[id(p0)], ("cos", cb)], writes=[("t1", tb)])
                        S.op("dve", lambda: nc.vector.tensor_tensor(out=t2[tb][:, :Wd], in0=p1[:, :Wd], in1=sinb[cb][:, :Wd], op=ALU.mult),
                             reads=[pkey[id(p1)], ("sin", cb)], writes=[("t2", tb)])
                        S.op("pool", lambda: nc.gpsimd.tensor_tensor(out=qkT[cb][:, qi * 4 + hd, :Wd], in0=t1[tb][:, :Wd], in1=t2[tb][:, :Wd], op=ALU.add),
                             reads=[("t1", tb), ("t2", tb)], writes=[("qkT", cb, qi * 4 + hd)])
                    dst = X["qT"] if qi == 0 else X["kT"]
                    S.dma("sp", dst.rearrange("(h p) t -> p h t", p=P)[:, :, tok0:tok0 + Wd], qkT[cb][:, qi * 4:qi * 4 + 4, :Wd],
                          reads=[("qkT", cb, qi * 4 + hd) for hd in range(4)], writes=[("d_qk", ci, qi)])
                for ti in range(ntl):
                    i = t0 + ti
                    pt = pa[pcount % 4]; pcount += 1
                    for kt in range(8):
                        S.op("pe", lambda: nc.tensor.matmul(pt[:], lhsT=ub[:, kt, ti * P:(ti + 1) * P], rhs=W["v"][:, kt, :], start=(kt == 0), stop=(kt == 7)),
                             reads=[("uT", cb, ti), "w_v"], writes=[pkey[id(pt)]], pe_chain=True)
                    S.op("act", lambda: nc.scalar.copy(out=vb[cb][:, ti, :], in_=pt[:]), reads=[pkey[id(pt)]], writes=[("vb", cb, ti)])
                    zb = i % 2
                    for kt in range(8):
                        S.op("pe", lambda: nc.tensor.matmul(pz_[zb][:, :], lhsT=ub[:, kt, ti * P:(ti + 1) * P], rhs=W["zv"][:, kt, :], start=(kt == 0), stop=(kt == 7)),
                             reads=[("uT", cb, ti), "w_zv"], writes=[("pz", zb)], pe_chain=True)
                    S.op("act", lambda: nc.scalar.activation(out=zg[zb][:], in_=pz_[zb][:, :], func=AF.Gelu_apprx_tanh), reads=[("pz", zb)], writes=[("zg", zb)])
                    S.op("dve", lambda: nc.vector.bn_stats(out=zst[:, zb, :], in_=zg[zb][:]), reads=[("zg", zb)], writes=[("zst", zb)])
                    S.op("dve", lambda: nc.vector.bn_aggr(out=zmv[:, zb, :], in_=zst[:, zb, :]), reads=[("zst", zb)], writes=[("zmv", zb)])
                    S.op("act", lambda: nc.scalar.activation(out=zrs[:, zb, 0:1], in_=zmv[:, zb, 1:2], func=AF.Sqrt, bias=LN_EPS, scale=1.0),
                         reads=[("zmv", zb)], writes=[("zrs0", zb)])
                    S.op("dve", lambda: nc.vector.reciprocal(out=zrs[:, zb, 1:2], in_=zrs[:, zb, 0:1]), reads=[("zrs0", zb)], writes=[("zrs1", zb)])
                    S.op("dve", lambda: nc.vector.tensor_scalar(out=zvn[zb][:], in0=zg[zb][:], scalar1=zmv[:, zb, 0:1], scalar2=zrs[:, zb, 1:2],
                                                                op0=ALU.subtract, op1=ALU.mult),
                         reads=[("zg", zb), ("zmv", zb), ("zrs1", zb)], writes=[("zvn", zb)])
                    for h in range(4):
                        S.op("pe", lambda: nc.tensor.matmul(pz_[zb][64 * (h % 2):64 * (h % 2) + 64, (h // 2) * P:(h // 2 + 1) * P],
                                                            lhsT=zvn[zb][:, h * 64:(h + 1) * 64], rhs=wsT[:, h, :], start=True, stop=True),
                             reads=[("zvn", zb), "wsT", ("zg", zb)], writes=[("pz", zb)], pe_chain=True)
                    S.op("dve", lambda: nc.vector.tensor_tensor(out=tg[zb][:], in0=pz_[zb][:, :].rearrange("p (a b) -> p a b", a=2), in1=bsT[:], op=ALU.add),
                         reads=[("pz", zb)] + bsT_keys, writes=[("tg", zb)])
                    S.op("pool", lambda: nc.gpsimd.tensor_tensor(out=ybT[cb][:, :, ti * P:(ti + 1) * P], in0=tg[zb][:], in1=zuT[cb][:, :, ti * P:(ti + 1) * P], op=ALU.mult),
                         reads=[("tg", zb), ("zuT", cb, 0), ("zuT", cb, 1)], writes=[("ybT", cb, ti)])
                S.dma("sp", X["v"][tok0:tok0 + Wd, :].rearrange("(a p) n -> p a n", p=P), vb[cb][:, :ntl, :],
                      reads=[("vb", cb, ti) for ti in range(ntl)], writes=[("d_v", ci)])
                S.dma("sp", X["ybT"].rearrange("(mt p) t -> p mt t", p=P)[:, :, tok0:tok0 + Wd], ybT[cb][:, :, :Wd],
                      reads=[("ybT", cb, ti) for ti in range(ntl)], writes=[("d_yb", ci)])


    def phase_attn(self, l):
        nc, S, I, X = self.nc, self.S, self.I, self.X
        last = (l == DEPTH - 1)
        lam_init = 0.8 - 0.6 * math.exp(-0.3 * l)
        with contextlib.ExitStack() as ph:
            lamb = self.sb(ph, "lamb", [P, 4, 64])
            lt = self.sb(ph, "lt", [P, 2, 64])
            ls = self.sb(ph, "ls", [P, 4])
            neglam = self.sb(ph, "neglam", [P, 1])
            gsc = self.sb(ph, "gsc", [P, 1])
            onesb = self.sb(ph, "onesb", [P, P], BF16)
            S.dma("sp", lamb[:], I["da_lam"][l].partition_broadcast(P), writes=["lamb"])
            S.dma("sp", gsc[:], I["da_subln_g"][l].rearrange("(p o) -> p o", o=1), writes=["gsc"])
            S.op("dve", lambda: nc.vector.memset(onesb[:], 1.0), writes=["onesb"])
            for a in range(2):
                S.op("dve", lambda: nc.vector.tensor_tensor(out=lt[:, a, :], in0=lamb[:, 2 * a, :], in1=lamb[:, 2 * a + 1, :], op=ALU.mult),
                     reads=["lamb"], writes=[("lt", a)])
                S.op("dve", lambda: nc.vector.tensor_reduce(out=ls[:, a:a + 1], in_=lt[:, a, :], axis=AX.X, op=ALU.add),
                     reads=[("lt", a)], writes=[("ls", a)])
                S.op("act", lambda: nc.scalar.activation(out=ls[:, 2 + a:3 + a], in_=ls[:, a:a + 1], func=AF.Exp), reads=[("ls", a)], writes=[("le", a)])
            S.op("dve", lambda: nc.vector.tensor_tensor(out=neglam[:], in0=ls[:, 3:4], in1=ls[:, 2:3], op=ALU.subtract),
                 reads=[("le", 0), ("le", 1)], writes=["neglam"])
            S.op("dve", lambda: nc.vector.tensor_scalar(out=neglam[:], in0=neglam[:], scalar1=-lam_init, scalar2=None, op0=ALU.add),
                 reads=["neglam"], writes=["neglam"])
            S.op("dve", lambda: nc.vector.tensor_scalar(out=gsc[:], in0=gsc[:], scalar1=(1.0 - lam_init), scalar2=None, op0=ALU.mult),
                 reads=["gsc"], writes=["gsc"])

            kTz = [[self.sb(ph, "kTz%d_%d" % (b, m), [P, NTOK], BF16) for m in range(2)] for b in range(2)]
            vh = [self.sb(ph, "vh%d" % b, [P, NT, P], BF16) for b in range(2)]
            for b in range(2):
                for m in range(2):
                    S.op("pool", lambda: nc.gpsimd.memset(kTz[b][m][:], 0.0), writes=[("kTz", b, m)])
            qt = [self.sb(ph, "qt%d" % i, [P, 512], BF16) for i in range(2)]
            NPT = 6
            pT = [self.sb(ph, "pT%d" % i, [P, 512], BF16) for i in range(NPT)]
            rd = [self.sb(ph, "rd%d" % i, [P, 512]) for i in range(2)]
            o0 = self.sb(ph, "o0", [P, 512])
            o1 = self.sb(ph, "o1", [P, 512])
            oo = self.sb(ph, "oo", [P, 512])
            sq = self.sb(ph, "sq", [P, 512], BF16)
            rt = self.sb(ph, "rt", [P, 512])
            yc = [self.sb(ph, "yc%d" % i, [P, 512], BF16) for i in range(2)]
            NSC = 4
            sc = [self.psum(ph, "sc%d" % i, [P, 512]) for i in range(NSC)]
            acco = [self.psum(ph, "acco%d" % i, [P, 512]) for i in range(2)]
            accd = [self.psum(ph, "accd%d" % i, [P, 512]) for i in range(2)]
            dacc = [[self.sb(ph, "dacc%d%d" % (a, m), [P, 512]) for m in range(2)] for a in range(2)]
            dsum = self.sb(ph, "dsum", [P, 512])
            ones32 = self.sb(ph, "ones32", [P, P])
            S.op("dve", lambda: nc.vector.memset(ones32[:], 1.0), writes=["ones32"])
            nq = 0
            nstep = 0
            for h in range(4):
                hb = h % 2
                for m in range(2):
                    S.dma("sp", kTz[hb][m][64 * m:64 * m + 64, :], X["kT"][h * P + 64 * m:h * P + 64 * m + 64, :], writes=[("kTz", hb, m)])
                S.dma("sp", vh[hb][:], X["v"][:, h * P:(h + 1) * P].rearrange("(j p) e -> p j e", p=P), writes=[("vh", hb)])
                qchunks = [(NCTX + 512 * c, 512, list(range(NT))) for c in range(8)]
                if not last:
                    qchunks = [(0, NCTX, [0, 1])] + qchunks
                for (tok0, Wq, keys) in qchunks:
                    qb = nq % 2
                    nq += 1
                    S.dma("sp", qt[qb][:, :Wq], X["qT"][h * P:(h + 1) * P, tok0:tok0 + Wq], writes=[("qt", qb)])
                    steps = [(j, m) for j in keys for m in range(2)]
                    LAG = 3

                    def emit_qk(si):
                        j, m = steps[si]
                        g = (nstep + si)
                        S.op("pe", lambda: nc.tensor.matmul(sc[g % NSC][:, :Wq], lhsT=kTz[hb][m][:, j * P:(j + 1) * P], rhs=qt[qb][:, :Wq], start=True, stop=True),
                             reads=[("kTz", hb, m), ("qt", qb)], writes=[("sc", g % NSC)], pe_chain=True)
                        S.op("act", lambda: nc.scalar.activation(out=pT[g % NPT][:, :Wq], in_=sc[g % NSC][:, :Wq], func=AF.Exp, scale=0.125),
                             reads=[("sc", g % NSC)], writes=[("pT", g % NPT)])

                    def emit_pv(si):
                        j, m = steps[si]
                        g = (nstep + si)
                        first = (j == keys[0])
                        lastk = (j == keys[-1])
                        S.op("pe", lambda: nc.tensor.matmul(acco[m][:, :Wq], lhsT=vh[hb][:, j, :], rhs=pT[g % NPT][:, :Wq], start=first, stop=lastk),
                             reads=[("vh", hb), ("pT", g % NPT)], writes=[("acco", m)], pe_chain=True)
                        a = (si // 2) % 2
                        en, eo = ("dve", nc.vector) if a == 0 else ("pool", nc.gpsimd)
                        if si < 4:
                            S.op(en, lambda: eo.tensor_copy(out=dacc[a][m][:, :Wq], in_=pT[g % NPT][:, :Wq]), reads=[("pT", g % NPT)], writes=[("dacc", a, m)])
                        else:
                            S.op(en, lambda: eo.tensor_tensor(out=dacc[a][m][:, :Wq], in0=dacc[a][m][:, :Wq], in1=pT[g % NPT][:, :Wq], op=ALU.add),
                                 reads=[("pT", g % NPT), ("dacc", a, m)], writes=[("dacc", a, m)])

                    for si in range(len(steps) + LAG):
                        if si < len(steps):
                            emit_qk(si)
                        if si - LAG >= 0:
                            emit_pv(si - LAG)
                    nstep += len(steps)
                    for m in range(2):
                        S.op("dve", lambda: nc.vector.tensor_tensor(out=dsum[:, :Wq], in0=dacc[0][m][:, :Wq], in1=dacc[1][m][:, :Wq], op=ALU.add),
                             reads=[("dacc", 0, m), ("dacc", 1, m)], writes=["dsum"])
                        S.op("pe", lambda: nc.tensor.matmul(accd[m][:, :Wq], lhsT=ones32[:], rhs=dsum[:, :Wq], start=True, stop=True),
                             reads=["ones32", "dsum"], writes=[("accd", m)], pe_chain=True)
                        S.op("dve", lambda: nc.vector.reciprocal(out=rd[m][:, :Wq], in_=accd[m][:, :Wq]), reads=[("accd", m)], writes=[("rd", m)])
                    S.op("dve", lambda: nc.vector.tensor_tensor(out=o0[:, :Wq], in0=acco[0][:, :Wq], in1=rd[0][:, :Wq], op=ALU.mult),
                         reads=[("acco", 0), ("rd", 0)], writes=["o0"])
                    S.op("dve", lambda: nc.vector.tensor_tensor(out=o1[:, :Wq], in0=acco[1][:, :Wq], in1=rd[1][:, :Wq], op=ALU.mult),
                         reads=[("acco", 1), ("rd", 1)], writes=["o1"])
                    S.op("dve", lambda: nc.vector.scalar_tensor_tensor(out=oo[:, :Wq], in0=o1[:, :Wq], scalar=neglam[:, 0:1], in1=o0[:, :Wq], op0=ALU.mult, op1=ALU.add),
                         reads=["o0", "o1", "neglam"], writes=["oo"])
                    S.op("pool", lambda: nc.gpsimd.tensor_tensor(out=sq[:, :Wq], in0=oo[:, :Wq], in1=oo[:, :Wq], op=ALU.mult), reads=["oo"], writes=["sq"])
                    g = nstep % NSC
                    S.op("pe", lambda: nc.tensor.matmul(sc[g][:, :Wq], lhsT=onesb[:], rhs=sq[:, :Wq], start=True, stop=True),
                         reads=["onesb", "sq"], writes=[("sc", g)], pe_chain=True)
                    S.op("act", lambda: nc.scalar.activation(out=rt[:, :Wq], in_=sc[g][:, :Wq], func=AF.Sqrt, scale=1.0 / 128, bias=LN_EPS),
                         reads=[("sc", g)], writes=["rt"])
                    nstep += 1
                    S.op("dve", lambda: nc.vector.reciprocal(out=rt[:, :Wq], in_=rt[:, :Wq]), reads=["rt"], writes=["rt"])
                    yb_ = nq % 2
                    S.op("dve", lambda: nc.vector.scalar_tensor_tensor(out=yc[yb_][:, :Wq], in0=oo[:, :Wq], scalar=gsc[:, 0:1], in1=rt[:, :Wq], op0=ALU.mult, op1=ALU.mult),
                         reads=["oo", "gsc", "rt"], writes=[("yc", yb_)])
                    S.dma("sp", X["ycT"][h * P:(h + 1) * P, tok0:tok0 + Wq], yc[yb_][:, :Wq], reads=[("yc", yb_)], writes=[("d_yc", yb_)])

    def phase_s5(self, l):
        nc, S, I, X = self.nc, self.S, self.I, self.X
        TWO_PI = 2.0 * math.pi
        INV2PI = 1.0 / TWO_PI
        MAGIC = 12582912.0
        V = nc.vector

        def dve(fn, reads, writes):
            return S.op("dve", fn, reads=reads, writes=writes)

        def reduce_angle(out, x, tmp, shift, kx, kout, ktmp):
            dve(lambda: V.tensor_scalar(out=tmp, in0=x, scalar1=INV2PI, scalar2=shift * INV2PI + MAGIC, op0=ALU.mult, op1=ALU.add), [kx], [ktmp])
            dve(lambda: V.tensor_scalar(out=tmp, in0=tmp, scalar1=-MAGIC, scalar2=None, op0=ALU.add), [ktmp], [ktmp])
            dve(lambda: V.scalar_tensor_tensor(out=out, in0=tmp, scalar=-TWO_PI, in1=x, op0=ALU.mult, op1=ALU.add), [ktmp, kx], [kout])

        def cossin(cos_o, sin_o, x, tmp, tmp2, kx, kc_, ks_, ktmp, ktmp2):
            reduce_angle(tmp2, x, tmp, 0.0, kx, ktmp2, ktmp)
            S.op("act", lambda: nc.scalar.activation(out=sin_o, in_=tmp2, func=AF.Sin), reads=[ktmp2], writes=[ks_])
            reduce_angle(tmp2, x, tmp, math.pi / 2, kx, ktmp2, ktmp)
            S.op("act", lambda: nc.scalar.activation(out=cos_o, in_=tmp2, func=AF.Sin, bias=halfpi[:, 0:1]), reads=[ktmp2, "halfpi"], writes=[kc_])

        with contextlib.ExitStack() as ph:
            aT_sb = self.sb(ph, "aT_sb", [P, 2, NTOK], BF16)
            yacc = self.sb(ph, "yacc", [P, 2, NTOK], F32)
            dcol = self.sb(ph, "dcol", [P, 2])
            halfpi = self.sb(ph, "halfpi", [P, 1])
            sigp = self.sb(ph, "sigp", [P, 2])
            nsig = self.sb(ph, "nsig", [P, 2])
            sigf = self.sb(ph, "sigf", [P, 2, P])
            Bmat = [self.sb(ph, "Bmat%d" % d, [P, 2, 1024], BF16) for d in range(2)]
            WpreR = [self.sb(ph, "WpreR%d" % d, [P, 2, 512]) for d in range(2)]
            WpreI = [self.sb(ph, "WpreI%d" % d, [P, 2, 512]) for d in range(2)]
            WpostR = [self.sb(ph, "WpostR%d" % d, [P, 8, P]) for d in range(2)]
            WpostI = [self.sb(ph, "WpostI%d" % d, [P, 8, P]) for d in range(2)]
            A128R = [self.sb(ph, "A128R%d" % d, [P, 8]) for d in range(2)]
            A128I = [self.sb(ph, "A128I%d" % d, [P, 8]) for d in range(2)]
            Cm = [self.sb(ph, "Cm%d" % d, [P, 16, P], BF16) for d in range(2)]
            Mdir = [self.sb(ph, "Mdir%d" % d, [P, P], BF16) for d in range(2)]
            E8 = self.sb(ph, "E8", [P, 8, P], BF16)
            cTh = [[self.sb(ph, "cTh%d%d" % (d, k), [P, P], BF16) for k in range(2)] for d in range(2)]
            cTl = [[self.sb(ph, "cTl%d%d" % (d, k), [P, P], BF16) for k in range(2)] for d in range(2)]

            S.dma("sp", aT_sb[:], X["aT"].rearrange("(kc p) t -> p kc t", p=P), writes=["aT_sb"])
            S.dma("sp", dcol[:], I["s5_d"][l].rearrange("(kc p) -> p kc", p=P), writes=["dcol"], allow_slow_non_contiguous=True)
            S.dma("sp", sigp[:], I["k_sigp"], writes=["sigp"])
            S.dma("sp", sigf[:], I["k_sigf"], writes=["sigf"])
            S.dma("pool", Mdir[0][:], I["k_mfwd"], writes=[("Mdir", 0)])
            S.dma("pool", Mdir[1][:], I["k_mbwd"], writes=[("Mdir", 1)])
            S.dma("pool", E8[:], I["k_e8"], writes=["E8"])
            Mneg = [self.sb(ph, "Mneg%d" % d, [P, P], BF16) for d in range(2)]
            CmN = [self.sb(ph, "CmN%d" % d, [P, 8, P], BF16) for d in range(2)]
            for d in range(2):
                dve(lambda: V.tensor_scalar(out=Mneg[d][:], in0=Mdir[d][:], scalar1=-1.0, scalar2=None, op0=ALU.mult), [("Mdir", d)], [("Mneg", d)])
            dve(lambda: V.memset(halfpi[:], math.pi / 2), [], ["halfpi"])
            dve(lambda: V.tensor_scalar(out=nsig[:], in0=sigp[:], scalar1=-1.0, scalar2=None, op0=ALU.mult), ["sigp"], ["nsig"])
            for d in range(2):
                for k in range(2):
                    S.op("pool", lambda: nc.gpsimd.memset(cTh[d][k][:], 0.0), writes=[("cTh", d, k)])
                    S.op("pool", lambda: nc.gpsimd.memset(cTl[d][k][:], 0.0), writes=[("cTl", d, k)])
            for kc in range(2):
                dve(lambda: V.tensor_scalar(out=yacc[:, kc, :], in0=aT_sb[:, kc, :], scalar1=dcol[:, kc:kc + 1], scalar2=None, op0=ALU.mult),
                    ["aT_sb", "dcol"], [("yacc", kc, i) for i in range(NT)])

            with contextlib.ExitStack() as su:
                R = {}
                for n in ("LR", "LI", "lrdt", "ang", "mag", "cs", "sn", "tA", "tB", "are", "aim", "zre", "zim", "x", "t3", "t4"):
                    R[n] = self.sb(su, "r_" + n, [P, 1024])
                LS = self.sb(su, "r_LS", [P, 16])
                braw = [self.sb(su, "braw%d" % i, [P, 512]) for i in range(2)]
                CmS = self.sb(su, "CmS", [P, 16, P])
                Cc = {}
                for n in ("LR", "LI", "LS", "lrdt", "ang", "angr", "t", "e", "x", "cs", "sn", "mg"):
                    Cc[n] = self.sb(su, "c_" + n, [P, 8])
                for d in range(2):
                    k_ = lambda n: ("su", n)
                    S.dma("sp", R["LR"][:], I["s5_lam_re"][l, d].rearrange("g p -> (g p)").partition_broadcast(P), writes=[k_("LR")])
                    S.dma("sp", R["LI"][:], I["s5_lam_im"][l, d].rearrange("g p -> (g p)").partition_broadcast(P), writes=[k_("LI")])
                    S.dma("sp", LS[:], I["s5_log_step"][l, d].partition_broadcast(P), writes=[k_("LS")])
                    S.op("act", lambda: nc.scalar.activation(out=LS[:], in_=LS[:], func=AF.Exp), reads=[k_("LS")], writes=[k_("LS")])
                    dtb = LS[:, :].unsqueeze(2).to_broadcast([P, 16, 64])
                    v3 = lambda t: t[:].rearrange("p (g q) -> p g q", g=16)
                    dve(lambda: V.tensor_tensor(out=v3(R["lrdt"]), in0=v3(R["LR"]), in1=dtb, op=ALU.mult), [k_("LR"), k_("LS")], [k_("lrdt")])
                    dve(lambda: V.tensor_tensor(out=v3(R["ang"]), in0=v3(R["LI"]), in1=dtb, op=ALU.mult), [k_("LI"), k_("LS")], [k_("ang")])
                    S.op("act", lambda: nc.scalar.activation(out=R["mag"][:], in_=R["lrdt"][:], func=AF.Exp), reads=[k_("lrdt")], writes=[k_("mag")])
                    reduce_angle(R["tB"][:], R["ang"][:], R["tA"][:], 0.0, k_("ang"), k_("tB"), k_("tA"))
                    dve(lambda: V.tensor_copy(out=R["ang"][:], in_=R["tB"][:]), [k_("tB")], [k_("ang")])
                    cossin(R["cs"][:], R["sn"][:], R["ang"][:], R["tA"][:], R["tB"][:], k_("ang"), k_("cs"), k_("sn"), k_("tA"), k_("tB"))
                    dve(lambda: V.tensor_tensor(out=R["are"][:], in0=R["mag"][:], in1=R["cs"][:], op=ALU.mult), [k_("mag"), k_("cs")], [k_("are")])
                    dve(lambda: V.tensor_tensor(out=R["aim"][:], in0=R["mag"][:], in1=R["sn"][:], op=ALU.mult), [k_("mag"), k_("sn")], [k_("aim")])
                    dve(lambda: V.tensor_tensor(out=R["tA"][:], in0=R["LR"][:], in1=R["LR"][:], op=ALU.mult), [k_("LR")], [k_("tA")])
                    dve(lambda: V.tensor_tensor(out=R["tB"][:], in0=R["LI"][:], in1=R["LI"][:], op=ALU.mult), [k_("LI")], [k_("tB")])
                    dve(lambda: V.tensor_tensor(out=R["cs"][:], in0=R["tA"][:], in1=R["tB"][:], op=ALU.add), [k_("tA"), k_("tB")], [k_("cs")])
                    dve(lambda: V.reciprocal(out=R["cs"][:], in_=R["cs"][:]), [k_("cs")], [k_("cs")])
                    dve(lambda: V.tensor_scalar(out=R["are"][:], in0=R["are"][:], scalar1=-1.0, scalar2=None, op0=ALU.add), [k_("are")], [k_("are")])
                    dve(lambda: V.tensor_tensor(out=R["tA"][:], in0=R["are"][:], in1=R["LR"][:], op=ALU.mult), [k_("are"), k_("LR")], [k_("tA")])
                    dve(lambda: V.tensor_tensor(out=R["tB"][:], in0=R["aim"][:], in1=R["LI"][:], op=ALU.mult), [k_("aim"), k_("LI")], [k_("tB")])
                    dve(lambda: V.tensor_tensor(out=R["tA"][:], in0=R["tA"][:], in1=R["tB"][:], op=ALU.add), [k_("tA"), k_("tB")], [k_("tA")])
                    dve(lambda: V.tensor_tensor(out=R["zre"][:], in0=R["tA"][:], in1=R["cs"][:], op=ALU.mult), [k_("tA"), k_("cs")], [k_("zre")])
                    dve(lambda: V.tensor_tensor(out=R["tA"][:], in0=R["aim"][:], in1=R["LR"][:], op=ALU.mult), [k_("aim"), k_("LR")], [k_("tA")])
                    dve(lambda: V.tensor_tensor(out=R["tB"][:], in0=R["are"][:], in1=R["LI"][:], op=ALU.mult), [k_("are"), k_("LI")], [k_("tB")])
                    dve(lambda: V.tensor_tensor(out=R["tA"][:], in0=R["tA"][:], in1=R["tB"][:], op=ALU.subtract), [k_("tA"), k_("tB")], [k_("tA")])
                    dve(lambda: V.tensor_tensor(out=R["zim"][:], in0=R["tA"][:], in1=R["cs"][:], op=ALU.mult), [k_("tA"), k_("cs")], [k_("zim")])
                    for kc in range(2):
                        hs = slice(kc * 512, (kc + 1) * 512)
                        for ri, src in enumerate((I["s5_b_re"], I["s5_b_im"])):
                            dve(lambda: V.memset(braw[ri][:], 0.0), [], [("braw", ri, gl) for gl in range(8)])
                            for gl in range(8):
                                g = kc * 8 + gl
                                S.dma("sp", braw[ri][16 * gl:16 * gl + 16, gl * 64:(gl + 1) * 64], src[l, d, g].rearrange("p c -> c p"),
                                      reads=[], writes=[("braw", ri, gl)], allow_slow_non_contiguous=True)
                        tA = R["tA"][:, 0:512]; tB = R["tB"][:, 0:512]
                        dve(lambda: V.tensor_tensor(out=tA, in0=braw[0][:], in1=R["zre"][:, hs], op=ALU.mult), [("braw", 0, gl) for gl in range(8)] + [k_("zre")], [k_("tA")])
                        dve(lambda: V.tensor_tensor(out=tB, in0=braw[1][:], in1=R["zim"][:, hs], op=ALU.mult), [("braw", 1, gl) for gl in range(8)] + [k_("zim")], [k_("tB")])
                        dve(lambda: V.tensor_tensor(out=Bmat[d][:, kc, 0:512], in0=tA, in1=tB, op=ALU.subtract), [k_("tA"), k_("tB")], [("Bmat", d, kc, 0)])
                        dve(lambda: V.tensor_tensor(out=tA, in0=braw[0][:], in1=R["zim"][:, hs], op=ALU.mult), [("braw", 0, gl) for gl in range(8)] + [k_("zim")], [k_("tA")])
                        dve(lambda: V.tensor_tensor(out=tB, in0=braw[1][:], in1=R["zre"][:, hs], op=ALU.mult), [("braw", 1, gl) for gl in range(8)] + [k_("zre")], [k_("tB")])
                        dve(lambda: V.tensor_tensor(out=Bmat[d][:, kc, 512:1024], in0=tA, in1=tB, op=ALU.add), [k_("tA"), k_("tB")], [("Bmat", d, kc, 1)])
                        S.op("act", lambda: nc.scalar.activation(out=R["mag"][:, 0:512], in_=R["lrdt"][:, hs], func=AF.Exp, scale=nsig[:, d:d + 1]),
                             reads=[k_("lrdt"), "nsig"], writes=[k_("mag")])
                        dve(lambda: V.tensor_scalar(out=R["x"][:, 0:512], in0=R["ang"][:, hs], scalar1=sigp[:, d:d + 1], scalar2=None, op0=ALU.mult),
                            [k_("ang"), "sigp"], [k_("x")])
                        cossin(R["cs"][:, 0:512], R["sn"][:, 0:512], R["x"][:, 0:512], R["t3"][:, 0:512], R["t4"][:, 0:512],
                               k_("x"), k_("cs"), k_("sn"), k_("t3"), k_("t4"))
                        dve(lambda: V.tensor_tensor(out=WpreR[d][:, kc, :], in0=R["mag"][:, 0:512], in1=R["cs"][:, 0:512], op=ALU.mult),
                            [k_("mag"), k_("cs")], [("WpreR", d, kc)])
                        dve(lambda: V.scalar_tensor_tensor(out=WpreI[d][:, kc, :], in0=R["mag"][:, 0:512], scalar=-1.0, in1=R["sn"][:, 0:512], op0=ALU.mult, op1=ALU.mult),
                            [k_("mag"), k_("sn")], [("WpreI", d, kc)])
                    S.dma("sp", Cc["LR"][:], I["s5_lam_re"][l, d].rearrange("g p -> (g p)").rearrange("(c q) -> q c", q=P), writes=[k_("cLR")], allow_slow_non_contiguous=True)
                    S.dma("sp", Cc["LI"][:], I["s5_lam_im"][l, d].rearrange("g p -> (g p)").rearrange("(c q) -> q c", q=P), writes=[k_("cLI")], allow_slow_non_contiguous=True)
                    for hh in range(2):
                        S.dma("sp", Cc["LS"][64 * hh:64 * hh + 64, :], I["s5_log_step"][l, d].rearrange("(c two) -> two c", two=2)[hh].partition_broadcast(64),
                              writes=[k_("cLS%d" % hh)], allow_slow_non_contiguous=True)
                    S.op("act", lambda: nc.scalar.activation(out=Cc["LS"][:], in_=Cc["LS"][:], func=AF.Exp), reads=[k_("cLS0"), k_("cLS1")], writes=[k_("cdt")])
                    dve(lambda: V.tensor_tensor(out=Cc["lrdt"][:], in0=Cc["LR"][:], in1=Cc["LS"][:], op=ALU.mult), [k_("cLR"), k_("cdt")], [k_("clrdt")])
                    dve(lambda: V.tensor_tensor(out=Cc["ang"][:], in0=Cc["LI"][:], in1=Cc["LS"][:], op=ALU.mult), [k_("cLI"), k_("cdt")], [k_("cang")])
                    reduce_angle(Cc["angr"][:], Cc["ang"][:], Cc["t"][:], 0.0, k_("cang"), k_("cangr"), k_("ct"))
                    E3 = R["tA"][:].rearrange("p (c t) -> p c t", c=8)
                    X3 = R["tB"][:].rearrange("p (c t) -> p c t", c=8)
                    sgb = sigf[:, d, :].unsqueeze(1).to_broadcast([P, 8, P])
                    dve(lambda: V.tensor_tensor(out=E3, in0=Cc["lrdt"][:, :].unsqueeze(2).to_broadcast([P, 8, P]), in1=sgb, op=ALU.mult),
                        [k_("clrdt"), "sigf"], [k_("tA")])
                    S.op("act", lambda: nc.scalar.activation(out=R["mag"][:], in_=R["tA"][:], func=AF.Exp), reads=[k_("tA")], writes=[k_("mag")])
                    dve(lambda: V.tensor_tensor(out=X3, in0=Cc["angr"][:, :].unsqueeze(2).to_broadcast([P, 8, P]), in1=sgb, op=ALU.mult),
                        [k_("cangr"), "sigf"], [k_("tB")])
                    dve(lambda: V.tensor_copy(out=R["x"][:], in_=R["tB"][:]), [k_("tB")], [k_("x")])
                    cossin(R["cs"][:], R["sn"][:], R["x"][:], R["t3"][:], R["t4"][:], k_("x"), k_("cs"), k_("sn"), k_("t3"), k_("t4"))
                    dve(lambda: V.tensor_tensor(out=WpostR[d][:].rearrange("p c t -> p (c t)"), in0=R["mag"][:], in1=R["cs"][:], op=ALU.mult),
                        [k_("mag"), k_("cs")], [("WpostR", d)])
                    dve(lambda: V.tensor_tensor(out=WpostI[d][:].rearrange("p c t -> p (c t)"), in0=R["mag"][:], in1=R["sn"][:], op=ALU.mult),
                        [k_("mag"), k_("sn")], [("WpostI", d)])
                    dve(lambda: V.tensor_scalar(out=Cc["e"][:], in0=Cc["lrdt"][:], scalar1=128.0, scalar2=None, op0=ALU.mult), [k_("clrdt")], [k_("ce")])
                    S.op("act", lambda: nc.scalar.activation(out=Cc["mg"][:], in_=Cc["e"][:], func=AF.Exp), reads=[k_("ce")], writes=[k_("cmg")])
                    dve(lambda: V.tensor_scalar(out=Cc["x"][:], in0=Cc["angr"][:], scalar1=128.0, scalar2=None, op0=ALU.mult), [k_("cangr")], [k_("cx")])
                    cossin(Cc["cs"][:], Cc["sn"][:], Cc["x"][:], Cc["t"][:], Cc["e"][:], k_("cx"), k_("ccs"), k_("csn"), k_("ct"), k_("ce"))
                    dve(lambda: V.tensor_tensor(out=A128R[d][:], in0=Cc["mg"][:], in1=Cc["cs"][:], op=ALU.mult), [k_("cmg"), k_("ccs")], [("A128R", d)])
                    dve(lambda: V.tensor_tensor(out=A128I[d][:], in0=Cc["mg"][:], in1=Cc["sn"][:], op=ALU.mult), [k_("cmg"), k_("csn")], [("A128I", d)])
                    cmk = [("CmS", g, ri) for g in range(16) for ri in range(2)]
                    dve(lambda: V.memset(CmS[:], 0.0), [], cmk)
                    for g in range(16):
                        kc, gl = g // 8, g % 8
                        j, hh = gl // 2, gl % 2
                        for ri, src in enumerate((I["s5_c_re"], I["s5_c_im"])):
                            S.dma("sp", CmS[64 * hh:64 * hh + 64, kc * 8 + ri * 4 + j, 16 * gl:16 * gl + 16], src[l, d, g].rearrange("c p -> p c"),
                                  reads=[], writes=[("CmS", g, ri)], allow_slow_non_contiguous=True)
                    Cm4 = Cm[d][:].rearrange("p (k r j) c -> p k r (j c)", k=2, r=2)
                    CmS4 = CmS[:].rearrange("p (k r j) c -> p k r (j c)", k=2, r=2)
                    dve(lambda: V.tensor_copy(out=Cm4[:, :, 0, :], in_=CmS4[:, :, 0, :]), cmk, [("Cm", d, 0)])
                    dve(lambda: V.tensor_scalar(out=Cm4[:, :, 1, :], in0=CmS4[:, :, 1, :], scalar1=-1.0, scalar2=None, op0=ALU.mult), cmk, [("Cm", d, 1)])
                    dve(lambda: V.tensor_scalar(out=CmN[d][:].rearrange("p (k j) c -> p k (j c)", k=2), in0=CmS4[:, :, 0, :], scalar1=-1.0, scalar2=None, op0=ALU.mult),
                        cmk, [("CmN", d)])
            S.barrier()

            P1 = [[self.sb(ph, "s5P1%d%d" % (d, k), [P, 2, 512], BF16) for k in range(2)] for d in range(2)]
            P2 = [[self.sb(ph, "s5P2%d%d" % (d, k), [P, 2, 512], BF16) for k in range(2)] for d in range(2)]
            Q1 = [[self.sb(ph, "s5Q1%d%d" % (d, k), [P, 2, 512], BF16) for k in range(2)] for d in range(2)]
            Q2 = [[self.sb(ph, "s5Q2%d%d" % (d, k), [P, 2, 512], BF16) for k in range(2)] for d in range(2)]
            tc1 = self.sb(ph, "s5tc1", [P, 2, 4])
            tc2 = self.sb(ph, "s5tc2", [P, 2, 4])
            cn = self.sb(ph, "s5cn", [P, 8])
            bu = self.psum(ph, "s5bu", [P, 2, 512])
            st_ = [self.psum(ph, "s5st%d" % d, [P, 8, P]) for d in range(2)]
            ctp = self.psum(ph, "s5ctp", [P, 512])
            yo = self.psum(ph, "s5yo", [P, 512])
            orders = [list(range(NT)), [1, 0] + list(range(NT - 1, 1, -1))]
            for step in range(NT):
                for d in range(2):
                    ti = orders[d][step]
                    cols = slice(ti * P, (ti + 1) * P)
                    tl = 127 if d == 0 else 0
                    st = st_[d]
                    for kc in range(2):
                        p1, p2, q1, q2 = P1[d][kc], P2[d][kc], Q1[d][kc], Q2[d][kc]
                        for ri in range(2):
                            S.op("pe", lambda: nc.tensor.matmul(bu[:, ri, :], lhsT=aT_sb[:, kc, cols], rhs=Bmat[d][:, kc, ri * 512:(ri + 1) * 512], start=True, stop=True),
                                 reads=["aT_sb"], writes=["bu"], pe_chain=True)
                        dve(lambda: V.tensor_tensor(out=p1[:], in0=bu[:], in1=WpreR[d][:, kc, :].unsqueeze(1).to_broadcast([P, 2, 512]), op=ALU.mult),
                            ["bu"], [("P1", d, kc)])
                        dve(lambda: V.tensor_tensor(out=p2[:], in0=bu[:], in1=WpreI[d][:, kc, :].unsqueeze(1).to_broadcast([P, 2, 512]), op=ALU.mult),
                            ["bu"], [("P2", d, kc)])
                        for jj in range(8):
                            j = jj % 4
                            js = slice(j * P, (j + 1) * P)
                            if jj < 4:
                                terms = [(p1[:, 0, js], Mdir[d]), (p2[:, 1, js], Mneg[d])]
                            else:
                                terms = [(p2[:, 0, js], Mdir[d]), (p1[:, 1, js], Mdir[d])]
                            for tix, (lh, rh) in enumerate(terms):
                                S.op("pe", lambda: nc.tensor.matmul(st[:, jj, :], lhsT=lh, rhs=rh[:], start=(tix == 0), stop=(tix == 1 and step == 0)),
                                     reads=[("P1", d, kc), ("P2", d, kc)], writes=[("st", d)], pe_chain=True)
                            if step > 0:
                                S.op("pe", lambda: nc.tensor.matmul(st[:, jj, :], lhsT=cTh[d][kc][:], rhs=E8[:, jj, :], start=False, stop=False),
                                     reads=[("cTh", d, kc)], writes=[("st", d)], pe_chain=True)
                                S.op("pe", lambda: nc.tensor.matmul(st[:, jj, :], lhsT=cTl[d][kc][:], rhs=E8[:, jj, :], start=False, stop=True),
                                     reads=[("cTl", d, kc)], writes=[("st", d)], pe_chain=True)
                        st3 = st[:].rearrange("p (r j) t -> p r (j t)", r=2)
                        wr = WpostR[d][:, kc * 4:(kc + 1) * 4, :].rearrange("p j t -> p (j t)").unsqueeze(1).to_broadcast([P, 2, 512])
                        wi = WpostI[d][:, kc * 4:(kc + 1) * 4, :].rearrange("p j t -> p (j t)").unsqueeze(1).to_broadcast([P, 2, 512])
                        dve(lambda: V.tensor_tensor(out=q1[:], in0=st3, in1=wr, op=ALU.mult), [("st", d)], [("Q1", d, kc)])
                        dve(lambda: V.tensor_tensor(out=q2[:], in0=st3, in1=wi, op=ALU.mult), [("st", d)], [("Q2", d, kc)])
                        if step < NT - 1:
                            sl = st[:, :, tl].rearrange("p (r j) -> p r j", r=2)
                            ar = A128R[d][:, kc * 4:(kc + 1) * 4].unsqueeze(1).to_broadcast([P, 2, 4])
                            ai = A128I[d][:, kc * 4:(kc + 1) * 4].unsqueeze(1).to_broadcast([P, 2, 4])
                            dve(lambda: V.tensor_tensor(out=tc1[:], in0=sl, in1=ar, op=ALU.mult), [("st", d)], ["tc1"])
                            dve(lambda: V.tensor_tensor(out=tc2[:], in0=sl, in1=ai, op=ALU.mult), [("st", d)], ["tc2"])
                            dve(lambda: V.tensor_tensor(out=cn[:, 0:4], in0=tc1[:, 0, :], in1=tc2[:, 1, :], op=ALU.subtract), ["tc1", "tc2"], ["cn0"])
                            dve(lambda: V.tensor_tensor(out=cn[:, 4:8], in0=tc2[:, 0, :], in1=tc1[:, 1, :], op=ALU.add), ["tc1", "tc2"], ["cn1"])
                            S.op("pe", lambda: nc.tensor.transpose(ctp[0:8, 0:P], cn[:, :], self.ident[:]), reads=["cn0", "cn1", "ident"], writes=["ctp"], pe_chain=True)
                            S.op("act", lambda: nc.scalar.copy(out=cTh[d][kc][0:8, :], in_=ctp[0:8, 0:P]), reads=["ctp"], writes=[("cTh", d, kc)])
                            dve(lambda: V.tensor_tensor(out=cTl[d][kc][0:8, :], in0=ctp[0:8, 0:P], in1=cTh[d][kc][0:8, :], op=ALU.subtract),
                                ["ctp", ("cTh", d, kc)], [("cTl", d, kc)])
                        rterms = []
                        for j in range(4):
                            js = slice(j * P, (j + 1) * P)
                            rterms += [(Cm[d][:, kc * 8 + j, :], q1[:, 0, js], ("Q1", d, kc)), (CmN[d][:, kc * 4 + j, :], q2[:, 1, js], ("Q2", d, kc)),
                                       (Cm[d][:, kc * 8 + 4 + j, :], q2[:, 0, js], ("Q2", d, kc)), (Cm[d][:, kc * 8 + 4 + j, :], q1[:, 1, js], ("Q1", d, kc))]
                        for tix, (lh, rh, rk) in enumerate(rterms):
                            S.op("pe", lambda: nc.tensor.matmul(yo[:, 0:P], lhsT=lh, rhs=rh, start=(tix == 0), stop=(tix == len(rterms) - 1)),
                                 reads=[rk], writes=["yo"], pe_chain=True)
                        dve(lambda: V.tensor_tensor(out=yacc[:, kc, cols], in0=yo[:, 0:P], in1=yacc[:, kc, cols], op=ALU.add),
                            ["yo", ("yacc", kc, ti)], [("yacc", kc, ti)])
            for kc in range(2):
                S.op("act", lambda: nc.scalar.activation(out=aT_sb[:, kc, :], in_=yacc[:, kc, :], func=AF.Gelu_apprx_tanh),
                     reads=[("yacc", kc, i) for i in range(NT)] + ["aT_sb"], writes=["aT_sb"])
            S.dma("sp", X["yaT"].rearrange("(kc p) t -> p kc t", p=P), aT_sb[:], reads=["aT_sb"], writes=["d_yaT"])

    def ln_stats(self, src, st6, mv, rs, key):
        nc, S = self.nc, self.S
        for hf in range(2):
            S.op("dve", lambda: nc.vector.bn_stats(out=st6[:, hf, :], in_=src[:, hf * 512:(hf + 1) * 512]), reads=[key], writes=[("st6", id(st6), hf)])
        S.op("dve", lambda: nc.vector.bn_aggr(out=mv[:, :], in_=st6[:, :, :].rearrange("p a b -> p (a b)")),
             reads=[("st6", id(st6), 0), ("st6", id(st6), 1)], writes=[("mv", id(mv))])
        S.op("act", lambda: nc.scalar.activation(out=rs[:, 0:1], in_=mv[:, 1:2], func=AF.Sqrt, bias=LN_EPS, scale=1.0),
             reads=[("mv", id(mv))], writes=[("rs0", id(rs))])
        S.op("dve", lambda: nc.vector.reciprocal(out=rs[:, 1:2], in_=rs[:, 0:1]), reads=[("rs0", id(rs))], writes=[("rs1", id(rs))])
        return [("mv", id(mv)), ("rs1", id(rs))]

    def phase_merge(self, l):
        nc, S, I, X = self.nc, self.S, self.I, self.X
        last = (l == DEPTH - 1)
        with contextlib.ExitStack() as ph:
            wG = self.sb(ph, "wG", [P, 8, 3072], BF16)
            for br in range(3):
                S.dma("pool", wG[:, :, br * 1024:(br + 1) * 1024],
                      I["w_in"][l][:, 2304 + br * 1024:2304 + (br + 1) * 1024].rearrange("(kt p) n -> p kt n", p=P), writes=[("wG", br)])
            wsm = {}
            for n, kts in (("w_glu_val", 2), ("w_glu_gate", 2), ("w_proj_gm", 2), ("w_proj_da", 4), ("w_out", 8)):
                wsm[n] = self.sb(ph, "m_" + n, [P, kts, D], BF16)
                S.dma("pool", wsm[n][:], I[n][l].rearrange("(kt p) n -> p kt n", p=P), writes=[n])
            lng = self.sb(ph, "lng", [P, D])
            lnb = self.sb(ph, "lnb", [P, D])
            S.dma("sp", lng[:], I["ln1_g"][l].partition_broadcast(P), writes=["lng"])
            S.dma("sp", lnb[:], I["ln1_b"][l].partition_broadcast(P), writes=["lnb"])
            ya = [self.sb(ph, "g_ya%d" % i, [P, 2, 512], BF16) for i in range(2)]
            yb = [self.sb(ph, "g_yb%d" % i, [P, 2, 512], BF16) for i in range(2)]
            yc = [self.sb(ph, "g_yc%d" % i, [P, 4, 512], BF16) for i in range(2)]
            uu = [self.sb(ph, "g_uu%d" % i, [P, 8, 512], BF16) for i in range(2)]
            mT = [self.sb(ph, "g_mT%d" % i, [P, 8, 512], BF16) for i in range(2)]
            fT = [self.sb(ph, "g_fT%d" % i, [P, 8, 512], BF16) for i in range(1)] * 2
            sg = [self.sb(ph, "g_sg%d" % i, [P, 4, 512]) for i in range(1)] * 2
            tm = [self.sb(ph, "g_tm%d" % i, [P, 4, 512]) for i in range(1)] * 2
            hx = [self.sb(ph, "g_hx%d" % i, [P, D]) for i in range(2)]
            tt_ = [self.sb(ph, "g_tt%d" % i, [P, D]) for i in range(2)]
            xn = [self.sb(ph, "g_xn%d" % i, [P, D], BF16) for i in range(2)]
            st6 = [self.sb(ph, "g_st6%d" % i, [P, 2, 6]) for i in range(2)]
            mv = [self.sb(ph, "g_mv%d" % i, [P, 2]) for i in range(2)]
            rs = [self.sb(ph, "g_rs%d" % i, [P, 2]) for i in range(2)]
            NPB = 5
            pb_ = [self.psum(ph, "g_p%d" % i, [P, 512]) for i in range(NPB)]
            po = self.psum(ph, "g_po", [P, 2, 512])
            tp = self.psum(ph, "g_tp", [P, 8, P], BF16)
            chunks = [(2 + 4 * c, 4) for c in range(8)]
            if not last:
                chunks = [(0, 2)] + chunks
            pc_ = 0
            tcount = 0

            def load(ci):
                t0, ntl = chunks[ci]
                Wd = ntl * P
                tok0 = t0 * P
                cb = ci % 2
                S.dma("sp", ya[cb][:, :, :Wd], X["yaT"].rearrange("(kt p) t -> p kt t", p=P)[:, :, tok0:tok0 + Wd], writes=[("ya", cb)])
                S.dma("sp", yb[cb][:, :, :Wd], X["ybT"].rearrange("(kt p) t -> p kt t", p=P)[:, :, tok0:tok0 + Wd], writes=[("yb", cb)])
                S.dma("sp", yc[cb][:, :, :Wd], X["ycT"].rearrange("(kt p) t -> p kt t", p=P)[:, :, tok0:tok0 + Wd], writes=[("yc", cb)])
                S.dma("sp", uu[cb][:, :, :Wd], X["uT"].rearrange("(kt p) t -> p kt t", p=P)[:, :, tok0:tok0 + Wd], writes=[("uu", cb)])

            def dc_step(ci, dc):
                nonlocal pc_
                t0, ntl = chunks[ci]
                Wd = ntl * P
                cb = ci % 2

                def prod(wn, kts, src, skey):
                    nonlocal pc_
                    pi = pc_ % NPB; pc_ += 1
                    pt = pb_[pi]
                    for kt in range(kts):
                        if wn[0] == "g":
                            br = "ABC".index(wn[1])
                            lhsT = wG[:, kt, br * 1024 + dc * P:br * 1024 + (dc + 1) * P]
                            wkey = ("wG", br)
                        else:
                            lhsT = wsm[wn][:, kt, dc * P:(dc + 1) * P]
                            wkey = wn
                        S.op("pe", lambda: nc.tensor.matmul(pt[:, :Wd], lhsT=lhsT, rhs=src[:, kt, :Wd], start=(kt == 0), stop=(kt == kts - 1)),
                             reads=[wkey, skey], writes=[("gp", pi)], pe_chain=True)
                    return pt, ("gp", pi)

                def sig(pt, pk, sgi):
                    S.op("act", lambda: nc.scalar.activation(out=sg[0][:, sgi, :Wd], in_=pt[:, :Wd], func=AF.Sigmoid), reads=[pk], writes=[("sg", sgi)])

                pt, pk = prod("w_glu_gate", 2, ya[cb], ("ya", cb)); sig(pt, pk, 0)
                pt, pk = prod("gA", 8, uu[cb], ("uu", cb)); sig(pt, pk, 1)
                pt, pk = prod("w_glu_val", 2, ya[cb], ("ya", cb))
                S.op("dve", lambda: nc.vector.tensor_tensor(out=tm[0][:, 0, :Wd], in0=pt[:, :Wd], in1=sg[0][:, 0, :Wd], op=ALU.mult),
                     reads=[pk, ("sg", 0)], writes=[("tm", 0)])
                S.op("pool", lambda: nc.gpsimd.tensor_tensor(out=tm[0][:, 0, :Wd], in0=tm[0][:, 0, :Wd], in1=sg[0][:, 1, :Wd], op=ALU.mult),
                     reads=[("tm", 0), ("sg", 1)], writes=[("tm", 0)])
                pt, pk = prod("gB", 8, uu[cb], ("uu", cb)); sig(pt, pk, 2)
                pt, pk = prod("w_proj_gm", 2, yb[cb], ("yb", cb))
                S.op("dve", lambda: nc.vector.tensor_tensor(out=tm[0][:, 1, :Wd], in0=pt[:, :Wd], in1=sg[0][:, 2, :Wd], op=ALU.mult),
                     reads=[pk, ("sg", 2)], writes=[("tm", 1)])
                pt, pk = prod("gC", 8, uu[cb], ("uu", cb)); sig(pt, pk, 3)
                pt, pk = prod("w_proj_da", 4, yc[cb], ("yc", cb))
                S.op("dve", lambda: nc.vector.tensor_tensor(out=tm[0][:, 2, :Wd], in0=pt[:, :Wd], in1=sg[0][:, 3, :Wd], op=ALU.mult),
                     reads=[pk, ("sg", 3)], writes=[("tm", 2)])
                S.op("pool", lambda: nc.gpsimd.tensor_tensor(out=tm[0][:, 3, :Wd], in0=tm[0][:, 0, :Wd], in1=tm[0][:, 1, :Wd], op=ALU.add),
                     reads=[("tm", 0), ("tm", 1)], writes=[("tm", 3)])
                S.op("pool", lambda: nc.gpsimd.tensor_tensor(out=mT[cb][:, dc, :Wd], in0=tm[0][:, 3, :Wd], in1=tm[0][:, 2, :Wd], op=ALU.add),
                     reads=[("tm", 3), ("tm", 2)], writes=[("mT", cb, dc)])

            def tail_tile(ci, ti):
                nonlocal tcount
                t0, ntl = chunks[ci]
                s = 1 if t0 == 0 else 0
                cb = ci % 2
                i = t0 + ti
                hb = tcount % 2
                tcount += 1
                S.dma("sp", hx[hb][:], self.hsrc(l, i), writes=[("ghx", hb)])
                for nh in range(2):
                    for kt in range(8):
                        S.op("pe", lambda: nc.tensor.matmul(po[:, nh, :], lhsT=mT[cb][:, kt, ti * P:(ti + 1) * P], rhs=wsm["w_out"][:, kt, nh * 512:(nh + 1) * 512],
                                                            start=(kt == 0), stop=(kt == 7)),
                             reads=[("mT", cb, kt), "w_out"], writes=[("po", nh)], pe_chain=True)
                pof = po[:].rearrange("p a b -> p (a b)")
                S.op("dve", lambda: nc.vector.tensor_tensor(out=tt_[hb][:], in0=pof, in1=self.gb[:, 2 * s + 0, :], op=ALU.mult),
                     reads=[("po", 0), ("po", 1)], writes=[("gtt", hb)])
                S.op("dve", lambda: nc.vector.scalar_tensor_tensor(out=hx[hb][:], in0=hx[hb][:], scalar=DN_ALPHA, in1=tt_[hb][:], op0=ALU.mult, op1=ALU.add),
                     reads=[("ghx", hb), ("gtt", hb)], writes=[("ghx", hb)])
                ks = self.ln_stats(hx[hb], st6[hb], mv[hb], rs[hb], ("ghx", hb))
                S.op("dve", lambda: nc.vector.tensor_scalar(out=tt_[hb][:], in0=hx[hb][:], scalar1=mv[hb][:, 0:1], scalar2=rs[hb][:, 1:2], op0=ALU.subtract, op1=ALU.mult),
                     reads=[("ghx", hb)] + ks, writes=[("gtt", hb)])
                S.op("pool", lambda: nc.gpsimd.tensor_tensor(out=tt_[hb][:], in0=tt_[hb][:], in1=lng[:], op=ALU.mult), reads=[("gtt", hb), "lng"], writes=[("gtt", hb)])
                S.op("pool", lambda: nc.gpsimd.tensor_tensor(out=hx[hb][:], in0=tt_[hb][:], in1=lnb[:], op=ALU.add), reads=[("gtt", hb), "lnb"], writes=[("ghx", hb)])
                S.dma("sp", X["h"][i * P:(i + 1) * P, :], hx[hb][:], reads=[("ghx", hb)], writes=[("d_h", hb)])
                ks = self.ln_stats(hx[hb], st6[hb], mv[hb], rs[hb], ("ghx", hb))
                S.op("dve", lambda: nc.vector.tensor_scalar(out=xn[hb][:], in0=hx[hb][:], scalar1=mv[hb][:, 0:1], scalar2=rs[hb][:, 1:2], op0=ALU.subtract, op1=ALU.mult),
                     reads=[("ghx", hb)] + ks, writes=[("gxn", hb)])
                for kt in range(8):
                    S.op("pe", lambda: nc.tensor.transpose(tp[:, kt, :], xn[hb][:, kt * P:(kt + 1) * P], self.identb[:]),
                         reads=[("gxn", hb), "identb"], writes=["gtp"], pe_chain=True)
                for kt in range(8):
                    S.op("act", lambda: nc.scalar.activation(out=fT[0][:, kt, ti * P:(ti + 1) * P], in_=tp[:, kt, :], func=AF.Identity,
                                                             scale=self.modc[:, s, 32 + kt:33 + kt], bias=self.modc[:, s, 24 + kt:25 + kt]),
                         reads=["gtp"], writes=[("gfT", 0, ti)])

            def tail_store(ci):
                t0, ntl = chunks[ci]
                Wd = ntl * P
                tok0 = t0 * P
                S.dma("sp", X["fT"].rearrange("(kt p) t -> p kt t", p=P)[:, :, tok0:tok0 + Wd], fT[0][:, :, :Wd],
                      reads=[("gfT", 0, ti) for ti in range(ntl)], writes=[("d_fT", 0)])

            nch = len(chunks)
            load(0)
            for dc in range(8):
                dc_step(0, dc)
            for ci in range(nch):
                ntl = chunks[ci][1]
                if ci + 1 < nch:
                    load(ci + 1)
                done = 0
                for dc in range(8):
                    if ci + 1 < nch:
                        dc_step(ci + 1, dc)
                    if dc % 2 == 1 and done < ntl:
                        tail_tile(ci, done)
                        done += 1
                while done < ntl:
                    tail_tile(ci, done)
                    done += 1
                tail_store(ci)

    def phase_moe(self, l):
        nc, S, I, X = self.nc, self.S, self.I, self.X
        V = nc.vector
        last = (l == DEPTH - 1)

        def dve(fn, reads, writes):
            return S.op("dve", fn, reads=reads, writes=writes)

        with contextlib.ExitStack() as ph:
            MAXT = 18
            wrt = self.sb(ph, "wrt", [P, 8, 64], BF16)
            S.dma("pool", wrt[:], I["w_router"][l].rearrange("(kt p) n -> p kt n", p=P), writes=["wrt"])
            brb = self.sb(ph, "brb", [P, 64])
            S.dma("sp", brb[:], I["b_router"][l].partition_broadcast(P), writes=["brb"])
            lng = self.sb(ph, "lng2", [P, D])
            lnb = self.sb(ph, "lnb2", [P, D])
            S.dma("sp", lng[:], I["ln2_g"][l].partition_broadcast(P), writes=["lng"])
            S.dma("sp", lnb[:], I["ln2_b"][l].partition_broadcast(P), writes=["lnb"])
            fTb = self.sb(ph, "fTb", [P, 8, MAXT * P], BF16)
            wts = self.sb(ph, "wts", [P, MAXT, 64])
            acc = self.sb(ph, "acc", [P, MAXT, D])
            NW = 3
            wg = [self.sb(ph, "wg%d" % i, [P, 8, 256], BF16) for i in range(NW)]
            wu = [self.sb(ph, "wu%d" % i, [P, 8, 256], BF16) for i in range(NW)]
            wd = [self.sb(ph, "wd%d" % i, [P, 2, D], BF16) for i in range(NW)]
            sgt = [self.sb(ph, "sgt%d" % i, [P, 512]) for i in range(2)]
            hT = [[self.sb(ph, "hT%d%d" % (i, m), [P, 512], BF16) for m in range(2)] for i in range(2)]
            r_sc = self.sb(ph, "r_sc", [P, 64]); r_sel = self.sb(ph, "r_sel", [P, 64]); r_eq = self.sb(ph, "r_eq", [P, 64])
            r_s2 = self.sb(ph, "r_s2", [P, 64]); r_m1 = self.sb(ph, "r_m1", [P, 8]); r_m2 = self.sb(ph, "r_m2", [P, 8])
            r_gs = self.sb(ph, "r_gs", [P, 8]); r_t8 = self.sb(ph, "r_t8", [P, 8]); r_gm = self.sb(ph, "r_gm", [P, 8])
            r_gt = self.sb(ph, "r_gt", [P, 8]); r_sm = self.sb(ph, "r_sm", [P, 64]); r_e8 = self.sb(ph, "r_e8", [P, 8])
            r_em = self.sb(ph, "r_em", [P, 64]); r_w = self.sb(ph, "r_w", [P, 64]); r_ss = self.sb(ph, "r_ss", [P, 2])
            hx = [self.sb(ph, "h_hx%d" % i, [P, D]) for i in range(2)]
            tt_ = [self.sb(ph, "h_tt%d" % i, [P, D]) for i in range(2)]
            st6 = [self.sb(ph, "h_st6%d" % i, [P, 2, 6]) for i in range(2)]
            mv = [self.sb(ph, "h_mv%d" % i, [P, 2]) for i in range(2)]
            rs = [self.sb(ph, "h_rs%d" % i, [P, 2]) for i in range(2)]
            pgu = [self.psum(ph, "h_pgu%d" % i, [P, 512]) for i in range(4)]
            py = [self.psum(ph, "h_py%d" % i, [P, 2, 512]) for i in range(2)]

            if last:
                blocks = [(2, 16), (18, 16)]
            else:
                blocks = [(0, 18), (18, 16)]
            wcount = 0
            ccount = 0
            fcount = 0
            ycount = 0
            for (t0, ntl) in blocks:
                BW = ntl * P
                tok0 = t0 * P
                for kt in range(8):
                    S.dma("sp", fTb[:, kt, :BW], X["fT"][kt * P:(kt + 1) * P, tok0:tok0 + BW], writes=[("fTb", kt)])
                fkeys = [("fTb", kt) for kt in range(8)]
                for ti in range(ntl):
                    cols = slice(ti * P, (ti + 1) * P)
                    prt = pgu[ti % 4]
                    pk = ("pgu", ti % 4)
                    for kt in range(8):
                        S.op("pe", lambda: nc.tensor.matmul(prt[:, 0:64], lhsT=fTb[:, kt, cols], rhs=wrt[:, kt, :], start=(kt == 0), stop=(kt == 7)),
                             reads=[("fTb", kt), "wrt"], writes=[pk], pe_chain=True)
                    S.op("act", lambda: nc.scalar.activation(out=r_sc[:], in_=prt[:, 0:64], func=AF.Sigmoid), reads=[pk], writes=["r_sc"])
                    dve(lambda: V.tensor_tensor(out=r_sel[:], in0=r_sc[:], in1=brb[:], op=ALU.add), ["r_sc", "brb"], ["r_sel"])
                    sel3 = r_sel[:].rearrange("p (g e) -> p g e", g=8)
                    dve(lambda: V.tensor_reduce(out=r_m1[:], in_=sel3, axis=AX.X, op=ALU.max), ["r_sel"], ["r_m1"])
                    dve(lambda: V.tensor_tensor(out=r_eq[:].rearrange("p (g e) -> p g e", g=8), in0=sel3, in1=r_m1[:, :].unsqueeze(2).to_broadcast([P, 8, 8]), op=ALU.is_equal),
                        ["r_sel", "r_m1"], ["r_eq"])
                    dve(lambda: V.scalar_tensor_tensor(out=r_s2[:], in0=r_eq[:], scalar=-4.0, in1=r_sel[:], op0=ALU.mult, op1=ALU.add), ["r_eq", "r_sel"], ["r_s2"])
                    dve(lambda: V.tensor_reduce(out=r_m2[:], in_=r_s2[:].rearrange("p (g e) -> p g e", g=8), axis=AX.X, op=ALU.max), ["r_s2"], ["r_m2"])
                    dve(lambda: V.tensor_tensor(out=r_gs[:], in0=r_m1[:], in1=r_m2[:], op=ALU.add), ["r_m1", "r_m2"], ["r_gs"])
                    dve(lambda: V.max(out=r_t8[:], in_=r_gs[:]), ["r_gs"], ["r_t8"])
                    dve(lambda: V.tensor_scalar(out=r_gm[:], in0=r_gs[:], scalar1=r_t8[:, 3:4], scalar2=None, op0=ALU.is_ge), ["r_gs", "r_t8"], ["r_gm"])
                    dve(lambda: V.tensor_scalar(out=r_gt[:], in0=r_gm[:], scalar1=4.0, scalar2=-4.0, op0=ALU.mult, op1=ALU.add), ["r_gm"], ["r_gt"])
                    sm3 = r_sm[:].rearrange("p (g e) -> p g e", g=8)
                    dve(lambda: V.tensor_tensor(out=sm3, in0=sel3, in1=r_gm[:, :].unsqueeze(2).to_broadcast([P, 8, 8]), op=ALU.mult), ["r_sel", "r_gm"], ["r_sm"])
                    dve(lambda: V.tensor_tensor(out=sm3, in0=sm3, in1=r_gt[:, :].unsqueeze(2).to_broadcast([P, 8, 8]), op=ALU.add), ["r_sm", "r_gt"], ["r_sm"])
                    dve(lambda: V.max(out=r_e8[:], in_=r_sm[:]), ["r_sm"], ["r_e8"])
                    dve(lambda: V.tensor_scalar(out=r_em[:], in0=r_sm[:], scalar1=r_e8[:, 7:8], scalar2=None, op0=ALU.is_ge), ["r_sm", "r_e8"], ["r_em"])
                    dve(lambda: V.tensor_tensor(out=r_w[:], in0=r_sc[:], in1=r_em[:], op=ALU.mult), ["r_sc", "r_em"], ["r_w"])
                    dve(lambda: V.tensor_reduce(out=r_ss[:, 0:1], in_=r_w[:], axis=AX.X, op=ALU.add), ["r_w"], ["r_ss0"])
                    dve(lambda: V.reciprocal(out=r_ss[:, 1:2], in_=r_ss[:, 0:1]), ["r_ss0"], ["r_ss1"])
                    dve(lambda: V.tensor_scalar(out=wts[:, ti, :], in0=r_w[:], scalar1=r_ss[:, 1:2], scalar2=2.5, op0=ALU.mult, op1=ALU.mult), ["r_w", "r_ss1"], [("wts", ti)])
                chunks = [(c * 512, min(512, BW - c * 512)) for c in range((BW + 511) // 512)]
                items = [(e, c0, Wd) for e in range(65) for (c0, Wd) in chunks]

                def wsrc(e):
                    if e < 64:
                        return (I["w_exp_gate"][l, e], I["w_exp_up"][l, e], I["w_exp_down"][l, e])
                    return (I["w_sh_gate"][l], I["w_sh_up"][l], I["w_sh_down"][l])

                def load_w(e):
                    ws = (wbase + e) % NW
                    srcs = wsrc(e)
                    S.dma("pool", wg[ws][:], srcs[0].rearrange("(kt p) n -> p kt n", p=P), writes=[("wg", ws)])
                    S.dma("pool", wu[ws][:], srcs[1].rearrange("(kt p) n -> p kt n", p=P), writes=[("wu", ws)])
                    S.dma("pool", wd[ws][:], srcs[2].rearrange("(kt p) n -> p kt n", p=P), writes=[("wd", ws)])

                def emit_gu(k):
                    nonlocal fcount
                    e, c0, Wd = items[k]
                    ws = (wbase + e) % NW
                    cb = k % 2
                    cs_ = slice(c0, c0 + Wd)
                    if c0 == 0 and e + 1 < 65:
                        load_w(e + 1)
                    for mt in range(2):
                        pg_i = 2 * mt
                        pu_i = 2 * mt + 1
                        for (pi, wt_, wk) in ((pg_i, wg[ws], ("wg", ws)), (pu_i, wu[ws], ("wu", ws))):
                            for kt in range(8):
                                S.op("pe", lambda: nc.tensor.matmul(pgu[pi][:, :Wd], lhsT=wt_[:, kt, mt * P:(mt + 1) * P], rhs=fTb[:, kt, cs_], start=(kt == 0), stop=(kt == 7)),
                                     reads=[wk, ("fTb", kt)], writes=[("pgu", pi)], pe_chain=True)
                        fb = fcount % 2
                        fcount += 1
                        S.op("act", lambda: nc.scalar.activation(out=sgt[fb][:, :Wd], in_=pgu[pg_i][:, :Wd], func=AF.Silu), reads=[("pgu", pg_i)], writes=[("sgt", fb)])
                        dve(lambda: V.tensor_tensor(out=hT[cb][mt][:, :Wd], in0=pgu[pu_i][:, :Wd], in1=sgt[fb][:, :Wd], op=ALU.mult),
                            [("pgu", pu_i), ("sgt", fb)], [("hT", cb, mt)])

                def emit_down(k):
                    nonlocal ycount
                    e, c0, Wd = items[k]
                    ws = (wbase + e) % NW
                    cb = k % 2
                    for tl_ in range(Wd // P):
                        ti = c0 // P + tl_
                        yb_ = ycount % 2
                        path = (ycount // 2) % 2
                        ab = ycount % 2
                        ycount += 1
                        for nh in range(2):
                            for mt in range(2):
                                S.op("pe", lambda: nc.tensor.matmul(py[yb_][:, nh, :], lhsT=hT[cb][mt][:, tl_ * P:(tl_ + 1) * P], rhs=wd[ws][:, mt, nh * 512:(nh + 1) * 512],
                                                                    start=(mt == 0), stop=(mt == 1)),
                                     reads=[("hT", cb, mt), ("wd", ws)], writes=[("py", yb_, nh)], pe_chain=True)
                        pyf = py[yb_][:].rearrange("p a b -> p (a b)")
                        pyk = [("py", yb_, 0), ("py", yb_, 1)]
                        wcol = wts[:, ti, min(e, 63):min(e, 63) + 1]
                        if e == 64:
                            if path == 0:
                                S.op("act", lambda: nc.scalar.copy(out=tt_[ab][:], in_=pyf), reads=pyk, writes=[("htt", ab)])
                                S.op("pool", lambda: nc.gpsimd.tensor_tensor(out=acc[:, ti, :], in0=tt_[ab][:], in1=acc[:, ti, :], op=ALU.add),
                                     reads=[("htt", ab), ("acc", ti)], writes=[("acc", ti)])
                            else:
                                dve(lambda: V.tensor_tensor(out=acc[:, ti, :], in0=pyf, in1=acc[:, ti, :], op=ALU.add), pyk + [("acc", ti)], [("acc", ti)])
                        elif path == 0:
                            dst = acc[:, ti, :] if e == 0 else tt_[ab][:]
                            dkey = ("acc", ti) if e == 0 else ("htt", ab)
                            S.op("act", lambda: nc.scalar.activation(out=dst, in_=pyf, func=AF.Identity, scale=wcol),
                                 reads=pyk + [("wts", ti)], writes=[dkey])
                            if e > 0:
                                S.op("pool", lambda: nc.gpsimd.tensor_tensor(out=acc[:, ti, :], in0=tt_[ab][:], in1=acc[:, ti, :], op=ALU.add),
                                     reads=[("htt", ab), ("acc", ti)], writes=[("acc", ti)])
                        else:
                            dst = acc[:, ti, :] if e == 0 else hx[ab][:]
                            dkey = ("acc", ti) if e == 0 else ("hhx", ab)
                            dve(lambda: V.tensor_tensor(out=dst, in0=pyf, in1=wcol.to_broadcast([P, D]), op=ALU.mult), pyk + [("wts", ti)], [dkey])
                            if e > 0:
                                dve(lambda: V.tensor_tensor(out=acc[:, ti, :], in0=hx[ab][:], in1=acc[:, ti, :], op=ALU.add),
                                    [("hhx", ab), ("acc", ti)], [("acc", ti)])

                wbase = wcount
                wcount += 65
                load_w(0)
                for k in range(len(items) + 1):
                    if k < len(items):
                        emit_gu(k)
                    if k >= 1:
                        emit_down(k - 1)
                for ti in range(ntl):
                    i = t0 + ti
                    s = 1 if i < 2 else 0
                    hb = i % 2
                    S.dma("sp", hx[hb][:], X["h"][i * P:(i + 1) * P, :], writes=[("hhx", hb)])
                    S.op("pool", lambda: nc.gpsimd.tensor_tensor(out=tt_[hb][:], in0=acc[:, ti, :], in1=self.gb[:, 2 * s + 1, :], op=ALU.mult),
                         reads=[("acc", ti)], writes=[("htt", hb)])
                    dve(lambda: V.scalar_tensor_tensor(out=hx[hb][:], in0=hx[hb][:], scalar=DN_ALPHA, in1=tt_[hb][:], op0=ALU.mult, op1=ALU.add),
                        [("hhx", hb), ("htt", hb)], [("hhx", hb)])
                    ks = self.ln_stats(hx[hb], st6[hb], mv[hb], rs[hb], ("hhx", hb))
                    dve(lambda: V.tensor_scalar(out=tt_[hb][:], in0=hx[hb][:], scalar1=mv[hb][:, 0:1], scalar2=rs[hb][:, 1:2], op0=ALU.subtract, op1=ALU.mult),
                        [("hhx", hb)] + ks, [("htt", hb)])
                    S.op("pool", lambda: nc.gpsimd.tensor_tensor(out=tt_[hb][:], in0=tt_[hb][:], in1=lng[:], op=ALU.mult), reads=[("htt", hb), "lng"], writes=[("htt", hb)])
                    S.op("pool", lambda: nc.gpsimd.tensor_tensor(out=hx[hb][:], in0=tt_[hb][:], in1=lnb[:], op=ALU.add), reads=[("htt", hb), "lnb"], writes=[("hhx", hb)])
                    if last:
                        dst = self.out[(i - 2) * P:(i - 1) * P, :]
                    else:
                        dst = X["h"][i * P:(i + 1) * P, :]
                    S.dma("sp", dst, hx[hb][:], reads=[("hhx", hb)], writes=[("d_h2", hb)])

_ROPE = None


def make_in_maps(inputs, ncores=8, used=None):
    global _ROPE
    if _ROPE is None:
        _ROPE = _rope_tables()
    cosT, sinT = _ROPE
    perm = _rope_perm()
    w_in = np.ascontiguousarray(inputs["w_in"], dtype=np.float32)
    qcols = np.concatenate([768 + h * 128 + perm for h in range(4)])
    kcols = np.concatenate([1280 + h * 128 + perm for h in range(4)])
    w_qkp = np.ascontiguousarray(w_in[:, :, np.concatenate([qcols, kcols])])
    shared = {k: np.ascontiguousarray(v, dtype=np.float32) for k, v in inputs.items() if k not in ("x", "c", "ctx")}
    shared["w_qkp"] = w_qkp
    shared["rope_cos"] = cosT
    shared["rope_sin"] = sinT
    for n, a in _consts().items():
        shared["k_" + n] = a
    maps = []
    for core in range(ncores):
        b = core % 4
        m = dict(shared)
        m["x"] = np.ascontiguousarray(inputs["x"][b], dtype=np.float32)
        m["ctx"] = np.ascontiguousarray(inputs["ctx"][b], dtype=np.float32)
        m["c"] = np.ascontiguousarray(inputs["c"][b], dtype=np.float32)
        if used is not None:
            m = {k: v for k, v in m.items() if k in used}
        maps.append(m)
    return maps


def kernel(**inputs):
    bld = Builder()
    nc = bld.build()
    maps = make_in_maps(inputs, used=set(bld.I.keys()))
    res = run_bass_kernel_spmd(nc, maps, core_ids=list(range(8)))
    out = np.stack([res.results[b]["out"] for b in range(4)], 0)
    return out.astype(np.float32)
```

```python
import contextlib, math
import numpy as np
import concourse.bass as bass
import concourse.mybir as mybir
from concourse.bass_utils import run_bass_kernel_spmd

F32 = mybir.dt.float32
BF16 = mybir.dt.bfloat16
AF = mybir.ActivationFunctionType
ALU = mybir.AluOpType
AX = mybir.AxisListType

P = 128
D = 1024
NCTX = 256
NLAT = 4096
NTOK = NCTX + NLAT
NT = NTOK // P
DEPTH = 2
LN_EPS = 1e-5
DN_ALPHA = (2 * DEPTH) ** 0.25
IN_WIDTH = 5376


class Sched:
    def __init__(self, nc, stack):
        self.nc = nc
        self.stack = stack
        self.eng = {"pe": nc.tensor, "act": nc.scalar, "dve": nc.vector,
                    "pool": nc.gpsimd, "sp": nc.sync}
        self.sem = {}
        self.cnt = {}
        for e in self.eng:
            self.sem[e] = stack.enter_context(nc.semaphore("s_" + e))
            self.cnt[e] = 0
        self.waited = {e: {} for e in self.eng}
        self.res = {}
        self.dsem = {}
        self.free_dsems = {}
        self.dtype_of = {}
        self.ninst = 0

    def _r(self, key):
        r = self.res.get(key)
        if r is None:
            r = self.res[key] = {"w": {}, "r": {}}
        return r

    def _need(self, deps, reads, writes):
        for k in reads:
            for s, v in self._r(k)["w"].items():
                if deps.get(s, 0) < v:
                    deps[s] = v
        for k in writes:
            r = self._r(k)
            for s, v in r["w"].items():
                if deps.get(s, 0) < v:
                    deps[s] = v
            for s, v in r["r"].items():
                if deps.get(s, 0) < v:
                    deps[s] = v

    def _emit_waits(self, e, deps, skip_self=False):
        w = self.waited[e]
        for s, v in deps.items():
            if skip_self and s == e:
                continue
            if w.get(s, 0) >= v:
                continue
            self.eng[e].wait_ge(self.sem[s], v)
            w[s] = v
            self.ninst += 1

    def _record(self, ev, reads, writes):
        s, v = ev
        for k in reads:
            r = self._r(k)["r"]
            if r.get(s, 0) < v:
                r[s] = v
        for k in writes:
            r = self._r(k)
            r["w"] = {s: v}
            r["r"] = {}

    def op(self, e, fn, reads=(), writes=(), pe_chain=False):
        deps = {}
        self._need(deps, reads, writes)
        self._emit_waits(e, deps, skip_self=(e == "pe" and pe_chain))
        ins = fn()
        self.cnt[e] += 1
        ins.then_inc(self.sem[e], 1)
        self._record((e, self.cnt[e]), reads, writes)
        self.ninst += 1
        return ins

    def dma(self, q, out, in_, reads=(), writes=(), semkey=None, **kw):
        if semkey is None:
            semkey = writes[0]
        sname = self.dsem.get(semkey)
        qt = "sw" if q == "pool" else "hw"
        if sname is None:
            pool_ = self.free_dsems.setdefault(qt, [])
            if pool_:
                sname = pool_.pop()
            else:
                sname = "d%d" % (len(self.sem))
                self.sem[sname] = self.stack.enter_context(self.nc.semaphore(sname))
                self.cnt[sname] = 0
                self.dtype_of[sname] = qt
            self.dsem[semkey] = sname
        assert self.dtype_of[sname] == qt, (semkey, sname, qt)
        deps = {}
        self._need(deps, reads, writes)
        self._emit_waits(q, deps)
        ins = self.eng[q].dma_start(out=out, in_=in_, **kw)
        self.cnt[sname] += 16
        ins.then_inc(self.sem[sname], 16)
        self._record((sname, self.cnt[sname]), reads, writes)
        self.ninst += 1
        return ins

    def barrier(self, final=False):
        engs = ["sp"] if final else list(self.eng)
        for e in engs:
            w = self.waited[e]
            for s, v in self.cnt.items():
                if v > 0 and w.get(s, 0) < v:
                    self.eng[e].wait_ge(self.sem[s], v)
                    w[s] = v
                    self.ninst += 1
        if not final:
            self.res = {}
            for sn in self.dsem.values():
                self.free_dsems.setdefault(self.dtype_of[sn], []).append(sn)
            self.dsem = {}


def _rope_tables():
    rows = NLAT // 64
    r, col = np.meshgrid(np.arange(rows), np.arange(64), indexing="ij")
    inv_freq = (10000.0 ** (-np.arange(0, 32, 2, dtype=np.float32) / 32)).astype(np.float32)
    ang = np.concatenate([r.reshape(-1, 1).astype(np.float32) * inv_freq,
                          col.reshape(-1, 1).astype(np.float32) * inv_freq], -1)
    cos = np.cos(ang).astype(np.float32)
    sin = np.sin(ang).astype(np.float32)
    cosT = np.ones((128, NTOK), np.float32)
    sinT = np.zeros((128, NTOK), np.float32)
    for m in range(2):
        for d in range(64):
            f = (d % 16) if d < 32 else 16 + (d % 16)
            sgn = -1.0 if (d % 32) < 16 else 1.0
            cosT[m * 64 + d, NCTX:] = cos[:, f]
            sinT[m * 64 + d, NCTX:] = sgn * sin[:, f]
    return cosT, sinT


def _rope_perm():
    perm = np.zeros(128, np.int64)
    for m in range(2):
        for d in range(64):
            pd = d + 16 if (d % 32) < 16 else d - 16
            perm[m * 64 + d] = m * 64 + pd
    return perm


def _consts():
    c = {}
    c["ident"] = np.eye(128, dtype=np.float32)
    tt = np.arange(128)
    c["mfwd"] = (tt[:, None] <= tt[None, :]).astype(np.float32)
    c["mbwd"] = (tt[:, None] >= tt[None, :]).astype(np.float32)
    sig = np.stack([tt, 127 - tt], 1).astype(np.float32)
    c["sigp"] = sig
    c["sigf"] = np.broadcast_to(np.stack([tt, 127 - tt], 0).astype(np.float32)[None], (128, 2, 128)).copy()
    e8 = np.zeros((128, 8, 128), np.float32)
    for k in range(8):
        e8[k, k, :] = 1.0
    c["e8"] = e8
    sel = np.zeros((128, 64, 128), np.float32)
    for k in range(64):
        sel[k, k, :] = 1.0
    c["sel"] = sel
    return c


class Builder:
    def __init__(self, nlayers=DEPTH, dbg=()):
        self.nlayers = nlayers
        self.dbg = set(dbg)
        self.nc = bass.Bass("TRN2", target_bir_lowering=False)
        self.dbg_out = {}

    def din(self, name, shape, dt=F32):
        return self.nc.dram_tensor(name, list(shape), dt, kind="ExternalInput").ap()

    def dscr(self, name, shape, dt):
        if name in self.dbg:
            t = self.nc.dram_tensor("dbg_" + name, list(shape), dt, kind="ExternalOutput").ap()
            self.dbg_out[name] = "dbg_" + name
            return t
        return self.nc.dram_tensor(name, list(shape), dt, kind="Internal").ap()

    def declare(self):
        L = DEPTH
        shapes = {"x": [NLAT, D], "ctx": [NCTX, D], "c": [D], "c_ctx": [D], "w_mod": [L, D, 6 * D], "b_mod": [L, 6 * D],
                  "w_in": [L, D, IN_WIDTH], "w_qkp": [L, D, 1024], "rope_cos": [128, NTOK], "rope_sin": [128, NTOK]}
        for n, sh in [("s5_lam_re", [L, 2, 16, 64]), ("s5_lam_im", [L, 2, 16, 64]), ("s5_log_step", [L, 2, 16]),
                      ("s5_b_re", [L, 2, 16, 64, 16]), ("s5_b_im", [L, 2, 16, 64, 16]),
                      ("s5_c_re", [L, 2, 16, 16, 64]), ("s5_c_im", [L, 2, 16, 16, 64]), ("s5_d", [L, 256]),
                      ("gm_w_s", [L, 4, 128, 128]), ("gm_b_s", [L, 4, 128]), ("da_lam", [L, 4, 64]),
                      ("da_subln_g", [L, 128]), ("w_glu_val", [L, 256, D]), ("w_glu_gate", [L, 256, D]),
                      ("w_proj_gm", [L, 256, D]), ("w_proj_da", [L, 512, D]), ("w_out", [L, D, D]),
                      ("ln1_g", [L, D]), ("ln1_b", [L, D]), ("ln2_g", [L, D]), ("ln2_b", [L, D]),
                      ("w_router", [L, D, 64]), ("b_router", [L, 64]),
                      ("w_exp_gate", [L, 64, D, 256]), ("w_exp_up", [L, 64, D, 256]), ("w_exp_down", [L, 64, 256, D]),
                      ("w_sh_gate", [L, D, 256]), ("w_sh_up", [L, D, 256]), ("w_sh_down", [L, 256, D])]:
            shapes[n] = sh
        for n, a in _consts().items():
            shapes["k_" + n] = list(a.shape)
        bld = self

        class Lazy(dict):
            def __missing__(self, n):
                self[n] = bld.din(n, shapes[n])
                return self[n]
        self.I = Lazy()
        self.out = self.nc.dram_tensor("out", [NLAT, D], F32, kind="ExternalOutput").ap()
        Sx = {}
        Sx["h"] = self.dscr("h", [NTOK, D], F32)
        Sx["uT"] = self.dscr("uT", [D, NTOK], BF16)
        Sx["aT"] = self.dscr("aT", [256, NTOK], BF16)
        Sx["qT"] = self.dscr("qT", [512, NTOK], BF16)
        Sx["kT"] = self.dscr("kT", [512, NTOK], BF16)
        Sx["v"] = self.dscr("v", [NTOK, 512], BF16)
        Sx["yaT"] = self.dscr("yaT", [256, NTOK], BF16)
        Sx["ybT"] = self.dscr("ybT", [256, NTOK], BF16)
        Sx["ycT"] = self.dscr("ycT", [512, NTOK], BF16)
        Sx["fT"] = self.dscr("fT", [D, NTOK], BF16)
        self.X = Sx

    def sb(self, ph, name, shape, dt=F32):
        return ph.enter_context(self.nc.sbuf_tensor(name + getattr(self, "sfx", ""), list(shape), dt))

    def psum(self, ph, name, shape, dt=F32):
        return ph.enter_context(self.nc.psum_tensor(name + getattr(self, "sfx", ""), list(shape), dt))

    def hsrc(self, l, i):
        if l == 0:
            if i < 2:
                return self.I["ctx"][i * P:(i + 1) * P, :]
            return self.I["x"][(i - 2) * P:(i - 1) * P, :]
        return self.X["h"][i * P:(i + 1) * P, :]

    def build(self):
        nc = self.nc
        self.declare()
        with contextlib.ExitStack() as st:
            self.S = S = Sched(nc, st)
            self.ident = self.sb(st, "ident", [P, P], F32)
            self.identb = self.sb(st, "identb", [P, P], BF16)
            S.dma("sp", self.ident[:], self.I["k_ident"], writes=["ident"])
            S.op("dve", lambda: nc.vector.tensor_copy(out=self.identb[:], in_=self.ident[:]), reads=["ident"], writes=["identb"])
            self.modc = self.sb(st, "modc", [P, 2, 48], F32)
            self.gb = self.sb(st, "gb", [P, 4, D], F32)
            for l in range(self.nlayers):
                self.layer(l)
            S.barrier(final=True)
        return nc

    def layer(self, l):
        S = self.S
        last = (l == DEPTH - 1)
        self.sfx = '_L%d' % l
        self.phase_mod(l)
        S.barrier()
        if 'noproj' not in self.dbg:
            self.phase_proj(l)
            S.barrier()
        if 'nos5' not in self.dbg:
            self.phase_s5(l)
            S.barrier()
        if 'noattn' not in self.dbg:
            self.phase_attn(l)
            S.barrier()
        if 'nomerge' not in self.dbg:
            self.phase_merge(l)
            S.barrier()
        if 'nomoe' not in self.dbg:
            self.phase_moe(l)
            S.barrier()

    def phase_mod(self, l):
        nc, S, I = self.nc, self.S, self.I
        with contextlib.ExitStack() as ph:
            cs = self.sb(ph, "cs", [P, 2, 8])
            scs = self.sb(ph, "scs", [P, 8, 2])
            rep = self.sb(ph, "rep", [P, 2, 8, P])
            bm = self.sb(ph, "bm", [P, 48])
            bmb = self.sb(ph, "bmb", [P, 2, D])
            wm = [self.sb(ph, "wm%d" % i, [P, 8, 512]) for i in range(2)]
            pmod = self.psum(ph, "pmod", [P, 48, 2])
            pg = [self.psum(ph, "pg%d" % i, [P, 512]) for i in range(2)]
            S.dma("sp", cs[:, 0, :], I["c"].rearrange("(k p) -> p k", p=P), writes=["cs0"], allow_slow_non_contiguous=True)
            S.dma("sp", cs[:, 1, :], I["c_ctx"].rearrange("(k p) -> p k", p=P), writes=["cs1"], allow_slow_non_contiguous=True)
            S.dma("sp", bm[:], I["b_mod"][l].rearrange("(j p) -> p j", p=P), writes=["bm"], allow_slow_non_contiguous=True)
            S.dma("sp", bmb[:, 0, :], I["b_mod"][l, 2 * D:3 * D].partition_broadcast(P), writes=["bmb0"])
            S.dma("sp", bmb[:, 1, :], I["b_mod"][l, 5 * D:6 * D].partition_broadcast(P), writes=["bmb1"])
            for s in range(2):
                S.op("act", lambda: nc.scalar.activation(out=scs[:, :, s], in_=cs[:, s, :], func=AF.Silu),
                     reads=["cs%d" % s], writes=[("scs", s)])
                S.op("dve", lambda: nc.vector.tensor_copy(out=rep[:, s, :, :], in_=scs[:, :, s].unsqueeze(2).to_broadcast([P, 8, P])),
                     reads=[("scs", s)], writes=[("rep", s)])
            for c in range(12):
                w = wm[c % 2]
                S.dma("sp", w[:], I["w_mod"][l][:, c * 512:(c + 1) * 512].rearrange("(kt p) n -> p kt n", p=P),
                      writes=[("wm", c % 2)])
                for j in range(4):
                    jj = c * 4 + j
                    for kt in range(8):
                        S.op("pe", lambda: nc.tensor.matmul(pmod[:, jj, :], lhsT=w[:, kt, j * P:(j + 1) * P], rhs=scs[:, kt, :],
                                                            start=(kt == 0), stop=(kt == 7)),
                             reads=[("wm", c % 2), ("scs", 0), ("scs", 1)], writes=["pmod"], pe_chain=True)
                if c in (4, 5, 10, 11):
                    gi = 0 if c < 6 else 1
                    half = c % 2
                    for s in range(2):
                        pgt = pg[s]
                        for kt in range(8):
                            S.op("pe", lambda: nc.tensor.matmul(pgt[:], lhsT=rep[:, s, kt, :], rhs=w[:, kt, :],
                                                                start=(kt == 0), stop=(kt == 7)),
                                 reads=[("wm", c % 2), ("rep", s)], writes=[("pg", s)], pe_chain=True)
                        S.op("dve", lambda: nc.vector.tensor_tensor(out=self.gb[:, 2 * s + gi, half * 512:(half + 1) * 512], in0=pgt[:],
                                                                    in1=bmb[:, gi, half * 512:(half + 1) * 512], op=ALU.add),
                             reads=[("pg", s), "bmb%d" % gi], writes=[("gb", 2 * s + gi, half)])
            for s in range(2):
                S.op("dve", lambda: nc.vector.tensor_tensor(out=self.modc[:, s, :], in0=pmod[:, :, s], in1=bm[:], op=ALU.add),
                     reads=["pmod", "bm"], writes=[("modc", s)])
                for c0 in (8, 32):
                    S.op("dve", lambda: nc.vector.tensor_scalar(out=self.modc[:, s, c0:c0 + 8], in0=self.modc[:, s, c0:c0 + 8], scalar1=1.0, scalar2=None, op0=ALU.add),
                         reads=[("modc", s)], writes=[("modc", s)])
            if "modc" in self.dbg:
                t = self.nc.dram_tensor("dbg_modc%d" % l, [P, 2, 48], F32, kind="ExternalOutput").ap()
                S.dma("sp", t, self.modc[:], reads=[("modc", 0), ("modc", 1)], writes=["dbg_modc"])
                t2 = self.nc.dram_tensor("dbg_gb%d" % l, [P, 4, D], F32, kind="ExternalOutput").ap()
                S.dma("sp", t2, self.gb[:], reads=[("gb", a, b) for a in range(4) for b in range(2)], writes=["dbg_gb"])

    def phase_proj(self, l):
        nc, S, I, X = self.nc, self.S, self.I, self.X
        with contextlib.ExitStack() as ph:
            wsplit = {"a": (0, 256), "zu": (256, 512), "zv": (512, 768), "q": (768, 1280), "k": (1280, 1792), "v": (1792, 2304)}
            W = {}
            for n, (c0, c1) in wsplit.items():
                W[n] = self.sb(ph, "w_" + n, [P, 8, c1 - c0], BF16)
                S.dma("pool", W[n][:], I["w_in"][l][:, c0:c1].rearrange("(kt p) n -> p kt n", p=P), writes=["w_" + n])
            for i, n in enumerate(("qp", "kp")):
                W[n] = self.sb(ph, "w_" + n, [P, 8, 512], BF16)
                S.dma("pool", W[n][:], I["w_qkp"][l][:, i * 512:(i + 1) * 512].rearrange("(kt p) n -> p kt n", p=P), writes=["w_" + n])
            wsn = self.sb(ph, "wsn", [P, 4, P], BF16)
            wsT = self.sb(ph, "wsT", [P, 4, P], BF16)
            bsT = self.sb(ph, "bsT", [P, 2, P], F32)
            S.dma("pool", wsn[:], I["gm_w_s"][l].rearrange("h t s -> t h s"), writes=["wsn"])
            for mt in range(2):
                for hh in range(2):
                    S.dma("sp", bsT[64 * hh:64 * hh + 64, mt, :], I["gm_b_s"][l, 2 * mt + hh, :].partition_broadcast(64),
                          writes=[("bsT", mt, hh)])
            bsT_keys = [("bsT", mt, hh) for mt in range(2) for hh in range(2)]

            NH = 3
            hx = [self.sb(ph, "hx%d" % i, [P, D]) for i in range(NH)]
            xn = [self.sb(ph, "xn%d" % i, [P, D], BF16) for i in range(2)]
            st6 = self.sb(ph, "st6", [P, 2, 2, 6])
            mv = self.sb(ph, "mv", [P, 2, 2])
            rs = self.sb(ph, "rs", [P, 2, 2])
            uT = [self.sb(ph, "uTb%d" % i, [P, 8, 512], BF16) for i in range(2)]
            cosb = [self.sb(ph, "cos%d" % i, [P, 512]) for i in range(2)]
            sinb = [self.sb(ph, "sin%d" % i, [P, 512]) for i in range(2)]
            aTb = [self.sb(ph, "aTb%d" % i, [P, 2, 512], BF16) for i in range(2)]
            zuT = [self.sb(ph, "zuT%d" % i, [P, 2, 512], BF16) for i in range(2)]
            ybT = [self.sb(ph, "ybT%d" % i, [P, 2, 512], BF16) for i in range(2)]
            qkT = [self.sb(ph, "qkT%d" % i, [P, 8, 512], BF16) for i in range(2)]
            vb = [self.sb(ph, "vb%d" % i, [P, 4, 512], BF16) for i in range(2)]
            zg = [self.sb(ph, "zg%d" % i, [P, 256]) for i in range(2)]
            zvn = [self.sb(ph, "zvn%d" % i, [P, 256], BF16) for i in range(2)]
            zst = self.sb(ph, "zst", [P, 2, 6])
            zmv = self.sb(ph, "zmv", [P, 2, 2])
            zrs = self.sb(ph, "zrs", [P, 2, 2])
            t1 = [self.sb(ph, "t1_%d" % i, [P, 512]) for i in range(2)]
            t2 = [self.sb(ph, "t2_%d" % i, [P, 512]) for i in range(2)]
            tg = [self.sb(ph, "tg%d" % i, [P, 2, P]) for i in range(2)]
            tp = [self.psum(ph, "tp%d" % i, [P, 8, P], BF16) for i in range(2)]
            for h in range(4):
                S.op("pe", lambda: nc.tensor.transpose(tp[0][:, h, :], wsn[:, h, :], self.identb[:]), reads=["wsn", "identb"], writes=[("tp", 0)], pe_chain=True)
            S.op("dve", lambda: nc.vector.tensor_copy(out=wsT[:], in_=tp[0][:, 0:4, :]), reads=[("tp", 0)], writes=["wsT"])
            pa = [self.psum(ph, "pa%d" % i, [P, 512]) for i in range(4)]
            pz_ = [self.psum(ph, "pz%d" % i, [P, 256]) for i in range(2)]

            chunks = [(0, 2)] + [(2 + 4 * c, 4) for c in range(8)]
            tcount = 0
            pcount = 0

            def emit_ln(ci):
                nonlocal tcount
                t0, ntl = chunks[ci]
                s = 1 if ci == 0 else 0
                Wd = ntl * P
                tok0 = t0 * P
                cb = ci % 2
                ub = uT[cb]
                S.dma("sp", cosb[cb][:, :Wd], I["rope_cos"][:, tok0:tok0 + Wd], writes=[("cos", cb)])
                S.dma("sp", sinb[cb][:, :Wd], I["rope_sin"][:, tok0:tok0 + Wd], writes=[("sin", cb)])
                for ti in range(ntl):
                    i = t0 + ti
                    hb = tcount % NH
                    xb = tcount % 2
                    tcount += 1
                    S.dma("sp", hx[hb][:], self.hsrc(l, i), writes=[("hx", hb)])
                    for hf in range(2):
                        S.op("dve", lambda: nc.vector.bn_stats(out=st6[:, xb, hf, :], in_=hx[hb][:, hf * 512:(hf + 1) * 512]),
                             reads=[("hx", hb)], writes=[("st6", xb, hf)])
                    S.op("dve", lambda: nc.vector.bn_aggr(out=mv[:, xb, :], in_=st6[:, xb, :, :].rearrange("p a b -> p (a b)")),
                         reads=[("st6", xb, 0), ("st6", xb, 1)], writes=[("mv", xb)])
                    S.op("act", lambda: nc.scalar.activation(out=rs[:, xb, 0:1], in_=mv[:, xb, 1:2], func=AF.Sqrt, bias=LN_EPS, scale=1.0),
                         reads=[("mv", xb)], writes=[("rs0", xb)])
                    S.op("dve", lambda: nc.vector.reciprocal(out=rs[:, xb, 1:2], in_=rs[:, xb, 0:1]), reads=[("rs0", xb)], writes=[("rs1", xb)])
                    S.op("dve", lambda: nc.vector.tensor_scalar(out=xn[xb][:], in0=hx[hb][:], scalar1=mv[:, xb, 0:1], scalar2=rs[:, xb, 1:2],
                                                                op0=ALU.subtract, op1=ALU.mult),
                         reads=[("hx", hb), ("mv", xb), ("rs1", xb)], writes=[("xn", xb)])
                    tpb = tp[xb]
                    for kt in range(8):
                        S.op("pe", lambda: nc.tensor.transpose(tpb[:, kt, :], xn[xb][:, kt * P:(kt + 1) * P], self.identb[:]),
                             reads=[("xn", xb), "identb"], writes=[("tp", xb)], pe_chain=True)
                    for kt in range(8):
                        S.op("act", lambda: nc.scalar.activation(out=ub[:, kt, ti * P:(ti + 1) * P], in_=tpb[:, kt, :], func=AF.Identity,
                                                                 scale=self.modc[:, s, 8 + kt:9 + kt], bias=self.modc[:, s, kt:kt + 1]),
                             reads=[("tp", xb), ("modc", s)], writes=[("uT", cb, ti)])
                ukeys_ = [("uT", cb, ti) for ti in range(ntl)]
                S.dma("sp", X["uT"].rearrange("(kt p) t -> p kt t", p=P)[:, :, tok0:tok0 + Wd], ub[:, :, :Wd], reads=ukeys_, writes=[("d_uT", ci % 2)])

            emit_ln(0)
            for ci, (t0, ntl) in enumerate(chunks):
                s = 1 if ci == 0 else 0
                Wd = ntl * P
                tok0 = t0 * P
                cb = ci % 2
                ub = uT[cb]
                ukeys = [("uT", cb, ti) for ti in range(ntl)]
                if ci + 1 < len(chunks):
                    emit_ln(ci + 1)

                def mm_fm(pt, wt, c0):
                    for kt in range(8):
                        S.op("pe", lambda: nc.tensor.matmul(pt[:, :Wd], lhsT=wt[:, kt, c0:c0 + P], rhs=ub[:, kt, :Wd], start=(kt == 0), stop=(kt == 7)),
                             reads=ukeys + [wt_key[id(wt)]], writes=[pkey[id(pt)]], pe_chain=True)

                wt_key = {id(W[n]): "w_" + n for n in W}
                pkey = {id(pa[i]): ("pa", i) for i in range(4)}

                for mt in range(2):
                    pt = pa[pcount % 4]; pcount += 1
                    mm_fm(pt, W["a"], mt * P)
                    S.op("act", lambda: nc.scalar.copy(out=aTb[cb][:, mt, :Wd], in_=pt[:, :Wd]), reads=[pkey[id(pt)]], writes=[("aTb", cb, mt)])
                S.dma("sp", X["aT"].rearrange("(mt p) t -> p mt t", p=P)[:, :, tok0:tok0 + Wd], aTb[cb][:, :, :Wd],
                      reads=[("aTb", cb, 0), ("aTb", cb, 1)], writes=[("d_aT", ci)])
                for mt in range(2):
                    pt = pa[pcount % 4]; pcount += 1
                    mm_fm(pt, W["zu"], mt * P)
                    S.op("act", lambda: nc.scalar.activation(out=zuT[cb][:, mt, :Wd], in_=pt[:, :Wd], func=AF.Gelu_apprx_tanh),
                         reads=[pkey[id(pt)]], writes=[("zuT", cb, mt)])
                for qi, (wn, wpn) in enumerate((("q", "qp"), ("k", "kp"))):
                    for hd in range(4):
                        p0 = pa[pcount % 4]; pcount += 1
                        p1 = pa[pcount % 4]; pcount += 1
                        mm_fm(p0, W[wn], hd * P)
                        mm_fm(p1, W[wpn], hd * P)
                        tb = (qi * 4 + hd) % 2
                        S.op("dve", lambda: nc.vector.tensor_tensor(out=t1[tb][:, :Wd], in0=p0[:, :Wd], in1=cosb[cb][:, :Wd], op=ALU.mult),
                             reads=[pkey[id(p0)], ("cos", cb)], writes=[("t1", tb)])
                        S.op("dve", lambda: nc.vector.tensor_tensor(out=t2[tb][:, :Wd], in0=p1[:, :Wd], in1=sinb[cb][:, :Wd], op=ALU.mult),
                             reads=[pkey[id(p1)], ("sin", cb)], writes=[("t2", tb)])
                        S.op("pool", lambda: nc.gpsimd.tensor_tensor(out=qkT[cb][:, qi * 4 + hd, :Wd], in0=t1[tb][:, :Wd], in1=t2[tb][:, :Wd], op=ALU.add),
                             reads=[("t1", tb), ("t2", tb)], writes=[("qkT", cb, qi * 4 + hd)])
                    dst = X["qT"] if qi == 0 else X["kT"]
                    S.dma("sp", dst.rearrange("(h p) t -> p h t", p=P)[:, :, tok0:tok0 + Wd], qkT[cb][:, qi * 4:qi * 4 + 4, :Wd],
                          reads=[("qkT", cb, qi * 4 + hd) for hd in range(4)], writes=[("d_qk", ci, qi)])
                for ti in range(ntl):
                    i = t0 + ti
                    pt = pa[pcount % 4]; pcount += 1
                    for kt in range(8):
                        S.op("pe", lambda: nc.tensor.matmul(pt[:], lhsT=ub[:, kt, ti * P:(ti + 1) * P], rhs=W["v"][:, kt, :], start=(kt == 0), stop=(kt == 7)),
                             reads=[("uT", cb, ti), "w_v"], writes=[pkey[id(pt)]], pe_chain=True)
                    S.op("act", lambda: nc.scalar.copy(out=vb[cb][:, ti, :], in_=pt[:]), reads=[pkey[id(pt)]], writes=[("vb", cb, ti)])
                    zb = i % 2
                    for kt in range(8):
                        S.op("pe", lambda: nc.tensor.matmul(pz_[zb][:, :], lhsT=ub[:, kt, ti * P:(ti + 1) * P], rhs=W["zv"][:, kt, :], start=(kt == 0), stop=(kt == 7)),
                             reads=[("uT", cb, ti), "w_zv"], writes=[("pz", zb)], pe_chain=True)
                    S.op("act", lambda: nc.scalar.activation(out=zg[zb][:], in_=pz_[zb][:, :], func=AF.Gelu_apprx_tanh), reads=[("pz", zb)], writes=[("zg", zb)])
                    S.op("dve", lambda: nc.vector.bn_stats(out=zst[:, zb, :], in_=zg[zb][:]), reads=[("zg", zb)], writes=[("zst", zb)])
                    S.op("dve", lambda: nc.vector.bn_aggr(out=zmv[:, zb, :], in_=zst[:, zb, :]), reads=[("zst", zb)], writes=[("zmv", zb)])
                    S.op("act", lambda: nc.scalar.activation(out=zrs[:, zb, 0:1], in_=zmv[:, zb, 1:2], func=AF.Sqrt, bias=LN_EPS, scale=1.0),
                         reads=[("zmv", zb)], writes=[("zrs0", zb)])
                    S.op("dve", lambda: nc.vector.reciprocal(out=zrs[:, zb, 1:2], in_=zrs[:, zb, 0:1]), reads=[("zrs0", zb)], writes=[("zrs1", zb)])
                    S.op("dve", lambda: nc.vector.tensor_scalar(out=zvn[zb][:], in0=zg[zb][:], scalar1=zmv[:, zb, 0:1], scalar2=zrs[:, zb, 1:2],
                                                                op0=ALU.subtract, op1=ALU.mult),
                         reads=[("zg", zb), ("zmv", zb), ("zrs1", zb)], writes=[("zvn", zb)])
                    for h in range(4):
                        S.op("pe", lambda: nc.tensor.matmul(pz_[zb][64 * (h % 2):64 * (h % 2) + 64, (h // 2) * P:(h // 2 + 1) * P],
                                                            lhsT=zvn[zb][:, h * 64:(h + 1) * 64], rhs=wsT[:, h, :], start=True, stop=True),
                             reads=[("zvn", zb), "wsT", ("zg", zb)], writes=[("pz", zb)], pe_chain=True)
                    S.op("dve", lambda: nc.vector.tensor_tensor(out=tg[zb][:], in0=pz_[zb][:, :].rearrange("p (a b) -> p a b", a=2), in1=bsT[:], op=ALU.add),
                         reads=[("pz", zb)] + bsT_keys, writes=[("tg", zb)])
                    S.op("pool", lambda: nc.gpsimd.tensor_tensor(out=ybT[cb][:, :, ti * P:(ti + 1) * P], in0=tg[zb][:], in1=zuT[cb][:, :, ti * P:(ti + 1) * P], op=ALU.mult),
                         reads=[("tg", zb), ("zuT", cb, 0), ("zuT", cb, 1)], writes=[("ybT", cb, ti)])
                S.dma("sp", X["v"][tok0:tok0 + Wd, :].rearrange("(a p) n -> p a n", p=P), vb[cb][:, :ntl, :],
                      reads=[("vb", cb, ti) for ti in range(ntl)], writes=[("d_v", ci)])
                S.dma("sp", X["ybT"].rearrange("(mt p) t -> p mt t", p=P)[:, :, tok0:tok0 + Wd], ybT[cb][:, :, :Wd],
                      reads=[("ybT", cb, ti) for ti in range(ntl)], writes=[("d_yb", ci)])


    def phase_attn(self, l):
        nc, S, I, X = self.nc, self.S, self.I, self.X
        last = (l == DEPTH - 1)
        lam_init = 0.8 - 0.6 * math.exp(-0.3 * l)
        with contextlib.ExitStack() as ph:
            lamb = self.sb(ph, "lamb", [P, 4, 64])
            lt = self.sb(ph, "lt", [P, 2, 64])
            ls = self.sb(ph, "ls", [P, 4])
            neglam = self.sb(ph, "neglam", [P, 1])
            gsc = self.sb(ph, "gsc", [P, 1])
            onesb = self.sb(ph, "onesb", [P, P], BF16)
            S.dma("sp", lamb[:], I["da_lam"][l].partition_broadcast(P), writes=["lamb"])
            S.dma("sp", gsc[:], I["da_subln_g"][l].rearrange("(p o) -> p o", o=1), writes=["gsc"])
            S.op("dve", lambda: nc.vector.memset(onesb[:], 1.0), writes=["onesb"])
            for a in range(2):
                S.op("dve", lambda: nc.vector.tensor_tensor(out=lt[:, a, :], in0=lamb[:, 2 * a, :], in1=lamb[:, 2 * a + 1, :], op=ALU.mult),
                     reads=["lamb"], writes=[("lt", a)])
                S.op("dve", lambda: nc.vector.tensor_reduce(out=ls[:, a:a + 1], in_=lt[:, a, :], axis=AX.X, op=ALU.add),
                     reads=[("lt", a)], writes=[("ls", a)])
                S.op("act", lambda: nc.scalar.activation(out=ls[:, 2 + a:3 + a], in_=ls[:, a:a + 1], func=AF.Exp), reads=[("ls", a)], writes=[("le", a)])
            S.op("dve", lambda: nc.vector.tensor_tensor(out=neglam[:], in0=ls[:, 3:4], in1=ls[:, 2:3], op=ALU.subtract),
                 reads=[("le", 0), ("le", 1)], writes=["neglam"])
            S.op("dve", lambda: nc.vector.tensor_scalar(out=neglam[:], in0=neglam[:], scalar1=-lam_init, scalar2=None, op0=ALU.add),
                 reads=["neglam"], writes=["neglam"])
            S.op("dve", lambda: nc.vector.tensor_scalar(out=gsc[:], in0=gsc[:], scalar1=(1.0 - lam_init), scalar2=None, op0=ALU.mult),
                 reads=["gsc"], writes=["gsc"])

            kTz = [[self.sb(ph, "kTz%d_%d" % (b, m), [P, NTOK], BF16) for m in range(2)] for b in range(2)]
            vh = [self.sb(ph, "vh%d" % b, [P, NT, P], BF16) for b in range(2)]
            for b in range(2):
                for m in range(2):
                    S.op("pool", lambda: nc.gpsimd.memset(kTz[b][m][:], 0.0), writes=[("kTz", b, m)])
            qt = [self.sb(ph, "qt%d" % i, [P, 512], BF16) for i in range(2)]
            NPT = 4
            pT = [self.sb(ph, "pT%d" % i, [P, 2, 512], BF16) for i in range(NPT)]
            rd = [self.sb(ph, "rd%d" % i, [P, 512]) for i in range(2)]
            o0 = self.sb(ph, "o0", [P, 512])
            o1 = self.sb(ph, "o1", [P, 512])
            oo = self.sb(ph, "oo", [P, 512])
            sq = self.sb(ph, "sq", [P, 512], BF16)
            rt = self.sb(ph, "rt", [P, 512])
            yc = [self.sb(ph, "yc%d" % i, [P, 512], BF16) for i in range(2)]
            NSC = 2
            sc = [self.psum(ph, "sc%d" % i, [P, 2, 512]) for i in range(NSC)]
            acco = [self.psum(ph, "acco%d" % i, [P, 512]) for i in range(2)]
            accd = [self.psum(ph, "accd%d" % i, [P, 512]) for i in range(2)]
            nq = 0
            nstep = 0
            for h in range(4):
                hb = h % 2
                for m in range(2):
                    S.dma("sp", kTz[hb][m][64 * m:64 * m + 64, :], X["kT"][h * P + 64 * m:h * P + 64 * m + 64, :], writes=[("kTz", hb, m)])
                S.dma("sp", vh[hb][:], X["v"][:, h * P:(h + 1) * P].rearrange("(j p) e -> p j e", p=P), writes=[("vh", hb)])
                qchunks = [(NCTX + 512 * c, 512, list(range(NT))) for c in range(8)]
                if not last:
                    qchunks = [(0, NCTX, [0, 1])] + qchunks
                for (tok0, Wq, keys) in qchunks:
                    qb = nq % 2
                    nq += 1
                    S.dma("sp", qt[qb][:, :Wq], X["qT"][h * P:(h + 1) * P, tok0:tok0 + Wq], writes=[("qt", qb)])
                    LAG = 2

                    def emit_qk(ki):
                        j = keys[ki]
                        g = nstep + ki
                        for m in range(2):
                            S.op("pe", lambda: nc.tensor.matmul(sc[g % NSC][:, m, :Wq], lhsT=kTz[hb][m][:, j * P:(j + 1) * P], rhs=qt[qb][:, :Wq], start=True, stop=True),
                                 reads=[("kTz", hb, m), ("qt", qb)], writes=[("sc", g % NSC, m)], pe_chain=True)
                        S.op("act", lambda: nc.scalar.activation(out=pT[g % NPT][:, :, :Wq], in_=sc[g % NSC][:, :, :Wq], func=AF.Exp, scale=0.125),
                             reads=[("sc", g % NSC, 0), ("sc", g % NSC, 1)], writes=[("pT", g % NPT)])

                    def emit_pv(ki):
                        j = keys[ki]
                        g = nstep + ki
                        first = (ki == 0)
                        lastk = (ki == len(keys) - 1)
                        for m in range(2):
                            S.op("pe", lambda: nc.tensor.matmul(acco[m][:, :Wq], lhsT=vh[hb][:, j, :], rhs=pT[g % NPT][:, m, :Wq], start=first, stop=lastk),
                                 reads=[("vh", hb), ("pT", g % NPT)], writes=[("acco", m)], pe_chain=True)
                            S.op("pe", lambda: nc.tensor.matmul(accd[m][:, :Wq], lhsT=onesb[:], rhs=pT[g % NPT][:, m, :Wq], start=first, stop=lastk),
                                 reads=["onesb", ("pT", g % NPT)], writes=[("accd", m)], pe_chain=True)

                    for ki in range(len(keys) + LAG):
                        if ki < len(keys):
                            emit_qk(ki)
                        if ki - LAG >= 0:
                            emit_pv(ki - LAG)
                    nstep += len(keys)
                    for m in range(2):
                        S.op("dve", lambda: nc.vector.reciprocal(out=rd[m][:, :Wq], in_=accd[m][:, :Wq]), reads=[("accd", m)], writes=[("rd", m)])
                    S.op("dve", lambda: nc.vector.tensor_tensor(out=o0[:, :Wq], in0=acco[0][:, :Wq], in1=rd[0][:, :Wq], op=ALU.mult),
                         reads=[("acco", 0), ("rd", 0)], writes=["o0"])
                    S.op("dve", lambda: nc.vector.tensor_tensor(out=o1[:, :Wq], in0=acco[1][:, :Wq], in1=rd[1][:, :Wq], op=ALU.mult),
                         reads=[("acco", 1), ("rd", 1)], writes=["o1"])
                    S.op("dve", lambda: nc.vector.scalar_tensor_tensor(out=oo[:, :Wq], in0=o1[:, :Wq], scalar=neglam[:, 0:1], in1=o0[:, :Wq], op0=ALU.mult, op1=ALU.add),
                         reads=["o0", "o1", "neglam"], writes=["oo"])
                    S.op("pool", lambda: nc.gpsimd.tensor_tensor(out=sq[:, :Wq], in0=oo[:, :Wq], in1=oo[:, :Wq], op=ALU.mult), reads=["oo"], writes=["sq"])
                    g = nstep % NSC
                    S.op("pe", lambda: nc.tensor.matmul(sc[g][:, 0, :Wq], lhsT=onesb[:], rhs=sq[:, :Wq], start=True, stop=True),
                         reads=["onesb", "sq"], writes=[("sc", g, 0)], pe_chain=True)
                    S.op("act", lambda: nc.scalar.activation(out=rt[:, :Wq], in_=sc[g][:, 0, :Wq], func=AF.Sqrt, scale=1.0 / 128, bias=LN_EPS),
                         reads=[("sc", g, 0)], writes=["rt"])
                    nstep += 1
                    S.op("dve", lambda: nc.vector.reciprocal(out=rt[:, :Wq], in_=rt[:, :Wq]), reads=["rt"], writes=["rt"])
                    yb_ = nq % 2
                    S.op("dve", lambda: nc.vector.scalar_tensor_tensor(out=yc[yb_][:, :Wq], in0=oo[:, :Wq], scalar=gsc[:, 0:1], in1=rt[:, :Wq], op0=ALU.mult, op1=ALU.mult),
                         reads=["oo", "gsc", "rt"], writes=[("yc", yb_)])
                    S.dma("sp", X["ycT"][h * P:(h + 1) * P, tok0:tok0 + Wq], yc[yb_][:, :Wq], reads=[("yc", yb_)], writes=[("d_yc", yb_)])

    def phase_s5(self, l):
        nc, S, I, X = self.nc, self.S, self.I, self.X
        TWO_PI = 2.0 * math.pi
        INV2PI = 1.0 / TWO_PI
        MAGIC = 12582912.0
        V = nc.vector

        def dve(fn, reads, writes):
            return S.op("dve", fn, reads=reads, writes=writes)

        def reduce_angle(out, x, tmp, shift, kx, kout, ktmp):
            dve(lambda: V.tensor_scalar(out=tmp, in0=x, scalar1=INV2PI, scalar2=shift * INV2PI + MAGIC, op0=ALU.mult, op1=ALU.add), [kx], [ktmp])
            dve(lambda: V.tensor_scalar(out=tmp, in0=tmp, scalar1=-MAGIC, scalar2=None, op0=ALU.add), [ktmp], [ktmp])
            dve(lambda: V.scalar_tensor_tensor(out=out, in0=tmp, scalar=-TWO_PI, in1=x, op0=ALU.mult, op1=ALU.add), [ktmp, kx], [kout])

        def cossin(cos_o, sin_o, x, tmp, tmp2, kx, kc_, ks_, ktmp, ktmp2):
            reduce_angle(tmp2, x, tmp, 0.0, kx, ktmp2, ktmp)
            S.op("act", lambda: nc.scalar.activation(out=sin_o, in_=tmp2, func=AF.Sin), reads=[ktmp2], writes=[ks_])
            reduce_angle(tmp2, x, tmp, math.pi / 2, kx, ktmp2, ktmp)
            S.op("act", lambda: nc.scalar.activation(out=cos_o, in_=tmp2, func=AF.Sin, bias=halfpi[:, 0:1]), reads=[ktmp2, "halfpi"], writes=[kc_])

        with contextlib.ExitStack() as ph:
            aT_sb = self.sb(ph, "aT_sb", [P, 2, NTOK], BF16)
            yacc = self.sb(ph, "yacc", [P, 2, NTOK], F32)
            dcol = self.sb(ph, "dcol", [P, 2])
            halfpi = self.sb(ph, "halfpi", [P, 1])
            sigp = self.sb(ph, "sigp", [P, 2])
            nsig = self.sb(ph, "nsig", [P, 2])
            sigf = self.sb(ph, "sigf", [P, 2, P])
            Bmat = [self.sb(ph, "Bmat%d" % d, [P, 2, 1024], BF16) for d in range(2)]
            WpreR = [self.sb(ph, "WpreR%d" % d, [P, 2, 512]) for d in range(2)]
            WpreI = [self.sb(ph, "WpreI%d" % d, [P, 2, 512]) for d in range(2)]
            WpostR = [self.sb(ph, "WpostR%d" % d, [P, 8, P]) for d in range(2)]
            WpostI = [self.sb(ph, "WpostI%d" % d, [P, 8, P]) for d in range(2)]
            A128R = [self.sb(ph, "A128R%d" % d, [P, 8]) for d in range(2)]
            A128I = [self.sb(ph, "A128I%d" % d, [P, 8]) for d in range(2)]
            Cm = [self.sb(ph, "Cm%d" % d, [P, 16, P], BF16) for d in range(2)]
            Mdir = [self.sb(ph, "Mdir%d" % d, [P, P], BF16) for d in range(2)]
            E8 = self.sb(ph, "E8", [P, 8, P], BF16)
            cTh = [[self.sb(ph, "cTh%d%d" % (d, k), [P, P], BF16) for k in range(2)] for d in range(2)]
            cTl = [[self.sb(ph, "cTl%d%d" % (d, k), [P, P], BF16) for k in range(2)] for d in range(2)]

            S.dma("sp", aT_sb[:], X["aT"].rearrange("(kc p) t -> p kc t", p=P), writes=["aT_sb"])
            S.dma("sp", dcol[:], I["s5_d"][l].rearrange("(kc p) -> p kc", p=P), writes=["dcol"], allow_slow_non_contiguous=True)
            S.dma("sp", sigp[:], I["k_sigp"], writes=["sigp"])
            S.dma("sp", sigf[:], I["k_sigf"], writes=["sigf"])
            S.dma("pool", Mdir[0][:], I["k_mfwd"], writes=[("Mdir", 0)])
            S.dma("pool", Mdir[1][:], I["k_mbwd"], writes=[("Mdir", 1)])
            S.dma("pool", E8[:], I["k_e8"], writes=["E8"])
            Mneg = [self.sb(ph, "Mneg%d" % d, [P, P], BF16) for d in range(2)]
            CmN = [self.sb(ph, "CmN%d" % d, [P, 8, P], BF16) for d in range(2)]
            for d in range(2):
                dve(lambda: V.tensor_scalar(out=Mneg[d][:], in0=Mdir[d][:], scalar1=-1.0, scalar2=None, op0=ALU.mult), [("Mdir", d)], [("Mneg", d)])
            dve(lambda: V.memset(halfpi[:], math.pi / 2), [], ["halfpi"])
            dve(lambda: V.tensor_scalar(out=nsig[:], in0=sigp[:], scalar1=-1.0, scalar2=None, op0=ALU.mult), ["sigp"], ["nsig"])
            for d in range(2):
                for k in range(2):
                    S.op("pool", lambda: nc.gpsimd.memset(cTh[d][k][:], 0.0), writes=[("cTh", d, k)])
                    S.op("pool", lambda: nc.gpsimd.memset(cTl[d][k][:], 0.0), writes=[("cTl", d, k)])
            for kc in range(2):
                dve(lambda: V.tensor_scalar(out=yacc[:, kc, :], in0=aT_sb[:, kc, :], scalar1=dcol[:, kc:kc + 1], scalar2=None, op0=ALU.mult),
                    ["aT_sb", "dcol"], [("yacc", kc, i) for i in range(NT)])

            with contextlib.ExitStack() as su:
                R = {}
                for n in ("LR", "LI", "lrdt", "ang", "mag", "cs", "sn", "tA", "tB", "are", "aim", "zre", "zim", "x", "t3", "t4"):
                    R[n] = self.sb(su, "r_" + n, [P, 1024])
                LS = self.sb(su, "r_LS", [P, 16])
                braw = [self.sb(su, "braw%d" % i, [P, 512]) for i in range(2)]
                CmS = self.sb(su, "CmS", [P, 16, P])
                Cc = {}
                for n in ("LR", "LI", "LS", "lrdt", "ang", "angr", "t", "e", "x", "cs", "sn", "mg"):
                    Cc[n] = self.sb(su, "c_" + n, [P, 8])
                for d in range(2):
                    k_ = lambda n: ("su", n)
                    S.dma("sp", R["LR"][:], I["s5_lam_re"][l, d].rearrange("g p -> (g p)").partition_broadcast(P), writes=[k_("LR")])
                    S.dma("sp", R["LI"][:], I["s5_lam_im"][l, d].rearrange("g p -> (g p)").partition_broadcast(P), writes=[k_("LI")])
                    S.dma("sp", LS[:], I["s5_log_step"][l, d].partition_broadcast(P), writes=[k_("LS")])
                    S.op("act", lambda: nc.scalar.activation(out=LS[:], in_=LS[:], func=AF.Exp), reads=[k_("LS")], writes=[k_("LS")])
                    dtb = LS[:, :].unsqueeze(2).to_broadcast([P, 16, 64])
                    v3 = lambda t: t[:].rearrange("p (g q) -> p g q", g=16)
                    dve(lambda: V.tensor_tensor(out=v3(R["lrdt"]), in0=v3(R["LR"]), in1=dtb, op=ALU.mult), [k_("LR"), k_("LS")], [k_("lrdt")])
                    dve(lambda: V.tensor_tensor(out=v3(R["ang"]), in0=v3(R["LI"]), in1=dtb, op=ALU.mult), [k_("LI"), k_("LS")], [k_("ang")])
                    S.op("act", lambda: nc.scalar.activation(out=R["mag"][:], in_=R["lrdt"][:], func=AF.Exp), reads=[k_("lrdt")], writes=[k_("mag")])
                    reduce_angle(R["tB"][:], R["ang"][:], R["tA"][:], 0.0, k_("ang"), k_("tB"), k_("tA"))
                    dve(lambda: V.tensor_copy(out=R["ang"][:], in_=R["tB"][:]), [k_("tB")], [k_("ang")])
                    cossin(R["cs"][:], R["sn"][:], R["ang"][:], R["tA"][:], R["tB"][:], k_("ang"), k_("cs"), k_("sn"), k_("tA"), k_("tB"))
                    dve(lambda: V.tensor_tensor(out=R["are"][:], in0=R["mag"][:], in1=R["cs"][:], op=ALU.mult), [k_("mag"), k_("cs")], [k_("are")])
                    dve(lambda: V.tensor_tensor(out=R["aim"][:], in0=R["mag"][:], in1=R["sn"][:], op=ALU.mult), [k_("mag"), k_("sn")], [k_("aim")])
                    dve(lambda: V.tensor_tensor(out=R["tA"][:], in0=R["LR"][:], in1=R["LR"][:], op=ALU.mult), [k_("LR")], [k_("tA")])
                    dve(lambda: V.tensor_tensor(out=R["tB"][:], in0=R["LI"][:], in1=R["LI"][:], op=ALU.mult), [k_("LI")], [k_("tB")])
                    dve(lambda: V.tensor_tensor(out=R["cs"][:], in0=R["tA"][:], in1=R["tB"][:], op=ALU.add), [k_("tA"), k_("tB")], [k_("cs")])
                    dve(lambda: V.reciprocal(out=R["cs"][:], in_=R["cs"][:]), [k_("cs")], [k_("cs")])
                    dve(lambda: V.tensor_scalar(out=R["are"][:], in0=R["are"][:], scalar1=-1.0, scalar2=None, op0=ALU.add), [k_("are")], [k_("are")])
                    dve(lambda: V.tensor_tensor(out=R["tA"][:], in0=R["are"][:], in1=R["LR"][:], op=ALU.mult), [k_("are"), k_("LR")], [k_("tA")])
                    dve(lambda: V.tensor_tensor(out=R["tB"][:], in0=R["aim"][:], in1=R["LI"][:], op=ALU.mult), [k_("aim"), k_("LI")], [k_("tB")])
                    dve(lambda: V.tensor_tensor(out=R["tA"][:], in0=R["tA"][:], in1=R["tB"][:], op=ALU.add), [k_("tA"), k_("tB")], [k_("tA")])
                    dve(lambda: V.tensor_tensor(out=R["zre"][:], in0=R["tA"][:], in1=R["cs"][:], op=ALU.mult), [k_("tA"), k_("cs")], [k_("zre")])
                    dve(lambda: V.tensor_tensor(out=R["tA"][:], in0=R["aim"][:], in1=R["LR"][:], op=ALU.mult), [k_("aim"), k_("LR")], [k_("tA")])
                    dve(lambda: V.tensor_tensor(out=R["tB"][:], in0=R["are"][:], in1=R["LI"][:], op=ALU.mult), [k_("are"), k_("LI")], [k_("tB")])
                    dve(lambda: V.tensor_tensor(out=R["tA"][:], in0=R["tA"][:], in1=R["tB"][:], op=ALU.subtract), [k_("tA"), k_("tB")], [k_("tA")])
                    dve(lambda: V.tensor_tensor(out=R["zim"][:], in0=R["tA"][:], in1=R["cs"][:], op=ALU.mult), [k_("tA"), k_("cs")], [k_("zim")])
                    for kc in range(2):
                        hs = slice(kc * 512, (kc + 1) * 512)
                        for ri, src in enumerate((I["s5_b_re"], I["s5_b_im"])):
                            dve(lambda: V.memset(braw[ri][:], 0.0), [], [("braw", ri, gl) for gl in range(8)])
                            for gl in range(8):
                                g = kc * 8 + gl
                                S.dma("sp", braw[ri][16 * gl:16 * gl + 16, gl * 64:(gl + 1) * 64], src[l, d, g].rearrange("p c -> c p"),
                                      reads=[], writes=[("braw", ri, gl)], allow_slow_non_contiguous=True)
                        tA = R["tA"][:, 0:512]; tB = R["tB"][:, 0:512]
                        dve(lambda: V.tensor_tensor(out=tA, in0=braw[0][:], in1=R["zre"][:, hs], op=ALU.mult), [("braw", 0, gl) for gl in range(8)] + [k_("zre")], [k_("tA")])
                        dve(lambda: V.tensor_tensor(out=tB, in0=braw[1][:], in1=R["zim"][:, hs], op=ALU.mult), [("braw", 1, gl) for gl in range(8)] + [k_("zim")], [k_("tB")])
                        dve(lambda: V.tensor_tensor(out=Bmat[d][:, kc, 0:512], in0=tA, in1=tB, op=ALU.subtract), [k_("tA"), k_("tB")], [("Bmat", d, kc, 0)])
                        dve(lambda: V.tensor_tensor(out=tA, in0=braw[0][:], in1=R["zim"][:, hs], op=ALU.mult), [("braw", 0, gl) for gl in range(8)] + [k_("zim")], [k_("tA")])
                        dve(lambda: V.tensor_tensor(out=tB, in0=braw[1][:], in1=R["zre"][:, hs], op=ALU.mult), [("braw", 1, gl) for gl in range(8)] + [k_("zre")], [k_("tB")])
                        dve(lambda: V.tensor_tensor(out=Bmat[d][:, kc, 512:1024], in0=tA, in1=tB, op=ALU.add), [k_("tA"), k_("tB")], [("Bmat", d, kc, 1)])
                        S.op("act", lambda: nc.scalar.activation(out=R["mag"][:, 0:512], in_=R["lrdt"][:, hs], func=AF.Exp, scale=nsig[:, d:d + 1]),
                             reads=[k_("lrdt"), "nsig"], writes=[k_("mag")])
                        dve(lambda: V.tensor_scalar(out=R["x"][:, 0:512], in0=R["ang"][:, hs], scalar1=sigp[:, d:d + 1], scalar2=None, op0=ALU.mult),
                            [k_("ang"), "sigp"], [k_("x")])
                        cossin(R["cs"][:, 0:512], R["sn"][:, 0:512], R["x"][:, 0:512], R["t3"][:, 0:512], R["t4"][:, 0:512],
                               k_("x"), k_("cs"), k_("sn"), k_("t3"), k_("t4"))
                        dve(lambda: V.tensor_tensor(out=WpreR[d][:, kc, :], in0=R["mag"][:, 0:512], in1=R["cs"][:, 0:512], op=ALU.mult),
                            [k_("mag"), k_("cs")], [("WpreR", d, kc)])
                        dve(lambda: V.scalar_tensor_tensor(out=WpreI[d][:, kc, :], in0=R["mag"][:, 0:512], scalar=-1.0, in1=R["sn"][:, 0:512], op0=ALU.mult, op1=ALU.mult),
                            [k_("mag"), k_("sn")], [("WpreI", d, kc)])
                    S.dma("sp", Cc["LR"][:], I["s5_lam_re"][l, d].rearrange("g p -> (g p)").rearrange("(c q) -> q c", q=P), writes=[k_("cLR")], allow_slow_non_contiguous=True)
                    S.dma("sp", Cc["LI"][:], I["s5_lam_im"][l, d].rearrange("g p -> (g p)").rearrange("(c q) -> q c", q=P), writes=[k_("cLI")], allow_slow_non_contiguous=True)
                    for hh in range(2):
                        S.dma("sp", Cc["LS"][64 * hh:64 * hh + 64, :], I["s5_log_step"][l, d].rearrange("(c two) -> two c", two=2)[hh].partition_broadcast(64),
                              writes=[k_("cLS%d" % hh)], allow_slow_non_contiguous=True)
                    S.op("act", lambda: nc.scalar.activation(out=Cc["LS"][:], in_=Cc["LS"][:], func=AF.Exp), reads=[k_("cLS0"), k_("cLS1")], writes=[k_("cdt")])
                    dve(lambda: V.tensor_tensor(out=Cc["lrdt"][:], in0=Cc["LR"][:], in1=Cc["LS"][:], op=ALU.mult), [k_("cLR"), k_("cdt")], [k_("clrdt")])
                    dve(lambda: V.tensor_tensor(out=Cc["ang"][:], in0=Cc["LI"][:], in1=Cc["LS"][:], op=ALU.mult), [k_("cLI"), k_("cdt")], [k_("cang")])
                    reduce_angle(Cc["angr"][:], Cc["ang"][:], Cc["t"][:], 0.0, k_("cang"), k_("cangr"), k_("ct"))
                    E3 = R["tA"][:].rearrange("p (c t) -> p c t", c=8)
                    X3 = R["tB"][:].rearrange("p (c t) -> p c t", c=8)
                    sgb = sigf[:, d, :].unsqueeze(1).to_broadcast([P, 8, P])
                    dve(lambda: V.tensor_tensor(out=E3, in0=Cc["lrdt"][:, :].unsqueeze(2).to_broadcast([P, 8, P]), in1=sgb, op=ALU.mult),
                        [k_("clrdt"), "sigf"], [k_("tA")])
                    S.op("act", lambda: nc.scalar.activation(out=R["mag"][:], in_=R["tA"][:], func=AF.Exp), reads=[k_("tA")], writes=[k_("mag")])
                    dve(lambda: V.tensor_tensor(out=X3, in0=Cc["angr"][:, :].unsqueeze(2).to_broadcast([P, 8, P]), in1=sgb, op=ALU.mult),
                        [k_("cangr"), "sigf"], [k_("tB")])
                    dve(lambda: V.tensor_copy(out=R["x"][:], in_=R["tB"][:]), [k_("tB")], [k_("x")])
                    cossin(R["cs"][:], R["sn"][:], R["x"][:], R["t3"][:], R["t4"][:], k_("x"), k_("cs"), k_("sn"), k_("t3"), k_("t4"))
                    dve(lambda: V.tensor_tensor(out=WpostR[d][:].rearrange("p c t -> p (c t)"), in0=R["mag"][:], in1=R["cs"][:], op=ALU.mult),
                        [k_("mag"), k_("cs")], [("WpostR", d)])
                    dve(lambda: V.tensor_tensor(out=WpostI[d][:].rearrange("p c t -> p (c t)"), in0=R["mag"][:], in1=R["sn"][:], op=ALU.mult),
                        [k_("mag"), k_("sn")], [("WpostI", d)])
                    dve(lambda: V.tensor_scalar(out=Cc["e"][:], in0=Cc["lrdt"][:], scalar1=128.0, scalar2=None, op0=ALU.mult), [k_("clrdt")], [k_("ce")])
                    S.op("act", lambda: nc.scalar.activation(out=Cc["mg"][:], in_=Cc["e"][:], func=AF.Exp), reads=[k_("ce")], writes=[k_("cmg")])
                    dve(lambda: V.tensor_scalar(out=Cc["x"][:], in0=Cc["angr"][:], scalar1=128.0, scalar2=None, op0=ALU.mult), [k_("cangr")], [k_("cx")])
                    cossin(Cc["cs"][:], Cc["sn"][:], Cc["x"][:], Cc["t"][:], Cc["e"][:], k_("cx"), k_("ccs"), k_("csn"), k_("ct"), k_("ce"))
                    dve(lambda: V.tensor_tensor(out=A128R[d][:], in0=Cc["mg"][:], in1=Cc["cs"][:], op=ALU.mult), [k_("cmg"), k_("ccs")], [("A128R", d)])
                    dve(lambda: V.tensor_tensor(out=A128I[d][:], in0=Cc["mg"][:], in1=Cc["sn"][:], op=ALU.mult), [k_("cmg"), k_("csn")], [("A128I", d)])
                    cmk = [("CmS", g, ri) for g in range(16) for ri in range(2)]
                    dve(lambda: V.memset(CmS[:], 0.0), [], cmk)
                    for g in range(16):
                        kc, gl = g // 8, g % 8
                        j, hh = gl // 2, gl % 2
                        for ri, src in enumerate((I["s5_c_re"], I["s5_c_im"])):
                            S.dma("sp", CmS[64 * hh:64 * hh + 64, kc * 8 + ri * 4 + j, 16 * gl:16 * gl + 16], src[l, d, g].rearrange("c p -> p c"),
                                  reads=[], writes=[("CmS", g, ri)], allow_slow_non_contiguous=True)
                    Cm4 = Cm[d][:].rearrange("p (k r j) c -> p k r (j c)", k=2, r=2)
                    CmS4 = CmS[:].rearrange("p (k r j) c -> p k r (j c)", k=2, r=2)
                    dve(lambda: V.tensor_copy(out=Cm4[:, :, 0, :], in_=CmS4[:, :, 0, :]), cmk, [("Cm", d, 0)])
                    dve(lambda: V.tensor_scalar(out=Cm4[:, :, 1, :], in0=CmS4[:, :, 1, :], scalar1=-1.0, scalar2=None, op0=ALU.mult), cmk, [("Cm", d, 1)])
                    dve(lambda: V.tensor_scalar(out=CmN[d][:].rearrange("p (k j) c -> p k (j c)", k=2), in0=CmS4[:, :, 0, :], scalar1=-1.0, scalar2=None, op0=ALU.mult),
                        cmk, [("CmN", d)])
            S.barrier()

            P1 = [[self.sb(ph, "s5P1%d%d" % (d, k), [P, 2, 512], BF16) for k in range(2)] for d in range(2)]
            P2 = [[self.sb(ph, "s5P2%d%d" % (d, k), [P, 2, 512], BF16) for k in range(2)] for d in range(2)]
            Q1 = [[self.sb(ph, "s5Q1%d%d" % (d, k), [P, 2, 512], BF16) for k in range(2)] for d in range(2)]
            Q2 = [[self.sb(ph, "s5Q2%d%d" % (d, k), [P, 2, 512], BF16) for k in range(2)] for d in range(2)]
            tc1 = self.sb(ph, "s5tc1", [P, 2, 4])
            tc2 = self.sb(ph, "s5tc2", [P, 2, 4])
            cn = self.sb(ph, "s5cn", [P, 8])
            bu = self.psum(ph, "s5bu", [P, 2, 512])
            st_ = [self.psum(ph, "s5st%d" % d, [P, 8, P]) for d in range(2)]
            ctp = self.psum(ph, "s5ctp", [P, 512])
            yo = self.psum(ph, "s5yo", [P, 512])
            orders = [list(range(NT)), [1, 0] + list(range(NT - 1, 1, -1))]
            for step in range(NT):
                for d in range(2):
                    ti = orders[d][step]
                    cols = slice(ti * P, (ti + 1) * P)
                    tl = 127 if d == 0 else 0
                    st = st_[d]
                    for kc in range(2):
                        p1, p2, q1, q2 = P1[d][kc], P2[d][kc], Q1[d][kc], Q2[d][kc]
                        for ri in range(2):
                            S.op("pe", lambda: nc.tensor.matmul(bu[:, ri, :], lhsT=aT_sb[:, kc, cols], rhs=Bmat[d][:, kc, ri * 512:(ri + 1) * 512], start=True, stop=True),
                                 reads=["aT_sb"], writes=["bu"], pe_chain=True)
                        dve(lambda: V.tensor_tensor(out=p1[:], in0=bu[:], in1=WpreR[d][:, kc, :].unsqueeze(1).to_broadcast([P, 2, 512]), op=ALU.mult),
                            ["bu"], [("P1", d, kc)])
                        dve(lambda: V.tensor_tensor(out=p2[:], in0=bu[:], in1=WpreI[d][:, kc, :].unsqueeze(1).to_broadcast([P, 2, 512]), op=ALU.mult),
                            ["bu"], [("P2", d, kc)])
                        for jj in range(8):
                            j = jj % 4
                            js = slice(j * P, (j + 1) * P)
                            if jj < 4:
                                terms = [(p1[:, 0, js], Mdir[d]), (p2[:, 1, js], Mneg[d])]
                            else:
                                terms = [(p2[:, 0, js], Mdir[d]), (p1[:, 1, js], Mdir[d])]
                            for tix, (lh, rh) in enumerate(terms):
                                S.op("pe", lambda: nc.tensor.matmul(st[:, jj, :], lhsT=lh, rhs=rh[:], start=(tix == 0), stop=(tix == 1 and step == 0)),
                                     reads=[("P1", d, kc), ("P2", d, kc)], writes=[("st", d)], pe_chain=True)
                            if step > 0:
                                S.op("pe", lambda: nc.tensor.matmul(st[:, jj, :], lhsT=cTh[d][kc][:], rhs=E8[:, jj, :], start=False, stop=False),
                                     reads=[("cTh", d, kc)], writes=[("st", d)], pe_chain=True)
                                S.op("pe", lambda: nc.tensor.matmul(st[:, jj, :], lhsT=cTl[d][kc][:], rhs=E8[:, jj, :], start=False, stop=True),
                                     reads=[("cTl", d, kc)], writes=[("st", d)], pe_chain=True)
                        st3 = st[:].rearrange("p (r j) t -> p r (j t)", r=2)
                        wr = WpostR[d][:, kc * 4:(kc + 1) * 4, :].rearrange("p j t -> p (j t)").unsqueeze(1).to_broadcast([P, 2, 512])
                        wi = WpostI[d][:, kc * 4:(kc + 1) * 4, :].rearrange("p j t -> p (j t)").unsqueeze(1).to_broadcast([P, 2, 512])
                        dve(lambda: V.tensor_tensor(out=q1[:], in0=st3, in1=wr, op=ALU.mult), [("st", d)], [("Q1", d, kc)])
                        dve(lambda: V.tensor_tensor(out=q2[:], in0=st3, in1=wi, op=ALU.mult), [("st", d)], [("Q2", d, kc)])
                        if step < NT - 1:
                            sl = st[:, :, tl].rearrange("p (r j) -> p r j", r=2)
                            ar = A128R[d][:, kc * 4:(kc + 1) * 4].unsqueeze(1).to_broadcast([P, 2, 4])
                            ai = A128I[d][:, kc * 4:(kc + 1) * 4].unsqueeze(1).to_broadcast([P, 2, 4])
                            dve(lambda: V.tensor_tensor(out=tc1[:], in0=sl, in1=ar, op=ALU.mult), [("st", d)], ["tc1"])
                            dve(lambda: V.tensor_tensor(out=tc2[:], in0=sl, in1=ai, op=ALU.mult), [("st", d)], ["tc2"])
                            dve(lambda: V.tensor_tensor(out=cn[:, 0:4], in0=tc1[:, 0, :], in1=tc2[:, 1, :], op=ALU.subtract), ["tc1", "tc2"], ["cn0"])
                            dve(lambda: V.tensor_tensor(out=cn[:, 4:8], in0=tc2[:, 0, :], in1=tc1[:, 1, :], op=ALU.add), ["tc1", "tc2"], ["cn1"])
                            S.op("pe", lambda: nc.tensor.transpose(ctp[0:8, 0:P], cn[:, :], self.ident[:]), reads=["cn0", "cn1", "ident"], writes=["ctp"], pe_chain=True)
                            S.op("act", lambda: nc.scalar.copy(out=cTh[d][kc][0:8, :], in_=ctp[0:8, 0:P]), reads=["ctp"], writes=[("cTh", d, kc)])
                            dve(lambda: V.tensor_tensor(out=cTl[d][kc][0:8, :], in0=ctp[0:8, 0:P], in1=cTh[d][kc][0:8, :], op=ALU.subtract),
                                ["ctp", ("cTh", d, kc)], [("cTl", d, kc)])
                        rterms = []
                        for j in range(4):
                            js = slice(j * P, (j + 1) * P)
                            rterms += [(Cm[d][:, kc * 8 + j, :], q1[:, 0, js], ("Q1", d, kc)), (CmN[d][:, kc * 4 + j, :], q2[:, 1, js], ("Q2", d, kc)),
                                       (Cm[d][:, kc * 8 + 4 + j, :], q2[:, 0, js], ("Q2", d, kc)), (Cm[d][:, kc * 8 + 4 + j, :], q1[:, 1, js], ("Q1", d, kc))]
                        for tix, (lh, rh, rk) in enumerate(rterms):
                            S.op("pe", lambda: nc.tensor.matmul(yo[:, 0:P], lhsT=lh, rhs=rh, start=(tix == 0), stop=(tix == len(rterms) - 1)),
                                 reads=[rk], writes=["yo"], pe_chain=True)
                        dve(lambda: V.tensor_tensor(out=yacc[:, kc, cols], in0=yo[:, 0:P], in1=yacc[:, kc, cols], op=ALU.add),
                            ["yo", ("yacc", kc, ti)], [("yacc", kc, ti)])
            for kc in range(2):
                S.op("act", lambda: nc.scalar.activation(out=aT_sb[:, kc, :], in_=yacc[:, kc, :], func=AF.Gelu_apprx_tanh),
                     reads=[("yacc", kc, i) for i in range(NT)] + ["aT_sb"], writes=["aT_sb"])
            S.dma("sp", X["yaT"].rearrange("(kc p) t -> p kc t", p=P), aT_sb[:], reads=["aT_sb"], writes=["d_yaT"])

    def ln_stats(self, src, st6, mv, rs, key):
        nc, S = self.nc, self.S
        for hf in range(2):
            S.op("dve", lambda: nc.vector.bn_stats(out=st6[:, hf, :], in_=src[:, hf * 512:(hf + 1) * 512]), reads=[key], writes=[("st6", id(st6), hf)])
        S.op("dve", lambda: nc.vector.bn_aggr(out=mv[:, :], in_=st6[:, :, :].rearrange("p a b -> p (a b)")),
             reads=[("st6", id(st6), 0), ("st6", id(st6), 1)], writes=[("mv", id(mv))])
        S.op("act", lambda: nc.scalar.activation(out=rs[:, 0:1], in_=mv[:, 1:2], func=AF.Sqrt, bias=LN_EPS, scale=1.0),
             reads=[("mv", id(mv))], writes=[("rs0", id(rs))])
        S.op("dve", lambda: nc.vector.reciprocal(out=rs[:, 1:2], in_=rs[:, 0:1]), reads=[("rs0", id(rs))], writes=[("rs1", id(rs))])
        return [("mv", id(mv)), ("rs1", id(rs))]

    def phase_merge(self, l):
        nc, S, I, X = self.nc, self.S, self.I, self.X
        last = (l == DEPTH - 1)
        with contextlib.ExitStack() as ph:
            wG = self.sb(ph, "wG", [P, 8, 3072], BF16)
            for br in range(3):
                S.dma("pool", wG[:, :, br * 1024:(br + 1) * 1024],
                      I["w_in"][l][:, 2304 + br * 1024:2304 + (br + 1) * 1024].rearrange("(kt p) n -> p kt n", p=P), writes=[("wG", br)])
            wsm = {}
            for n, kts in (("w_glu_val", 2), ("w_glu_gate", 2), ("w_proj_gm", 2), ("w_proj_da", 4), ("w_out", 8)):
                wsm[n] = self.sb(ph, "m_" + n, [P, kts, D], BF16)
                S.dma("pool", wsm[n][:], I[n][l].rearrange("(kt p) n -> p kt n", p=P), writes=[n])
            lng = self.sb(ph, "lng", [P, D])
            lnb = self.sb(ph, "lnb", [P, D])
            S.dma("sp", lng[:], I["ln1_g"][l].partition_broadcast(P), writes=["lng"])
            S.dma("sp", lnb[:], I["ln1_b"][l].partition_broadcast(P), writes=["lnb"])
            ya = [self.sb(ph, "g_ya%d" % i, [P, 2, 512], BF16) for i in range(2)]
            yb = [self.sb(ph, "g_yb%d" % i, [P, 2, 512], BF16) for i in range(2)]
            yc = [self.sb(ph, "g_yc%d" % i, [P, 4, 512], BF16) for i in range(2)]
            uu = [self.sb(ph, "g_uu%d" % i, [P, 8, 512], BF16) for i in range(2)]
            mT = [self.sb(ph, "g_mT%d" % i, [P, 8, 512], BF16) for i in range(2)]
            fT = [self.sb(ph, "g_fT%d" % i, [P, 8, 512], BF16) for i in range(1)] * 2
            sg = [self.sb(ph, "g_sg%d" % i, [P, 4, 512]) for i in range(1)] * 2
            tm = [self.sb(ph, "g_tm%d" % i, [P, 4, 512]) for i in range(1)] * 2
            hx = [self.sb(ph, "g_hx%d" % i, [P, D]) for i in range(2)]
            tt_ = [self.sb(ph, "g_tt%d" % i, [P, D]) for i in range(2)]
            xn = [self.sb(ph, "g_xn%d" % i, [P, D], BF16) for i in range(2)]
            st6 = [self.sb(ph, "g_st6%d" % i, [P, 2, 6]) for i in range(2)]
            mv = [self.sb(ph, "g_mv%d" % i, [P, 2]) for i in range(2)]
            rs = [self.sb(ph, "g_rs%d" % i, [P, 2]) for i in range(2)]
            NPB = 5
            pb_ = [self.psum(ph, "g_p%d" % i, [P, 512]) for i in range(NPB)]
            po = self.psum(ph, "g_po", [P, 2, 512])
            tp = self.psum(ph, "g_tp", [P, 8, P], BF16)
            chunks = [(2 + 4 * c, 4) for c in range(8)]
            if not last:
                chunks = [(0, 2)] + chunks
            pc_ = 0
            tcount = 0

            def load(ci):
                t0, ntl = chunks[ci]
                Wd = ntl * P
                tok0 = t0 * P
                cb = ci % 2
                S.dma("sp", ya[cb][:, :, :Wd], X["yaT"].rearrange("(kt p) t -> p kt t", p=P)[:, :, tok0:tok0 + Wd], writes=[("ya", cb)])
                S.dma("sp", yb[cb][:, :, :Wd], X["ybT"].rearrange("(kt p) t -> p kt t", p=P)[:, :, tok0:tok0 + Wd], writes=[("yb", cb)])
                S.dma("sp", yc[cb][:, :, :Wd], X["ycT"].rearrange("(kt p) t -> p kt t", p=P)[:, :, tok0:tok0 + Wd], writes=[("yc", cb)])
                S.dma("sp", uu[cb][:, :, :Wd], X["uT"].rearrange("(kt p) t -> p kt t", p=P)[:, :, tok0:tok0 + Wd], writes=[("uu", cb)])

            def dc_step(ci, dc):
                nonlocal pc_
                t0, ntl = chunks[ci]
                Wd = ntl * P
                cb = ci % 2

                def prod(wn, kts, src, skey):
                    nonlocal pc_
                    pi = pc_ % NPB; pc_ += 1
                    pt = pb_[pi]
                    for kt in range(kts):
                        if wn[0] == "g":
                            br = "ABC".index(wn[1])
                            lhsT = wG[:, kt, br * 1024 + dc * P:br * 1024 + (dc + 1) * P]
                            wkey = ("wG", br)
                        else:
                            lhsT = wsm[wn][:, kt, dc * P:(dc + 1) * P]
                            wkey = wn
                        S.op("pe", lambda: nc.tensor.matmul(pt[:, :Wd], lhsT=lhsT, rhs=src[:, kt, :Wd], start=(kt == 0), stop=(kt == kts - 1)),
                             reads=[wkey, skey], writes=[("gp", pi)], pe_chain=True)
                    return pt, ("gp", pi)

                def sig(pt, pk, sgi):
                    S.op("act", lambda: nc.scalar.activation(out=sg[0][:, sgi, :Wd], in_=pt[:, :Wd], func=AF.Sigmoid), reads=[pk], writes=[("sg", sgi)])

                pt, pk = prod("w_glu_gate", 2, ya[cb], ("ya", cb)); sig(pt, pk, 0)
                pt, pk = prod("gA", 8, uu[cb], ("uu", cb)); sig(pt, pk, 1)
                pt, pk = prod("w_glu_val", 2, ya[cb], ("ya", cb))
                S.op("dve", lambda: nc.vector.tensor_tensor(out=tm[0][:, 0, :Wd], in0=pt[:, :Wd], in1=sg[0][:, 0, :Wd], op=ALU.mult),
                     reads=[pk, ("sg", 0)], writes=[("tm", 0)])
                S.op("pool", lambda: nc.gpsimd.tensor_tensor(out=tm[0][:, 0, :Wd], in0=tm[0][:, 0, :Wd], in1=sg[0][:, 1, :Wd], op=ALU.mult),
                     reads=[("tm", 0), ("sg", 1)], writes=[("tm", 0)])
                pt, pk = prod("gB", 8, uu[cb], ("uu", cb)); sig(pt, pk, 2)
                pt, pk = prod("w_proj_gm", 2, yb[cb], ("yb", cb))
                S.op("dve", lambda: nc.vector.tensor_tensor(out=tm[0][:, 1, :Wd], in0=pt[:, :Wd], in1=sg[0][:, 2, :Wd], op=ALU.mult),
                     reads=[pk, ("sg", 2)], writes=[("tm", 1)])
                pt, pk = prod("gC", 8, uu[cb], ("uu", cb)); sig(pt, pk, 3)
                pt, pk = prod("w_proj_da", 4, yc[cb], ("yc", cb))
                S.op("dve", lambda: nc.vector.tensor_tensor(out=tm[0][:, 2, :Wd], in0=pt[:, :Wd], in1=sg[0][:, 3, :Wd], op=ALU.mult),
                     reads=[pk, ("sg", 3)], writes=[("tm", 2)])
                S.op("pool", lambda: nc.gpsimd.tensor_tensor(out=tm[0][:, 3, :Wd], in0=tm[0][:, 0, :Wd], in1=tm[0][:, 1, :Wd], op=ALU.add),
                     reads=[("tm", 0), ("tm", 1)], writes=[("tm", 3)])
                S.op("pool", lambda: nc.gpsimd.tensor_tensor(out=mT[cb][:, dc, :Wd], in0=tm[0][:, 3, :Wd], in1=tm[0][:, 2, :Wd], op=ALU.add),
                     reads=[("tm", 3), ("tm", 2)], writes=[("mT", cb, dc)])

            def tail_tile(ci, ti):
                nonlocal tcount
                t0, ntl = chunks[ci]
                s = 1 if t0 == 0 else 0
                cb = ci % 2
                i = t0 + ti
                hb = tcount % 2
                tcount += 1
                S.dma("sp", hx[hb][:], self.hsrc(l, i), writes=[("ghx", hb)])
                for nh in range(2):
                    for kt in range(8):
                        S.op("pe", lambda: nc.tensor.matmul(po[:, nh, :], lhsT=mT[cb][:, kt, ti * P:(ti + 1) * P], rhs=wsm["w_out"][:, kt, nh * 512:(nh + 1) * 512],
                                                            start=(kt == 0), stop=(kt == 7)),
                             reads=[("mT", cb, kt), "w_out"], writes=[("po", nh)], pe_chain=True)
                pof = po[:].rearrange("p a b -> p (a b)")
                S.op("dve", lambda: nc.vector.tensor_tensor(out=tt_[hb][:], in0=pof, in1=self.gb[:, 2 * s + 0, :], op=ALU.mult),
                     reads=[("po", 0), ("po", 1)], writes=[("gtt", hb)])
                S.op("dve", lambda: nc.vector.scalar_tensor_tensor(out=hx[hb][:], in0=hx[hb][:], scalar=DN_ALPHA, in1=tt_[hb][:], op0=ALU.mult, op1=ALU.add),
                     reads=[("ghx", hb), ("gtt", hb)], writes=[("ghx", hb)])
                ks = self.ln_stats(hx[hb], st6[hb], mv[hb], rs[hb], ("ghx", hb))
                S.op("dve", lambda: nc.vector.tensor_scalar(out=tt_[hb][:], in0=hx[hb][:], scalar1=mv[hb][:, 0:1], scalar2=rs[hb][:, 1:2], op0=ALU.subtract, op1=ALU.mult),
                     reads=[("ghx", hb)] + ks, writes=[("gtt", hb)])
                S.op("pool", lambda: nc.gpsimd.tensor_tensor(out=tt_[hb][:], in0=tt_[hb][:], in1=lng[:], op=ALU.mult), reads=[("gtt", hb), "lng"], writes=[("gtt", hb)])
                S.op("pool", lambda: nc.gpsimd.tensor_tensor(out=hx[hb][:], in0=tt_[hb][:], in1=lnb[:], op=ALU.add), reads=[("gtt", hb), "lnb"], writes=[("ghx", hb)])
                S.dma("sp", X["h"][i * P:(i + 1) * P, :], hx[hb][:], reads=[("ghx", hb)], writes=[("d_h", hb)])
                ks = self.ln_stats(hx[hb], st6[hb], mv[hb], rs[hb], ("ghx", hb))
                S.op("dve", lambda: nc.vector.tensor_scalar(out=xn[hb][:], in0=hx[hb][:], scalar1=mv[hb][:, 0:1], scalar2=rs[hb][:, 1:2], op0=ALU.subtract, op1=ALU.mult),
                     reads=[("ghx", hb)] + ks, writes=[("gxn", hb)])
                for kt in range(8):
                    S.op("pe", lambda: nc.tensor.transpose(tp[:, kt, :], xn[hb][:, kt * P:(kt + 1) * P], self.identb[:]),
                         reads=[("gxn", hb), "identb"], writes=["gtp"], pe_chain=True)
                for kt in range(8):
                    S.op("act", lambda: nc.scalar.activation(out=fT[0][:, kt, ti * P:(ti + 1) * P], in_=tp[:, kt, :], func=AF.Identity,
                                                             scale=self.modc[:, s, 32 + kt:33 + kt], bias=self.modc[:, s, 24 + kt:25 + kt]),
                         reads=["gtp"], writes=[("gfT", 0, ti)])

            def tail_store(ci):
                t0, ntl = chunks[ci]
                Wd = ntl * P
                tok0 = t0 * P
                S.dma("sp", X["fT"].rearrange("(kt p) t -> p kt t", p=P)[:, :, tok0:tok0 + Wd], fT[0][:, :, :Wd],
                      reads=[("gfT", 0, ti) for ti in range(ntl)], writes=[("d_fT", 0)])

            nch = len(chunks)
            load(0)
            for dc in range(8):
                dc_step(0, dc)
            for ci in range(nch):
                ntl = chunks[ci][1]
                if ci + 1 < nch:
                    load(ci + 1)
                done = 0
                for dc in range(8):
                    if ci + 1 < nch:
                        dc_step(ci + 1, dc)
                    if dc % 2 == 1 and done < ntl:
                        tail_tile(ci, done)
                        done += 1
                while done < ntl:
                    tail_tile(ci, done)
                    done += 1
                tail_store(ci)

    def phase_moe(self, l):
        nc, S, I, X = self.nc, self.S, self.I, self.X
        V = nc.vector
        last = (l == DEPTH - 1)

        def dve(fn, reads, writes):
            return S.op("dve", fn, reads=reads, writes=writes)

        with contextlib.ExitStack() as ph:
            MAXT = 18
            wrt = self.sb(ph, "wrt", [P, 8, 64], BF16)
            S.dma("pool", wrt[:], I["w_router"][l].rearrange("(kt p) n -> p kt n", p=P), writes=["wrt"])
            brb = self.sb(ph, "brb", [P, 64])
            S.dma("sp", brb[:], I["b_router"][l].partition_broadcast(P), writes=["brb"])
            lng = self.sb(ph, "lng2", [P, D])
            lnb = self.sb(ph, "lnb2", [P, D])
            S.dma("sp", lng[:], I["ln2_g"][l].partition_broadcast(P), writes=["lng"])
            S.dma("sp", lnb[:], I["ln2_b"][l].partition_broadcast(P), writes=["lnb"])
            fTb = self.sb(ph, "fTb", [P, 8, MAXT * P], BF16)
            wts = self.sb(ph, "wts", [P, MAXT, 64])
            acc = self.sb(ph, "acc", [P, MAXT, D])
            NW = 3
            wg = [self.sb(ph, "wg%d" % i, [P, 8, 256], BF16) for i in range(NW)]
            wu = [self.sb(ph, "wu%d" % i, [P, 8, 256], BF16) for i in range(NW)]
            wd = [self.sb(ph, "wd%d" % i, [P, 2, D], BF16) for i in range(NW)]
            sgt = [self.sb(ph, "sgt%d" % i, [P, 512]) for i in range(2)]
            hT = [[self.sb(ph, "hT%d%d" % (i, m), [P, 512], BF16) for m in range(2)] for i in range(2)]
            r_sc = self.sb(ph, "r_sc", [P, 64]); r_sel = self.sb(ph, "r_sel", [P, 64]); r_eq = self.sb(ph, "r_eq", [P, 64])
            r_s2 = self.sb(ph, "r_s2", [P, 64]); r_m1 = self.sb(ph, "r_m1", [P, 8]); r_m2 = self.sb(ph, "r_m2", [P, 8])
            r_gs = self.sb(ph, "r_gs", [P, 8]); r_t8 = self.sb(ph, "r_t8", [P, 8]); r_gm = self.sb(ph, "r_gm", [P, 8])
            r_gt = self.sb(ph, "r_gt", [P, 8]); r_sm = self.sb(ph, "r_sm", [P, 64]); r_e8 = self.sb(ph, "r_e8", [P, 8])
            r_em = self.sb(ph, "r_em", [P, 64]); r_w = self.sb(ph, "r_w", [P, 64]); r_ss = self.sb(ph, "r_ss", [P, 2])
            hx = [self.sb(ph, "h_hx%d" % i, [P, D]) for i in range(2)]
            tt_ = [self.sb(ph, "h_tt%d" % i, [P, D]) for i in range(2)]
            st6 = [self.sb(ph, "h_st6%d" % i, [P, 2, 6]) for i in range(2)]
            mv = [self.sb(ph, "h_mv%d" % i, [P, 2]) for i in range(2)]
            rs = [self.sb(ph, "h_rs%d" % i, [P, 2]) for i in range(2)]
            pgu = [self.psum(ph, "h_pgu%d" % i, [P, 512]) for i in range(4)]
            py = [self.psum(ph, "h_py%d" % i, [P, 2, 512]) for i in range(2)]

            if last:
                blocks = [(2, 16), (18, 16)]
            else:
                blocks = [(0, 18), (18, 16)]
            wcount = 0
            ccount = 0
            fcount = 0
            ycount = 0
            for (t0, ntl) in blocks:
                BW = ntl * P
                tok0 = t0 * P
                for kt in range(8):
                    S.dma("sp", fTb[:, kt, :BW], X["fT"][kt * P:(kt + 1) * P, tok0:tok0 + BW], writes=[("fTb", kt)])
                fkeys = [("fTb", kt) for kt in range(8)]
                for ti in range(ntl):
                    cols = slice(ti * P, (ti + 1) * P)
                    prt = pgu[ti % 4]
                    pk = ("pgu", ti % 4)
                    for kt in range(8):
                        S.op("pe", lambda: nc.tensor.matmul(prt[:, 0:64], lhsT=fTb[:, kt, cols], rhs=wrt[:, kt, :], start=(kt == 0), stop=(kt == 7)),
                             reads=[("fTb", kt), "wrt"], writes=[pk], pe_chain=True)
                    S.op("act", lambda: nc.scalar.activation(out=r_sc[:], in_=prt[:, 0:64], func=AF.Sigmoid), reads=[pk], writes=["r_sc"])
                    dve(lambda: V.tensor_tensor(out=r_sel[:], in0=r_sc[:], in1=brb[:], op=ALU.add), ["r_sc", "brb"], ["r_sel"])
                    sel3 = r_sel[:].rearrange("p (g e) -> p g e", g=8)
                    dve(lambda: V.tensor_reduce(out=r_m1[:], in_=sel3, axis=AX.X, op=ALU.max), ["r_sel"], ["r_m1"])
                    dve(lambda: V.tensor_tensor(out=r_eq[:].rearrange("p (g e) -> p g e", g=8), in0=sel3, in1=r_m1[:, :].unsqueeze(2).to_broadcast([P, 8, 8]), op=ALU.is_equal),
                        ["r_sel", "r_m1"], ["r_eq"])
                    dve(lambda: V.scalar_tensor_tensor(out=r_s2[:], in0=r_eq[:], scalar=-4.0, in1=r_sel[:], op0=ALU.mult, op1=ALU.add), ["r_eq", "r_sel"], ["r_s2"])
                    dve(lambda: V.tensor_reduce(out=r_m2[:], in_=r_s2[:].rearrange("p (g e) -> p g e", g=8), axis=AX.X, op=ALU.max), ["r_s2"], ["r_m2"])
                    dve(lambda: V.tensor_tensor(out=r_gs[:], in0=r_m1[:], in1=r_m2[:], op=ALU.add), ["r_m1", "r_m2"], ["r_gs"])
                    dve(lambda: V.max(out=r_t8[:], in_=r_gs[:]), ["r_gs"], ["r_t8"])
                    dve(lambda: V.tensor_scalar(out=r_gm[:], in0=r_gs[:], scalar1=r_t8[:, 3:4], scalar2=None, op0=ALU.is_ge), ["r_gs", "r_t8"], ["r_gm"])
                    dve(lambda: V.tensor_scalar(out=r_gt[:], in0=r_gm[:], scalar1=4.0, scalar2=-4.0, op0=ALU.mult, op1=ALU.add), ["r_gm"], ["r_gt"])
                    sm3 = r_sm[:].rearrange("p (g e) -> p g e", g=8)
                    dve(lambda: V.tensor_tensor(out=sm3, in0=sel3, in1=r_gm[:, :].unsqueeze(2).to_broadcast([P, 8, 8]), op=ALU.mult), ["r_sel", "r_gm"], ["r_sm"])
                    dve(lambda: V.tensor_tensor(out=sm3, in0=sm3, in1=r_gt[:, :].unsqueeze(2).to_broadcast([P, 8, 8]), op=ALU.add), ["r_sm", "r_gt"], ["r_sm"])
                    dve(lambda: V.max(out=r_e8[:], in_=r_sm[:]), ["r_sm"], ["r_e8"])
                    dve(lambda: V.tensor_scalar(out=r_em[:], in0=r_sm[:], scalar1=r_e8[:, 7:8], scalar2=None, op0=ALU.is_ge), ["r_sm", "r_e8"], ["r_em"])
                    dve(lambda: V.tensor_tensor(out=r_w[:], in0=r_sc[:], in1=r_em[:], op=ALU.mult), ["r_sc", "r_em"], ["r_w"])
                    dve(lambda: V.tensor_reduce(out=r_ss[:, 0:1], in_=r_w[:], axis=AX.X, op=ALU.add), ["r_w"], ["r_ss0"])
                    dve(lambda: V.reciprocal(out=r_ss[:, 1:2], in_=r_ss[:, 0:1]), ["r_ss0"], ["r_ss1"])
                    dve(lambda: V.tensor_scalar(out=wts[:, ti, :], in0=r_w[:], scalar1=r_ss[:, 1:2], scalar2=2.5, op0=ALU.mult, op1=ALU.mult), ["r_w", "r_ss1"], [("wts", ti)])
                chunks = [(c * 512, min(512, BW - c * 512)) for c in range((BW + 511) // 512)]
                items = [(e, c0, Wd) for e in range(65) for (c0, Wd) in chunks]

                def wsrc(e):
                    if e < 64:
                        return (I["w_exp_gate"][l, e], I["w_exp_up"][l, e], I["w_exp_down"][l, e])
                    return (I["w_sh_gate"][l], I["w_sh_up"][l], I["w_sh_down"][l])

                def load_w(e):
                    ws = (wbase + e) % NW
                    srcs = wsrc(e)
                    S.dma("pool", wg[ws][:], srcs[0].rearrange("(kt p) n -> p kt n", p=P), writes=[("wg", ws)])
                    S.dma("pool", wu[ws][:], srcs[1].rearrange("(kt p) n -> p kt n", p=P), writes=[("wu", ws)])
                    S.dma("pool", wd[ws][:], srcs[2].rearrange("(kt p) n -> p kt n", p=P), writes=[("wd", ws)])

                def emit_gu(k):
                    nonlocal fcount
                    e, c0, Wd = items[k]
                    ws = (wbase + e) % NW
                    cb = k % 2
                    cs_ = slice(c0, c0 + Wd)
                    if c0 == 0 and e + 1 < 65:
                        load_w(e + 1)
                    for mt in range(2):
                        pg_i = 2 * mt
                        pu_i = 2 * mt + 1
                        for (pi, wt_, wk) in ((pg_i, wg[ws], ("wg", ws)), (pu_i, wu[ws], ("wu", ws))):
                            for kt in range(8):
                                S.op("pe", lambda: nc.tensor.matmul(pgu[pi][:, :Wd], lhsT=wt_[:, kt, mt * P:(mt + 1) * P], rhs=fTb[:, kt, cs_], start=(kt == 0), stop=(kt == 7)),
                                     reads=[wk, ("fTb", kt)], writes=[("pgu", pi)], pe_chain=True)
                        fb = fcount % 2
                        fcount += 1
                        S.op("act", lambda: nc.scalar.activation(out=sgt[fb][:, :Wd], in_=pgu[pg_i][:, :Wd], func=AF.Silu), reads=[("pgu", pg_i)], writes=[("sgt", fb)])
                        dve(lambda: V.tensor_tensor(out=hT[cb][mt][:, :Wd], in0=pgu[pu_i][:, :Wd], in1=sgt[fb][:, :Wd], op=ALU.mult),
                            [("pgu", pu_i), ("sgt", fb)], [("hT", cb, mt)])

                def emit_down(k):
                    nonlocal ycount
                    e, c0, Wd = items[k]
                    ws = (wbase + e) % NW
                    cb = k % 2
                    for tl_ in range(Wd // P):
                        ti = c0 // P + tl_
                        yb_ = ycount % 2
                        path = (ycount // 2) % 2
                        ab = ycount % 2
                        ycount += 1
                        for nh in range(2):
                            for mt in range(2):
                                S.op("pe", lambda: nc.tensor.matmul(py[yb_][:, nh, :], lhsT=hT[cb][mt][:, tl_ * P:(tl_ + 1) * P], rhs=wd[ws][:, mt, nh * 512:(nh + 1) * 512],
                                                                    start=(mt == 0), stop=(mt == 1)),
                                     reads=[("hT", cb, mt), ("wd", ws)], writes=[("py", yb_, nh)], pe_chain=True)
                        pyf = py[yb_][:].rearrange("p a b -> p (a b)")
                        pyk = [("py", yb_, 0), ("py", yb_, 1)]
                        wcol = wts[:, ti, min(e, 63):min(e, 63) + 1]
                        if e == 64:
                            if path == 0:
                                S.op("act", lambda: nc.scalar.copy(out=tt_[ab][:], in_=pyf), reads=pyk, writes=[("htt", ab)])
                                S.op("pool", lambda: nc.gpsimd.tensor_tensor(out=acc[:, ti, :], in0=tt_[ab][:], in1=acc[:, ti, :], op=ALU.add),
                                     reads=[("htt", ab), ("acc", ti)], writes=[("acc", ti)])
                            else:
                                dve(lambda: V.tensor_tensor(out=acc[:, ti, :], in0=pyf, in1=acc[:, ti, :], op=ALU.add), pyk + [("acc", ti)], [("acc", ti)])
                        elif path == 0:
                            dst = acc[:, ti, :] if e == 0 else tt_[ab][:]
                            dkey = ("acc", ti) if e == 0 else ("htt", ab)
                            S.op("act", lambda: nc.scalar.activation(out=dst, in_=pyf, func=AF.Identity, scale=wcol),
                                 reads=pyk + [("wts", ti)], writes=[dkey])
                            if e > 0:
                                S.op("pool", lambda: nc.gpsimd.tensor_tensor(out=acc[:, ti, :], in0=tt_[ab][:], in1=acc[:, ti, :], op=ALU.add),
                                     reads=[("htt", ab), ("acc", ti)], writes=[("acc", ti)])
                        else:
                            dst = acc[:, ti, :] if e == 0 else hx[ab][:]
                            dkey = ("acc", ti) if e == 0 else ("hhx", ab)
                            dve(lambda: V.tensor_tensor(out=dst, in0=pyf, in1=wcol.to_broadcast([P, D]), op=ALU.mult), pyk + [("wts", ti)], [dkey])
                            if e > 0:
                                dve(lambda: V.tensor_tensor(out=acc[:, ti, :], in0=hx[ab][:], in1=acc[:, ti, :], op=ALU.add),
                                    [("hhx", ab), ("acc", ti)], [("acc", ti)])

                wbase = wcount
                wcount += 65
                load_w(0)
                for k in range(len(items) + 1):
                    if k < len(items):
                        emit_gu(k)
                    if k >= 1:
                        emit_down(k - 1)
                for ti in range(ntl):
                    i = t0 + ti
                    s = 1 if i < 2 else 0
                    hb = i % 2
                    S.dma("sp", hx[hb][:], X["h"][i * P:(i + 1) * P, :], writes=[("hhx", hb)])
                    S.op("pool", lambda: nc.gpsimd.tensor_tensor(out=tt_[hb][:], in0=acc[:, ti, :], in1=self.gb[:, 2 * s + 1, :], op=ALU.mult),
                         reads=[("acc", ti)], writes=[("htt", hb)])
                    dve(lambda: V.scalar_tensor_tensor(out=hx[hb][:], in0=hx[hb][:], scalar=DN_ALPHA, in1=tt_[hb][:], op0=ALU.mult, op1=ALU.add),
                        [("hhx", hb), ("htt", hb)], [("hhx", hb)])
                    ks = self.ln_stats(hx[hb], st6[hb], mv[hb], rs[hb], ("hhx", hb))
                    dve(lambda: V.tensor_scalar(out=tt_[hb][:], in0=hx[hb][:], scalar1=mv[hb][:, 0:1], scalar2=rs[hb][:, 1:2], op0=ALU.subtract, op1=ALU.mult),
                        [("hhx", hb)] + ks, [("htt", hb)])
                    S.op("pool", lambda: nc.gpsimd.tensor_tensor(out=tt_[hb][:], in0=tt_[hb][:], in1=lng[:], op=ALU.mult), reads=[("htt", hb), "lng"], writes=[("htt", hb)])
                    S.op("pool", lambda: nc.gpsimd.tensor_tensor(out=hx[hb][:], in0=tt_[hb][:], in1=lnb[:], op=ALU.add), reads=[("htt", hb), "lnb"], writes=[("hhx", hb)])
                    if last:
                        dst = self.out[(i - 2) * P:(i - 1) * P, :]
                    else:
                        dst = X["h"][i * P:(i + 1) * P, :]
                    S.dma("sp", dst, hx[hb][:], reads=[("hhx", hb)], writes=[("d_h2", hb)])

_ROPE = None


def make_in_maps(inputs, ncores=8, used=None):
    global _ROPE
    if _ROPE is None:
        _ROPE = _rope_tables()
    cosT, sinT = _ROPE
    perm = _rope_perm()
    w_in = np.ascontiguousarray(inputs["w_in"], dtype=np.float32)
    qcols = np.concatenate([768 + h * 128 + perm for h in range(4)])
    kcols = np.concatenate([1280 + h * 128 + perm for h in range(4)])
    w_qkp = np.ascontiguousarray(w_in[:, :, np.concatenate([qcols, kcols])])
    shared = {k: np.ascontiguousarray(v, dtype=np.float32) for k, v in inputs.items() if k not in ("x", "c", "ctx")}
    shared["w_qkp"] = w_qkp
    shared["rope_cos"] = cosT
    shared["rope_sin"] = sinT
    for n, a in _consts().items():
        shared["k_" + n] = a
    maps = []
    for core in range(ncores):
        b = core % 4
        m = dict(shared)
        m["x"] = np.ascontiguousarray(inputs["x"][b], dtype=np.float32)
        m["ctx"] = np.ascontiguousarray(inputs["ctx"][b], dtype=np.float32)
        m["c"] = np.ascontiguousarray(inputs["c"][b], dtype=np.float32)
        if used is not None:
            m = {k: v for k, v in m.items() if k in used}
        maps.append(m)
    return maps


def kernel(**inputs):
    bld = Builder()
    nc = bld.build()
    maps = make_in_maps(inputs, used=set(bld.I.keys()))
    res = run_bass_kernel_spmd(nc, maps, core_ids=list(range(8)))
    out = np.stack([res.results[b]["out"] for b in range(4)], 0)
    return out.astype(np.float32)
```

```python
import contextlib, math
import numpy as np
import concourse.bass as bass
import concourse.mybir as mybir
from concourse.bass_utils import run_bass_kernel_spmd

F32 = mybir.dt.float32
BF16 = mybir.dt.bfloat16
AF = mybir.ActivationFunctionType
ALU = mybir.AluOpType
AX = mybir.AxisListType

P = 128
D = 1024
NCTX = 256
NLAT = 4096
NTOK = NCTX + NLAT
NT = NTOK // P
DEPTH = 2
LN_EPS = 1e-5
DN_ALPHA = (2 * DEPTH) ** 0.25
IN_WIDTH = 5376


class Sched:
    def __init__(self, nc, stack):
        self.nc = nc
        self.stack = stack
        self.eng = {"pe": nc.tensor, "act": nc.scalar, "dve": nc.vector,
                    "pool": nc.gpsimd, "sp": nc.sync}
        self.sem = {}
        self.cnt = {}
        for e in self.eng:
            self.sem[e] = stack.enter_context(nc.semaphore("s_" + e))
            self.cnt[e] = 0
        self.waited = {e: {} for e in self.eng}
        self.res = {}
        self.dsem = {}
        self.free_dsems = {}
        self.dtype_of = {}
        self.ninst = 0

    def _r(self, key):
        r = self.res.get(key)
        if r is None:
            r = self.res[key] = {"w": {}, "r": {}}
        return r

    def _need(self, deps, reads, writes):
        for k in reads:
            for s, v in self._r(k)["w"].items():
                if deps.get(s, 0) < v:
                    deps[s] = v
        for k in writes:
            r = self._r(k)
            for s, v in r["w"].items():
                if deps.get(s, 0) < v:
                    deps[s] = v
            for s, v in r["r"].items():
                if deps.get(s, 0) < v:
                    deps[s] = v

    def _emit_waits(self, e, deps, skip_self=False):
        w = self.waited[e]
        for s, v in deps.items():
            if skip_self and s == e:
                continue
            if w.get(s, 0) >= v:
                continue
            self.eng[e].wait_ge(self.sem[s], v)
            w[s] = v
            self.ninst += 1

    def _record(self, ev, reads, writes):
        s, v = ev
        for k in reads:
            r = self._r(k)["r"]
            if r.get(s, 0) < v:
                r[s] = v
        for k in writes:
            r = self._r(k)
            r["w"] = {s: v}
            r["r"] = {}

    def op(self, e, fn, reads=(), writes=(), pe_chain=False):
        deps = {}
        self._need(deps, reads, writes)
        self._emit_waits(e, deps, skip_self=(e == "pe" and pe_chain))
        ins = fn()
        self.cnt[e] += 1
        ins.then_inc(self.sem[e], 1)
        self._record((e, self.cnt[e]), reads, writes)
        self.ninst += 1
        return ins

    def dma(self, q, out, in_, reads=(), writes=(), semkey=None, **kw):
        if semkey is None:
            semkey = writes[0]
        sname = self.dsem.get(semkey)
        qt = "sw" if q == "pool" else "hw"
        if sname is None:
            pool_ = self.free_dsems.setdefault(qt, [])
            if pool_:
                sname = pool_.pop()
            else:
                sname = "d%d" % (len(self.sem))
                self.sem[sname] = self.stack.enter_context(self.nc.semaphore(sname))
                self.cnt[sname] = 0
                self.dtype_of[sname] = qt
            self.dsem[semkey] = sname
        assert self.dtype_of[sname] == qt, (semkey, sname, qt)
        deps = {}
        self._need(deps, reads, writes)
        self._emit_waits(q, deps)
        ins = self.eng[q].dma_start(out=out, in_=in_, **kw)
        self.cnt[sname] += 16
        ins.then_inc(self.sem[sname], 16)
        self._record((sname, self.cnt[sname]), reads, writes)
        self.ninst += 1
        return ins

    def barrier(self, final=False):
        engs = ["sp"] if final else list(self.eng)
        for e in engs:
            w = self.waited[e]
            for s, v in self.cnt.items():
                if v > 0 and w.get(s, 0) < v:
                    self.eng[e].wait_ge(self.sem[s], v)
                    w[s] = v
                    self.ninst += 1
        if not final:
            self.res = {}
            for sn in self.dsem.values():
                self.free_dsems.setdefault(self.dtype_of[sn], []).append(sn)
            self.dsem = {}


def _rope_tables():
    rows = NLAT // 64
    r, col = np.meshgrid(np.arange(rows), np.arange(64), indexing="ij")
    inv_freq = (10000.0 ** (-np.arange(0, 32, 2, dtype=np.float32) / 32)).astype(np.float32)
    ang = np.concatenate([r.reshape(-1, 1).astype(np.float32) * inv_freq,
                          col.reshape(-1, 1).astype(np.float32) * inv_freq], -1)
    cos = np.cos(ang).astype(np.float32)
    sin = np.sin(ang).astype(np.float32)
    cosT = np.ones((128, NTOK), np.float32)
    sinT = np.zeros((128, NTOK), np.float32)
    for m in range(2):
        for d in range(64):
            f = (d % 16) if d < 32 else 16 + (d % 16)
            sgn = -1.0 if (d % 32) < 16 else 1.0
            cosT[m * 64 + d, NCTX:] = cos[:, f]
            sinT[m * 64 + d, NCTX:] = sgn * sin[:, f]
    return cosT, sinT


def _rope_perm():
    perm = np.zeros(128, np.int64)
    for m in range(2):
        for d in range(64):
            pd = d + 16 if (d % 32) < 16 else d - 16
            perm[m * 64 + d] = m * 64 + pd
    return perm


def _consts():
    c = {}
    c["ident"] = np.eye(128, dtype=np.float32)
    tt = np.arange(128)
    c["mfwd"] = (tt[:, None] <= tt[None, :]).astype(np.float32)
    c["mbwd"] = (tt[:, None] >= tt[None, :]).astype(np.float32)
    sig = np.stack([tt, 127 - tt], 1).astype(np.float32)
    c["sigp"] = sig
    c["sigf"] = np.broadcast_to(np.stack([tt, 127 - tt], 0).astype(np.float32)[None], (128, 2, 128)).copy()
    e8 = np.zeros((128, 8, 128), np.float32)
    for k in range(8):
        e8[k, k, :] = 1.0
    c["e8"] = e8
    sel = np.zeros((128, 64, 128), np.float32)
    for k in range(64):
        sel[k, k, :] = 1.0
    c["sel"] = sel
    return c


class Builder:
    def __init__(self, nlayers=DEPTH, dbg=()):
        self.nlayers = nlayers
        self.dbg = set(dbg)
        self.nc = bass.Bass("TRN2", target_bir_lowering=False)
        self.dbg_out = {}

    def din(self, name, shape, dt=F32):
        return self.nc.dram_tensor(name, list(shape), dt, kind="ExternalInput").ap()

    def dscr(self, name, shape, dt):
        if name in self.dbg:
            t = self.nc.dram_tensor("dbg_" + name, list(shape), dt, kind="ExternalOutput").ap()
            self.dbg_out[name] = "dbg_" + name
            return t
        return self.nc.dram_tensor(name, list(shape), dt, kind="Internal").ap()

    def declare(self):
        L = DEPTH
        shapes = {"x": [NLAT, D], "ctx": [NCTX, D], "c": [D], "c_ctx": [D], "w_mod": [L, D, 6 * D], "b_mod": [L, 6 * D],
                  "w_in": [L, D, IN_WIDTH], "w_qkp": [L, D, 1024], "rope_cos": [128, NTOK], "rope_sin": [128, NTOK]}
        for n, sh in [("s5_lam_re", [L, 2, 16, 64]), ("s5_lam_im", [L, 2, 16, 64]), ("s5_log_step", [L, 2, 16]),
                      ("s5_b_re", [L, 2, 16, 64, 16]), ("s5_b_im", [L, 2, 16, 64, 16]),
                      ("s5_c_re", [L, 2, 16, 16, 64]), ("s5_c_im", [L, 2, 16, 16, 64]), ("s5_d", [L, 256]),
                      ("gm_w_s", [L, 4, 128, 128]), ("gm_b_s", [L, 4, 128]), ("da_lam", [L, 4, 64]),
                      ("da_subln_g", [L, 128]), ("w_glu_val", [L, 256, D]), ("w_glu_gate", [L, 256, D]),
                      ("w_proj_gm", [L, 256, D]), ("w_proj_da", [L, 512, D]), ("w_out", [L, D, D]),
                      ("ln1_g", [L, D]), ("ln1_b", [L, D]), ("ln2_g", [L, D]), ("ln2_b", [L, D]),
                      ("w_router", [L, D, 64]), ("b_router", [L, 64]),
                      ("w_exp_gate", [L, 64, D, 256]), ("w_exp_up", [L, 64, D, 256]), ("w_exp_down", [L, 64, 256, D]),
                      ("w_sh_gate", [L, D, 256]), ("w_sh_up", [L, D, 256]), ("w_sh_down", [L, 256, D])]:
            shapes[n] = sh
        for n, a in _consts().items():
            shapes["k_" + n] = list(a.shape)
        bld = self

        class Lazy(dict):
            def __missing__(self, n):
                self[n] = bld.din(n, shapes[n])
                return self[n]
        self.I = Lazy()
        self.out = self.nc.dram_tensor("out", [NLAT, D], F32, kind="ExternalOutput").ap()
        Sx = {}
        Sx["h"] = self.dscr("h", [NTOK, D], F32)
        Sx["uT"] = self.dscr("uT", [D, NTOK], BF16)
        Sx["aT"] = self.dscr("aT", [256, NTOK], BF16)
        Sx["qT"] = self.dscr("qT", [512, NTOK], BF16)
        Sx["kT"] = self.dscr("kT", [512, NTOK], BF16)
        Sx["v"] = self.dscr("v", [NTOK, 512], BF16)
        Sx["yaT"] = self.dscr("yaT", [256, NTOK], BF16)
        Sx["ybT"] = self.dscr("ybT", [256, NTOK], BF16)
        Sx["ycT"] = self.dscr("ycT", [512, NTOK], BF16)
        Sx["fT"] = self.dscr("fT", [D, NTOK], BF16)
        self.X = Sx

    def sb(self, ph, name, shape, dt=F32):
        return ph.enter_context(self.nc.sbuf_tensor(name + getattr(self, "sfx", ""), list(shape), dt))

    def psum(self, ph, name, shape, dt=F32):
        return ph.enter_context(self.nc.psum_tensor(name + getattr(self, "sfx", ""), list(shape), dt))

    def hsrc(self, l, i):
        if l == 0:
            if i < 2:
                return self.I["ctx"][i * P:(i + 1) * P, :]
            return self.I["x"][(i - 2) * P:(i - 1) * P, :]
        return self.X["h"][i * P:(i + 1) * P, :]

    def build(self):
        nc = self.nc
        self.declare()
        with contextlib.ExitStack() as st:
            self.S = S = Sched(nc, st)
            self.ident = self.sb(st, "ident", [P, P], F32)
            self.identb = self.sb(st, "identb", [P, P], BF16)
            S.dma("sp", self.ident[:], self.I["k_ident"], writes=["ident"])
            S.op("dve", lambda: nc.vector.tensor_copy(out=self.identb[:], in_=self.ident[:]), reads=["ident"], writes=["identb"])
            self.modc = self.sb(st, "modc", [P, 2, 48], F32)
            self.gb = self.sb(st, "gb", [P, 4, D], F32)
            for l in range(self.nlayers):
                self.layer(l)
            S.barrier(final=True)
        return nc

    def layer(self, l):
        S = self.S
        last = (l == DEPTH - 1)
        self.sfx = '_L%d' % l
        self.phase_mod(l)
        S.barrier()
        if 'noproj' not in self.dbg:
            self.phase_proj(l)
            S.barrier()
        if 'nos5' not in self.dbg:
            self.phase_s5(l)
            S.barrier()
        if 'noattn' not in self.dbg:
            self.phase_attn(l)
            S.barrier()
        if 'nomerge' not in self.dbg:
            self.phase_merge(l)
            S.barrier()
        if 'nomoe' not in self.dbg:
            self.phase_moe(l)
            S.barrier()

    def phase_mod(self, l):
        nc, S, I = self.nc, self.S, self.I
        with contextlib.ExitStack() as ph:
            cs = self.sb(ph, "cs", [P, 2, 8])
            scs = self.sb(ph, "scs", [P, 8, 2])
            rep = self.sb(ph, "rep", [P, 2, 8, P])
            bm = self.sb(ph, "bm", [P, 48])
            bmb = self.sb(ph, "bmb", [P, 2, D])
            wm = [self.sb(ph, "wm%d" % i, [P, 8, 512]) for i in range(2)]
            pmod = self.psum(ph, "pmod", [P, 48, 2])
            pg = [self.psum(ph, "pg%d" % i, [P, 512]) for i in range(2)]
            S.dma("sp", cs[:, 0, :], I["c"].rearrange("(k p) -> p k", p=P), writes=["cs0"], allow_slow_non_contiguous=True)
            S.dma("sp", cs[:, 1, :], I["c_ctx"].rearrange("(k p) -> p k", p=P), writes=["cs1"], allow_slow_non_contiguous=True)
            S.dma("sp", bm[:], I["b_mod"][l].rearrange("(j p) -> p j", p=P), writes=["bm"], allow_slow_non_contiguous=True)
            S.dma("sp", bmb[:, 0, :], I["b_mod"][l, 2 * D:3 * D].partition_broadcast(P), writes=["bmb0"])
            S.dma("sp", bmb[:, 1, :], I["b_mod"][l, 5 * D:6 * D].partition_broadcast(P), writes=["bmb1"])
            for s in range(2):
                S.op("act", lambda: nc.scalar.activation(out=scs[:, :, s], in_=cs[:, s, :], func=AF.Silu),
                     reads=["cs%d" % s], writes=[("scs", s)])
                S.op("dve", lambda: nc.vector.tensor_copy(out=rep[:, s, :, :], in_=scs[:, :, s].unsqueeze(2).to_broadcast([P, 8, P])),
                     reads=[("scs", s)], writes=[("rep", s)])
            for c in range(12):
                w = wm[c % 2]
                S.dma("sp", w[:], I["w_mod"][l][:, c * 512:(c + 1) * 512].rearrange("(kt p) n -> p kt n", p=P),
                      writes=[("wm", c % 2)])
                for j in range(4):
                    jj = c * 4 + j
                    for kt in range(8):
                        S.op("pe", lambda: nc.tensor.matmul(pmod[:, jj, :], lhsT=w[:, kt, j * P:(j + 1) * P], rhs=scs[:, kt, :],
                                                            start=(kt == 0), stop=(kt == 7)),
                             reads=[("wm", c % 2), ("scs", 0), ("scs", 1)], writes=["pmod"], pe_chain=True)
                if c in (4, 5, 10, 11):
                    gi = 0 if c < 6 else 1
                    half = c % 2
                    for s in range(2):
                        pgt = pg[s]
                        for kt in range(8):
                            S.op("pe", lambda: nc.tensor.matmul(pgt[:], lhsT=rep[:, s, kt, :], rhs=w[:, kt, :],
                                                                start=(kt == 0), stop=(kt == 7)),
                                 reads=[("wm", c % 2), ("rep", s)], writes=[("pg", s)], pe_chain=True)
                        S.op("dve", lambda: nc.vector.tensor_tensor(out=self.gb[:, 2 * s + gi, half * 512:(half + 1) * 512], in0=pgt[:],
                                                                    in1=bmb[:, gi, half * 512:(half + 1) * 512], op=ALU.add),
                             reads=[("pg", s), "bmb%d" % gi], writes=[("gb", 2 * s + gi, half)])
            for s in range(2):
                S.op("dve", lambda: nc.vector.tensor_tensor(out=self.modc[:, s, :], in0=pmod[:, :, s], in1=bm[:], op=ALU.add),
                     reads=["pmod", "bm"], writes=[("modc", s)])
                for c0 in (8, 32):
                    S.op("dve", lambda: nc.vector.tensor_scalar(out=self.modc[:, s, c0:c0 + 8], in0=self.modc[:, s, c0:c0 + 8], scalar1=1.0, scalar2=None, op0=ALU.add),
                         reads=[("modc", s)], writes=[("modc", s)])
            if "modc" in self.dbg:
                t = self.nc.dram_tensor("dbg_modc%d" % l, [P, 2, 48], F32, kind="ExternalOutput").ap()
                S.dma("sp", t, self.modc[:], reads=[("modc", 0), ("modc", 1)], writes=["dbg_modc"])
                t2 = self.nc.dram_tensor("dbg_gb%d" % l, [P, 4, D], F32, kind="ExternalOutput").ap()
                S.dma("sp", t2, self.gb[:], reads=[("gb", a, b) for a in range(4) for b in range(2)], writes=["dbg_gb"])

    def phase_proj(self, l):
        nc, S, I, X = self.nc, self.S, self.I, self.X
        with contextlib.ExitStack() as ph:
            wsplit = {"a": (0, 256), "zu": (256, 512), "zv": (512, 768), "q": (768, 1280), "k": (1280, 1792), "v": (1792, 2304)}
            W = {}
            for n, (c0, c1) in wsplit.items():
                W[n] = self.sb(ph, "w_" + n, [P, 8, c1 - c0], BF16)
                S.dma("pool", W[n][:], I["w_in"][l][:, c0:c1].rearrange("(kt p) n -> p kt n", p=P), writes=["w_" + n])
            for i, n in enumerate(("qp", "kp")):
                W[n] = self.sb(ph, "w_" + n, [P, 8, 512], BF16)
                S.dma("pool", W[n][:], I["w_qkp"][l][:, i * 512:(i + 1) * 512].rearrange("(kt p) n -> p kt n", p=P), writes=["w_" + n])
            wsn = self.sb(ph, "wsn", [P, 4, P], BF16)
            wsT = self.sb(ph, "wsT", [P, 4, P], BF16)
            bsT = self.sb(ph, "bsT", [P, 2, P], F32)
            S.dma("pool", wsn[:], I["gm_w_s"][l].rearrange("h t s -> t h s"), writes=["wsn"])
            for mt in range(2):
                for hh in range(2):
                    S.dma("sp", bsT[64 * hh:64 * hh + 64, mt, :], I["gm_b_s"][l, 2 * mt + hh, :].partition_broadcast(64),
                          writes=[("bsT", mt, hh)])
            bsT_keys = [("bsT", mt, hh) for mt in range(2) for hh in range(2)]

            NH = 3
            hx = [self.sb(ph, "hx%d" % i, [P, D]) for i in range(NH)]
            xn = [self.sb(ph, "xn%d" % i, [P, D], BF16) for i in range(2)]
            st6 = self.sb(ph, "st6", [P, 2, 2, 6])
            mv = self.sb(ph, "mv", [P, 2, 2])
            rs = self.sb(ph, "rs", [P, 2, 2])
            uT = [self.sb(ph, "uTb%d" % i, [P, 8, 512], BF16) for i in range(2)]
            cosb = [self.sb(ph, "cos%d" % i, [P, 512]) for i in range(2)]
            sinb = [self.sb(ph, "sin%d" % i, [P, 512]) for i in range(2)]
            aTb = [self.sb(ph, "aTb%d" % i, [P, 2, 512], BF16) for i in range(2)]
            zuT = [self.sb(ph, "zuT%d" % i, [P, 2, 512], BF16) for i in range(2)]
            ybT = [self.sb(ph, "ybT%d" % i, [P, 2, 512], BF16) for i in range(2)]
            qkT = [self.sb(ph, "qkT%d" % i, [P, 8, 512], BF16) for i in range(2)]
            vb = [self.sb(ph, "vb%d" % i, [P, 4, 512], BF16) for i in range(2)]
            zg = [self.sb(ph, "zg%d" % i, [P, 256]) for i in range(2)]
            zvn = [self.sb(ph, "zvn%d" % i, [P, 256], BF16) for i in range(2)]
            zst = self.sb(ph, "zst", [P, 2, 6])
            zmv = self.sb(ph, "zmv", [P, 2, 2])
            zrs = self.sb(ph, "zrs", [P, 2, 2])
            t1 = [self.sb(ph, "t1_%d" % i, [P, 512]) for i in range(2)]
            t2 = [self.sb(ph, "t2_%d" % i, [P, 512]) for i in range(2)]
            tg = [self.sb(ph, "tg%d" % i, [P, 2, P]) for i in range(2)]
            tp = [self.psum(ph, "tp%d" % i, [P, 8, P], BF16) for i in range(2)]
            for h in range(4):
                S.op("pe", lambda: nc.tensor.transpose(tp[0][:, h, :], wsn[:, h, :], self.identb[:]), reads=["wsn", "identb"], writes=[("tp", 0)], pe_chain=True)
            S.op("dve", lambda: nc.vector.tensor_copy(out=wsT[:], in_=tp[0][:, 0:4, :]), reads=[("tp", 0)], writes=["wsT"])
            pa = [self.psum(ph, "pa%d" % i, [P, 512]) for i in range(4)]
            pz_ = [self.psum(ph, "pz%d" % i, [P, 256]) for i in range(2)]

            chunks = [(0, 2)] + [(2 + 4 * c, 4) for c in range(8)]
            tcount = 0
            pcount = 0

            def emit_ln(ci):
                nonlocal tcount
                t0, ntl = chunks[ci]
                s = 1 if ci == 0 else 0
                Wd = ntl * P
                tok0 = t0 * P
                cb = ci % 2
                ub = uT[cb]
                S.dma("sp", cosb[cb][:, :Wd], I["rope_cos"][:, tok0:tok0 + Wd], writes=[("cos", cb)])
                S.dma("sp", sinb[cb][:, :Wd], I["rope_sin"][:, tok0:tok0 + Wd], writes=[("sin", cb)])
                for ti in range(ntl):
                    i = t0 + ti
                    hb = tcount % NH
                    xb = tcount % 2
                    tcount += 1
                    S.dma("sp", hx[hb][:], self.hsrc(l, i), writes=[("hx", hb)])
                    for hf in range(2):
                        S.op("dve", lambda: nc.vector.bn_stats(out=st6[:, xb, hf, :], in_=hx[hb][:, hf * 512:(hf + 1) * 512]),
                             reads=[("hx", hb)], writes=[("st6", xb, hf)])
                    S.op("dve", lambda: nc.vector.bn_aggr(out=mv[:, xb, :], in_=st6[:, xb, :, :].rearrange("p a b -> p (a b)")),
                         reads=[("st6", xb, 0), ("st6", xb, 1)], writes=[("mv", xb)])
                    S.op("act", lambda: nc.scalar.activation(out=rs[:, xb, 0:1], in_=mv[:, xb, 1:2], func=AF.Sqrt, bias=LN_EPS, scale=1.0),
                         reads=[("mv", xb)], writes=[("rs0", xb)])
                    S.op("dve", lambda: nc.vector.reciprocal(out=rs[:, xb, 1:2], in_=rs[:, xb, 0:1]), reads=[("rs0", xb)], writes=[("rs1", xb)])
                    S.op("dve", lambda: nc.vector.tensor_scalar(out=xn[xb][:], in0=hx[hb][:], scalar1=mv[:, xb, 0:1], scalar2=rs[:, xb, 1:2],
                                                                op0=ALU.subtract, op1=ALU.mult),
                         reads=[("hx", hb), ("mv", xb), ("rs1", xb)], writes=[("xn", xb)])
                    tpb = tp[xb]
                    for kt in range(8):
                        S.op("pe", lambda: nc.tensor.transpose(tpb[:, kt, :], xn[xb][:, kt * P:(kt + 1) * P], self.identb[:]),
                             reads=[("xn", xb), "identb"], writes=[("tp", xb)], pe_chain=True)
                    for kt in range(8):
                        S.op("act", lambda: nc.scalar.activation(out=ub[:, kt, ti * P:(ti + 1) * P], in_=tpb[:, kt, :], func=AF.Identity,
                                                                 scale=self.modc[:, s, 8 + kt:9 + kt], bias=self.modc[:, s, kt:kt + 1]),
                             reads=[("tp", xb), ("modc", s)], writes=[("uT", cb, ti)])
                ukeys_ = [("uT", cb, ti) for ti in range(ntl)]
                S.dma("sp", X["uT"].rearrange("(kt p) t -> p kt t", p=P)[:, :, tok0:tok0 + Wd], ub[:, :, :Wd], reads=ukeys_, writes=[("d_uT", ci % 2)])

            emit_ln(0)
            for ci, (t0, ntl) in enumerate(chunks):
                s = 1 if ci == 0 else 0
                Wd = ntl * P
                tok0 = t0 * P
                cb = ci % 2
                ub = uT[cb]
                ukeys = [("uT", cb, ti) for ti in range(ntl)]
                if ci + 1 < len(chunks):
                    emit_ln(ci + 1)

                def mm_fm(pt, wt, c0):
                    for kt in range(8):
                        S.op("pe", lambda: nc.tensor.matmul(pt[:, :Wd], lhsT=wt[:, kt, c0:c0 + P], rhs=ub[:, kt, :Wd], start=(kt == 0), stop=(kt == 7)),
                             reads=ukeys + [wt_key[id(wt)]], writes=[pkey[id(pt)]], pe_chain=True)

                wt_key = {id(W[n]): "w_" + n for n in W}
                pkey = {id(pa[i]): ("pa", i) for i in range(4)}

                for mt in range(2):
                    pt = pa[pcount % 4]; pcount += 1
                    mm_fm(pt, W["a"], mt * P)
                    S.op("act", lambda: nc.scalar.copy(out=aTb[cb][:, mt, :Wd], in_=pt[:, :Wd]), reads=[pkey[id(pt)]], writes=[("aTb", cb, mt)])
                S.dma("sp", X["aT"].rearrange("(mt p) t -> p mt t", p=P)[:, :, tok0:tok0 + Wd], aTb[cb][:, :, :Wd],
                      reads=[("aTb", cb, 0), ("aTb", cb, 1)], writes=[("d_aT", ci)])
                for mt in range(2):
                    pt = pa[pcount % 4]; pcount += 1
                    mm_fm(pt, W["zu"], mt * P)
                    S.op("act", lambda: nc.scalar.activation(out=zuT[cb][:, mt, :Wd], in_=pt[:, :Wd], func=AF.Gelu_apprx_tanh),
                         reads=[pkey[id(pt)]], writes=[("zuT", cb, mt)])
                for qi, (wn, wpn) in enumerate((("q", "qp"), ("k", "kp"))):
                    for hd in range(4):
                        p0 = pa[pcount % 4]; pcount += 1
                        p1 = pa[pcount % 4]; pcount += 1
                        mm_fm(p0, W[wn], hd * P)
                        mm_fm(p1, W[wpn], hd * P)
                        tb = (qi * 4 + hd) % 2
                        S.op("dve", lambda: nc.vector.tensor_tensor(out=t1[tb][:, :Wd], in0=p0[:, :Wd], in1=cosb[cb][:, :Wd], op=ALU.mult),
                             reads=[pkey[id(p0)], ("cos", cb)], writes=[("t1", tb)])
                        S.op("dve", lambda: nc.vector.tensor_tensor(out=t2[tb][:, :Wd], in0=p1[:, :Wd], in1=sinb[cb][:, :Wd], op=ALU.mult),
                             reads=[pkey[id(p1)], ("sin", cb)], writes=[("t2", tb)])
                        S.op("pool", lambda: nc.gpsimd.tensor_tensor(out=qkT[cb][:, qi * 4 + hd, :Wd], in0=t1[tb][:, :Wd], in1=t2[tb][:, :Wd], op=ALU.add),
                             reads=[("t1", tb), ("t2", tb)], writes=[("qkT", cb, qi * 4 + hd)])
                    dst = X["qT"] if qi == 0 else X["kT"]
                    S.dma("sp", dst.rearrange("(h p) t -> p h t", p=P)[:, :, tok0:tok0 + Wd], qkT[cb][:, qi * 4:qi * 4 + 4, :Wd],
                          reads=[("qkT", cb, qi * 4 + hd) for hd in range(4)], writes=[("d_qk", ci, qi)])
                for ti in range(ntl):
                    i = t0 + ti
                    pt = pa[pcount % 4]; pcount += 1
                    for kt in range(8):
                        S.op("pe", lambda: nc.tensor.matmul(pt[:], lhsT=ub[:, kt, ti * P:(ti + 1) * P], rhs=W["v"][:, kt, :], start=(kt == 0), stop=(kt == 7)),
                             reads=[("uT", cb, ti), "w_v"], writes=[pkey[id(pt)]], pe_chain=True)
                    S.op("act", lambda: nc.scalar.copy(out=vb[cb][:, ti, :], in_=pt[:]), reads=[pkey[id(pt)]], writes=[("vb", cb, ti)])
                    zb = i % 2
                    for kt in range(8):
                        S.op("pe", lambda: nc.tensor.matmul(pz_[zb][:, :], lhsT=ub[:, kt, ti * P:(ti + 1) * P], rhs=W["zv"][:, kt, :], start=(kt == 0), stop=(kt == 7)),
                             reads=[("uT", cb, ti), "w_zv"], writes=[("pz", zb)], pe_chain=True)
                    S.op("act", lambda: nc.scalar.activation(out=zg[zb][:], in_=pz_[zb][:, :], func=AF.Gelu_apprx_tanh), reads=[("pz", zb)], writes=[("zg", zb)])
                    S.op("dve", lambda: nc.vector.bn_stats(out=zst[:, zb, :], in_=zg[zb][:]), reads=[("zg", zb)], writes=[("zst", zb)])
                    S.op("dve", lambda: nc.vector.bn_aggr(out=zmv[:, zb, :], in_=zst[:, zb, :]), reads=[("zst", zb)], writes=[("zmv", zb)])
                    S.op("act", lambda: nc.scalar.activation(out=zrs[:, zb, 0:1], in_=zmv[:, zb, 1:2], func=AF.Sqrt, bias=LN_EPS, scale=1.0),
                         reads=[("zmv", zb)], writes=[("zrs0", zb)])
                    S.op("dve", lambda: nc.vector.reciprocal(out=zrs[:, zb, 1:2], in_=zrs[:, zb, 0:1]), reads=[("zrs0", zb)], writes=[("zrs1", zb)])
                    S.op("dve", lambda: nc.vector.tensor_scalar(out=zvn[zb][:], in0=zg[zb][:], scalar1=zmv[:, zb, 0:1], scalar2=zrs[:, zb, 1:2],
                                                                op0=ALU.subtract, op1=ALU.mult),
                         reads=[("zg", zb), ("zmv", zb), ("zrs1", zb)], writes=[("zvn", zb)])
                    for h in range(4):
                        S.op("pe", lambda: nc.tensor.matmul(pz_[zb][64 * (h % 2):64 * (h % 2) + 64, (h // 2) * P:(h // 2 + 1) * P],
                                                            lhsT=zvn[zb][:, h * 64:(h + 1) * 64], rhs=wsT[:, h, :], start=True, stop=True),
                             reads=[("zvn", zb), "wsT", ("zg", zb)], writes=[("pz", zb)], pe_chain=True)
                    S.op("dve", lambda: nc.vector.tensor_tensor(out=tg[zb][:], in0=pz_[zb][:, :].rearrange("p (a b) -> p a b", a=2), in1=bsT[:], op=ALU.add),
                         reads=[("pz", zb)] + bsT_keys, writes=[("tg", zb)])
                    S.op("pool", lambda: nc.gpsimd.tensor_tensor(out=ybT[cb][:, :, ti * P:(ti + 1) * P], in0=tg[zb][:], in1=zuT[cb][:, :, ti * P:(ti + 1) * P], op=ALU.mult),
                         reads=[("tg", zb), ("zuT", cb, 0), ("zuT", cb, 1)], writes=[("ybT", cb, ti)])
                S.dma("sp", X["v"][tok0:tok0 + Wd, :].rearrange("(a p) n -> p a n", p=P), vb[cb][:, :ntl, :],
                      reads=[("vb", cb, ti) for ti in range(ntl)], writes=[("d_v", ci)])
                S.dma("sp", X["ybT"].rearrange("(mt p) t -> p mt t", p=P)[:, :, tok0:tok0 + Wd], ybT[cb][:, :, :Wd],
                      reads=[("ybT", cb, ti) for ti in range(ntl)], writes=[("d_yb", ci)])


    def phase_attn(self, l):
        nc, S, I, X = self.nc, self.S, self.I, self.X
        last = (l == DEPTH - 1)
        lam_init = 0.8 - 0.6 * math.exp(-0.3 * l)
        with contextlib.ExitStack() as ph:
            lamb = self.sb(ph, "lamb", [P, 4, 64])
            lt = self.sb(ph, "lt", [P, 2, 64])
            ls = self.sb(ph, "ls", [P, 4])
            neglam = self.sb(ph, "neglam", [P, 1])
            gsc = self.sb(ph, "gsc", [P, 1])
            onesb = self.sb(ph, "onesb", [P, P], BF16)
            S.dma("sp", lamb[:], I["da_lam"][l].partition_broadcast(P), writes=["lamb"])
            S.dma("sp", gsc[:], I["da_subln_g"][l].rearrange("(p o) -> p o", o=1), writes=["gsc"])
            S.op("dve", lambda: nc.vector.memset(onesb[:], 1.0), writes=["onesb"])
            epsc = self.sb(ph, "epsc", [P, 1])
            S.op("dve", lambda: nc.vector.memset(epsc[:], LN_EPS), writes=["epsc"])
            for a in range(2):
                S.op("dve", lambda: nc.vector.tensor_tensor(out=lt[:, a, :], in0=lamb[:, 2 * a, :], in1=lamb[:, 2 * a + 1, :], op=ALU.mult),
                     reads=["lamb"], writes=[("lt", a)])
                S.op("dve", lambda: nc.vector.tensor_reduce(out=ls[:, a:a + 1], in_=lt[:, a, :], axis=AX.X, op=ALU.add),
                     reads=[("lt", a)], writes=[("ls", a)])
                S.op("act", lambda: nc.scalar.activation(out=ls[:, 2 + a:3 + a], in_=ls[:, a:a + 1], func=AF.Exp), reads=[("ls", a)], writes=[("le", a)])
            S.op("dve", lambda: nc.vector.tensor_tensor(out=neglam[:], in0=ls[:, 3:4], in1=ls[:, 2:3], op=ALU.subtract),
                 reads=[("le", 0), ("le", 1)], writes=["neglam"])
            S.op("dve", lambda: nc.vector.tensor_scalar(out=neglam[:], in0=neglam[:], scalar1=-lam_init, scalar2=None, op0=ALU.add),
                 reads=["neglam"], writes=["neglam"])
            S.op("dve", lambda: nc.vector.tensor_scalar(out=gsc[:], in0=gsc[:], scalar1=(1.0 - lam_init), scalar2=None, op0=ALU.mult),
                 reads=["gsc"], writes=["gsc"])

            kTz = [[self.sb(ph, "kTz%d_%d" % (b, m), [P, NTOK], BF16) for m in range(2)] for b in range(2)]
            vh = [self.sb(ph, "vh%d" % b, [P, NT, P], BF16) for b in range(2)]
            for b in range(2):
                for m in range(2):
                    S.op("pool", lambda: nc.gpsimd.memset(kTz[b][m][:], 0.0), writes=[("kTz", b, m)])
            qt = [self.sb(ph, "qt%d" % i, [P, 512], BF16) for i in range(2)]
            NPT = 4
            pT = [self.sb(ph, "pT%d" % i, [P, 2, 512], BF16) for i in range(NPT)]
            rd = [self.sb(ph, "rd%d" % i, [P, 512]) for i in range(2)]
            o0 = self.sb(ph, "o0", [P, 512])
            o1 = self.sb(ph, "o1", [P, 512])
            oo = self.sb(ph, "oo", [P, 512])
            sq = self.sb(ph, "sq", [P, 512], BF16)
            rt = self.sb(ph, "rt", [P, 512])
            yc = [self.sb(ph, "yc%d" % i, [P, 512], BF16) for i in range(2)]
            NSC = 2
            sc = [self.psum(ph, "sc%d" % i, [P, 2, 512]) for i in range(NSC)]
            acco = [self.psum(ph, "acco%d" % i, [P, 512]) for i in range(2)]
            accd = [self.psum(ph, "accd%d" % i, [P, 512]) for i in range(2)]
            nq = 0
            nstep = 0
            for h in range(4):
                hb = h % 2
                for m in range(2):
                    S.dma("sp", kTz[hb][m][64 * m:64 * m + 64, :], X["kT"][h * P + 64 * m:h * P + 64 * m + 64, :], writes=[("kTz", hb, m)])
                S.dma("sp", vh[hb][:], X["v"][:, h * P:(h + 1) * P].rearrange("(j p) e -> p j e", p=P), writes=[("vh", hb)])
                qchunks = [(NCTX + 512 * c, 512, list(range(NT))) for c in range(8)]
                if not last:
                    qchunks = [(0, NCTX, [0, 1])] + qchunks
                for (tok0, Wq, keys) in qchunks:
                    qb = nq % 2
                    nq += 1
                    S.dma("sp", qt[qb][:, :Wq], X["qT"][h * P:(h + 1) * P, tok0:tok0 + Wq], writes=[("qt", qb)])
                    LAG = 2

                    def emit_qk(ki):
                        j = keys[ki]
                        g = nstep + ki
                        for m in range(2):
                            S.op("pe", lambda: nc.tensor.matmul(sc[g % NSC][:, m, :Wq], lhsT=kTz[hb][m][:, j * P:(j + 1) * P], rhs=qt[qb][:, :Wq], start=True, stop=True),
                                 reads=[("kTz", hb, m), ("qt", qb)], writes=[("sc", g % NSC, m)], pe_chain=True)
                        S.op("act", lambda: nc.scalar.activation(out=pT[g % NPT][:, :, :Wq], in_=sc[g % NSC][:, :, :Wq], func=AF.Exp, scale=0.125),
                             reads=[("sc", g % NSC, 0), ("sc", g % NSC, 1)], writes=[("pT", g % NPT)])

                    def emit_pv(ki):
                        j = keys[ki]
                        g = nstep + ki
                        first = (ki == 0)
                        lastk = (ki == len(keys) - 1)
                        for m in range(2):
                            S.op("pe", lambda: nc.tensor.matmul(acco[m][:, :Wq], lhsT=vh[hb][:, j, :], rhs=pT[g % NPT][:, m, :Wq], start=first, stop=lastk),
                                 reads=[("vh", hb), ("pT", g % NPT)], writes=[("acco", m)], pe_chain=True)
                            S.op("pe", lambda: nc.tensor.matmul(accd[m][:, :Wq], lhsT=onesb[:], rhs=pT[g % NPT][:, m, :Wq], start=first, stop=lastk),
                                 reads=["onesb", ("pT", g % NPT)], writes=[("accd", m)], pe_chain=True)

                    for ki in range(len(keys) + LAG):
                        if ki < len(keys):
                            emit_qk(ki)
                        if ki - LAG >= 0:
                            emit_pv(ki - LAG)
                    nstep += len(keys)
                    for m in range(2):
                        S.op("act", lambda: nc.scalar.activation(out=rd[m][:, :Wq], in_=accd[m][:, :Wq], func=AF.Ln), reads=[("accd", m)], writes=[("rd", m)])
                        S.op("act", lambda: nc.scalar.activation(out=rd[m][:, :Wq], in_=rd[m][:, :Wq], func=AF.Exp, scale=-1.0), reads=[("rd", m)], writes=[("rd", m)])
                    S.op("dve", lambda: nc.vector.tensor_tensor(out=o0[:, :Wq], in0=acco[0][:, :Wq], in1=rd[0][:, :Wq], op=ALU.mult),
                         reads=[("acco", 0), ("rd", 0)], writes=["o0"])
                    S.op("dve", lambda: nc.vector.tensor_tensor(out=o1[:, :Wq], in0=acco[1][:, :Wq], in1=rd[1][:, :Wq], op=ALU.mult),
                         reads=[("acco", 1), ("rd", 1)], writes=["o1"])
                    S.op("dve", lambda: nc.vector.scalar_tensor_tensor(out=oo[:, :Wq], in0=o1[:, :Wq], scalar=neglam[:, 0:1], in1=o0[:, :Wq], op0=ALU.mult, op1=ALU.add),
                         reads=["o0", "o1", "neglam"], writes=["oo"])
                    S.op("pool", lambda: nc.gpsimd.tensor_tensor(out=sq[:, :Wq], in0=oo[:, :Wq], in1=oo[:, :Wq], op=ALU.mult), reads=["oo"], writes=["sq"])
                    g = nstep % NSC
                    S.op("pe", lambda: nc.tensor.matmul(sc[g][:, 0, :Wq], lhsT=onesb[:], rhs=sq[:, :Wq], start=True, stop=True),
                         reads=["onesb", "sq"], writes=[("sc", g, 0)], pe_chain=True)
                    S.op("act", lambda: nc.scalar.activation(out=rt[:, :Wq], in_=sc[g][:, 0, :Wq], func=AF.Ln, scale=1.0 / 128, bias=epsc[:, 0:1]),
                         reads=[("sc", g, 0), "epsc"], writes=["rt"])
                    nstep += 1
                    S.op("act", lambda: nc.scalar.activation(out=rt[:, :Wq], in_=rt[:, :Wq], func=AF.Exp, scale=-0.5), reads=["rt"], writes=["rt"])
                    yb_ = nq % 2
                    S.op("dve", lambda: nc.vector.scalar_tensor_tensor(out=yc[yb_][:, :Wq], in0=oo[:, :Wq], scalar=gsc[:, 0:1], in1=rt[:, :Wq], op0=ALU.mult, op1=ALU.mult),
                         reads=["oo", "gsc", "rt"], writes=[("yc", yb_)])
                    S.dma("sp", X["ycT"][h * P:(h + 1) * P, tok0:tok0 + Wq], yc[yb_][:, :Wq], reads=[("yc", yb_)], writes=[("d_yc", yb_)])

    def phase_s5(self, l):
        nc, S, I, X = self.nc, self.S, self.I, self.X
        TWO_PI = 2.0 * math.pi
        INV2PI = 1.0 / TWO_PI
        MAGIC = 12582912.0
        V = nc.vector

        def dve(fn, reads, writes):
            return S.op("dve", fn, reads=reads, writes=writes)

        def reduce_angle(out, x, tmp, shift, kx, kout, ktmp):
            dve(lambda: V.tensor_scalar(out=tmp, in0=x, scalar1=INV2PI, scalar2=shift * INV2PI + MAGIC, op0=ALU.mult, op1=ALU.add), [kx], [ktmp])
            dve(lambda: V.tensor_scalar(out=tmp, in0=tmp, scalar1=-MAGIC, scalar2=None, op0=ALU.add), [ktmp], [ktmp])
            dve(lambda: V.scalar_tensor_tensor(out=out, in0=tmp, scalar=-TWO_PI, in1=x, op0=ALU.mult, op1=ALU.add), [ktmp, kx], [kout])

        def cossin(cos_o, sin_o, x, tmp, tmp2, kx, kc_, ks_, ktmp, ktmp2):
            reduce_angle(tmp2, x, tmp, 0.0, kx, ktmp2, ktmp)
            S.op("act", lambda: nc.scalar.activation(out=sin_o, in_=tmp2, func=AF.Sin), reads=[ktmp2], writes=[ks_])
            reduce_angle(tmp2, x, tmp, math.pi / 2, kx, ktmp2, ktmp)
            S.op("act", lambda: nc.scalar.activation(out=cos_o, in_=tmp2, func=AF.Sin, bias=halfpi[:, 0:1]), reads=[ktmp2, "halfpi"], writes=[kc_])

        with contextlib.ExitStack() as ph:
            aT_sb = self.sb(ph, "aT_sb", [P, 2, NTOK], BF16)
            yacc = self.sb(ph, "yacc", [P, 2, NTOK], F32)
            dcol = self.sb(ph, "dcol", [P, 2])
            halfpi = self.sb(ph, "halfpi", [P, 1])
            sigp = self.sb(ph, "sigp", [P, 2])
            nsig = self.sb(ph, "nsig", [P, 2])
            sigf = self.sb(ph, "sigf", [P, 2, P])
            Bmat = [self.sb(ph, "Bmat%d" % d, [P, 2, 1024], BF16) for d in range(2)]
            WpreR = [self.sb(ph, "WpreR%d" % d, [P, 2, 512]) for d in range(2)]
            WpreI = [self.sb(ph, "WpreI%d" % d, [P, 2, 512]) for d in range(2)]
            WpostR = [self.sb(ph, "WpostR%d" % d, [P, 8, P]) for d in range(2)]
            WpostI = [self.sb(ph, "WpostI%d" % d, [P, 8, P]) for d in range(2)]
            A128R = [self.sb(ph, "A128R%d" % d, [P, 8]) for d in range(2)]
            A128I = [self.sb(ph, "A128I%d" % d, [P, 8]) for d in range(2)]
            Cm = [self.sb(ph, "Cm%d" % d, [P, 16, P], BF16) for d in range(2)]
            Mdir = [self.sb(ph, "Mdir%d" % d, [P, P], BF16) for d in range(2)]
            E8 = self.sb(ph, "E8", [P, 8, P], BF16)
            cTh = [[self.sb(ph, "cTh%d%d" % (d, k), [P, P], BF16) for k in range(2)] for d in range(2)]
            cTl = [[self.sb(ph, "cTl%d%d" % (d, k), [P, P], BF16) for k in range(2)] for d in range(2)]

            S.dma("sp", aT_sb[:], X["aT"].rearrange("(kc p) t -> p kc t", p=P), writes=["aT_sb"])
            S.dma("sp", dcol[:], I["s5_d"][l].rearrange("(kc p) -> p kc", p=P), writes=["dcol"], allow_slow_non_contiguous=True)
            S.dma("sp", sigp[:], I["k_sigp"], writes=["sigp"])
            S.dma("sp", sigf[:], I["k_sigf"], writes=["sigf"])
            S.dma("pool", Mdir[0][:], I["k_mfwd"], writes=[("Mdir", 0)])
            S.dma("pool", Mdir[1][:], I["k_mbwd"], writes=[("Mdir", 1)])
            S.dma("pool", E8[:], I["k_e8"], writes=["E8"])
            Mneg = [self.sb(ph, "Mneg%d" % d, [P, P], BF16) for d in range(2)]
            CmN = [self.sb(ph, "CmN%d" % d, [P, 8, P], BF16) for d in range(2)]
            for d in range(2):
                dve(lambda: V.tensor_scalar(out=Mneg[d][:], in0=Mdir[d][:], scalar1=-1.0, scalar2=None, op0=ALU.mult), [("Mdir", d)], [("Mneg", d)])
            dve(lambda: V.memset(halfpi[:], math.pi / 2), [], ["halfpi"])
            dve(lambda: V.tensor_scalar(out=nsig[:], in0=sigp[:], scalar1=-1.0, scalar2=None, op0=ALU.mult), ["sigp"], ["nsig"])
            for d in range(2):
                for k in range(2):
                    S.op("pool", lambda: nc.gpsimd.memset(cTh[d][k][:], 0.0), writes=[("cTh", d, k)])
                    S.op("pool", lambda: nc.gpsimd.memset(cTl[d][k][:], 0.0), writes=[("cTl", d, k)])
            for kc in range(2):
                dve(lambda: V.tensor_scalar(out=yacc[:, kc, :], in0=aT_sb[:, kc, :], scalar1=dcol[:, kc:kc + 1], scalar2=None, op0=ALU.mult),
                    ["aT_sb", "dcol"], [("yacc", kc, i) for i in range(NT)])

            with contextlib.ExitStack() as su:
                R = {}
                for n in ("LR", "LI", "lrdt", "ang", "mag", "cs", "sn", "tA", "tB", "are", "aim", "zre", "zim", "x", "t3", "t4"):
                    R[n] = self.sb(su, "r_" + n, [P, 1024])
                LS = self.sb(su, "r_LS", [P, 16])
                braw = [self.sb(su, "braw%d" % i, [P, 512]) for i in range(2)]
                CmS = self.sb(su, "CmS", [P, 16, P])
                Cc = {}
                for n in ("LR", "LI", "LS", "lrdt", "ang", "angr", "t", "e", "x", "cs", "sn", "mg"):
                    Cc[n] = self.sb(su, "c_" + n, [P, 8])
                for d in range(2):
                    k_ = lambda n: ("su", n)
                    S.dma("sp", R["LR"][:], I["s5_lam_re"][l, d].rearrange("g p -> (g p)").partition_broadcast(P), writes=[k_("LR")])
                    S.dma("sp", R["LI"][:], I["s5_lam_im"][l, d].rearrange("g p -> (g p)").partition_broadcast(P), writes=[k_("LI")])
                    S.dma("sp", LS[:], I["s5_log_step"][l, d].partition_broadcast(P), writes=[k_("LS")])
                    S.op("act", lambda: nc.scalar.activation(out=LS[:], in_=LS[:], func=AF.Exp), reads=[k_("LS")], writes=[k_("LS")])
                    dtb = LS[:, :].unsqueeze(2).to_broadcast([P, 16, 64])
                    v3 = lambda t: t[:].rearrange("p (g q) -> p g q", g=16)
                    dve(lambda: V.tensor_tensor(out=v3(R["lrdt"]), in0=v3(R["LR"]), in1=dtb, op=ALU.mult), [k_("LR"), k_("LS")], [k_("lrdt")])
                    dve(lambda: V.tensor_tensor(out=v3(R["ang"]), in0=v3(R["LI"]), in1=dtb, op=ALU.mult), [k_("LI"), k_("LS")], [k_("ang")])
                    S.op("act", lambda: nc.scalar.activation(out=R["mag"][:], in_=R["lrdt"][:], func=AF.Exp), reads=[k_("lrdt")], writes=[k_("mag")])
                    reduce_angle(R["tB"][:], R["ang"][:], R["tA"][:], 0.0, k_("ang"), k_("tB"), k_("tA"))
                    dve(lambda: V.tensor_copy(out=R["ang"][:], in_=R["tB"][:]), [k_("tB")], [k_("ang")])
                    cossin(R["cs"][:], R["sn"][:], R["ang"][:], R["tA"][:], R["tB"][:], k_("ang"), k_("cs"), k_("sn"), k_("tA"), k_("tB"))
                    dve(lambda: V.tensor_tensor(out=R["are"][:], in0=R["mag"][:], in1=R["cs"][:], op=ALU.mult), [k_("mag"), k_("cs")], [k_("are")])
                    dve(lambda: V.tensor_tensor(out=R["aim"][:], in0=R["mag"][:], in1=R["sn"][:], op=ALU.mult), [k_("mag"), k_("sn")], [k_("aim")])
                    dve(lambda: V.tensor_tensor(out=R["tA"][:], in0=R["LR"][:], in1=R["LR"][:], op=ALU.mult), [k_("LR")], [k_("tA")])
                    dve(lambda: V.tensor_tensor(out=R["tB"][:], in0=R["LI"][:], in1=R["LI"][:], op=ALU.mult), [k_("LI")], [k_("tB")])
                    dve(lambda: V.tensor_tensor(out=R["cs"][:], in0=R["tA"][:], in1=R["tB"][:], op=ALU.add), [k_("tA"), k_("tB")], [k_("cs")])
                    dve(lambda: V.reciprocal(out=R["cs"][:], in_=R["cs"][:]), [k_("cs")], [k_("cs")])
                    dve(lambda: V.tensor_scalar(out=R["are"][:], in0=R["are"][:], scalar1=-1.0, scalar2=None, op0=ALU.add), [k_("are")], [k_("are")])
                    dve(lambda: V.tensor_tensor(out=R["tA"][:], in0=R["are"][:], in1=R["LR"][:], op=ALU.mult), [k_("are"), k_("LR")], [k_("tA")])
                    dve(lambda: V.tensor_tensor(out=R["tB"][:], in0=R["aim"][:], in1=R["LI"][:], op=ALU.mult), [k_("aim"), k_("LI")], [k_("tB")])
                    dve(lambda: V.tensor_tensor(out=R["tA"][:], in0=R["tA"][:], in1=R["tB"][:], op=ALU.add), [k_("tA"), k_("tB")], [k_("tA")])
                    dve(lambda: V.tensor_tensor(out=R["zre"][:], in0=R["tA"][:], in1=R["cs"][:], op=ALU.mult), [k_("tA"), k_("cs")], [k_("zre")])
                    dve(lambda: V.tensor_tensor(out=R["tA"][:], in0=R["aim"][:], in1=R["LR"][:], op=ALU.mult), [k_("aim"), k_("LR")], [k_("tA")])
                    dve(lambda: V.tensor_tensor(out=R["tB"][:], in0=R["are"][:], in1=R["LI"][:], op=ALU.mult), [k_("are"), k_("LI")], [k_("tB")])
                    dve(lambda: V.tensor_tensor(out=R["tA"][:], in0=R["tA"][:], in1=R["tB"][:], op=ALU.subtract), [k_("tA"), k_("tB")], [k_("tA")])
                    dve(lambda: V.tensor_tensor(out=R["zim"][:], in0=R["tA"][:], in1=R["cs"][:], op=ALU.mult), [k_("tA"), k_("cs")], [k_("zim")])
                    for kc in range(2):
                        hs = slice(kc * 512, (kc + 1) * 512)
                        for ri, src in enumerate((I["s5_b_re"], I["s5_b_im"])):
                            dve(lambda: V.memset(braw[ri][:], 0.0), [], [("braw", ri, gl) for gl in range(8)])
                            for gl in range(8):
                                g = kc * 8 + gl
                                S.dma("sp", braw[ri][16 * gl:16 * gl + 16, gl * 64:(gl + 1) * 64], src[l, d, g].rearrange("p c -> c p"),
                                      reads=[], writes=[("braw", ri, gl)], allow_slow_non_contiguous=True)
                        tA = R["tA"][:, 0:512]; tB = R["tB"][:, 0:512]
                        dve(lambda: V.tensor_tensor(out=tA, in0=braw[0][:], in1=R["zre"][:, hs], op=ALU.mult), [("braw", 0, gl) for gl in range(8)] + [k_("zre")], [k_("tA")])
                        dve(lambda: V.tensor_tensor(out=tB, in0=braw[1][:], in1=R["zim"][:, hs], op=ALU.mult), [("braw", 1, gl) for gl in range(8)] + [k_("zim")], [k_("tB")])
                        dve(lambda: V.tensor_tensor(out=Bmat[d][:, kc, 0:512], in0=tA, in1=tB, op=ALU.subtract), [k_("tA"), k_("tB")], [("Bmat", d, kc, 0)])
                        dve(lambda: V.tensor_tensor(out=tA, in0=braw[0][:], in1=R["zim"][:, hs], op=ALU.mult), [("braw", 0, gl) for gl in range(8)] + [k_("zim")], [k_("tA")])
                        dve(lambda: V.tensor_tensor(out=tB, in0=braw[1][:], in1=R["zre"][:, hs], op=ALU.mult), [("braw", 1, gl) for gl in range(8)] + [k_("zre")], [k_("tB")])
                        dve(lambda: V.tensor_tensor(out=Bmat[d][:, kc, 512:1024], in0=tA, in1=tB, op=ALU.add), [k_("tA"), k_("tB")], [("Bmat", d, kc, 1)])
                        S.op("act", lambda: nc.scalar.activation(out=R["mag"][:, 0:512], in_=R["lrdt"][:, hs], func=AF.Exp, scale=nsig[:, d:d + 1]),
                             reads=[k_("lrdt"), "nsig"], writes=[k_("mag")])
                        dve(lambda: V.tensor_scalar(out=R["x"][:, 0:512], in0=R["ang"][:, hs], scalar1=sigp[:, d:d + 1], scalar2=None, op0=ALU.mult),
                            [k_("ang"), "sigp"], [k_("x")])
                        cossin(R["cs"][:, 0:512], R["sn"][:, 0:512], R["x"][:, 0:512], R["t3"][:, 0:512], R["t4"][:, 0:512],
                               k_("x"), k_("cs"), k_("sn"), k_("t3"), k_("t4"))
                        dve(lambda: V.tensor_tensor(out=WpreR[d][:, kc, :], in0=R["mag"][:, 0:512], in1=R["cs"][:, 0:512], op=ALU.mult),
                            [k_("mag"), k_("cs")], [("WpreR", d, kc)])
                        dve(lambda: V.scalar_tensor_tensor(out=WpreI[d][:, kc, :], in0=R["mag"][:, 0:512], scalar=-1.0, in1=R["sn"][:, 0:512], op0=ALU.mult, op1=ALU.mult),
                            [k_("mag"), k_("sn")], [("WpreI", d, kc)])
                    S.dma("sp", Cc["LR"][:], I["s5_lam_re"][l, d].rearrange("g p -> (g p)").rearrange("(c q) -> q c", q=P), writes=[k_("cLR")], allow_slow_non_contiguous=True)
                    S.dma("sp", Cc["LI"][:], I["s5_lam_im"][l, d].rearrange("g p -> (g p)").rearrange("(c q) -> q c", q=P), writes=[k_("cLI")], allow_slow_non_contiguous=True)
                    for hh in range(2):
                        S.dma("sp", Cc["LS"][64 * hh:64 * hh + 64, :], I["s5_log_step"][l, d].rearrange("(c two) -> two c", two=2)[hh].partition_broadcast(64),
                              writes=[k_("cLS%d" % hh)], allow_slow_non_contiguous=True)
                    S.op("act", lambda: nc.scalar.activation(out=Cc["LS"][:], in_=Cc["LS"][:], func=AF.Exp), reads=[k_("cLS0"), k_("cLS1")], writes=[k_("cdt")])
                    dve(lambda: V.tensor_tensor(out=Cc["lrdt"][:], in0=Cc["LR"][:], in1=Cc["LS"][:], op=ALU.mult), [k_("cLR"), k_("cdt")], [k_("clrdt")])
                    dve(lambda: V.tensor_tensor(out=Cc["ang"][:], in0=Cc["LI"][:], in1=Cc["LS"][:], op=ALU.mult), [k_("cLI"), k_("cdt")], [k_("cang")])
                    reduce_angle(Cc["angr"][:], Cc["ang"][:], Cc["t"][:], 0.0, k_("cang"), k_("cangr"), k_("ct"))
                    E3 = R["tA"][:].rearrange("p (c t) -> p c t", c=8)
                    X3 = R["tB"][:].rearrange("p (c t) -> p c t", c=8)
                    sgb = sigf[:, d, :].unsqueeze(1).to_broadcast([P, 8, P])
                    dve(lambda: V.tensor_tensor(out=E3, in0=Cc["lrdt"][:, :].unsqueeze(2).to_broadcast([P, 8, P]), in1=sgb, op=ALU.mult),
                        [k_("clrdt"), "sigf"], [k_("tA")])
                    S.op("act", lambda: nc.scalar.activation(out=R["mag"][:], in_=R["tA"][:], func=AF.Exp), reads=[k_("tA")], writes=[k_("mag")])
                    dve(lambda: V.tensor_tensor(out=X3, in0=Cc["angr"][:, :].unsqueeze(2).to_broadcast([P, 8, P]), in1=sgb, op=ALU.mult),
                        [k_("cangr"), "sigf"], [k_("tB")])
                    dve(lambda: V.tensor_copy(out=R["x"][:], in_=R["tB"][:]), [k_("tB")], [k_("x")])
                    cossin(R["cs"][:], R["sn"][:], R["x"][:], R["t3"][:], R["t4"][:], k_("x"), k_("cs"), k_("sn"), k_("t3"), k_("t4"))
                    dve(lambda: V.tensor_tensor(out=WpostR[d][:].rearrange("p c t -> p (c t)"), in0=R["mag"][:], in1=R["cs"][:], op=ALU.mult),
                        [k_("mag"), k_("cs")], [("WpostR", d)])
                    dve(lambda: V.tensor_tensor(out=WpostI[d][:].rearrange("p c t -> p (c t)"), in0=R["mag"][:], in1=R["sn"][:], op=ALU.mult),
                        [k_("mag"), k_("sn")], [("WpostI", d)])
                    dve(lambda: V.tensor_scalar(out=Cc["e"][:], in0=Cc["lrdt"][:], scalar1=128.0, scalar2=None, op0=ALU.mult), [k_("clrdt")], [k_("ce")])
                    S.op("act", lambda: nc.scalar.activation(out=Cc["mg"][:], in_=Cc["e"][:], func=AF.Exp), reads=[k_("ce")], writes=[k_("cmg")])
                    dve(lambda: V.tensor_scalar(out=Cc["x"][:], in0=Cc["angr"][:], scalar1=128.0, scalar2=None, op0=ALU.mult), [k_("cangr")], [k_("cx")])
                    cossin(Cc["cs"][:], Cc["sn"][:], Cc["x"][:], Cc["t"][:], Cc["e"][:], k_("cx"), k_("ccs"), k_("csn"), k_("ct"), k_("ce"))
                    dve(lambda: V.tensor_tensor(out=A128R[d][:], in0=Cc["mg"][:], in1=Cc["cs"][:], op=ALU.mult), [k_("cmg"), k_("ccs")], [("A128R", d)])
                    dve(lambda: V.tensor_tensor(out=A128I[d][:], in0=Cc["mg"][:], in1=Cc["sn"][:], op=ALU.mult), [k_("cmg"), k_("csn")], [("A128I", d)])
                    cmk = [("CmS", g, ri) for g in range(16) for ri in range(2)]
                    dve(lambda: V.memset(CmS[:], 0.0), [], cmk)
                    for g in range(16):
                        kc, gl = g // 8, g % 8
                        j, hh = gl // 2, gl % 2
                        for ri, src in enumerate((I["s5_c_re"], I["s5_c_im"])):
                            S.dma("sp", CmS[64 * hh:64 * hh + 64, kc * 8 + ri * 4 + j, 16 * gl:16 * gl + 16], src[l, d, g].rearrange("c p -> p c"),
                                  reads=[], writes=[("CmS", g, ri)], allow_slow_non_contiguous=True)
                    Cm4 = Cm[d][:].rearrange("p (k r j) c -> p k r (j c)", k=2, r=2)
                    CmS4 = CmS[:].rearrange("p (k r j) c -> p k r (j c)", k=2, r=2)
                    dve(lambda: V.tensor_copy(out=Cm4[:, :, 0, :], in_=CmS4[:, :, 0, :]), cmk, [("Cm", d, 0)])
                    dve(lambda: V.tensor_scalar(out=Cm4[:, :, 1, :], in0=CmS4[:, :, 1, :], scalar1=-1.0, scalar2=None, op0=ALU.mult), cmk, [("Cm", d, 1)])
                    dve(lambda: V.tensor_scalar(out=CmN[d][:].rearrange("p (k j) c -> p k (j c)", k=2), in0=CmS4[:, :, 0, :], scalar1=-1.0, scalar2=None, op0=ALU.mult),
                        cmk, [("CmN", d)])
            S.barrier()

            P1 = [[self.sb(ph, "s5P1%d%d" % (d, k), [P, 2, 512], BF16) for k in range(2)] for d in range(2)]
            P2 = [[self.sb(ph, "s5P2%d%d" % (d, k), [P, 2, 512], BF16) for k in range(2)] for d in range(2)]
            Q1 = [[self.sb(ph, "s5Q1%d%d" % (d, k), [P, 2, 512], BF16) for k in range(2)] for d in range(2)]
            Q2 = [[self.sb(ph, "s5Q2%d%d" % (d, k), [P, 2, 512], BF16) for k in range(2)] for d in range(2)]
            tc1 = self.sb(ph, "s5tc1", [P, 2, 4])
            tc2 = self.sb(ph, "s5tc2", [P, 2, 4])
            cn = self.sb(ph, "s5cn", [P, 8])
            bu = self.psum(ph, "s5bu", [P, 2, 512])
            st_ = [self.psum(ph, "s5st%d" % d, [P, 8, P]) for d in range(2)]
            ctp = self.psum(ph, "s5ctp", [P, 512])
            yo = self.psum(ph, "s5yo", [P, 512])
            orders = [list(range(NT)), [1, 0] + list(range(NT - 1, 1, -1))]
            for step in range(NT):
                for d in range(2):
                    ti = orders[d][step]
                    cols = slice(ti * P, (ti + 1) * P)
                    tl = 127 if d == 0 else 0
                    st = st_[d]
                    for kc in range(2):
                        p1, p2, q1, q2 = P1[d][kc], P2[d][kc], Q1[d][kc], Q2[d][kc]
                        for ri in range(2):
                            S.op("pe", lambda: nc.tensor.matmul(bu[:, ri, :], lhsT=aT_sb[:, kc, cols], rhs=Bmat[d][:, kc, ri * 512:(ri + 1) * 512], start=True, stop=True),
                                 reads=["aT_sb"], writes=["bu"], pe_chain=True)
                        dve(lambda: V.tensor_tensor(out=p1[:], in0=bu[:], in1=WpreR[d][:, kc, :].unsqueeze(1).to_broadcast([P, 2, 512]), op=ALU.mult),
                            ["bu"], [("P1", d, kc)])
                        dve(lambda: V.tensor_tensor(out=p2[:], in0=bu[:], in1=WpreI[d][:, kc, :].unsqueeze(1).to_broadcast([P, 2, 512]), op=ALU.mult),
                            ["bu"], [("P2", d, kc)])
                        for jj in range(8):
                            j = jj % 4
                            js = slice(j * P, (j + 1) * P)
                            if jj < 4:
                                terms = [(p1[:, 0, js], Mdir[d]), (p2[:, 1, js], Mneg[d])]
                            else:
                                terms = [(p2[:, 0, js], Mdir[d]), (p1[:, 1, js], Mdir[d])]
                            for tix, (lh, rh) in enumerate(terms):
                                S.op("pe", lambda: nc.tensor.matmul(st[:, jj, :], lhsT=lh, rhs=rh[:], start=(tix == 0), stop=(tix == 1 and step == 0)),
                                     reads=[("P1", d, kc), ("P2", d, kc)], writes=[("st", d)], pe_chain=True)
                            if step > 0:
                                S.op("pe", lambda: nc.tensor.matmul(st[:, jj, :], lhsT=cTh[d][kc][:], rhs=E8[:, jj, :], start=False, stop=False),
                                     reads=[("cTh", d, kc)], writes=[("st", d)], pe_chain=True)
                                S.op("pe", lambda: nc.tensor.matmul(st[:, jj, :], lhsT=cTl[d][kc][:], rhs=E8[:, jj, :], start=False, stop=True),
                                     reads=[("cTl", d, kc)], writes=[("st", d)], pe_chain=True)
                        st3 = st[:].rearrange("p (r j) t -> p r (j t)", r=2)
                        wr = WpostR[d][:, kc * 4:(kc + 1) * 4, :].rearrange("p j t -> p (j t)").unsqueeze(1).to_broadcast([P, 2, 512])
                        wi = WpostI[d][:, kc * 4:(kc + 1) * 4, :].rearrange("p j t -> p (j t)").unsqueeze(1).to_broadcast([P, 2, 512])
                        dve(lambda: V.tensor_tensor(out=q1[:], in0=st3, in1=wr, op=ALU.mult), [("st", d)], [("Q1", d, kc)])
                        dve(lambda: V.tensor_tensor(out=q2[:], in0=st3, in1=wi, op=ALU.mult), [("st", d)], [("Q2", d, kc)])
                        if step < NT - 1:
                            sl = st[:, :, tl].rearrange("p (r j) -> p r j", r=2)
                            ar = A128R[d][:, kc * 4:(kc + 1) * 4].unsqueeze(1).to_broadcast([P, 2, 4])
                            ai = A128I[d][:, kc * 4:(kc + 1) * 4].unsqueeze(1).to_broadcast([P, 2, 4])
                            dve(lambda: V.tensor_tensor(out=tc1[:], in0=sl, in1=ar, op=ALU.mult), [("st", d)], ["tc1"])
                            dve(lambda: V.tensor_tensor(out=tc2[:], in0=sl, in1=ai, op=ALU.mult), [("st", d)], ["tc2"])
                            dve(lambda: V.tensor_tensor(out=cn[:, 0:4], in0=tc1[:, 0, :], in1=tc2[:, 1, :], op=ALU.subtract), ["tc1", "tc2"], ["cn0"])
                            dve(lambda: V.tensor_tensor(out=cn[:, 4:8], in0=tc2[:, 0, :], in1=tc1[:, 1, :], op=ALU.add), ["tc1", "tc2"], ["cn1"])
                            S.op("pe", lambda: nc.tensor.transpose(ctp[0:8, 0:P], cn[:, :], self.ident[:]), reads=["cn0", "cn1", "ident"], writes=["ctp"], pe_chain=True)
                            S.op("act", lambda: nc.scalar.copy(out=cTh[d][kc][0:8, :], in_=ctp[0:8, 0:P]), reads=["ctp"], writes=[("cTh", d, kc)])
                            dve(lambda: V.tensor_tensor(out=cTl[d][kc][0:8, :], in0=ctp[0:8, 0:P], in1=cTh[d][kc][0:8, :], op=ALU.subtract),
                                ["ctp", ("cTh", d, kc)], [("cTl", d, kc)])
                        rterms = []
                        for j in range(4):
                            js = slice(j * P, (j + 1) * P)
                            rterms += [(Cm[d][:, kc * 8 + j, :], q1[:, 0, js], ("Q1", d, kc)), (CmN[d][:, kc * 4 + j, :], q2[:, 1, js], ("Q2", d, kc)),
                                       (Cm[d][:, kc * 8 + 4 + j, :], q2[:, 0, js], ("Q2", d, kc)), (Cm[d][:, kc * 8 + 4 + j, :], q1[:, 1, js], ("Q1", d, kc))]
                        for tix, (lh, rh, rk) in enumerate(rterms):
                            S.op("pe", lambda: nc.tensor.matmul(yo[:, 0:P], lhsT=lh, rhs=rh, start=(tix == 0), stop=(tix == len(rterms) - 1)),
                                 reads=[rk], writes=["yo"], pe_chain=True)
                        dve(lambda: V.tensor_tensor(out=yacc[:, kc, cols], in0=yo[:, 0:P], in1=yacc[:, kc, cols], op=ALU.add),
                            ["yo", ("yacc", kc, ti)], [("yacc", kc, ti)])
            for kc in range(2):
                S.op("act", lambda: nc.scalar.activation(out=aT_sb[:, kc, :], in_=yacc[:, kc, :], func=AF.Gelu_apprx_tanh),
                     reads=[("yacc", kc, i) for i in range(NT)] + ["aT_sb"], writes=["aT_sb"])
            S.dma("sp", X["yaT"].rearrange("(kc p) t -> p kc t", p=P), aT_sb[:], reads=["aT_sb"], writes=["d_yaT"])

    def ln_stats(self, src, st6, mv, rs, key):
        nc, S = self.nc, self.S
        for hf in range(2):
            S.op("dve", lambda: nc.vector.bn_stats(out=st6[:, hf, :], in_=src[:, hf * 512:(hf + 1) * 512]), reads=[key], writes=[("st6", id(st6), hf)])
        S.op("dve", lambda: nc.vector.bn_aggr(out=mv[:, :], in_=st6[:, :, :].rearrange("p a b -> p (a b)")),
             reads=[("st6", id(st6), 0), ("st6", id(st6), 1)], writes=[("mv", id(mv))])
        S.op("act", lambda: nc.scalar.activation(out=rs[:, 0:1], in_=mv[:, 1:2], func=AF.Sqrt, bias=LN_EPS, scale=1.0),
             reads=[("mv", id(mv))], writes=[("rs0", id(rs))])
        S.op("dve", lambda: nc.vector.reciprocal(out=rs[:, 1:2], in_=rs[:, 0:1]), reads=[("rs0", id(rs))], writes=[("rs1", id(rs))])
        return [("mv", id(mv)), ("rs1", id(rs))]

    def phase_merge(self, l):
        nc, S, I, X = self.nc, self.S, self.I, self.X
        last = (l == DEPTH - 1)
        with contextlib.ExitStack() as ph:
            wG = self.sb(ph, "wG", [P, 8, 3072], BF16)
            for br in range(3):
                S.dma("pool", wG[:, :, br * 1024:(br + 1) * 1024],
                      I["w_in"][l][:, 2304 + br * 1024:2304 + (br + 1) * 1024].rearrange("(kt p) n -> p kt n", p=P), writes=[("wG", br)])
            wsm = {}
            for n, kts in (("w_glu_val", 2), ("w_glu_gate", 2), ("w_proj_gm", 2), ("w_proj_da", 4), ("w_out", 8)):
                wsm[n] = self.sb(ph, "m_" + n, [P, kts, D], BF16)
                S.dma("pool", wsm[n][:], I[n][l].rearrange("(kt p) n -> p kt n", p=P), writes=[n])
            lng = self.sb(ph, "lng", [P, D])
            lnb = self.sb(ph, "lnb", [P, D])
            S.dma("sp", lng[:], I["ln1_g"][l].partition_broadcast(P), writes=["lng"])
            S.dma("sp", lnb[:], I["ln1_b"][l].partition_broadcast(P), writes=["lnb"])
            ya = [self.sb(ph, "g_ya%d" % i, [P, 2, 512], BF16) for i in range(2)]
            yb = [self.sb(ph, "g_yb%d" % i, [P, 2, 512], BF16) for i in range(2)]
            yc = [self.sb(ph, "g_yc%d" % i, [P, 4, 512], BF16) for i in range(2)]
            uu = [self.sb(ph, "g_uu%d" % i, [P, 8, 512], BF16) for i in range(2)]
            mT = [self.sb(ph, "g_mT%d" % i, [P, 8, 512], BF16) for i in range(2)]
            fT = [self.sb(ph, "g_fT%d" % i, [P, 8, 512], BF16) for i in range(1)] * 2
            sg = [self.sb(ph, "g_sg%d" % i, [P, 4, 512]) for i in range(1)] * 2
            tm = [self.sb(ph, "g_tm%d" % i, [P, 4, 512]) for i in range(1)] * 2
            hx = [self.sb(ph, "g_hx%d" % i, [P, D]) for i in range(2)]
            tt_ = [self.sb(ph, "g_tt%d" % i, [P, D]) for i in range(2)]
            xn = [self.sb(ph, "g_xn%d" % i, [P, D], BF16) for i in range(2)]
            st6 = [self.sb(ph, "g_st6%d" % i, [P, 2, 6]) for i in range(2)]
            mv = [self.sb(ph, "g_mv%d" % i, [P, 2]) for i in range(2)]
            rs = [self.sb(ph, "g_rs%d" % i, [P, 2]) for i in range(2)]
            NPB = 5
            pb_ = [self.psum(ph, "g_p%d" % i, [P, 512]) for i in range(NPB)]
            po = self.psum(ph, "g_po", [P, 2, 512])
            tp = self.psum(ph, "g_tp", [P, 8, P], BF16)
            chunks = [(2 + 4 * c, 4) for c in range(8)]
            if not last:
                chunks = [(0, 2)] + chunks
            pc_ = 0
            tcount = 0

            def load(ci):
                t0, ntl = chunks[ci]
                Wd = ntl * P
                tok0 = t0 * P
                cb = ci % 2
                S.dma("sp", ya[cb][:, :, :Wd], X["yaT"].rearrange("(kt p) t -> p kt t", p=P)[:, :, tok0:tok0 + Wd], writes=[("ya", cb)])
                S.dma("sp", yb[cb][:, :, :Wd], X["ybT"].rearrange("(kt p) t -> p kt t", p=P)[:, :, tok0:tok0 + Wd], writes=[("yb", cb)])
                S.dma("sp", yc[cb][:, :, :Wd], X["ycT"].rearrange("(kt p) t -> p kt t", p=P)[:, :, tok0:tok0 + Wd], writes=[("yc", cb)])
                S.dma("sp", uu[cb][:, :, :Wd], X["uT"].rearrange("(kt p) t -> p kt t", p=P)[:, :, tok0:tok0 + Wd], writes=[("uu", cb)])

            def dc_step(ci, dc):
                nonlocal pc_
                t0, ntl = chunks[ci]
                Wd = ntl * P
                cb = ci % 2

                def prod(wn, kts, src, skey):
                    nonlocal pc_
                    pi = pc_ % NPB; pc_ += 1
                    pt = pb_[pi]
                    for kt in range(kts):
                        if wn[0] == "g":
                            br = "ABC".index(wn[1])
                            lhsT = wG[:, kt, br * 1024 + dc * P:br * 1024 + (dc + 1) * P]
                            wkey = ("wG", br)
                        else:
                            lhsT = wsm[wn][:, kt, dc * P:(dc + 1) * P]
                            wkey = wn
                        S.op("pe", lambda: nc.tensor.matmul(pt[:, :Wd], lhsT=lhsT, rhs=src[:, kt, :Wd], start=(kt == 0), stop=(kt == kts - 1)),
                             reads=[wkey, skey], writes=[("gp", pi)], pe_chain=True)
                    return pt, ("gp", pi)

                def sig(pt, pk, sgi):
                    S.op("act", lambda: nc.scalar.activation(out=sg[0][:, sgi, :Wd], in_=pt[:, :Wd], func=AF.Sigmoid), reads=[pk], writes=[("sg", sgi)])

                pt, pk = prod("w_glu_gate", 2, ya[cb], ("ya", cb)); sig(pt, pk, 0)
                pt, pk = prod("gA", 8, uu[cb], ("uu", cb)); sig(pt, pk, 1)
                pt, pk = prod("w_glu_val", 2, ya[cb], ("ya", cb))
                S.op("dve", lambda: nc.vector.tensor_tensor(out=tm[0][:, 0, :Wd], in0=pt[:, :Wd], in1=sg[0][:, 0, :Wd], op=ALU.mult),
                     reads=[pk, ("sg", 0)], writes=[("tm", 0)])
                S.op("pool", lambda: nc.gpsimd.tensor_tensor(out=tm[0][:, 0, :Wd], in0=tm[0][:, 0, :Wd], in1=sg[0][:, 1, :Wd], op=ALU.mult),
                     reads=[("tm", 0), ("sg", 1)], writes=[("tm", 0)])
                pt, pk = prod("gB", 8, uu[cb], ("uu", cb)); sig(pt, pk, 2)
                pt, pk = prod("w_proj_gm", 2, yb[cb], ("yb", cb))
                S.op("dve", lambda: nc.vector.tensor_tensor(out=tm[0][:, 1, :Wd], in0=pt[:, :Wd], in1=sg[0][:, 2, :Wd], op=ALU.mult),
                     reads=[pk, ("sg", 2)], writes=[("tm", 1)])
                pt, pk = prod("gC", 8, uu[cb], ("uu", cb)); sig(pt, pk, 3)
                pt, pk = prod("w_proj_da", 4, yc[cb], ("yc", cb))
                S.op("dve", lambda: nc.vector.tensor_tensor(out=tm[0][:, 2, :Wd], in0=pt[:, :Wd], in1=sg[0][:, 3, :Wd], op=ALU.mult),
                     reads=[pk, ("sg", 3)], writes=[("tm", 2)])
                S.op("pool", lambda: nc.gpsimd.tensor_tensor(out=tm[0][:, 3, :Wd], in0=tm[0][:, 0, :Wd], in1=tm[0][:, 1, :Wd], op=ALU.add),
                     reads=[("tm", 0), ("tm", 1)], writes=[("tm", 3)])
                S.op("pool", lambda: nc.gpsimd.tensor_tensor(out=mT[cb][:, dc, :Wd], in0=tm[0][:, 3, :Wd], in1=tm[0][:, 2, :Wd], op=ALU.add),
                     reads=[("tm", 3), ("tm", 2)], writes=[("mT", cb, dc)])

            def tail_tile(ci, ti):
                nonlocal tcount
                t0, ntl = chunks[ci]
                s = 1 if t0 == 0 else 0
                cb = ci % 2
                i = t0 + ti
                hb = tcount % 2
                tcount += 1
                S.dma("sp", hx[hb][:], self.hsrc(l, i), writes=[("ghx", hb)])
                for nh in range(2):
                    for kt in range(8):
                        S.op("pe", lambda: nc.tensor.matmul(po[:, nh, :], lhsT=mT[cb][:, kt, ti * P:(ti + 1) * P], rhs=wsm["w_out"][:, kt, nh * 512:(nh + 1) * 512],
                                                            start=(kt == 0), stop=(kt == 7)),
                             reads=[("mT", cb, kt), "w_out"], writes=[("po", nh)], pe_chain=True)
                pof = po[:].rearrange("p a b -> p (a b)")
                S.op("dve", lambda: nc.vector.tensor_tensor(out=tt_[hb][:], in0=pof, in1=self.gb[:, 2 * s + 0, :], op=ALU.mult),
                     reads=[("po", 0), ("po", 1)], writes=[("gtt", hb)])
                S.op("dve", lambda: nc.vector.scalar_tensor_tensor(out=hx[hb][:], in0=hx[hb][:], scalar=DN_ALPHA, in1=tt_[hb][:], op0=ALU.mult, op1=ALU.add),
                     reads=[("ghx", hb), ("gtt", hb)], writes=[("ghx", hb)])
                ks = self.ln_stats(hx[hb], st6[hb], mv[hb], rs[hb], ("ghx", hb))
                S.op("dve", lambda: nc.vector.tensor_scalar(out=tt_[hb][:], in0=hx[hb][:], scalar1=mv[hb][:, 0:1], scalar2=rs[hb][:, 1:2], op0=ALU.subtract, op1=ALU.mult),
                     reads=[("ghx", hb)] + ks, writes=[("gtt", hb)])
                S.op("pool", lambda: nc.gpsimd.tensor_tensor(out=tt_[hb][:], in0=tt_[hb][:], in1=lng[:], op=ALU.mult), reads=[("gtt", hb), "lng"], writes=[("gtt", hb)])
                S.op("pool", lambda: nc.gpsimd.tensor_tensor(out=hx[hb][:], in0=tt_[hb][:], in1=lnb[:], op=ALU.add), reads=[("gtt", hb), "lnb"], writes=[("ghx", hb)])
                S.dma("sp", X["h"][i * P:(i + 1) * P, :], hx[hb][:], reads=[("ghx", hb)], writes=[("d_h", hb)])
                ks = self.ln_stats(hx[hb], st6[hb], mv[hb], rs[hb], ("ghx", hb))
                S.op("dve", lambda: nc.vector.tensor_scalar(out=xn[hb][:], in0=hx[hb][:], scalar1=mv[hb][:, 0:1], scalar2=rs[hb][:, 1:2], op0=ALU.subtract, op1=ALU.mult),
                     reads=[("ghx", hb)] + ks, writes=[("gxn", hb)])
                for kt in range(8):
                    S.op("pe", lambda: nc.tensor.transpose(tp[:, kt, :], xn[hb][:, kt * P:(kt + 1) * P], self.identb[:]),
                         reads=[("gxn", hb), "identb"], writes=["gtp"], pe_chain=True)
                for kt in range(8):
                    S.op("act", lambda: nc.scalar.activation(out=fT[0][:, kt, ti * P:(ti + 1) * P], in_=tp[:, kt, :], func=AF.Identity,
                                                             scale=self.modc[:, s, 32 + kt:33 + kt], bias=self.modc[:, s, 24 + kt:25 + kt]),
                         reads=["gtp"], writes=[("gfT", 0, ti)])

            def tail_store(ci):
                t0, ntl = chunks[ci]
                Wd = ntl * P
                tok0 = t0 * P
                S.dma("sp", X["fT"].rearrange("(kt p) t -> p kt t", p=P)[:, :, tok0:tok0 + Wd], fT[0][:, :, :Wd],
                      reads=[("gfT", 0, ti) for ti in range(ntl)], writes=[("d_fT", 0)])

            nch = len(chunks)
            load(0)
            for dc in range(8):
                dc_step(0, dc)
            for ci in range(nch):
                ntl = chunks[ci][1]
                if ci + 1 < nch:
                    load(ci + 1)
                done = 0
                for dc in range(8):
                    if ci + 1 < nch:
                        dc_step(ci + 1, dc)
                    if dc % 2 == 1 and done < ntl:
                        tail_tile(ci, done)
                        done += 1
                while done < ntl:
                    tail_tile(ci, done)
                    done += 1
                tail_store(ci)

    def phase_moe(self, l):
        nc, S, I, X = self.nc, self.S, self.I, self.X
        V = nc.vector
        last = (l == DEPTH - 1)

        def dve(fn, reads, writes):
            return S.op("dve", fn, reads=reads, writes=writes)

        with contextlib.ExitStack() as ph:
            MAXT = 18
            wrt = self.sb(ph, "wrt", [P, 8, 64], BF16)
            S.dma("pool", wrt[:], I["w_router"][l].rearrange("(kt p) n -> p kt n", p=P), writes=["wrt"])
            brb = self.sb(ph, "brb", [P, 64])
            S.dma("sp", brb[:], I["b_router"][l].partition_broadcast(P), writes=["brb"])
            lng = self.sb(ph, "lng2", [P, D])
            lnb = self.sb(ph, "lnb2", [P, D])
            S.dma("sp", lng[:], I["ln2_g"][l].partition_broadcast(P), writes=["lng"])
            S.dma("sp", lnb[:], I["ln2_b"][l].partition_broadcast(P), writes=["lnb"])
            fTb = self.sb(ph, "fTb", [P, 8, MAXT * P], BF16)
            wts = self.sb(ph, "wts", [P, MAXT, 64])
            acc = self.sb(ph, "acc", [P, MAXT, D])
            NW = 3
            wg = [self.sb(ph, "wg%d" % i, [P, 8, 256], BF16) for i in range(NW)]
            wu = [self.sb(ph, "wu%d" % i, [P, 8, 256], BF16) for i in range(NW)]
            wd = [self.sb(ph, "wd%d" % i, [P, 2, D], BF16) for i in range(NW)]
            sgt = [self.sb(ph, "sgt%d" % i, [P, 512]) for i in range(2)]
            hT = [[self.sb(ph, "hT%d%d" % (i, m), [P, 512], BF16) for m in range(2)] for i in range(2)]
            r_sc = self.sb(ph, "r_sc", [P, 64]); r_sel = self.sb(ph, "r_sel", [P, 64]); r_eq = self.sb(ph, "r_eq", [P, 64])
            r_s2 = self.sb(ph, "r_s2", [P, 64]); r_m1 = self.sb(ph, "r_m1", [P, 8]); r_m2 = self.sb(ph, "r_m2", [P, 8])
            r_gs = self.sb(ph, "r_gs", [P, 8]); r_t8 = self.sb(ph, "r_t8", [P, 8]); r_gm = self.sb(ph, "r_gm", [P, 8])
            r_gt = self.sb(ph, "r_gt", [P, 8]); r_sm = self.sb(ph, "r_sm", [P, 64]); r_e8 = self.sb(ph, "r_e8", [P, 8])
            r_em = self.sb(ph, "r_em", [P, 64]); r_w = self.sb(ph, "r_w", [P, 64]); r_ss = self.sb(ph, "r_ss", [P, 2])
            hx = [self.sb(ph, "h_hx%d" % i, [P, D]) for i in range(2)]
            tt_ = [self.sb(ph, "h_tt%d" % i, [P, D]) for i in range(2)]
            st6 = [self.sb(ph, "h_st6%d" % i, [P, 2, 6]) for i in range(2)]
            mv = [self.sb(ph, "h_mv%d" % i, [P, 2]) for i in range(2)]
            rs = [self.sb(ph, "h_rs%d" % i, [P, 2]) for i in range(2)]
            pgu = [self.psum(ph, "h_pgu%d" % i, [P, 512]) for i in range(4)]
            py = [self.psum(ph, "h_py%d" % i, [P, 2, 512]) for i in range(2)]

            if last:
                blocks = [(2, 16), (18, 16)]
            else:
                blocks = [(0, 18), (18, 16)]
            wcount = 0
            ccount = 0
            fcount = 0
            ycount = 0
            for (t0, ntl) in blocks:
                BW = ntl * P
                tok0 = t0 * P
                for kt in range(8):
                    S.dma("sp", fTb[:, kt, :BW], X["fT"][kt * P:(kt + 1) * P, tok0:tok0 + BW], writes=[("fTb", kt)])
                fkeys = [("fTb", kt) for kt in range(8)]
                for ti in range(ntl):
                    cols = slice(ti * P, (ti + 1) * P)
                    prt = pgu[ti % 4]
                    pk = ("pgu", ti % 4)
                    for kt in range(8):
                        S.op("pe", lambda: nc.tensor.matmul(prt[:, 0:64], lhsT=fTb[:, kt, cols], rhs=wrt[:, kt, :], start=(kt == 0), stop=(kt == 7)),
                             reads=[("fTb", kt), "wrt"], writes=[pk], pe_chain=True)
                    S.op("act", lambda: nc.scalar.activation(out=r_sc[:], in_=prt[:, 0:64], func=AF.Sigmoid), reads=[pk], writes=["r_sc"])
                    dve(lambda: V.tensor_tensor(out=r_sel[:], in0=r_sc[:], in1=brb[:], op=ALU.add), ["r_sc", "brb"], ["r_sel"])
                    sel3 = r_sel[:].rearrange("p (g e) -> p g e", g=8)
                    dve(lambda: V.tensor_reduce(out=r_m1[:], in_=sel3, axis=AX.X, op=ALU.max), ["r_sel"], ["r_m1"])
                    dve(lambda: V.tensor_tensor(out=r_eq[:].rearrange("p (g e) -> p g e", g=8), in0=sel3, in1=r_m1[:, :].unsqueeze(2).to_broadcast([P, 8, 8]), op=ALU.is_equal),
                        ["r_sel", "r_m1"], ["r_eq"])
                    dve(lambda: V.scalar_tensor_tensor(out=r_s2[:], in0=r_eq[:], scalar=-4.0, in1=r_sel[:], op0=ALU.mult, op1=ALU.add), ["r_eq", "r_sel"], ["r_s2"])
                    dve(lambda: V.tensor_reduce(out=r_m2[:], in_=r_s2[:].rearrange("p (g e) -> p g e", g=8), axis=AX.X, op=ALU.max), ["r_s2"], ["r_m2"])
                    dve(lambda: V.tensor_tensor(out=r_gs[:], in0=r_m1[:], in1=r_m2[:], op=ALU.add), ["r_m1", "r_m2"], ["r_gs"])
                    dve(lambda: V.max(out=r_t8[:], in_=r_gs[:]), ["r_gs"], ["r_t8"])
                    dve(lambda: V.tensor_scalar(out=r_gm[:], in0=r_gs[:], scalar1=r_t8[:, 3:4], scalar2=None, op0=ALU.is_ge), ["r_gs", "r_t8"], ["r_gm"])
                    dve(lambda: V.tensor_scalar(out=r_gt[:], in0=r_gm[:], scalar1=4.0, scalar2=-4.0, op0=ALU.mult, op1=ALU.add), ["r_gm"], ["r_gt"])
                    sm3 = r_sm[:].rearrange("p (g e) -> p g e", g=8)
                    dve(lambda: V.tensor_tensor(out=sm3, in0=sel3, in1=r_gm[:, :].unsqueeze(2).to_broadcast([P, 8, 8]), op=ALU.mult), ["r_sel", "r_gm"], ["r_sm"])
                    dve(lambda: V.tensor_tensor(out=sm3, in0=sm3, in1=r_gt[:, :].unsqueeze(2).to_broadcast([P, 8, 8]), op=ALU.add), ["r_sm", "r_gt"], ["r_sm"])
                    dve(lambda: V.max(out=r_e8[:], in_=r_sm[:]), ["r_sm"], ["r_e8"])
                    dve(lambda: V.tensor_scalar(out=r_em[:], in0=r_sm[:], scalar1=r_e8[:, 7:8], scalar2=None, op0=ALU.is_ge), ["r_sm", "r_e8"], ["r_em"])
                    dve(lambda: V.tensor_tensor(out=r_w[:], in0=r_sc[:], in1=r_em[:], op=ALU.mult), ["r_sc", "r_em"], ["r_w"])
                    dve(lambda: V.tensor_reduce(out=r_ss[:, 0:1], in_=r_w[:], axis=AX.X, op=ALU.add), ["r_w"], ["r_ss0"])
                    dve(lambda: V.reciprocal(out=r_ss[:, 1:2], in_=r_ss[:, 0:1]), ["r_ss0"], ["r_ss1"])
                    dve(lambda: V.tensor_scalar(out=wts[:, ti, :], in0=r_w[:], scalar1=r_ss[:, 1:2], scalar2=2.5, op0=ALU.mult, op1=ALU.mult), ["r_w", "r_ss1"], [("wts", ti)])
                chunks = [(c * 512, min(512, BW - c * 512)) for c in range((BW + 511) // 512)]
                items = [(e, c0, Wd) for e in range(65) for (c0, Wd) in chunks]

                def wsrc(e):
                    if e < 64:
                        return (I["w_exp_gate"][l, e], I["w_exp_up"][l, e], I["w_exp_down"][l, e])
                    return (I["w_sh_gate"][l], I["w_sh_up"][l], I["w_sh_down"][l])

                def load_w(e):
                    ws = (wbase + e) % NW
                    srcs = wsrc(e)
                    S.dma("pool", wg[ws][:], srcs[0].rearrange("(kt p) n -> p kt n", p=P), writes=[("wg", ws)])
                    S.dma("pool", wu[ws][:], srcs[1].rearrange("(kt p) n -> p kt n", p=P), writes=[("wu", ws)])
                    S.dma("pool", wd[ws][:], srcs[2].rearrange("(kt p) n -> p kt n", p=P), writes=[("wd", ws)])

                def emit_gu(k):
                    nonlocal fcount
                    e, c0, Wd = items[k]
                    ws = (wbase + e) % NW
                    cb = k % 2
                    cs_ = slice(c0, c0 + Wd)
                    if c0 == 0 and e + 1 < 65:
                        load_w(e + 1)
                    for mt in range(2):
                        pg_i = 2 * mt
                        pu_i = 2 * mt + 1
                        for (pi, wt_, wk) in ((pg_i, wg[ws], ("wg", ws)), (pu_i, wu[ws], ("wu", ws))):
                            for kt in range(8):
                                S.op("pe", lambda: nc.tensor.matmul(pgu[pi][:, :Wd], lhsT=wt_[:, kt, mt * P:(mt + 1) * P], rhs=fTb[:, kt, cs_], start=(kt == 0), stop=(kt == 7)),
                                     reads=[wk, ("fTb", kt)], writes=[("pgu", pi)], pe_chain=True)
                        fb = fcount % 2
                        fcount += 1
                        S.op("act", lambda: nc.scalar.activation(out=sgt[fb][:, :Wd], in_=pgu[pg_i][:, :Wd], func=AF.Silu), reads=[("pgu", pg_i)], writes=[("sgt", fb)])
                        dve(lambda: V.tensor_tensor(out=hT[cb][mt][:, :Wd], in0=pgu[pu_i][:, :Wd], in1=sgt[fb][:, :Wd], op=ALU.mult),
                            [("pgu", pu_i), ("sgt", fb)], [("hT", cb, mt)])

                def emit_down(k):
                    nonlocal ycount
                    e, c0, Wd = items[k]
                    ws = (wbase + e) % NW
                    cb = k % 2
                    for tl_ in range(Wd // P):
                        ti = c0 // P + tl_
                        yb_ = ycount % 2
                        path = (ycount // 2) % 2
                        ab = ycount % 2
                        ycount += 1
                        for nh in range(2):
                            for mt in range(2):
                                S.op("pe", lambda: nc.tensor.matmul(py[yb_][:, nh, :], lhsT=hT[cb][mt][:, tl_ * P:(tl_ + 1) * P], rhs=wd[ws][:, mt, nh * 512:(nh + 1) * 512],
                                                                    start=(mt == 0), stop=(mt == 1)),
                                     reads=[("hT", cb, mt), ("wd", ws)], writes=[("py", yb_, nh)], pe_chain=True)
                        pyf = py[yb_][:].rearrange("p a b -> p (a b)")
                        pyk = [("py", yb_, 0), ("py", yb_, 1)]
                        wcol = wts[:, ti, min(e, 63):min(e, 63) + 1]
                        if e == 64:
                            if path == 0:
                                S.op("act", lambda: nc.scalar.copy(out=tt_[ab][:], in_=pyf), reads=pyk, writes=[("htt", ab)])
                                S.op("pool", lambda: nc.gpsimd.tensor_tensor(out=acc[:, ti, :], in0=tt_[ab][:], in1=acc[:, ti, :], op=ALU.add),
                                     reads=[("htt", ab), ("acc", ti)], writes=[("acc", ti)])
                            else:
                                dve(lambda: V.tensor_tensor(out=acc[:, ti, :], in0=pyf, in1=acc[:, ti, :], op=ALU.add), pyk + [("acc", ti)], [("acc", ti)])
                        elif path == 0:
                            dst = acc[:, ti, :] if e == 0 else tt_[ab][:]
                            dkey = ("acc", ti) if e == 0 else ("htt", ab)
                            S.op("act", lambda: nc.scalar.activation(out=dst, in_=pyf, func=AF.Identity, scale=wcol),
                                 reads=pyk + [("wts", ti)], writes=[dkey])
                            if e > 0:
                                S.op("pool", lambda: nc.gpsimd.tensor_tensor(out=acc[:, ti, :], in0=tt_[ab][:], in1=acc[:, ti, :], op=ALU.add),
                                     reads=[("htt", ab), ("acc", ti)], writes=[("acc", ti)])
                        else:
                            dst = acc[:, ti, :] if e == 0 else hx[ab][:]
                            dkey = ("acc", ti) if e == 0 else ("hhx", ab)
                            dve(lambda: V.tensor_tensor(out=dst, in0=pyf, in1=wcol.to_broadcast([P, D]), op=ALU.mult), pyk + [("wts", ti)], [dkey])
                            if e > 0:
                                dve(lambda: V.tensor_tensor(out=acc[:, ti, :], in0=hx[ab][:], in1=acc[:, ti, :], op=ALU.add),
                                    [("hhx", ab), ("acc", ti)], [("acc", ti)])

                wbase = wcount
                wcount += 65
                load_w(0)
                for k in range(len(items) + 1):
                    if k < len(items):
                        emit_gu(k)
                    if k >= 1:
                        emit_down(k - 1)
                for ti in range(ntl):
                    i = t0 + ti
                    s = 1 if i < 2 else 0
                    hb = i % 2
                    S.dma("sp", hx[hb][:], X["h"][i * P:(i + 1) * P, :], writes=[("hhx", hb)])
                    S.op("pool", lambda: nc.gpsimd.tensor_tensor(out=tt_[hb][:], in0=acc[:, ti, :], in1=self.gb[:, 2 * s + 1, :], op=ALU.mult),
                         reads=[("acc", ti)], writes=[("htt", hb)])
                    dve(lambda: V.scalar_tensor_tensor(out=hx[hb][:], in0=hx[hb][:], scalar=DN_ALPHA, in1=tt_[hb][:], op0=ALU.mult, op1=ALU.add),
                        [("hhx", hb), ("htt", hb)], [("hhx", hb)])
                    ks = self.ln_stats(hx[hb], st6[hb], mv[hb], rs[hb], ("hhx", hb))
                    dve(lambda: V.tensor_scalar(out=tt_[hb][:], in0=hx[hb][:], scalar1=mv[hb][:, 0:1], scalar2=rs[hb][:, 1:2], op0=ALU.subtract, op1=ALU.mult),
                        [("hhx", hb)] + ks, [("htt", hb)])
                    S.op("pool", lambda: nc.gpsimd.tensor_tensor(out=tt_[hb][:], in0=tt_[hb][:], in1=lng[:], op=ALU.mult), reads=[("htt", hb), "lng"], writes=[("htt", hb)])
                    S.op("pool", lambda: nc.gpsimd.tensor_tensor(out=hx[hb][:], in0=tt_[hb][:], in1=lnb[:], op=ALU.add), reads=[("htt", hb), "lnb"], writes=[("hhx", hb)])
                    if last:
                        dst = self.out[(i - 2) * P:(i - 1) * P, :]
                    else:
                        dst = X["h"][i * P:(i + 1) * P, :]
                    S.dma("sp", dst, hx[hb][:], reads=[("hhx", hb)], writes=[("d_h2", hb)])

_ROPE = None


def make_in_maps(inputs, ncores=8, used=None):
    global _ROPE
    if _ROPE is None:
        _ROPE = _rope_tables()
    cosT, sinT = _ROPE
    perm = _rope_perm()
    w_in = np.ascontiguousarray(inputs["w_in"], dtype=np.float32)
    qcols = np.concatenate([768 + h * 128 + perm for h in range(4)])
    kcols = np.concatenate([1280 + h * 128 + perm for h in range(4)])
    w_qkp = np.ascontiguousarray(w_in[:, :, np.concatenate([qcols, kcols])])
    shared = {k: np.ascontiguousarray(v, dtype=np.float32) for k, v in inputs.items() if k not in ("x", "c", "ctx")}
    shared["w_qkp"] = w_qkp
    shared["rope_cos"] = cosT
    shared["rope_sin"] = sinT
    for n, a in _consts().items():
        shared["k_" + n] = a
    maps = []
    for core in range(ncores):
        b = core % 4
        m = dict(shared)
        m["x"] = np.ascontiguousarray(inputs["x"][b], dtype=np.float32)
        m["ctx"] = np.ascontiguousarray(inputs["ctx"][b], dtype=np.float32)
        m["c"] = np.ascontiguousarray(inputs["c"][b], dtype=np.float32)
        if used is not None:
            m = {k: v for k, v in m.items() if k in used}
        maps.append(m)
    return maps


def kernel(**inputs):
    bld = Builder()
    nc = bld.build()
    maps = make_in_maps(inputs, used=set(bld.I.keys()))
    res = run_bass_kernel_spmd(nc, maps, core_ids=list(range(8)))
    out = np.stack([res.results[b]["out"] for b in range(4)], 0)
    return out.astype(np.float32)
```
